# Optimizing a Trainium2 kernel written in Bass

```python
import jax, jax.numpy as jnp
from jax import lax
import numpy as np

D_MODEL = 1024
BATCH = 32
SEQ = 2048
DEPTH = 1

H_R = 8
N_R = 64
C_R = H_R * N_R
DECAY_LORA = 64
AAA_LORA = 64
GATE_LORA = 128
RW_SIZES = (C_R, C_R, C_R, DECAY_LORA, AAA_LORA, GATE_LORA)
RW_COLS = sum(RW_SIZES)
LNX_EPS = 64e-5

H_M = 4
DK_M = 64
DV_M = 128
C_MQK = H_M * DK_M
C_MV = H_M * DV_M
ML_SIZES = (C_MQK, C_MQK, C_MV, C_MV, H_M, H_M)
ML_COLS = sum(ML_SIZES)
QK_CONV = 4
CHUNK = 64
GATE_CAP = 15.0

N_IN = RW_COLS + ML_COLS
D_MIX = C_R + C_MV

D_FF = 2816
FFN_CONV = 3
NORM_EPS = 1e-6

kernel_name = "hymba_rwkv7_mlstm_convffn"


def _split(t, sizes):
    idx = np.cumsum(sizes)[:-1].tolist()
    return jnp.split(t, idx, axis=-1)


def rmsnorm(x, g):
    xf = x.astype(jnp.float32)
    y = xf * lax.rsqrt(jnp.mean(xf * xf, axis=-1, keepdims=True) + NORM_EPS)
    return y.astype(x.dtype) * g


def head_layernorm(y, eps):
    yf = y.astype(jnp.float32)
    mu = jnp.mean(yf, axis=-1, keepdims=True)
    var = jnp.mean(jnp.square(yf - mu), axis=-1, keepdims=True)
    return (yf - mu) * lax.rsqrt(var + eps)


def head_rmsnorm(y):
    yf = y.astype(jnp.float32)
    return yf * lax.rsqrt(jnp.mean(yf * yf, axis=-1, keepdims=True) + NORM_EPS)


def token_shift(t):
    return jnp.pad(t, ((0, 0), (1, 0), (0, 0)))[:, :-1]


def causal_dwconv(t, w, b):
    K = w.shape[0]
    T = t.shape[1]
    tp = jnp.pad(t, ((0, 0), (K - 1, 0), (0, 0)))
    out = b + tp[:, 0:T] * w[0]
    for j in range(1, K):
        out = out + tp[:, j:j + T] * w[j]
    return out


def softcap(t):
    return GATE_CAP * jnp.tanh(t / GATE_CAP)


def rwkv7_scan(r, w, k, v, a, b):
    Bn, T, H, N = r.shape

    def step(S, inp):
        r_t, w_t, k_t, v_t, a_t, b_t = inp
        sa = jnp.einsum('bhij,bhj->bhi', S, a_t)
        S = S * w_t[:, :, None, :] + sa[..., None] * b_t[:, :, None, :] + v_t[..., :, None] * k_t[..., None, :]
        y = jnp.einsum('bhij,bhj->bhi', S, r_t)
        return S, y

    S0 = jnp.zeros((Bn, H, N, N), jnp.float32)
    xs = tuple(jnp.moveaxis(t, 1, 0) for t in (r, w, k, v, a, b))
    _, y = lax.scan(step, S0, xs)
    return jnp.moveaxis(y, 0, 1)


def rwkv7_group(p, mu, w0, w_up_decay, a0, w_up_a, w_up_g, k_k, k_a, r_k, lnx_w, lnx_b):
    Bn, T, _ = p.shape
    p = p + (token_shift(p) - p) * mu
    r, k, v, w_lo, a_lo, g_lo = _split(p, RW_SIZES)
    log_w = -jax.nn.softplus(-(w0 + jnp.tanh(w_lo) @ w_up_decay)) - 0.5
    decay = jnp.exp(-jnp.exp(log_w.astype(jnp.float32)))
    a = jax.nn.sigmoid(a0 + a_lo @ w_up_a)
    g = jax.nn.sigmoid(g_lo) @ w_up_g
    heads = lambda t: t.reshape(Bn, T, H_R, N_R)
    kk = heads(k * k_k).astype(jnp.float32)
    kk = kk / jnp.maximum(jnp.sqrt(jnp.sum(kk * kk, axis=-1, keepdims=True)), 1e-12)
    k = k * (1 + (a - 1) * k_a)
    r_h, k_h, v_h, a_h = heads(r), heads(k), heads(v), heads(a)
    y = rwkv7_scan(r_h, heads(decay), k_h, v_h, -kk, kk * a_h)
    y = head_layernorm(y, LNX_EPS).reshape(Bn, T, C_R) * lnx_w + lnx_b
    bonus = jnp.sum(r_h * k_h * r_k, axis=-1, keepdims=True) * v_h
    return ((y + bonus.reshape(Bn, T, C_R)) * g).astype(p.dtype)


def mlstm_chunkwise(q, k, v, ig, logf):
    Bn, T, H, _ = q.shape
    nc = T // CHUNK

    def to_chunks(t):
        t = t.reshape(Bn, nc, CHUNK, H, *t.shape[3:])
        return jnp.moveaxis(t, (1, 3), (0, 2))

    causal = jnp.tril(jnp.ones((CHUNK, CHUNK), dtype=bool))

    def step(carry, inp):
        C, n, m = carry
        qc, kc, vc, ic, fc = inp
        b = jnp.cumsum(fc, axis=-1)
        log_d = jnp.where(causal, b[..., :, None] - b[..., None, :] + ic[..., None, :], -jnp.inf)
        m_inter = b + m[..., None]
        m_t = jnp.maximum(jnp.max(log_d, axis=-1), m_inter)
        d = jnp.exp(log_d - m_t[..., None])
        s = jnp.einsum('bhtd,bhsd->bhts', qc, kc) * d
        sc = jnp.exp(m_inter - m_t)
        num = jnp.einsum('bhts,bhsv->bhtv', s, vc) + sc[..., None] * jnp.einsum('bhtd,bhdv->bhtv', qc, C)
        den = jnp.sum(s, axis=-1) + sc * jnp.einsum('bhtd,bhd->bht', qc, n)
        h = num / jnp.maximum(jnp.abs(den), jnp.exp(-m_t))[..., None]
        b_last = b[..., -1]
        a_w = b_last[..., None] - b + ic
        m_new = jnp.maximum(b_last + m, jnp.max(a_w, axis=-1))
        w_s = jnp.exp(a_w - m_new[..., None])
        dec = jnp.exp(b_last + m - m_new)
        C = dec[..., None, None] * C + jnp.einsum('bhs,bhsd,bhsv->bhdv', w_s, kc, vc)
        n = dec[..., None] * n + jnp.einsum('bhs,bhsd->bhd', w_s, kc)
        return (C, n, m_new), h

    carry0 = (jnp.zeros((Bn, H, q.shape[-1], v.shape[-1]), jnp.float32),
              jnp.zeros((Bn, H, q.shape[-1]), jnp.float32),
              jnp.zeros((Bn, H), jnp.float32))
    xs = tuple(to_chunks(t) for t in (q, k, v, ig, logf))
    _, h = lax.scan(step, carry0, xs)
    h = jnp.moveaxis(h, (0, 2), (1, 3))
    return h.reshape(Bn, T, H, v.shape[-1])


def mlstm_group(p, qk_conv_w, qk_conv_b, i_bias, f_bias, mh_norm_g):
    Bn, T, _ = p.shape
    q, k, v, o, ig, fg = _split(p, ML_SIZES)
    qk = jax.nn.silu(causal_dwconv(jnp.concatenate([q, k], axis=-1), qk_conv_w, qk_conv_b))
    q, k = qk[..., :C_MQK], qk[..., C_MQK:]
    ig = softcap((ig + i_bias).astype(jnp.float32))
    logf = jax.nn.log_sigmoid(softcap((fg + f_bias).astype(jnp.float32)))
    h = mlstm_chunkwise(q.reshape(Bn, T, H_M, DK_M) * (DK_M ** -0.5),
                        k.reshape(Bn, T, H_M, DK_M),
                        v.reshape(Bn, T, H_M, DV_M), ig, logf)
    h = head_rmsnorm(h).reshape(Bn, T, C_MV) * mh_norm_g * jax.nn.sigmoid(o)
    return h.astype(p.dtype)


def setup_inputs(seed: int = 0) -> dict:
    key = jax.random.key(seed)
    ks = jax.random.split(key, 26)
    f32 = jnp.float32
    nrm = lambda k, s, sc: jax.random.normal(k, s, f32) * sc
    gain = lambda k, s: 1.0 + 0.02 * jax.random.normal(k, s, f32)
    L = DEPTH
    return {
        "x": jax.random.normal(ks[0], (BATCH, SEQ, D_MODEL), f32),
        "norm1_g": gain(ks[1], (L, D_MODEL)),
        "w_in": nrm(ks[2], (L, D_MODEL, N_IN), D_MODEL ** -0.5),
        "rw_mu": jax.random.uniform(ks[3], (L, RW_COLS), f32),
        "w0": jax.random.uniform(ks[4], (L, C_R), f32, -6.0, 0.0),
        "w_up_decay": nrm(ks[5], (L, DECAY_LORA, C_R), 0.1 * DECAY_LORA ** -0.5),
        "a0": nrm(ks[6], (L, C_R), 0.1),
        "w_up_a": nrm(ks[7], (L, AAA_LORA, C_R), 0.1 * AAA_LORA ** -0.5),
        "w_up_g": nrm(ks[8], (L, GATE_LORA, C_R), GATE_LORA ** -0.5),
        "k_k": 0.85 + 0.02 * jax.random.normal(ks[9], (L, C_R), f32),
        "k_a": gain(ks[10], (L, C_R)),
        "r_k": nrm(ks[11], (L, H_R, N_R), 0.1),
        "lnx_w": gain(ks[12], (L, C_R)),
        "lnx_b": nrm(ks[13], (L, C_R), 0.02),
        "qk_conv_w": nrm(ks[14], (L, QK_CONV, 2 * C_MQK), QK_CONV ** -0.5),
        "qk_conv_b": nrm(ks[15], (L, 2 * C_MQK), 0.02),
        "i_bias": nrm(ks[16], (L, H_M), 0.1),
        "f_bias": jax.random.uniform(ks[17], (L, H_M), f32, 3.0, 6.0),
        "mh_norm_g": gain(ks[18], (L, C_MV)),
        "w_out": nrm(ks[19], (L, D_MIX, D_MODEL), D_MIX ** -0.5),
        "norm2_g": gain(ks[20], (L, D_MODEL)),
        "w_ffn_up": nrm(ks[21], (L, D_MODEL, 2 * D_FF), D_MODEL ** -0.5),
        "ffn_conv_w": nrm(ks[22], (L, FFN_CONV, D_FF), FFN_CONV ** -0.5),
        "ffn_conv_b": nrm(ks[23], (L, D_FF), 0.02),
        "w_ffn_down": nrm(ks[24], (L, D_FF, D_MODEL), D_FF ** -0.5),
        "norm_f_g": gain(ks[25], (D_MODEL,)),
    }


def reference(x, norm1_g, w_in, rw_mu, w0, w_up_decay, a0, w_up_a, w_up_g, k_k, k_a, r_k,
              lnx_w, lnx_b, qk_conv_w, qk_conv_b, i_bias, f_bias, mh_norm_g, w_out,
              norm2_g, w_ffn_up, ffn_conv_w, ffn_conv_b, w_ffn_down, norm_f_g):
    for l in range(DEPTH):
        h = rmsnorm(x, norm1_g[l])
        proj = h @ w_in[l]
        p_rw, p_ml = proj[..., :RW_COLS], proj[..., RW_COLS:]
        y_rw = rwkv7_group(p_rw, rw_mu[l], w0[l], w_up_decay[l], a0[l], w_up_a[l], w_up_g[l],
                           k_k[l], k_a[l], r_k[l], lnx_w[l], lnx_b[l])
        y_ml = mlstm_group(p_ml, qk_conv_w[l], qk_conv_b[l], i_bias[l], f_bias[l],
                           mh_norm_g[l])
        x = x + jnp.concatenate([y_rw, y_ml], axis=-1) @ w_out[l]
        h = rmsnorm(x, norm2_g[l])
        u = h @ w_ffn_up[l]
        a, b = u[..., :D_FF], u[..., D_FF:]
        a = causal_dwconv(a, ffn_conv_w[l], ffn_conv_b[l])
        x = x + (jax.nn.silu(a) * b) @ w_ffn_down[l]
    return rmsnorm(x, norm_f_g)
```

```python
import numpy as np
from contextlib import ExitStack
import concourse.bass as bass
import concourse.mybir as mybir
from concourse.bass_utils import run_bass_kernel_spmd

F32 = mybir.dt.float32
BF16 = mybir.dt.bfloat16
ALU = mybir.AluOpType
AF = mybir.ActivationFunctionType
AX = mybir.AxisListType

N_CORES = 8
D = 1024
N_IN = 3336
RWC = 1792
MLO = 2 * RWC
WIN_COLS = 2 * RWC + 1544
DFF = 2816
NB = DFF // 128
CDEC = float(np.exp(-0.5))


class Res:
    __slots__ = ("name", "lw", "rd")

    def __init__(self, name):
        self.name = name
        self.lw = None
        self.rd = []


class Sched:
    EPOCH = 30000
    NDMA = 24

    def __init__(self, nc, es):
        self.nc = nc
        self.es = es
        self.engs = {"pe": nc.tensor, "act": nc.scalar, "dve": nc.vector,
                     "pool": nc.gpsimd, "sp": nc.sync}
        self.sems = {e: [] for e in self.engs}
        self.cnt = {e: 0 for e in self.engs}
        self.waited = {e: {} for e in self.engs}
        self.dma_sems = [es.enter_context(nc.semaphore(f"dq{i}")) for i in range(self.NDMA)]
        self.dma_cnt = [0] * self.NDMA
        self.dma_i = 0
        for e in self.engs:
            self._new_epoch(e)

    def _new_epoch(self, e):
        s = self.es.enter_context(self.nc.semaphore(f"s_{e}_{len(self.sems[e])}"))
        self.sems[e].append(s)
        self.cnt[e] = 0

    def _wait(self, e, dep):
        if dep[0] == "dma":
            _, idx, val = dep
            key = ("dma", idx)
            sem = self.dma_sems[idx]
        else:
            de, ep, val = dep
            key = (de, ep)
            sem = self.sems[de][ep]
        if self.waited[e].get(key, 0) >= val:
            return
        self.engs[e].wait_ge(sem, val)
        self.waited[e][key] = val

    def _deps(self, e, reads, writes):
        deps = []
        for r in reads:
            if r.lw is not None and not (r.lw[0] == e and e == "pe"):
                deps.append(r.lw)
        for w in writes:
            if w.lw is not None and w.lw[0] != e:
                deps.append(w.lw)
            for d in w.rd:
                if d[0] != e:
                    deps.append(d)
        return deps

    def _mark(self, tag, reads, writes):
        for r in reads:
            r.rd.append(tag)
            if len(r.rd) > 64:
                r.rd = r.rd[-48:]
        for w in writes:
            w.lw = tag
            w.rd = []

    def op(self, e, fn, reads=(), writes=()):
        for d in self._deps(e, reads, writes):
            self._wait(e, d)
        if self.cnt[e] >= self.EPOCH:
            self._new_epoch(e)
        ins = fn(self.engs[e])
        ep = len(self.sems[e]) - 1
        ins.then_inc(self.sems[e][ep], 1)
        self.cnt[e] += 1
        tag = (e, ep, self.cnt[e])
        self._mark(tag, reads, writes)
        return tag

    def dma(self, q, out, in_, reads=(), writes=(), slow=False):
        for d in self._deps(q, reads, writes):
            self._wait(q, d)
        idx = self.dma_i
        self.dma_i = (self.dma_i + 1) % self.NDMA
        kw = {"allow_slow_non_contiguous": True} if slow else {}
        self.engs[q].dma_start(out=out, in_=in_, **kw).then_inc(self.dma_sems[idx], 16)
        self.dma_cnt[idx] += 16
        tag = ("dma", idx, self.dma_cnt[idx])
        self._mark(tag, reads, writes)
        return tag

    def barrier(self):
        for e in self.engs:
            for d in self.engs:
                if d != e:
                    ep = len(self.sems[d]) - 1
                    if self.cnt[d] > 0:
                        self._wait(e, (d, ep, self.cnt[d]))
                    elif ep > 0:
                        self._wait(e, (d, ep - 1, self.EPOCH))
            for i in range(self.NDMA):
                if self.dma_cnt[i]:
                    self._wait(e, ("dma", i, self.dma_cnt[i]))


import os
VAR = int(os.environ.get('KVAR', '0'))


class _Stop(Exception):
    pass


def build(NSEQ, T, dbg=False, stop=0):
    nc = bass.Bass("TRN2", target_bir_lowering=False)
    try:
        _build(nc, NSEQ, T, dbg, stop)
    except _Stop:
        pass
    return nc


def _build(nc, NSEQ, T, dbg, stop):
    NT = T // 128
    NTOK = NSEQ * T
    di = lambda n, s: nc.dram_tensor(n, s, F32, kind="ExternalInput").ap()
    x_d = di("x", [NTOK, D])
    norm1_g = di("norm1_g", [128, 8]); w_in = di("w_in", [D, N_IN]); rw_mu = di("rw_mu", [RWC])
    w0 = di("w0", [512]); w_up_decay = di("w_up_decay", [64, 512]); a0 = di("a0", [512])
    w_up_a = di("w_up_a", [64, 512]); w_up_g = di("w_up_g", [128, 512]); k_k = di("k_k", [512])
    k_a = di("k_a", [512]); r_k = di("r_k", [512]); lnx_w = di("lnx_w", [512]); lnx_b = di("lnx_b", [512])
    qk_conv_w = di("qk_conv_w", [128, 16]); qk_conv_b = di("qk_conv_b", [128, 4])
    i_bias = di("i_bias", [4]); f_bias = di("f_bias", [4]); mh_norm_g = di("mh_norm_g", [512])
    w_out = di("w_out", [D, D]); norm2_g = di("norm2_g", [128, 8]); w_ffn_up = di("w_ffn_up", [D, 2 * DFF])
    ffn_conv_w = di("ffn_conv_w", [128, NB * 3]); ffn_conv_b = di("ffn_conv_b", [128, NB])
    w_ffn_down = di("w_ffn_down", [DFF, D]); norm_f_g = di("norm_f_g", [D])
    consts = di("consts", [128, 640])
    out_d = nc.dram_tensor("out", [NTOK, D], F32, kind="ExternalOutput").ap()
    x1_d = nc.dram_tensor("x1s", [NTOK, D], F32, kind="Internal").ap()
    dbg_d = {}
    if dbg:
        for nm in ("yrw", "yml", "y", "hm"):
            dbg_d[nm] = nc.dram_tensor("d_" + nm, [NTOK, 512], F32, kind="ExternalOutput").ap()

    es0 = ExitStack()
    with es0:
        S = Sched(nc, es0)

        def chk(n):
            if stop == n:
                S.barrier()
                raise _Stop()

        def mk(es):
            def sb(n, s, d=F32):
                return es.enter_context(nc.sbuf_tensor(n, s, d)), Res(n)

            def ps(n, s, d=F32):
                return es.enter_context(nc.psum_tensor(n, s, d)), Res(n)
            return sb, ps

        sb0, ps0 = mk(es0)
        CST, rCST = sb0("CST", [128, 640])
        S.dma("sp", CST[:], consts, writes=[rCST])
        IDF = CST[:, 0:128]; MU = CST[:, 128:256]; MUI = CST[:, 256:384]; ML = CST[:, 384:512]; ONES = CST[:, 512:640]
        CSB, rCSB = sb0("CSB", [128, 640], BF16)
        S.op("dve", lambda e: e.tensor_copy(out=CSB[:], in_=CST[:]), reads=[rCST], writes=[rCSB])
        IDB = CSB[:, 0:128]
        MUI8, rMUI8 = sb0("MUI8", [128, 128])
        S.op("dve", lambda e: e.tensor_scalar(out=MUI8[:], in0=MUI, scalar1=0.125, scalar2=None, op0=ALU.mult),
             reads=[rCST], writes=[rMUI8])
        NPF = 6
        PF = [ps0(f"PF{i}", [128, 512]) for i in range(NPF)]
        PT = [ps0(f"PT{i}", [128, 1024], BF16) for i in range(2)]
        pf_i = [0]
        pt_i = [0]

        def pf():
            p = PF[pf_i[0] % NPF]
            pf_i[0] += 1
            return p

        def ptb():
            p = PT[pt_i[0] % 2]
            pt_i[0] += 1
            return p

        def bc3(ap2, a, b):
            return ap2.rearrange("p (a o) -> p a o", o=1).to_broadcast([ap2.shape[0], a, b])

        def bcm(ap2, a):
            return ap2.rearrange("p (o n) -> p o n", o=1).to_broadcast([ap2.shape[0], a, ap2.shape[1]])

        def v3(ap, a):
            return ap.rearrange("p (a b) -> p a b", a=a)

        def rmsnorm_to_bf16(X, rX, HN, rHN, ST, rST):
            S.op("pool", lambda e: e.memset(ST[:, 0:1], 0.0), writes=[rST])
            S.op("act", lambda e: e.activation(out=HN[:], in_=X, func=AF.Square, accum_out=ST[:, 0:1]),
                 reads=[rX, rST], writes=[rHN, rST])
            S.op("dve", lambda e: e.tensor_scalar(out=ST[:, 1:2], in0=ST[:, 0:1], scalar1=1.0 / D, scalar2=1e-6,
                                                  op0=ALU.mult, op1=ALU.add), reads=[rST], writes=[rST])
            S.op("act", lambda e: e.activation(out=ST[:, 2:3], in_=ST[:, 1:2], func=AF.Sqrt), reads=[rST], writes=[rST])
            S.op("dve", lambda e: e.reciprocal(out=ST[:, 3:4], in_=ST[:, 2:3]), reads=[rST], writes=[rST])
            S.op("act", lambda e: e.activation(out=HN[:], in_=X, func=AF.Copy, scale=ST[:, 3:4]),
                 reads=[rX, rST], writes=[rHN])

        es1 = ExitStack()
        with es1:
            sb1, _ = mk(es1)
            WIN, rWIN = sb1("WIN", [128, 8, WIN_COLS], BF16)
            WOUT, rWOUT = sb1("WOUT", [128, 8, D], BF16)
            WLOR, rWLOR = sb1("WLOR", [128, 512], BF16)
            WG, rWG = sb1("WG", [128, 512], BF16)
            VEC, rVEC = sb1("VEC", [128, 8, 512])
            CW, rCW = sb1("CW", [128, 4, 4])
            CB, rCB = sb1("CB", [128, 4])
            GB, rGB = sb1("GB", [128, 8])
            for i, v in enumerate((w0, a0, k_k, k_a, r_k, lnx_w, lnx_b, mh_norm_g)):
                S.dma("sp", VEC[:, i, :], v.partition_broadcast(128), writes=[rVEC])
            S.dma("sp", CW[:].rearrange("p b j -> p (b j)"), qk_conv_w, writes=[rCW])
            S.dma("sp", CB[:], qk_conv_b, writes=[rCB])
            S.dma("sp", GB[:, 0:4], i_bias.partition_broadcast(128), writes=[rGB])
            S.dma("sp", GB[:, 4:8], f_bias.partition_broadcast(128), writes=[rGB])
            W0V = VEC[:, 0, :]; A0V = VEC[:, 1, :]; KKV = VEC[:, 2, :]; KAV = VEC[:, 3, :]
            RKV = VEC[:, 4, :]; LWV = VEC[:, 5, :]; LBV = VEC[:, 6, :]; MHG = VEC[:, 7, :]
            esA = ExitStack()
            with esA:
                sbA, _ = mk(esA)
                MUT, rMUT = sbA("MUT", [128, RWC])
                OMM, rOMM = sbA("OMM", [128, RWC])
                G1, rG1 = sbA("G1", [128, 8])
                STG = [sbA(f"STG{i}", [128, N_IN]) for i in range(2)]
                S.dma("sp", MUT[:], rw_mu.partition_broadcast(128), writes=[rMUT])
                S.dma("sp", G1[:], norm1_g, writes=[rG1])
                S.op("dve", lambda e: e.tensor_scalar(out=OMM[:], in0=MUT[:], scalar1=-1.0, scalar2=1.0,
                                                      op0=ALU.mult, op1=ALU.add), reads=[rMUT], writes=[rOMM])
                for k in range(8):
                    st, rst = STG[k % 2]
                    S.dma("sp", st[:], w_in[k * 128:(k + 1) * 128, :], writes=[rst])
                    S.op("act", lambda e: e.activation(out=st[:], in_=st[:], func=AF.Copy, scale=G1[:, k:k + 1]),
                         reads=[rst, rG1], writes=[rst])
                    S.op("dve", lambda e: e.tensor_tensor(out=WIN[:, k, 0:RWC], in0=st[:, 0:RWC], in1=OMM[:], op=ALU.mult),
                         reads=[rst, rOMM], writes=[rWIN])
                    S.op("pool", lambda e: e.tensor_tensor(out=WIN[:, k, RWC:2 * RWC], in0=st[:, 0:RWC], in1=MUT[:], op=ALU.mult),
                         reads=[rst, rMUT], writes=[rWIN])
                    S.op("act", lambda e: e.activation(out=WIN[:, k, MLO:WIN_COLS], in_=st[:, RWC:N_IN], func=AF.Copy),
                         reads=[rst], writes=[rWIN])
                for k in range(8):
                    st, rst = STG[k % 2]
                    S.dma("sp", st[:, 0:D], w_out[k * 128:(k + 1) * 128, :], writes=[rst])
                    S.op("dve", lambda e: e.tensor_copy(out=WOUT[:, k, :], in_=st[:, 0:D]), reads=[rst], writes=[rWOUT])
                st, rst = STG[0]
                S.dma("sp", st[0:64, 0:512], w_up_decay, writes=[rst])
                S.dma("sp", st[64:128, 0:512], w_up_a, writes=[rst])
                S.dma("sp", st[:, 512:1024], w_up_g, writes=[rst])
                S.op("dve", lambda e: e.tensor_copy(out=WLOR[:], in_=st[:, 0:512]), reads=[rst], writes=[rWLOR])
                S.op("dve", lambda e: e.tensor_copy(out=WG[:], in_=st[:, 512:1024]), reads=[rst], writes=[rWG])
                S.barrier()
            chk(1)
            esB = ExitStack()
            with esB:
                sbB, _ = mk(esB)
                X = [sbB(f"X{i}", [128, D]) for i in range(2)]
                HN, rHN = sbB("HN", [128, D], BF16)
                HT = [sbB(f"HT{i}", [128, 8, 129], BF16) for i in range(2)]
                ST, rST = sbB("ST", [128, 8])
                R, rR = sbB("R", [128, 512]); K, rK = sbB("K", [128, 512]); V, rV = sbB("V", [128, 512])
                LT, rLT = sbB("LT", [128, 256], BF16)
                SG, rSG = sbB("SG", [128, 512]); AA, rAA = sbB("AA", [128, 512])
                ECL, rECL = sbB("ECL", [128, 512]); ENCL, rENCL = sbB("ENCL", [128, 512])
                KKN, rKKN = sbB("KKN", [128, 512]); KM, rKM = sbB("KM", [128, 512])
                TA, rTA = sbB("TA", [128, 512]); TB, rTB = sbB("TB", [128, 512])
                ECLM, rECLM = TA, rTA
                BON, rBON = SG, rSG
                S8, rS8 = sbB("S8", [128, 64])
                WL, rWL = sbB("WL", [128, 4])
                RBAR, rRBAR = sbB("RBAR", [128, 512], BF16); ABAR, rABAR = sbB("ABAR", [128, 512], BF16)
                BTIL, rBTIL = sbB("BTIL", [128, 512], BF16); KTIL, rKTIL = sbB("KTIL", [128, 512], BF16)
                VB, rVB = sbB("VB", [128, 512], BF16)
                RBT, rRBT = sbB("RBT", [128, 4, 128], BF16); ABT, rABT = sbB("ABT", [128, 4, 128], BF16)
                BTT, rBTT = sbB("BTT", [128, 4, 128], BF16); KTT, rKTT = sbB("KTT", [128, 4, 128], BF16)
                AAK, rAAK = sbB("AAK", [128, 8, 128], BF16); ARKT, rARKT = sbB("ARKT", [128, 8, 128], BF16)
                ARBT, rARBT = sbB("ARBT", [128, 8, 128], BF16)
                PQ = [[sbB(f"PQ{i}", [128, 4, 128], BF16) for i in range(4)]] * 2
                TT = [[sbB(f"TT{i}", [128, 4, 128], BF16) for i in range(2)]] * 2
                ABPT, rABPT = sbB("ABPT", [128, 4, 128], BF16); AAKPT, rAAKPT = sbB("AAKPT", [128, 8, 128], BF16)
                UB, rUB = sbB("UB", [128, 512], BF16)
                YF, rYF = ECL, rECL
                HF = [sbB("HF", [128, 4, 64])] * NSEQ
                HB, rHB = sbB("HB", [128, 4, 64], BF16)
                QKC, rQKC = sbB("QKC", [128, 4, 131])
                ACC, rACC = v3(TA[:], 4), rTA
                QKT, rQKT = ABT, rABT
                KP, rKP = sbB("KP", [128, 4, 64], BF16)
                VE, rVE = sbB("VE", [128, 4, 129], BF16)
                G8, rG8 = sbB("G8", [128, 48])
                DG, rDG = v3(TB[:], 4), rTB
                MST = [sbB("MST", [128, 4])] * NSEQ
                CF = [sbB("CF", [128, 2, 129])] * NSEQ
                CBF, rCBF = sbB("CBF", [128, 2, 129], BF16)
                PTB, rPTB = RBT, rRBT
                HM, rHM = v3(KKN[:], 4), rKKN
                SO, rSO = KM, rKM
                MIX, rMIX = sbB("MIX", [128, D], BF16)
                MIXT, rMIXT = AAKPT, rAAKPT
                DBG, rDBG = sbB("DBG", [128, 512]) if dbg else (None, None)

                S.op("pool", lambda e: e.memset(VE[:], 1.0), writes=[rVE])

                def dbg_out(nm, ap, rr, r0):
                    if dbg:
                        S.dma("sp", dbg_d[nm][r0:r0 + 128, :], ap, reads=[rr])

                tile_i = 0
                for s in range(NSEQ):
                    HFs, rHFs = HF[s]
                    CFs, rCFs = CF[s]
                    Ms, rMs = MST[s]
                    S.op("pool", lambda e: e.memset(HFs[:], 0.0), writes=[rHFs])
                    S.op("pool", lambda e: e.memset(CFs[:], 0.0), writes=[rCFs])
                    S.op("pool", lambda e: e.memset(Ms[:], 0.0), writes=[rMs])
                    for c in range(NT):
                        r0 = s * T + c * 128
                        Xc, rXc = X[tile_i % 2]
                        HTc, rHTc = HT[tile_i % 2]
                        HTp, rHTp = HT[(tile_i + 1) % 2]
                        tile_i += 1
                        S.dma("sp", Xc[:], x_d[r0:r0 + 128, :], writes=[rXc])
                        rmsnorm_to_bf16(Xc[:], rXc, HN, rHN, ST, rST)
                        pt, rpt = ptb()
                        S.op("pe", lambda e: [e.transpose(out=pt[:, k * 128:(k + 1) * 128], in_=HN[:, k * 128:(k + 1) * 128],
                                                          identity=IDB) for k in range(8)][-1],
                             reads=[rHN, rCSB], writes=[rpt])
                        S.op("dve", lambda e: e.tensor_copy(out=HTc[:, :, 1:129], in_=v3(pt[:], 8)), reads=[rpt], writes=[rHTc])
                        if c == 0:
                            S.op("pool", lambda e: e.memset(HTc[:, :, 0:1], 0.0), writes=[rHTc])
                        else:
                            S.op("pool", lambda e: e.tensor_copy(out=HTc[:, :, 0:1], in_=HTp[:, :, 128:129]),
                                 reads=[rHTp], writes=[rHTc])
                        chk(2)
                        cur = lambda k: HTc[:, k, 1:129]
                        prv = lambda k: HTc[:, k, 0:128]

                        def proj_tok(p, col, n, shifted):
                            def f(e):
                                last = None
                                nm = 16 if shifted else 8
                                i = 0
                                for k in range(8):
                                    last = e.matmul(p, lhsT=cur(k), rhs=WIN[:, k, col:col + n], start=(i == 0), stop=(i == nm - 1)); i += 1
                                    if shifted:
                                        last = e.matmul(p, lhsT=prv(k), rhs=WIN[:, k, RWC + col:RWC + col + n], start=False, stop=(i == nm - 1)); i += 1
                                return last
                            return f

                        def proj_feat(p, col, shifted):
                            def f(e):
                                last = None
                                nm = 16 if shifted else 8
                                i = 0
                                for k in range(8):
                                    last = e.matmul(p, lhsT=WIN[:, k, col:col + 128], rhs=cur(k), start=(i == 0), stop=(i == nm - 1)); i += 1
                                    if shifted:
                                        last = e.matmul(p, lhsT=WIN[:, k, RWC + col:RWC + col + 128], rhs=prv(k), start=False, stop=(i == nm - 1)); i += 1
                                return last
                            return f

                        for g, (dst, rdst) in enumerate(((R, rR), (K, rK), (V, rV))):
                            p, rp = pf()
                            S.op("pe", proj_tok(p[:], g * 512, 512, True), reads=[rHTc, rWIN], writes=[rp])
                            S.op("act", lambda e: e.activation(out=dst[:], in_=p[:], func=AF.Copy), reads=[rp], writes=[rdst])
                        pl, rpl = pf()
                        S.op("pe", proj_feat(pl[:, 0:128], 1536, True), reads=[rHTc, rWIN], writes=[rpl])
                        S.op("pe", proj_feat(pl[:, 128:256], 1664, True), reads=[rHTc, rWIN], writes=[rpl])
                        S.op("act", lambda e: e.activation(out=LT[0:64, 0:128], in_=pl[0:64, 0:128], func=AF.Tanh), reads=[rpl], writes=[rLT])
                        S.op("act", lambda e: e.activation(out=LT[64:128, 0:128], in_=pl[64:128, 0:128], func=AF.Copy), reads=[rpl], writes=[rLT])
                        S.op("act", lambda e: e.activation(out=LT[:, 128:256], in_=pl[:, 128:256], func=AF.Sigmoid), reads=[rpl], writes=[rLT])
                        pw, rpw = pf()
                        S.op("pe", lambda e: e.matmul(pw[:], lhsT=LT[0:64, 0:128], rhs=WLOR[0:64, :], start=True, stop=True),
                             reads=[rLT, rWLOR], writes=[rpw])
                        S.op("dve", lambda e: e.tensor_tensor(out=SG[:], in0=pw[:], in1=W0V, op=ALU.add), reads=[rpw, rVEC], writes=[rSG])
                        S.op("act", lambda e: e.activation(out=SG[:], in_=SG[:], func=AF.Sigmoid), reads=[rSG], writes=[rSG])
                        pa, rpa = pf()
                        S.op("pe", lambda e: e.matmul(pa[:], lhsT=LT[64:128, 0:128], rhs=WLOR[64:128, :], start=True, stop=True),
                             reads=[rLT, rWLOR], writes=[rpa])
                        S.op("dve", lambda e: e.tensor_tensor(out=AA[:], in0=pa[:], in1=A0V, op=ALU.add), reads=[rpa, rVEC], writes=[rAA])
                        S.op("act", lambda e: e.activation(out=AA[:], in_=AA[:], func=AF.Sigmoid), reads=[rAA], writes=[rAA])
                        chk(3)
                        pc, rpc = pf()
                        S.op("pe", lambda e: e.matmul(pc[:], lhsT=MUI, rhs=SG[:], start=True, stop=True), reads=[rCST, rSG], writes=[rpc])
                        S.op("act", lambda e: e.activation(out=ECL[:], in_=pc[:], func=AF.Exp, scale=-CDEC), reads=[rpc], writes=[rECL])
                        S.op("act", lambda e: e.activation(out=ENCL[:], in_=pc[:], func=AF.Exp, scale=CDEC), reads=[rpc], writes=[rENCL])
                        S.op("dve", lambda e: e.tensor_tensor(out=TA[:], in0=pc[:], in1=SG[:], op=ALU.subtract), reads=[rpc, rSG], writes=[rTA])
                        S.op("act", lambda e: e.activation(out=ECLM[:], in_=TA[:], func=AF.Exp, scale=-CDEC), reads=[rTA], writes=[rECLM])
                        pwl, rpwl = pf()
                        S.op("pe", lambda e: [e.matmul(pwl[(h % 2) * 64:(h % 2) * 64 + 64, h // 2:h // 2 + 1], lhsT=SG[:, h * 64:(h + 1) * 64], rhs=ONES[:, 0:1], start=True, stop=True)
                                              for h in range(8)][-1], reads=[rSG, rCST], writes=[rpwl])
                        S.op("act", lambda e: e.activation(out=WL[:], in_=pwl[:, 0:4], func=AF.Exp, scale=-CDEC), reads=[rpwl], writes=[rWL])
                        S.op("dve", lambda e: e.tensor_tensor(out=KKN[:], in0=K[:], in1=KKV, op=ALU.mult), reads=[rK, rVEC], writes=[rKKN])
                        S.op("pool", lambda e: e.tensor_tensor(out=TB[:], in0=KKN[:], in1=KKN[:], op=ALU.mult), reads=[rKKN], writes=[rTB])
                        S.op("dve", lambda e: e.tensor_reduce(out=S8[:, 0:8], in_=v3(TB[:], 8), axis=AX.X, op=ALU.add), reads=[rTB], writes=[rS8])
                        S.op("act", lambda e: e.activation(out=S8[:, 8:16], in_=S8[:, 0:8], func=AF.Sqrt), reads=[rS8], writes=[rS8])
                        S.op("dve", lambda e: e.tensor_scalar(out=S8[:, 8:16], in0=S8[:, 8:16], scalar1=1e-12, scalar2=None, op0=ALU.max),
                             reads=[rS8], writes=[rS8])
                        S.op("dve", lambda e: e.reciprocal(out=S8[:, 16:24], in_=S8[:, 8:16]), reads=[rS8], writes=[rS8])
                        S.op("dve", lambda e: e.tensor_tensor(out=v3(KKN[:], 8), in0=v3(KKN[:], 8), in1=bc3(S8[:, 16:24], 8, 64), op=ALU.mult),
                             reads=[rKKN, rS8], writes=[rKKN])
                        S.op("dve", lambda e: e.scalar_tensor_tensor(out=TB[:], in0=AA[:], scalar=-1.0, in1=KAV, op0=ALU.add, op1=ALU.mult),
                             reads=[rAA, rVEC], writes=[rTB])
                        S.op("dve", lambda e: e.scalar_tensor_tensor(out=KM[:], in0=TB[:], scalar=1.0, in1=K[:], op0=ALU.add, op1=ALU.mult),
                             reads=[rTB, rK], writes=[rKM])
                        S.op("dve", lambda e: e.tensor_tensor(out=RBAR[:], in0=R[:], in1=ECL[:], op=ALU.mult), reads=[rR, rECL], writes=[rRBAR])
                        S.op("dve", lambda e: e.scalar_tensor_tensor(out=ABAR[:], in0=KKN[:], scalar=-1.0, in1=ECLM[:], op0=ALU.mult, op1=ALU.mult),
                             reads=[rKKN, rECLM], writes=[rABAR])
                        S.op("pool", lambda e: e.tensor_tensor(out=TA[:], in0=KKN[:], in1=AA[:], op=ALU.mult), reads=[rKKN, rAA], writes=[rTA])
                        S.op("pool", lambda e: e.tensor_tensor(out=BTIL[:], in0=TA[:], in1=ENCL[:], op=ALU.mult), reads=[rTA, rENCL], writes=[rBTIL])
                        S.op("dve", lambda e: e.tensor_tensor(out=KTIL[:], in0=KM[:], in1=ENCL[:], op=ALU.mult), reads=[rKM, rENCL], writes=[rKTIL])
                        S.op("act", lambda e: e.activation(out=VB[:], in_=V[:], func=AF.Copy), reads=[rV], writes=[rVB])
                        S.op("pool", lambda e: e.tensor_tensor(out=TB[:], in0=R[:], in1=KM[:], op=ALU.mult), reads=[rR, rKM], writes=[rTB])
                        S.op("pool", lambda e: e.tensor_tensor(out=TB[:], in0=TB[:], in1=RKV, op=ALU.mult), reads=[rTB, rVEC], writes=[rTB])
                        S.op("dve", lambda e: e.tensor_reduce(out=S8[:, 24:32], in_=v3(TB[:], 8), axis=AX.X, op=ALU.add), reads=[rTB], writes=[rS8])
                        S.op("dve", lambda e: e.tensor_tensor(out=v3(BON[:], 8), in0=v3(V[:], 8), in1=bc3(S8[:, 24:32], 8, 64), op=ALU.mult),
                             reads=[rV, rS8], writes=[rBON])
                        for src, rsrc, dst, rdst in ((RBAR, rRBAR, RBT, rRBT), (ABAR, rABAR, ABT, rABT),
                                                     (BTIL, rBTIL, BTT, rBTT), (KTIL, rKTIL, KTT, rKTT)):
                            pt, rpt = ptb()
                            S.op("pe", lambda e: [e.transpose(out=pt[:, b * 128:(b + 1) * 128], in_=src[:, b * 128:(b + 1) * 128],
                                                              identity=IDB) for b in range(4)][-1], reads=[rsrc, rCSB], writes=[rpt])
                            S.op("act", lambda e: e.activation(out=dst[:], in_=v3(pt[:, 0:512], 4), func=AF.Copy), reads=[rpt], writes=[rdst])

                        chk(4)

                        def hop(Tt, h):
                            return Tt[(h % 2) * 64:(h % 2) * 64 + 64, h // 2, :]

                        for g in range(2):
                            hs = [g, g + 2, g + 4, g + 6]
                            P0, rP0 = PQ[g][0]; Q0, rQ0 = PQ[g][1]

                            def mm4(p, A, Bm):
                                return lambda e: [e.matmul(p[:, j * 128:(j + 1) * 128], lhsT=hop(A, h), rhs=hop(Bm, h), start=True, stop=True)
                                                  for j, h in enumerate(hs) if (VAR != 3 or h % 2 == 0) and (VAR != 4 or h % 2 == 1)][-1]
                            for (A, rA, Bm, rB, dst, rdst, mask, eng) in (
                                    (ABT, rABT, BTT, rBTT, P0[:], rP0, ML, "dve"),
                                    (BTT, rBTT, ABT, rABT, Q0[:], rQ0, MU, "dve"),
                                    (ABT, rABT, KTT, rKTT, AAK[:, 4 * g:4 * g + 4, :], rAAK, ML, "dve"),
                                    (KTT, rKTT, RBT, rRBT, ARKT[:, 4 * g:4 * g + 4, :], rARKT, MUI, "dve"),
                                    (BTT, rBTT, RBT, rRBT, ARBT[:, 4 * g:4 * g + 4, :], rARBT, MUI, "dve")):
                                p, rp = pf()
                                S.op("pe", mm4(p, A, Bm), reads=[rA, rB], writes=[rp])
                                if VAR in (1, 3, 4):
                                    pass
                                elif VAR == 2:
                                    for j4 in range(4):
                                        S.op(eng, lambda e: e.tensor_tensor(out=dst[:, j4, :], in0=p[:, j4 * 128:(j4 + 1) * 128], in1=mask, op=ALU.mult),
                                             reads=[rp, rCST], writes=[rdst])
                                else:
                                    S.op(eng, lambda e: e.tensor_tensor(out=dst, in0=v3(p[:], 4), in1=bcm(mask, 4), op=ALU.mult),
                                         reads=[rp, rCST], writes=[rdst])
                            chk(41)
                            Tc, rTc = TT[g][0]
                            S.op("pool", lambda e: e.tensor_tensor(out=Tc[:], in0=Q0[:], in1=bcm(IDB, 4), op=ALU.add),
                                 reads=[rQ0, rCSB], writes=[rTc])
                            Pp, rPp, Qp, rQp = P0, rP0, Q0, rQ0
                            ti = 0
                            for lev in range(1, 7):
                                Pn, rPn = PQ[g][2 * (lev % 2)]
                                Qn, rQn = PQ[g][2 * (lev % 2) + 1]
                                p, rp = pf()
                                S.op("pe", lambda e: [e.matmul(p[:, j * 128:(j + 1) * 128], lhsT=Qp[:, j, :], rhs=Pp[:, j, :], start=True, stop=True)
                                                      for j in range(4)][-1], reads=[rPp, rQp], writes=[rp])
                                S.op("act", lambda e: e.activation(out=Pn[:], in_=v3(p[:], 4), func=AF.Copy), reads=[rp], writes=[rPn])
                                if lev < 6:
                                    p2, rp2 = pf()
                                    S.op("pe", lambda e: [e.matmul(p2[:, j * 128:(j + 1) * 128], lhsT=Pp[:, j, :], rhs=Qp[:, j, :], start=True, stop=True)
                                                          for j in range(4)][-1], reads=[rPp, rQp], writes=[rp2])
                                    S.op("act", lambda e: e.activation(out=Qn[:], in_=v3(p2[:], 4), func=AF.Copy), reads=[rp2], writes=[rQn])
                                Tn, rTn = TT[g][(ti + 1) % 2]
                                p3, rp3 = pf()
                                S.op("pe", lambda e: [e.matmul(p3[:, j * 128:(j + 1) * 128], lhsT=Pn[:, j, :], rhs=Tc[:, j, :], start=True, stop=True)
                                                      for j in range(4)][-1], reads=[rPn, rTc], writes=[rp3])
                                S.op("dve", lambda e: e.tensor_tensor(out=Tn[:], in0=v3(p3[:], 4), in1=Tc[:], op=ALU.add),
                                     reads=[rp3, rTc], writes=[rTn])
                                Tc, rTc = Tn, rTn
                                ti += 1
                                Pp, rPp, Qp, rQp = Pn, rPn, Qn, rQn
                            chk(42)
                            p, rp = pf()
                            S.op("pe", lambda e: [e.matmul(p[g * 64:g * 64 + 64, j * 128:(j + 1) * 128], lhsT=ABAR[:, h * 64:(h + 1) * 64], rhs=Tc[:, j, :],
                                                           start=True, stop=True) for j, h in enumerate(hs)][-1],
                                 reads=[rABAR, rTc], writes=[rp])
                            S.op("act", lambda e: e.activation(out=ABPT[g * 64:g * 64 + 64, :, :], in_=v3(p[g * 64:g * 64 + 64, :], 4), func=AF.Copy),
                                 reads=[rp], writes=[rABPT])
                            chk(43)
                            p, rp = pf()
                            S.op("pe", lambda e: [e.matmul(p[:, j * 128:(j + 1) * 128], lhsT=AAK[:, 4 * g + j, :], rhs=Tc[:, j, :],
                                                           start=True, stop=True) for j, h in enumerate(hs)][-1],
                                 reads=[rAAK, rTc], writes=[rp])
                            S.op("dve", lambda e: e.tensor_copy(out=AAKPT[:, 4 * g:4 * g + 4, :], in_=v3(p[:], 4)), reads=[rp], writes=[rAAKPT])

                        chk(5)
                        S.op("act", lambda e: e.activation(out=HB[:], in_=HFs[:], func=AF.Copy), reads=[rHFs], writes=[rHB])
                        pu, rpu = pf()

                        def fu(e):
                            last = None
                            for h in range(8):
                                e.matmul(pu[:, h * 64:(h + 1) * 64], lhsT=hop(ABPT, h), rhs=hop(HB, h), start=True, stop=False)
                                last = e.matmul(pu[:, h * 64:(h + 1) * 64], lhsT=AAKPT[:, (h % 2) * 4 + h // 2, :], rhs=VB[:, h * 64:(h + 1) * 64], start=False, stop=True)
                            return last
                        S.op("pe", fu, reads=[rABPT, rHB, rAAKPT, rVB], writes=[rpu])
                        S.op("act", lambda e: e.activation(out=UB[:], in_=pu[:], func=AF.Copy), reads=[rpu], writes=[rUB])
                        py, rpy = pf()

                        def fy(e):
                            last = None
                            for h in range(8):
                                o = py[:, h * 64:(h + 1) * 64]
                                e.matmul(o, lhsT=hop(RBT, h), rhs=hop(HB, h), start=True, stop=False)
                                e.matmul(o, lhsT=ARBT[:, (h % 2) * 4 + h // 2, :], rhs=UB[:, h * 64:(h + 1) * 64], start=False, stop=False)
                                last = e.matmul(o, lhsT=ARKT[:, (h % 2) * 4 + h // 2, :], rhs=VB[:, h * 64:(h + 1) * 64], start=False, stop=True)
                            return last
                        S.op("pe", fy, reads=[rRBT, rHB, rARBT, rUB, rARKT, rVB], writes=[rpy])
                        ph, rph = pf()

                        def fh(e):
                            last = None
                            for h in range(8):
                                o = ph[(h % 2) * 64:(h % 2) * 64 + 64, (h // 2) * 64:(h // 2 + 1) * 64]
                                e.matmul(o, lhsT=BTIL[:, h * 64:(h + 1) * 64], rhs=UB[:, h * 64:(h + 1) * 64], start=True, stop=False)
                                last = e.matmul(o, lhsT=KTIL[:, h * 64:(h + 1) * 64], rhs=VB[:, h * 64:(h + 1) * 64], start=False, stop=True)
                            return last
                        S.op("pe", fh, reads=[rBTIL, rKTIL, rUB, rVB], writes=[rph])
                        S.op("dve", lambda e: e.tensor_tensor(out=HFs[:], in0=v3(ph[:, 0:256], 4), in1=HFs[:], op=ALU.add),
                             reads=[rph, rHFs], writes=[rHFs])
                        S.op("dve", lambda e: e.tensor_tensor(out=HFs[:], in0=HFs[:], in1=bc3(WL[:], 4, 64), op=ALU.mult),
                             reads=[rHFs, rWL], writes=[rHFs])
                        S.op("act", lambda e: e.activation(out=YF[:], in_=py[:], func=AF.Copy), reads=[rpy], writes=[rYF])
                        dbg_out("y", YF[:], rYF, r0)
                        S.op("dve", lambda e: e.tensor_reduce(out=S8[:, 32:40], in_=v3(YF[:], 8), axis=AX.X, op=ALU.add), reads=[rYF], writes=[rS8])
                        S.op("dve", lambda e: e.tensor_scalar(out=S8[:, 32:40], in0=S8[:, 32:40], scalar1=1.0 / 64, scalar2=None, op0=ALU.mult),
                             reads=[rS8], writes=[rS8])
                        S.op("dve", lambda e: e.tensor_tensor(out=v3(YF[:], 8), in0=v3(YF[:], 8), in1=bc3(S8[:, 32:40], 8, 64), op=ALU.subtract),
                             reads=[rYF, rS8], writes=[rYF])
                        S.op("pool", lambda e: e.tensor_tensor(out=TB[:], in0=YF[:], in1=YF[:], op=ALU.mult), reads=[rYF], writes=[rTB])
                        S.op("dve", lambda e: e.tensor_reduce(out=S8[:, 40:48], in_=v3(TB[:], 8), axis=AX.X, op=ALU.add), reads=[rTB], writes=[rS8])
                        S.op("dve", lambda e: e.tensor_scalar(out=S8[:, 40:48], in0=S8[:, 40:48], scalar1=1.0 / 64, scalar2=64e-5,
                                                              op0=ALU.mult, op1=ALU.add), reads=[rS8], writes=[rS8])
                        S.op("act", lambda e: e.activation(out=S8[:, 40:48], in_=S8[:, 40:48], func=AF.Sqrt), reads=[rS8], writes=[rS8])
                        S.op("dve", lambda e: e.reciprocal(out=S8[:, 48:56], in_=S8[:, 40:48]), reads=[rS8], writes=[rS8])
                        S.op("dve", lambda e: e.tensor_tensor(out=v3(YF[:], 8), in0=v3(YF[:], 8), in1=bc3(S8[:, 48:56], 8, 64), op=ALU.mult),
                             reads=[rYF, rS8], writes=[rYF])
                        S.op("pool", lambda e: e.tensor_tensor(out=YF[:], in0=YF[:], in1=LWV, op=ALU.mult), reads=[rYF, rVEC], writes=[rYF])
                        S.op("pool", lambda e: e.tensor_tensor(out=YF[:], in0=YF[:], in1=LBV, op=ALU.add), reads=[rYF, rVEC], writes=[rYF])
                        S.op("pool", lambda e: e.tensor_tensor(out=YF[:], in0=YF[:], in1=BON[:], op=ALU.add), reads=[rYF, rBON], writes=[rYF])
                        pg, rpg = pf()
                        S.op("pe", lambda e: e.matmul(pg[:], lhsT=LT[:, 128:256], rhs=WG[:], start=True, stop=True), reads=[rLT, rWG], writes=[rpg])
                        if dbg:
                            S.op("dve", lambda e: e.tensor_tensor(out=DBG[:], in0=YF[:], in1=pg[:], op=ALU.mult), reads=[rYF, rpg], writes=[rDBG])
                            dbg_out("yrw", DBG[:], rDBG, r0)
                        S.op("dve", lambda e: e.tensor_tensor(out=MIX[:, 0:512], in0=YF[:], in1=pg[:], op=ALU.mult), reads=[rYF, rpg], writes=[rMIX])

                        chk(6)
                        pqk, rpqk = pf()
                        for b in range(4):
                            S.op("pe", proj_feat(pqk[:, b * 128:(b + 1) * 128], MLO - 0 + b * 128 if False else 0, False) if False else
                                 (lambda e, b=b: [e.matmul(pqk[:, b * 128:(b + 1) * 128], lhsT=WIN[:, k, MLO + b * 128:MLO + (b + 1) * 128], rhs=cur(k),
                                                          start=(k == 0), stop=(k == 7)) for k in range(8)][-1]),
                                 reads=[rHTc, rWIN], writes=[rpqk])
                        if c == 0:
                            S.op("pool", lambda e: e.memset(QKC[:, :, 0:3], 0.0), writes=[rQKC])
                        else:
                            S.op("pool", lambda e: e.tensor_copy(out=QKC[:, :, 0:3], in_=QKC[:, :, 128:131]), reads=[rQKC], writes=[rQKC])
                        S.op("act", lambda e: e.activation(out=QKC[:, :, 3:131], in_=v3(pqk[:], 4), func=AF.Copy), reads=[rpqk], writes=[rQKC])
                        for b in range(4):
                            eng = "dve"
                            S.op(eng, lambda e, b=b: e.tensor_scalar(out=ACC[:, b, :], in0=QKC[:, b, 0:128], scalar1=CW[:, b, 0:1], scalar2=CB[:, b:b + 1],
                                                                     op0=ALU.mult, op1=ALU.add), reads=[rQKC, rCW, rCB], writes=[rACC])
                            for j in range(1, 4):
                                S.op(eng, lambda e, b=b, j=j: e.scalar_tensor_tensor(out=ACC[:, b, :], in0=QKC[:, b, j:j + 128], scalar=CW[:, b, j:j + 1],
                                                                                    in1=ACC[:, b, :], op0=ALU.mult, op1=ALU.add),
                                     reads=[rQKC, rCW, rACC], writes=[rACC])
                        S.op("act", lambda e: e.activation(out=QKT[:], in_=ACC, func=AF.Silu), reads=[rACC], writes=[rQKT])
                        pv, rpv = pf()
                        S.op("pe", proj_tok(pv[:], MLO + 512 - 0, 512, False) if False else
                             (lambda e: [e.matmul(pv[:], lhsT=cur(k), rhs=WIN[:, k, MLO + 512:MLO + 1024], start=(k == 0), stop=(k == 7)) for k in range(8)][-1]),
                             reads=[rHTc, rWIN], writes=[rpv])
                        S.op("act", lambda e: e.activation(out=VE[:, :, 0:128], in_=v3(pv[:], 4), func=AF.Copy), reads=[rpv], writes=[rVE])
                        po, rpo = pf()
                        S.op("pe", lambda e: [e.matmul(po[:], lhsT=cur(k), rhs=WIN[:, k, MLO + 1024:MLO + 1536], start=(k == 0), stop=(k == 7)) for k in range(8)][-1],
                             reads=[rHTc, rWIN], writes=[rpo])
                        S.op("act", lambda e: e.activation(out=SO[:], in_=po[:], func=AF.Sigmoid), reads=[rpo], writes=[rSO])
                        pgt, rpgt = pf()
                        S.op("pe", lambda e: [e.matmul(pgt[:, 0:8], lhsT=cur(k), rhs=WIN[:, k, MLO + 1536:MLO + 1544], start=(k == 0), stop=(k == 7)) for k in range(8)][-1],
                             reads=[rHTc, rWIN], writes=[rpgt])
                        S.op("dve", lambda e: e.tensor_tensor(out=G8[:, 0:8], in0=pgt[:, 0:8], in1=GB[:], op=ALU.add), reads=[rpgt, rGB], writes=[rG8])
                        S.op("act", lambda e: e.activation(out=G8[:, 0:8], in_=G8[:, 0:8], func=AF.Tanh, scale=1.0 / 15), reads=[rG8], writes=[rG8])
                        S.op("act", lambda e: e.activation(out=G8[:, 8:12], in_=G8[:, 4:8], func=AF.Exp, scale=-15.0), reads=[rG8], writes=[rG8])
                        S.op("dve", lambda e: e.tensor_scalar(out=G8[:, 8:12], in0=G8[:, 8:12], scalar1=1.0, scalar2=None, op0=ALU.add), reads=[rG8], writes=[rG8])
                        S.op("act", lambda e: e.activation(out=G8[:, 12:16], in_=G8[:, 8:12], func=AF.Ln), reads=[rG8], writes=[rG8])
                        pb, rpb = pf()
                        S.op("pe", lambda e: e.matmul(pb[:, 0:4], lhsT=MUI, rhs=G8[:, 12:16], start=True, stop=True), reads=[rCST, rG8], writes=[rpb])
                        S.op("pe", lambda e: e.matmul(pb[:, 4:8], lhsT=ONES, rhs=G8[:, 12:16], start=True, stop=True), reads=[rCST, rG8], writes=[rpb])
                        S.op("dve", lambda e: e.scalar_tensor_tensor(out=G8[:, 16:20], in0=G8[:, 0:4], scalar=15.0, in1=pb[:, 0:4], op0=ALU.mult, op1=ALU.add),
                             reads=[rG8, rpb], writes=[rG8])
                        S.op("dve", lambda e: e.tensor_tensor(out=DG, in0=bcm(IDF, 4), in1=bc3(G8[:, 16:20], 4, 128), op=ALU.mult),
                             reads=[rCST, rG8], writes=[rDG])
                        pgm, rpgm = pf()
                        S.op("pe", lambda e: e.matmul(pgm[:], lhsT=ONES, rhs=TB[:], start=True, stop=True),
                             reads=[rCST, rDG], writes=[rpgm])
                        S.op("dve", lambda e: e.tensor_reduce(out=G8[:, 20:24], in_=v3(pgm[:], 4), axis=AX.X, op=ALU.max), reads=[rpgm], writes=[rG8])
                        S.op("dve", lambda e: e.tensor_tensor(out=G8[:, 20:24], in0=G8[:, 20:24], in1=Ms[:], op=ALU.max), reads=[rG8, rMs], writes=[rG8])
                        S.op("dve", lambda e: e.tensor_tensor(out=G8[:, 40:44], in0=G8[:, 16:20], in1=G8[:, 20:24], op=ALU.subtract), reads=[rG8], writes=[rG8])
                        S.op("act", lambda e: e.activation(out=G8[:, 24:28], in_=G8[:, 40:44], func=AF.Exp), reads=[rG8], writes=[rG8])
                        S.op("dve", lambda e: e.tensor_tensor(out=G8[:, 44:48], in0=Ms[:], in1=G8[:, 20:24], op=ALU.subtract), reads=[rG8, rMs], writes=[rG8])
                        S.op("act", lambda e: e.activation(out=G8[:, 28:32], in_=G8[:, 44:48], func=AF.Exp), reads=[rG8], writes=[rG8])
                        S.op("dve", lambda e: e.tensor_scalar(out=G8[:, 36:40], in0=G8[:, 28:32], scalar1=0.125, scalar2=None, op0=ALU.mult), reads=[rG8], writes=[rG8])
                        S.op("dve", lambda e: e.tensor_tensor(out=G8[:, 40:44], in0=pb[:, 0:4], in1=G8[:, 20:24], op=ALU.subtract), reads=[rpb, rG8], writes=[rG8])
                        S.op("act", lambda e: e.activation(out=G8[:, 32:36], in_=G8[:, 40:44], func=AF.Exp), reads=[rG8], writes=[rG8])
                        S.op("dve", lambda e: e.tensor_tensor(out=Ms[:], in0=G8[:, 20:24], in1=pb[:, 4:8], op=ALU.subtract), reads=[rG8, rpb], writes=[rMs])
                        pt, rpt = ptb()
                        S.op("pe", lambda e: [e.transpose(out=pt[:, b * 128:(b + 1) * 128], in_=QKT[:, 2 + b, :], identity=IDB) for b in range(2)][-1],
                             reads=[rQKT, rCSB], writes=[rpt])
                        S.op("dve", lambda e: e.tensor_tensor(out=KP[:], in0=v3(pt[:, 0:256], 4), in1=bc3(G8[:, 24:28], 4, 64), op=ALU.mult),
                             reads=[rpt, rG8], writes=[rKP])
                        psts = [pf(), pf()]
                        for par in range(2):
                            pst, rpst = psts[par]
                            S.op("pe", lambda e: [e.matmul(pst[:, (h // 2) * 128:(h // 2 + 1) * 128], lhsT=QKT[par * 64:par * 64 + 64, 2 + h // 2, :],
                                                           rhs=QKT[par * 64:par * 64 + 64, h // 2, :], start=True, stop=True) for h in (par, par + 2)][-1],
                                 reads=[rQKT], writes=[rpst])
                        for h in range(4):
                            pst, rpst = psts[h % 2]
                            S.op("dve", lambda e, h=h: e.scalar_tensor_tensor(out=PTB[:, h, :], in0=pst[:, (h // 2) * 128:(h // 2 + 1) * 128], scalar=G8[:, 24 + h:25 + h],
                                                                             in1=MUI8[:], op0=ALU.mult, op1=ALU.mult), reads=[rpst, rG8, rMUI8], writes=[rPTB])
                        for h in range(4):
                            po_ = (h % 2) * 64
                            S.op("pool", lambda e, h=h: e.tensor_scalar(out=CBF[po_:po_ + 64, h // 2, :], in0=CFs[po_:po_ + 64, h // 2, :],
                                                                        scalar1=G8[po_:po_ + 64, 36 + h:37 + h], scalar2=None, op0=ALU.mult),
                                 reads=[rCFs, rG8], writes=[rCBF])
                        pn = [pf(), pf()]
                        for i2 in range(2):
                            pnn, rpnn = pn[i2]

                            def fn(e, i2=i2, pnn=pnn):
                                last = None
                                for j in range(2):
                                    h = 2 * i2 + j
                                    o = pnn[:, j * 129:(j + 1) * 129]
                                    e.matmul(o, lhsT=PTB[:, h, :], rhs=VE[:, h, :], start=True, stop=False)
                                    last = e.matmul(o, lhsT=QKT[(h % 2) * 64:(h % 2) * 64 + 64, h // 2, :], rhs=CBF[(h % 2) * 64:(h % 2) * 64 + 64, h // 2, :], start=False, stop=True)
                                return last
                            S.op("pe", fn, reads=[rPTB, rVE, rQKT, rCBF], writes=[rpnn])
                        pcc, rpcc = pf()
                        S.op("pe", lambda e: [e.matmul(pcc[(h % 2) * 64:(h % 2) * 64 + 64, (h // 2) * 129:(h // 2 + 1) * 129], lhsT=KP[:, h, :], rhs=VE[:, h, :],
                                                       start=True, stop=True) for h in range(4)][-1], reads=[rKP, rVE], writes=[rpcc])
                        for h in range(4):
                            po_ = (h % 2) * 64
                            S.op("dve", lambda e, h=h: e.scalar_tensor_tensor(out=CFs[po_:po_ + 64, h // 2, :], in0=CFs[po_:po_ + 64, h // 2, :],
                                                                             scalar=G8[po_:po_ + 64, 28 + h:29 + h],
                                                                             in1=pcc[po_:po_ + 64, (h // 2) * 129:(h // 2 + 1) * 129], op0=ALU.mult, op1=ALU.add),
                                 reads=[rCFs, rG8, rpcc], writes=[rCFs])
                        for i2 in range(2):
                            pnn, rpnn = pn[i2]
                            S.op("dve", lambda e, i2=i2, pnn=pnn: e.tensor_copy(out=S8[:, 56 + 2 * i2:58 + 2 * i2],
                                                                              in_=pnn[:, 0:258].rearrange("p (a b) -> p a b", a=2)[:, :, 128:129].rearrange("p a b -> p (a b)")),
                                 reads=[rpnn], writes=[rS8])
                        S.op("dve", lambda e: e.tensor_scalar(out=G8[:, 40:44], in0=S8[:, 56:60], scalar1=-1.0, scalar2=None, op0=ALU.mult), reads=[rS8], writes=[rG8])
                        S.op("dve", lambda e: e.tensor_tensor(out=S8[:, 56:60], in0=S8[:, 56:60], in1=G8[:, 40:44], op=ALU.max), reads=[rS8, rG8], writes=[rS8])
                        S.op("dve", lambda e: e.tensor_tensor(out=S8[:, 56:60], in0=S8[:, 56:60], in1=G8[:, 32:36], op=ALU.max), reads=[rS8, rG8], writes=[rS8])
                        S.op("dve", lambda e: e.reciprocal(out=S8[:, 60:64], in_=S8[:, 56:60]), reads=[rS8], writes=[rS8])
                        for i2 in range(2):
                            pnn, rpnn = pn[i2]
                            S.op("dve", lambda e, i2=i2, pnn=pnn: e.tensor_tensor(out=HM[:, 2 * i2:2 * i2 + 2, :],
                                                                                in0=pnn[:, 0:258].rearrange("p (a b) -> p a b", a=2)[:, :, 0:128],
                                                                                in1=bc3(S8[:, 60 + 2 * i2:62 + 2 * i2], 2, 128), op=ALU.mult),
                                 reads=[rpnn, rS8], writes=[rHM])
                        HM2 = KKN[:]
                        dbg_out("hm", HM2, rHM, r0)
                        S.op("pool", lambda e: e.tensor_tensor(out=TB[:], in0=HM2, in1=HM2, op=ALU.mult), reads=[rHM], writes=[rTB])
                        S.op("dve", lambda e: e.tensor_reduce(out=G8[:, 40:44], in_=v3(TB[:], 4), axis=AX.X, op=ALU.add), reads=[rTB], writes=[rG8])
                        S.op("dve", lambda e: e.tensor_scalar(out=G8[:, 40:44], in0=G8[:, 40:44], scalar1=1.0 / 128, scalar2=1e-6, op0=ALU.mult, op1=ALU.add),
                             reads=[rG8], writes=[rG8])
                        S.op("act", lambda e: e.activation(out=G8[:, 40:44], in_=G8[:, 40:44], func=AF.Sqrt), reads=[rG8], writes=[rG8])
                        S.op("dve", lambda e: e.reciprocal(out=G8[:, 44:48], in_=G8[:, 40:44]), reads=[rG8], writes=[rG8])
                        S.op("dve", lambda e: e.tensor_tensor(out=HM, in0=HM, in1=bc3(G8[:, 44:48], 4, 128), op=ALU.mult), reads=[rHM, rG8], writes=[rHM])
                        S.op("pool", lambda e: e.tensor_tensor(out=TB[:], in0=HM2, in1=MHG, op=ALU.mult), reads=[rHM, rVEC], writes=[rTB])
                        if dbg:
                            S.op("dve", lambda e: e.tensor_tensor(out=DBG[:], in0=TB[:], in1=SO[:], op=ALU.mult), reads=[rTB, rSO], writes=[rDBG])
                            dbg_out("yml", DBG[:], rDBG, r0)
                        S.op("pool", lambda e: e.tensor_tensor(out=MIX[:, 512:1024], in0=TB[:], in1=SO[:], op=ALU.mult), reads=[rTB, rSO], writes=[rMIX])

                        chk(7)
                        pt, rpt = ptb()
                        S.op("pe", lambda e: [e.transpose(out=pt[:, k * 128:(k + 1) * 128], in_=MIX[:, k * 128:(k + 1) * 128], identity=IDB) for k in range(8)][-1],
                             reads=[rMIX, rCSB], writes=[rpt])
                        S.op("act", lambda e: e.activation(out=MIXT[:], in_=v3(pt[:], 8), func=AF.Copy), reads=[rpt], writes=[rMIXT])
                        for g in range(2):
                            p, rp = pf()
                            S.op("pe", lambda e, g=g, p=p: [e.matmul(p[:], lhsT=MIXT[:, k, :], rhs=WOUT[:, k, g * 512:(g + 1) * 512], start=(k == 0), stop=(k == 7))
                                                          for k in range(8)][-1], reads=[rMIXT, rWOUT], writes=[rp])
                            S.op("dve", lambda e, g=g, p=p: e.tensor_tensor(out=Xc[:, g * 512:(g + 1) * 512], in0=p[:], in1=Xc[:, g * 512:(g + 1) * 512], op=ALU.add),
                                 reads=[rp, rXc], writes=[rXc])
                        S.dma("sp", x1_d[r0:r0 + 128, :], Xc[:], reads=[rXc])
                        chk(8)
                S.barrier()
            S.barrier()

        es2 = ExitStack()
        with es2:
            sb2, _ = mk(es2)
            TOKS = 512 if T % 512 == 0 else 256
            NSUB = TOKS // 128
            WUP, rWUP = sb2("WUP", [128, 8, 2 * DFF], BF16)
            WDN, rWDN = sb2("WDN", [128, NB, D], BF16)
            FCW, rFCW = sb2("FCW", [128, NB, 3])
            FCB, rFCB = sb2("FCB", [128, NB])
            GF, rGF = sb2("GF", [128, D])
            S.dma("sp", FCW[:].rearrange("p b j -> p (b j)"), ffn_conv_w, writes=[rFCW])
            S.dma("sp", FCB[:], ffn_conv_b, writes=[rFCB])
            S.dma("sp", GF[:], norm_f_g.partition_broadcast(128), writes=[rGF])
            esC = ExitStack()
            with esC:
                sbC, _ = mk(esC)
                G2, rG2 = sbC("G2", [128, 8])
                STG2 = [sbC(f"STH{i}", [128, DFF]) for i in range(2)]
                S.dma("sp", G2[:], norm2_g, writes=[rG2])
                i = 0
                for k in range(8):
                    for hlf in range(2):
                        st, rst = STG2[i % 2]; i += 1
                        S.dma("sp", st[:], w_ffn_up[k * 128:(k + 1) * 128, hlf * DFF:(hlf + 1) * DFF], writes=[rst])
                        eng = "act" if hlf == 0 else "dve"
                        if eng == "act":
                            S.op("act", lambda e: e.activation(out=WUP[:, k, hlf * DFF:(hlf + 1) * DFF], in_=st[:], func=AF.Copy, scale=G2[:, k:k + 1]),
                                 reads=[rst, rG2], writes=[rWUP])
                        else:
                            S.op("dve", lambda e: e.tensor_scalar(out=WUP[:, k, hlf * DFF:(hlf + 1) * DFF], in0=st[:], scalar1=G2[:, k:k + 1], scalar2=None, op0=ALU.mult),
                                 reads=[rst, rG2], writes=[rWUP])
                for b in range(0, NB, 2):
                    st, rst = STG2[i % 2]; i += 1
                    S.dma("sp", st[:, 0:2 * D].rearrange("p (a n) -> p a n", a=2), w_ffn_down[b * 128:(b + 2) * 128, :].rearrange("(a p) n -> p a n", p=128),
                          writes=[rst])
                    S.op("pool" if (b // 2) % 2 else "dve", lambda e: e.tensor_copy(out=WDN[:, b:b + 2, :], in_=st[:, 0:2 * D].rearrange("p (a n) -> p a n", a=2)),
                         reads=[rst], writes=[rWDN])
                S.barrier()
            chk(9)
            esD = ExitStack()
            with esD:
                sbD, _ = mk(esD)
                XS = [sbD(f"XS{i}", [128, D]) for i in range(NSUB)]
                HN2, rHN2 = sbD("HN2", [128, D], BF16)
                H2T, rH2T = sbD("H2T", [128, 8, TOKS], BF16)
                GT, rGT = sbD("GT", [128, NB, TOKS], BF16)
                AC = [sbD(f"AC{i}", [128, TOKS + 2]) for i in range(2)]
                AQ = [sbD(f"AQ{i}", [128, TOKS]) for i in range(2)]
                CAR, rCAR = sbD("CAR", [128, NB, 2])
                ST2, rST2 = sbD("ST2", [128, 8])
                it = 0
                for s in range(NSEQ):
                    S.op("pool", lambda e: e.memset(CAR[:], 0.0), writes=[rCAR])
                    for c in range(T // TOKS):
                        r0 = s * T + c * TOKS
                        for sub in range(NSUB):
                            Xs, rXs = XS[sub]
                            S.dma("sp", Xs[:], x1_d[r0 + sub * 128:r0 + (sub + 1) * 128, :], writes=[rXs])
                            rmsnorm_to_bf16(Xs[:], rXs, HN2, rHN2, ST2, rST2)
                            pt, rpt = ptb()
                            S.op("pe", lambda e: [e.transpose(out=pt[:, k * 128:(k + 1) * 128], in_=HN2[:, k * 128:(k + 1) * 128], identity=IDB) for k in range(8)][-1],
                                 reads=[rHN2, rCSB], writes=[rpt])
                            S.op("dve", lambda e, sub=sub: e.tensor_copy(out=H2T[:, :, sub * 128:(sub + 1) * 128], in_=v3(pt[:], 8)), reads=[rpt], writes=[rH2T])
                        for b in range(NB):
                            ACb, rACb = AC[b % 2]
                            AQb, rAQb = AQ[b % 2]
                            pa, rpa = pf()
                            pbk, rpbk = pf()
                            S.op("pe", lambda e, b=b, pa=pa: [e.matmul(pa[:, 0:TOKS], lhsT=WUP[:, k, b * 128:(b + 1) * 128], rhs=H2T[:, k, :], start=(k == 0), stop=(k == 7))
                                                             for k in range(8)][-1], reads=[rWUP, rH2T], writes=[rpa])
                            S.op("pe", lambda e, b=b, pbk=pbk: [e.matmul(pbk[:, 0:TOKS], lhsT=WUP[:, k, DFF + b * 128:DFF + (b + 1) * 128], rhs=H2T[:, k, :], start=(k == 0), stop=(k == 7))
                                                               for k in range(8)][-1], reads=[rWUP, rH2T], writes=[rpbk])
                            S.op("pool", lambda e, b=b: e.tensor_copy(out=ACb[:, 0:2], in_=CAR[:, b, :]), reads=[rCAR], writes=[rACb])
                            S.op("act", lambda e: e.activation(out=ACb[:, 2:TOKS + 2], in_=pa[:, 0:TOKS], func=AF.Copy), reads=[rpa], writes=[rACb])
                            S.op("pool", lambda e, b=b: e.tensor_copy(out=CAR[:, b, :], in_=ACb[:, TOKS:TOKS + 2]), reads=[rACb], writes=[rCAR])
                            S.op("dve", lambda e, b=b: e.tensor_scalar(out=AQb[:], in0=ACb[:, 0:TOKS], scalar1=FCW[:, b, 0:1], scalar2=FCB[:, b:b + 1], op0=ALU.mult, op1=ALU.add),
                                 reads=[rACb, rFCW, rFCB], writes=[rAQb])
                            S.op("dve", lambda e, b=b: e.scalar_tensor_tensor(out=AQb[:], in0=ACb[:, 1:TOKS + 1], scalar=FCW[:, b, 1:2], in1=AQb[:], op0=ALU.mult, op1=ALU.add),
                                 reads=[rACb, rFCW, rAQb], writes=[rAQb])
                            S.op("dve", lambda e, b=b: e.scalar_tensor_tensor(out=AQb[:], in0=ACb[:, 2:TOKS + 2], scalar=FCW[:, b, 2:3], in1=AQb[:], op0=ALU.mult, op1=ALU.add),
                                 reads=[rACb, rFCW, rAQb], writes=[rAQb])
                            S.op("act", lambda e: e.activation(out=AQb[:], in_=AQb[:], func=AF.Silu), reads=[rAQb], writes=[rAQb])
                            S.op("dve", lambda e, b=b: e.tensor_tensor(out=GT[:, b, :], in0=AQb[:], in1=pbk[:, 0:TOKS], op=ALU.mult), reads=[rAQb, rpbk], writes=[rGT])
                        for sub in range(NSUB):
                            Xs, rXs = XS[sub]
                            X2c, rX2c = Xs, rXs
                            for g in range(2):
                                p, rp = pf()
                                S.op("pe", lambda e, g=g, p=p, sub=sub: [e.matmul(p[:], lhsT=GT[:, b, sub * 128:(sub + 1) * 128], rhs=WDN[:, b, g * 512:(g + 1) * 512],
                                                                                 start=(b == 0), stop=(b == NB - 1)) for b in range(NB)][-1], reads=[rGT, rWDN], writes=[rp])
                                S.op("dve", lambda e, g=g, p=p: e.tensor_tensor(out=X2c[:, g * 512:(g + 1) * 512], in0=p[:], in1=Xs[:, g * 512:(g + 1) * 512], op=ALU.add),
                                     reads=[rp, rXs], writes=[rX2c])
                            S.op("pool", lambda e: e.memset(ST2[:, 4:5], 0.0), writes=[rST2])
                            S.op("act", lambda e: e.activation(out=HN2[:], in_=X2c[:], func=AF.Square, accum_out=ST2[:, 4:5]), reads=[rX2c, rST2], writes=[rHN2, rST2])
                            S.op("dve", lambda e: e.tensor_scalar(out=ST2[:, 5:6], in0=ST2[:, 4:5], scalar1=1.0 / D, scalar2=1e-6, op0=ALU.mult, op1=ALU.add),
                                 reads=[rST2], writes=[rST2])
                            S.op("act", lambda e: e.activation(out=ST2[:, 6:7], in_=ST2[:, 5:6], func=AF.Sqrt), reads=[rST2], writes=[rST2])
                            S.op("dve", lambda e: e.reciprocal(out=ST2[:, 7:8], in_=ST2[:, 6:7]), reads=[rST2], writes=[rST2])
                            S.op("act", lambda e: e.activation(out=X2c[:], in_=X2c[:], func=AF.Copy, scale=ST2[:, 7:8]), reads=[rX2c, rST2], writes=[rX2c])
                            S.op("pool", lambda e: e.tensor_tensor(out=X2c[:], in0=X2c[:], in1=GF[:], op=ALU.mult), reads=[rX2c, rGF], writes=[rX2c])
                            S.dma("sp", out_d[r0 + sub * 128:r0 + (sub + 1) * 128, :], X2c[:], reads=[rX2c])
                S.barrier()
            S.barrier()
    return nc


def make_consts():
    c = np.zeros((128, 640), np.float32)
    i = np.arange(128)
    c[:, 0:128] = np.eye(128)
    c[:, 128:256] = (i[:, None] < i[None, :])
    c[:, 256:384] = (i[:, None] <= i[None, :])
    c[:, 384:512] = (i[:, None] > i[None, :])
    c[:, 512:640] = 1.0
    return c


def make_in_maps(inputs, n_cores, nseq):
    f = lambda a: np.ascontiguousarray(np.asarray(a, np.float32))
    x = f(inputs["x"])
    T = x.shape[1]
    shared = {}
    for k, v in inputs.items():
        if k == "x":
            continue
        a = f(v)
        if k != "norm_f_g":
            a = a[0]
        if k == "r_k":
            a = a.reshape(512)
        elif k in ("norm1_g", "norm2_g", "qk_conv_b", "ffn_conv_b"):
            a = a.reshape(-1, 128).T
        elif k in ("qk_conv_w", "ffn_conv_w"):
            j = a.shape[0]
            a = a.reshape(j, -1, 128).transpose(2, 1, 0).reshape(128, -1)
        shared[k] = np.ascontiguousarray(a)
    shared["consts"] = make_consts()
    maps = []
    for c in range(n_cores):
        m = dict(shared)
        m["x"] = np.ascontiguousarray(x[c * nseq:(c + 1) * nseq].reshape(nseq * T, D))
        maps.append(m)
    return maps


def kernel(**inputs):
    x = np.asarray(inputs["x"])
    B, T, _ = x.shape
    nseq = B // N_CORES
    nc = build(nseq, T)
    maps = make_in_maps(inputs, N_CORES, nseq)
    res = run_bass_kernel_spmd(nc, maps, core_ids=list(range(N_CORES)))
    out = np.concatenate([r["out"].reshape(nseq, T, D) for r in res.results], axis=0)
    return out.astype(np.float32)
```

```python
import numpy as np
from contextlib import ExitStack
import concourse.bass as bass
import concourse.mybir as mybir
from concourse.bass_utils import run_bass_kernel_spmd

F32 = mybir.dt.float32
BF16 = mybir.dt.bfloat16
ALU = mybir.AluOpType
AF = mybir.ActivationFunctionType
AX = mybir.AxisListType

N_CORES = 8
D = 1024
N_IN = 3336
RWC = 1792
MLO = 2 * RWC
WIN_COLS = 2 * RWC + 1544
DFF = 2816
NB = DFF // 128
CDEC = float(np.exp(-0.5))


class Res:
    __slots__ = ("name", "lw", "rd")

    def __init__(self, name):
        self.name = name
        self.lw = None
        self.rd = []


class Sched:
    EPOCH = 30000
    NDMA = 24

    def __init__(self, nc, es):
        self.nc = nc
        self.es = es
        self.engs = {"pe": nc.tensor, "act": nc.scalar, "dve": nc.vector,
                     "pool": nc.gpsimd, "sp": nc.sync}
        self.sems = {e: [] for e in self.engs}
        self.cnt = {e: 0 for e in self.engs}
        self.waited = {e: {} for e in self.engs}
        self.dma_sems = [es.enter_context(nc.semaphore(f"dq{i}")) for i in range(self.NDMA)]
        self.dma_cnt = [0] * self.NDMA
        self.dma_i = 0
        for e in self.engs:
            self._new_epoch(e)

    def _new_epoch(self, e):
        s = self.es.enter_context(self.nc.semaphore(f"s_{e}_{len(self.sems[e])}"))
        self.sems[e].append(s)
        self.cnt[e] = 0

    def _wait(self, e, dep):
        if dep[0] == "dma":
            _, idx, val = dep
            key = ("dma", idx)
            sem = self.dma_sems[idx]
        else:
            de, ep, val = dep
            key = (de, ep)
            sem = self.sems[de][ep]
        if self.waited[e].get(key, 0) >= val:
            return
        self.engs[e].wait_ge(sem, val)
        self.waited[e][key] = val

    def _deps(self, e, reads, writes):
        deps = []
        for r in reads:
            if r.lw is not None and not (r.lw[0] == e and e == "pe"):
                deps.append(r.lw)
        for w in writes:
            if w.lw is not None and w.lw[0] != e:
                deps.append(w.lw)
            for d in w.rd:
                if d[0] != e:
                    deps.append(d)
        return deps

    def _mark(self, tag, reads, writes):
        for r in reads:
            r.rd.append(tag)
            if len(r.rd) > 64:
                r.rd = r.rd[-48:]
        for w in writes:
            w.lw = tag
            w.rd = []

    def op(self, e, fn, reads=(), writes=()):
        for d in self._deps(e, reads, writes):
            self._wait(e, d)
        if self.cnt[e] >= self.EPOCH:
            self._new_epoch(e)
        ins = fn(self.engs[e])
        ep = len(self.sems[e]) - 1
        ins.then_inc(self.sems[e][ep], 1)
        self.cnt[e] += 1
        tag = (e, ep, self.cnt[e])
        self._mark(tag, reads, writes)
        return tag

    def dma(self, q, out, in_, reads=(), writes=(), slow=False):
        for d in self._deps(q, reads, writes):
            self._wait(q, d)
        idx = self.dma_i
        self.dma_i = (self.dma_i + 1) % self.NDMA
        kw = {"allow_slow_non_contiguous": True} if slow else {}
        self.engs[q].dma_start(out=out, in_=in_, **kw).then_inc(self.dma_sems[idx], 16)
        self.dma_cnt[idx] += 16
        tag = ("dma", idx, self.dma_cnt[idx])
        self._mark(tag, reads, writes)
        return tag

    def barrier(self):
        for e in self.engs:
            for d in self.engs:
                if d != e:
                    ep = len(self.sems[d]) - 1
                    if self.cnt[d] > 0:
                        self._wait(e, (d, ep, self.cnt[d]))
                    elif ep > 0:
                        self._wait(e, (d, ep - 1, self.EPOCH))
            for i in range(self.NDMA):
                if self.dma_cnt[i]:
                    self._wait(e, ("dma", i, self.dma_cnt[i]))


import os
VAR = int(os.environ.get('KVAR', '0'))


class _Stop(Exception):
    pass


def build(NSEQ, T, dbg=False, stop=0):
    nc = bass.Bass("TRN2", target_bir_lowering=False)
    try:
        _build(nc, NSEQ, T, dbg, stop)
    except _Stop:
        pass
    return nc


def _build(nc, NSEQ, T, dbg, stop):
    NT = T // 128
    NTOK = NSEQ * T
    di = lambda n, s: nc.dram_tensor(n, s, F32, kind="ExternalInput").ap()
    x_d = di("x", [NTOK, D])
    norm1_g = di("norm1_g", [128, 8]); w_in = di("w_in", [D, N_IN]); rw_mu = di("rw_mu", [RWC])
    w0 = di("w0", [512]); w_up_decay = di("w_up_decay", [64, 512]); a0 = di("a0", [512])
    w_up_a = di("w_up_a", [64, 512]); w_up_g = di("w_up_g", [128, 512]); k_k = di("k_k", [512])
    k_a = di("k_a", [512]); r_k = di("r_k", [512]); lnx_w = di("lnx_w", [512]); lnx_b = di("lnx_b", [512])
    qk_conv_w = di("qk_conv_w", [128, 16]); qk_conv_b = di("qk_conv_b", [128, 4])
    i_bias = di("i_bias", [4]); f_bias = di("f_bias", [4]); mh_norm_g = di("mh_norm_g", [512])
    w_out = di("w_out", [D, D]); norm2_g = di("norm2_g", [128, 8]); w_ffn_up = di("w_ffn_up", [D, 2 * DFF])
    ffn_conv_w = di("ffn_conv_w", [128, NB * 3]); ffn_conv_b = di("ffn_conv_b", [128, NB])
    w_ffn_down = di("w_ffn_down", [DFF, D]); norm_f_g = di("norm_f_g", [D])
    consts = di("consts", [128, 640])
    out_d = nc.dram_tensor("out", [NTOK, D], F32, kind="ExternalOutput").ap()
    x1_d = nc.dram_tensor("x1s", [NTOK, D], F32, kind="Internal").ap()
    dbg_d = {}
    if dbg:
        for nm in ("yrw", "yml", "y", "hm"):
            dbg_d[nm] = nc.dram_tensor("d_" + nm, [NTOK, 512], F32, kind="ExternalOutput").ap()

    es0 = ExitStack()
    with es0:
        S = Sched(nc, es0)

        def chk(n):
            if stop == n:
                S.barrier()
                raise _Stop()

        def mk(es):
            def sb(n, s, d=F32):
                return es.enter_context(nc.sbuf_tensor(n, s, d)), Res(n)

            def ps(n, s, d=F32):
                return es.enter_context(nc.psum_tensor(n, s, d)), Res(n)
            return sb, ps

        sb0, ps0 = mk(es0)
        CST, rCST = sb0("CST", [128, 640])
        S.dma("sp", CST[:], consts, writes=[rCST])
        IDF = CST[:, 0:128]; MU = CST[:, 128:256]; MUI = CST[:, 256:384]; ML = CST[:, 384:512]; ONES = CST[:, 512:640]
        CSB, rCSB = sb0("CSB", [128, 640], BF16)
        S.op("dve", lambda e: e.tensor_copy(out=CSB[:], in_=CST[:]), reads=[rCST], writes=[rCSB])
        IDB = CSB[:, 0:128]
        MUI8, rMUI8 = sb0("MUI8", [128, 128])
        S.op("dve", lambda e: e.tensor_scalar(out=MUI8[:], in0=MUI, scalar1=0.125, scalar2=None, op0=ALU.mult),
             reads=[rCST], writes=[rMUI8])
        NPF = 6
        PF = [ps0(f"PF{i}", [128, 512]) for i in range(NPF)]
        PT = [ps0(f"PT{i}", [128, 1024], BF16) for i in range(2)]
        pf_i = [0]
        pt_i = [0]

        POOLS = {"all": [0, 1, 2, 3, 4, 5], "g0": [0, 1], "g1": [2, 3], "m": [4, 5], "r": [0, 1, 2, 3]}
        pool_i = {k: 0 for k in POOLS}

        def pf(pool="all"):
            lst = POOLS[pool]
            p = PF[lst[pool_i[pool] % len(lst)]]
            pool_i[pool] += 1
            return p

        def interleave(*gens):
            gens = list(gens)
            while gens:
                for gg in list(gens):
                    try:
                        next(gg)
                        yield
                    except StopIteration:
                        gens.remove(gg)

        def ptb():
            return PT[0]

        def bc3(ap2, a, b):
            return ap2.rearrange("p (a o) -> p a o", o=1).to_broadcast([ap2.shape[0], a, b])

        def bcm(ap2, a):
            return ap2.rearrange("p (o n) -> p o n", o=1).to_broadcast([ap2.shape[0], a, ap2.shape[1]])

        def v3(ap, a):
            return ap.rearrange("p (a b) -> p a b", a=a)

        def rmsnorm_to_bf16(X, rX, HN, rHN, ST, rST):
            S.op("pool", lambda e: e.memset(ST[:, 0:1], 0.0), writes=[rST])
            S.op("act", lambda e: e.activation(out=HN[:], in_=X, func=AF.Square, accum_out=ST[:, 0:1]),
                 reads=[rX, rST], writes=[rHN, rST])
            S.op("dve", lambda e: e.tensor_scalar(out=ST[:, 1:2], in0=ST[:, 0:1], scalar1=1.0 / D, scalar2=1e-6,
                                                  op0=ALU.mult, op1=ALU.add), reads=[rST], writes=[rST])
            S.op("act", lambda e: e.activation(out=ST[:, 2:3], in_=ST[:, 1:2], func=AF.Sqrt), reads=[rST], writes=[rST])
            S.op("dve", lambda e: e.reciprocal(out=ST[:, 3:4], in_=ST[:, 2:3]), reads=[rST], writes=[rST])
            S.op("act", lambda e: e.activation(out=HN[:], in_=X, func=AF.Copy, scale=ST[:, 3:4]),
                 reads=[rX, rST], writes=[rHN])

        es1 = ExitStack()
        with es1:
            sb1, _ = mk(es1)
            WIN, rWIN = sb1("WIN", [128, 8, WIN_COLS], BF16)
            WOUT, rWOUT = sb1("WOUT", [128, 8, D], BF16)
            WLOR, rWLOR = sb1("WLOR", [128, 512], BF16)
            WG, rWG = sb1("WG", [128, 512], BF16)
            VEC, rVEC = sb1("VEC", [128, 8, 512])
            CW, rCW = sb1("CW", [128, 4, 4])
            CB, rCB = sb1("CB", [128, 4])
            GB, rGB = sb1("GB", [128, 8])
            for i, v in enumerate((w0, a0, k_k, k_a, r_k, lnx_w, lnx_b, mh_norm_g)):
                S.dma("sp", VEC[:, i, :], v.partition_broadcast(128), writes=[rVEC])
            S.dma("sp", CW[:].rearrange("p b j -> p (b j)"), qk_conv_w, writes=[rCW])
            S.dma("sp", CB[:], qk_conv_b, writes=[rCB])
            S.dma("sp", GB[:, 0:4], i_bias.partition_broadcast(128), writes=[rGB])
            S.dma("sp", GB[:, 4:8], f_bias.partition_broadcast(128), writes=[rGB])
            W0V = VEC[:, 0, :]; A0V = VEC[:, 1, :]; KKV = VEC[:, 2, :]; KAV = VEC[:, 3, :]
            RKV = VEC[:, 4, :]; LWV = VEC[:, 5, :]; LBV = VEC[:, 6, :]; MHG = VEC[:, 7, :]
            esA = ExitStack()
            with esA:
                sbA, _ = mk(esA)
                MUT, rMUT = sbA("MUT", [128, RWC])
                OMM, rOMM = sbA("OMM", [128, RWC])
                G1, rG1 = sbA("G1", [128, 8])
                STG = [sbA(f"STG{i}", [128, N_IN]) for i in range(2)]
                S.dma("sp", MUT[:], rw_mu.partition_broadcast(128), writes=[rMUT])
                S.dma("sp", G1[:], norm1_g, writes=[rG1])
                S.op("dve", lambda e: e.tensor_scalar(out=OMM[:], in0=MUT[:], scalar1=-1.0, scalar2=1.0,
                                                      op0=ALU.mult, op1=ALU.add), reads=[rMUT], writes=[rOMM])
                for k in range(8):
                    st, rst = STG[k % 2]
                    S.dma("sp", st[:], w_in[k * 128:(k + 1) * 128, :], writes=[rst])
                    S.op("act", lambda e: e.activation(out=st[:], in_=st[:], func=AF.Copy, scale=G1[:, k:k + 1]),
                         reads=[rst, rG1], writes=[rst])
                    S.op("dve", lambda e: e.tensor_tensor(out=WIN[:, k, 0:RWC], in0=st[:, 0:RWC], in1=OMM[:], op=ALU.mult),
                         reads=[rst, rOMM], writes=[rWIN])
                    S.op("pool", lambda e: e.tensor_tensor(out=WIN[:, k, RWC:2 * RWC], in0=st[:, 0:RWC], in1=MUT[:], op=ALU.mult),
                         reads=[rst, rMUT], writes=[rWIN])
                    S.op("act", lambda e: e.activation(out=WIN[:, k, MLO:WIN_COLS], in_=st[:, RWC:N_IN], func=AF.Copy),
                         reads=[rst], writes=[rWIN])
                for k in range(8):
                    st, rst = STG[k % 2]
                    S.dma("sp", st[:, 0:D], w_out[k * 128:(k + 1) * 128, :], writes=[rst])
                    S.op("dve", lambda e: e.tensor_copy(out=WOUT[:, k, :], in_=st[:, 0:D]), reads=[rst], writes=[rWOUT])
                st, rst = STG[0]
                S.dma("sp", st[0:64, 0:512], w_up_decay, writes=[rst])
                S.dma("sp", st[64:128, 0:512], w_up_a, writes=[rst])
                S.dma("sp", st[:, 512:1024], w_up_g, writes=[rst])
                S.op("dve", lambda e: e.tensor_copy(out=WLOR[:], in_=st[:, 0:512]), reads=[rst], writes=[rWLOR])
                S.op("dve", lambda e: e.tensor_copy(out=WG[:], in_=st[:, 512:1024]), reads=[rst], writes=[rWG])
                S.barrier()
            chk(1)
            esB = ExitStack()
            with esB:
                sbB, _ = mk(esB)
                X = [sbB(f"X{i}", [128, D]) for i in range(2)]
                HN, rHN = sbB("HN", [128, D], BF16)
                HT = [sbB(f"HT{i}", [128, 8, 129], BF16) for i in range(2)]
                ST, rST = sbB("ST", [128, 8])
                R, rR = sbB("R", [128, 512]); K, rK = sbB("K", [128, 512]); V, rV = sbB("V", [128, 512])
                SG, rSG = sbB("SG", [128, 512]); AA, rAA = sbB("AA", [128, 512])
                ECL, rECL = sbB("ECL", [128, 512]); ENCL, rENCL = sbB("ENCL", [128, 512])
                KKN, rKKN = sbB("KKN", [128, 512]); KM, rKM = sbB("KM", [128, 512])
                TA, rTA = sbB("TA", [128, 512]); TB, rTB = sbB("TB", [128, 512])
                ECLM, rECLM = TA, rTA
                BON, rBON = SG, rSG
                S8, rS8 = sbB("S8", [128, 64])
                WL, rWL = sbB("WL", [128, 4])
                RBAR, rRBAR = sbB("RBAR", [128, 512], BF16); ABAR, rABAR = sbB("ABAR", [128, 512], BF16)
                BTIL, rBTIL = sbB("BTIL", [128, 512], BF16); KTIL, rKTIL = sbB("KTIL", [128, 512], BF16)
                VB, rVB = sbB("VB", [128, 512], BF16)
                RBT, rRBT = sbB("RBT", [128, 4, 128], BF16); ABT, rABT = sbB("ABT", [128, 4, 128], BF16)
                BTT, rBTT = sbB("BTT", [128, 4, 128], BF16); KTT, rKTT = sbB("KTT", [128, 4, 128], BF16)
                AAK, rAAK = sbB("AAK", [128, 8, 128], BF16); ARKT, rARKT = sbB("ARKT", [128, 8, 128], BF16)
                ARBT, rARBT = sbB("ARBT", [128, 8, 128], BF16)
                PQ = [[sbB(f"PQ{g}{i}", [128, 4, 128], BF16) for i in range(4)] for g in range(2)]
                TT = [[sbB(f"TT{g}{i}", [128, 4, 128], BF16) for i in range(2)] for g in range(2)]
                ABPT, rABPT = sbB("ABPT", [128, 4, 128], BF16); AAKPT, rAAKPT = sbB("AAKPT", [128, 8, 128], BF16)
                UB, rUB = sbB("UB", [128, 512], BF16)
                YF, rYF = ECL, rECL
                HF = [sbB("HF", [128, 4, 64])] * NSEQ
                HB, rHB = sbB("HB", [128, 4, 64], BF16)
                QKC, rQKC = sbB("QKC", [128, 4, 131])
                ACC_, rACC = sbB("ACC", [128, 512]); ACC = v3(ACC_[:], 4)
                QKT, rQKT = sbB("QKT", [128, 4, 128], BF16)
                KP, rKP = sbB("KP", [128, 4, 64], BF16)
                VE, rVE = sbB("VE", [128, 4, 129], BF16)
                G8, rG8 = sbB("G8", [128, 48])
                TM, rTM = sbB("TM", [128, 512]); DG, rDG = v3(TM[:], 4), rTM
                MST = [sbB("MST", [128, 4])] * NSEQ
                CF = [sbB("CF", [128, 2, 129])] * NSEQ
                CBF, rCBF = sbB("CBF", [128, 2, 129], BF16)
                PTB, rPTB = sbB("PTB", [128, 4, 128], BF16)
                HM_, rHM = sbB("HM", [128, 512]); HM = v3(HM_[:], 4)
                SO, rSO = sbB("SO", [128, 512])
                MIX, rMIX = sbB("MIX", [128, D], BF16)
                MIXT, rMIXT = AAKPT, rAAKPT
                DBG, rDBG = sbB("DBG", [128, 512]) if dbg else (None, None)

                S.op("pool", lambda e: e.memset(VE[:], 1.0), writes=[rVE])

                def dbg_out(nm, ap, rr, r0):
                    if dbg:
                        S.dma("sp", dbg_d[nm][r0:r0 + 128, :], ap, reads=[rr])

                LT2 = [sbB(f"LTb{i}", [128, 256], BF16) for i in range(2)]
                tiles = [(s_, c_) for s_ in range(NSEQ) for c_ in range(NT)]
                EP, rEP = PT[1]
                EPF = EP[:].bitcast(F32)

                def mk_proj(HTx):
                    cur = lambda k: HTx[:, k, 1:129]
                    prv = lambda k: HTx[:, k, 0:128]

                    def proj_tok(p, col, n, shifted):
                        def f(e):
                            last = None
                            nm = 16 if shifted else 8
                            i = 0
                            for k in range(8):
                                last = e.matmul(p, lhsT=cur(k), rhs=WIN[:, k, col:col + n], start=(i == 0), stop=(i == nm - 1)); i += 1
                                if shifted:
                                    last = e.matmul(p, lhsT=prv(k), rhs=WIN[:, k, RWC + col:RWC + col + n], start=False, stop=(i == nm - 1)); i += 1
                            return last
                        return f

                    def proj_feat(p, col, shifted):
                        def f(e):
                            last = None
                            nm = 16 if shifted else 8
                            i = 0
                            for k in range(8):
                                last = e.matmul(p, lhsT=WIN[:, k, col:col + 128], rhs=cur(k), start=(i == 0), stop=(i == nm - 1)); i += 1
                                if shifted:
                                    last = e.matmul(p, lhsT=WIN[:, k, RWC + col:RWC + col + 128], rhs=prv(k), start=False, stop=(i == nm - 1)); i += 1
                            return last
                        return f
                    return cur, prv, proj_tok, proj_feat

                def early(ti):
                    s_, c_ = tiles[ti]
                    r0_ = s_ * T + c_ * 128
                    Xn, rXn = X[ti % 2]
                    HTn, rHTn = HT[ti % 2]
                    HTq, rHTq = HT[(ti + 1) % 2]
                    LTn, rLTn = LT2[ti % 2]
                    _, _, ptok, pfeat = mk_proj(HTn)
                    S.dma("sp", Xn[:], x_d[r0_:r0_ + 128, :], writes=[rXn])
                    yield
                    rmsnorm_to_bf16(Xn[:], rXn, HN, rHN, ST, rST)
                    yield
                    yield S.op("pe", lambda e: [e.transpose(out=EP[:, k * 128:(k + 1) * 128], in_=HN[:, k * 128:(k + 1) * 128],
                                                            identity=IDB) for k in range(8)][-1], reads=[rHN, rCSB], writes=[rEP])
                    yield S.op("dve", lambda e: e.tensor_copy(out=HTn[:, :, 1:129], in_=v3(EP[:], 8)), reads=[rEP], writes=[rHTn])
                    if c_ == 0:
                        yield S.op("pool", lambda e: e.memset(HTn[:, :, 0:1], 0.0), writes=[rHTn])
                    else:
                        yield S.op("pool", lambda e: e.tensor_copy(out=HTn[:, :, 0:1], in_=HTq[:, :, 128:129]), reads=[rHTq], writes=[rHTn])
                    yield S.op("pe", pfeat(EPF[:, 0:128], 1536, True), reads=[rHTn, rWIN], writes=[rEP])
                    yield S.op("pe", pfeat(EPF[:, 128:256], 1664, True), reads=[rHTn, rWIN], writes=[rEP])
                    yield S.op("act", lambda e: e.activation(out=LTn[0:64, 0:128], in_=EPF[0:64, 0:128], func=AF.Tanh), reads=[rEP], writes=[rLTn])
                    yield S.op("act", lambda e: e.activation(out=LTn[:, 128:256], in_=EPF[:, 128:256], func=AF.Sigmoid), reads=[rEP], writes=[rLTn])
                    yield S.op("act", lambda e: e.activation(out=LTn[64:128, 0:128], in_=EPF[64:128, 0:128], func=AF.Copy), reads=[rEP], writes=[rLTn])
                    for g_, (dst, rdst) in ((1, (K, rK)), (0, (R, rR)), (2, (V, rV))):
                        yield S.op("pe", ptok(EPF, g_ * 512, 512, True), reads=[rHTn, rWIN], writes=[rEP])
                        yield S.op("act", lambda e: e.activation(out=dst[:], in_=EPF, func=AF.Copy), reads=[rEP], writes=[rdst])

                for ti, (s, c) in enumerate(tiles):
                    if True:
                        HFs, rHFs = HF[s]
                        CFs, rCFs = CF[s]
                        Ms, rMs = MST[s]
                        if c == 0:
                            S.op("pool", lambda e: e.memset(HFs[:], 0.0), writes=[rHFs])
                            S.op("pool", lambda e: e.memset(CFs[:], 0.0), writes=[rCFs])
                            S.op("pool", lambda e: e.memset(Ms[:], 0.0), writes=[rMs])
                        r0 = s * T + c * 128
                        Xc, rXc = X[ti % 2]
                        HTc, rHTc = HT[ti % 2]
                        LT, rLT = LT2[ti % 2]
                        cur, prv, proj_tok, proj_feat = mk_proj(HTc)
                        if ti == 0:
                            for _ in early(0):
                                pass
                        chk(2)
                        pw, rpw = pf()
                        S.op("pe", lambda e: e.matmul(pw[:], lhsT=LT[0:64, 0:128], rhs=WLOR[0:64, :], start=True, stop=True),
                             reads=[rLT, rWLOR], writes=[rpw])
                        S.op("dve", lambda e: e.tensor_tensor(out=SG[:], in0=pw[:], in1=W0V, op=ALU.add), reads=[rpw, rVEC], writes=[rSG])
                        S.op("act", lambda e: e.activation(out=SG[:], in_=SG[:], func=AF.Sigmoid), reads=[rSG], writes=[rSG])
                        pa, rpa = pf()
                        S.op("pe", lambda e: e.matmul(pa[:], lhsT=LT[64:128, 0:128], rhs=WLOR[64:128, :], start=True, stop=True),
                             reads=[rLT, rWLOR], writes=[rpa])
                        S.op("dve", lambda e: e.tensor_tensor(out=AA[:], in0=pa[:], in1=A0V, op=ALU.add), reads=[rpa, rVEC], writes=[rAA])
                        S.op("act", lambda e: e.activation(out=AA[:], in_=AA[:], func=AF.Sigmoid), reads=[rAA], writes=[rAA])
                        chk(3)
                        pc, rpc = pf()
                        S.op("pe", lambda e: e.matmul(pc[:], lhsT=MUI, rhs=SG[:], start=True, stop=True), reads=[rCST, rSG], writes=[rpc])
                        S.op("act", lambda e: e.activation(out=ECL[:], in_=pc[:], func=AF.Exp, scale=-CDEC), reads=[rpc], writes=[rECL])
                        S.op("act", lambda e: e.activation(out=ENCL[:], in_=pc[:], func=AF.Exp, scale=CDEC), reads=[rpc], writes=[rENCL])
                        S.op("dve", lambda e: e.tensor_tensor(out=TA[:], in0=pc[:], in1=SG[:], op=ALU.subtract), reads=[rpc, rSG], writes=[rTA])
                        S.op("act", lambda e: e.activation(out=ECLM[:], in_=TA[:], func=AF.Exp, scale=-CDEC), reads=[rTA], writes=[rECLM])
                        pwl, rpwl = pf()
                        S.op("pe", lambda e: [e.matmul(pwl[(h % 2) * 64:(h % 2) * 64 + 64, h // 2:h // 2 + 1], lhsT=SG[:, h * 64:(h + 1) * 64], rhs=ONES[:, 0:1], start=True, stop=True)
                                              for h in range(8)][-1], reads=[rSG, rCST], writes=[rpwl])
                        S.op("act", lambda e: e.activation(out=WL[:], in_=pwl[:, 0:4], func=AF.Exp, scale=-CDEC), reads=[rpwl], writes=[rWL])
                        S.op("dve", lambda e: e.tensor_tensor(out=KKN[:], in0=K[:], in1=KKV, op=ALU.mult), reads=[rK, rVEC], writes=[rKKN])
                        S.op("pool", lambda e: e.tensor_tensor(out=TB[:], in0=KKN[:], in1=KKN[:], op=ALU.mult), reads=[rKKN], writes=[rTB])
                        S.op("dve", lambda e: e.tensor_reduce(out=S8[:, 0:8], in_=v3(TB[:], 8), axis=AX.X, op=ALU.add), reads=[rTB], writes=[rS8])
                        S.op("act", lambda e: e.activation(out=S8[:, 8:16], in_=S8[:, 0:8], func=AF.Sqrt), reads=[rS8], writes=[rS8])
                        S.op("dve", lambda e: e.tensor_scalar(out=S8[:, 8:16], in0=S8[:, 8:16], scalar1=1e-12, scalar2=None, op0=ALU.max),
                             reads=[rS8], writes=[rS8])
                        S.op("dve", lambda e: e.reciprocal(out=S8[:, 16:24], in_=S8[:, 8:16]), reads=[rS8], writes=[rS8])
                        S.op("dve", lambda e: e.tensor_tensor(out=v3(KKN[:], 8), in0=v3(KKN[:], 8), in1=bc3(S8[:, 16:24], 8, 64), op=ALU.mult),
                             reads=[rKKN, rS8], writes=[rKKN])
                        S.op("dve", lambda e: e.scalar_tensor_tensor(out=TB[:], in0=AA[:], scalar=-1.0, in1=KAV, op0=ALU.add, op1=ALU.mult),
                             reads=[rAA, rVEC], writes=[rTB])
                        S.op("dve", lambda e: e.scalar_tensor_tensor(out=KM[:], in0=TB[:], scalar=1.0, in1=K[:], op0=ALU.add, op1=ALU.mult),
                             reads=[rTB, rK], writes=[rKM])
                        S.op("dve", lambda e: e.tensor_tensor(out=RBAR[:], in0=R[:], in1=ECL[:], op=ALU.mult), reads=[rR, rECL], writes=[rRBAR])
                        S.op("dve", lambda e: e.scalar_tensor_tensor(out=ABAR[:], in0=KKN[:], scalar=-1.0, in1=ECLM[:], op0=ALU.mult, op1=ALU.mult),
                             reads=[rKKN, rECLM], writes=[rABAR])
                        S.op("pool", lambda e: e.tensor_tensor(out=TA[:], in0=KKN[:], in1=AA[:], op=ALU.mult), reads=[rKKN, rAA], writes=[rTA])
                        S.op("pool", lambda e: e.tensor_tensor(out=BTIL[:], in0=TA[:], in1=ENCL[:], op=ALU.mult), reads=[rTA, rENCL], writes=[rBTIL])
                        S.op("dve", lambda e: e.tensor_tensor(out=KTIL[:], in0=KM[:], in1=ENCL[:], op=ALU.mult), reads=[rKM, rENCL], writes=[rKTIL])
                        S.op("act", lambda e: e.activation(out=VB[:], in_=V[:], func=AF.Copy), reads=[rV], writes=[rVB])
                        S.op("pool", lambda e: e.tensor_tensor(out=TB[:], in0=R[:], in1=KM[:], op=ALU.mult), reads=[rR, rKM], writes=[rTB])
                        S.op("pool", lambda e: e.tensor_tensor(out=TB[:], in0=TB[:], in1=RKV, op=ALU.mult), reads=[rTB, rVEC], writes=[rTB])
                        S.op("dve", lambda e: e.tensor_reduce(out=S8[:, 24:32], in_=v3(TB[:], 8), axis=AX.X, op=ALU.add), reads=[rTB], writes=[rS8])
                        S.op("dve", lambda e: e.tensor_tensor(out=v3(BON[:], 8), in0=v3(V[:], 8), in1=bc3(S8[:, 24:32], 8, 64), op=ALU.mult),
                             reads=[rV, rS8], writes=[rBON])
                        for src, rsrc, dst, rdst in ((RBAR, rRBAR, RBT, rRBT), (ABAR, rABAR, ABT, rABT),
                                                     (BTIL, rBTIL, BTT, rBTT), (KTIL, rKTIL, KTT, rKTT)):
                            pt, rpt = ptb()
                            S.op("pe", lambda e: [e.transpose(out=pt[:, b * 128:(b + 1) * 128], in_=src[:, b * 128:(b + 1) * 128],
                                                              identity=IDB) for b in range(4)][-1], reads=[rsrc, rCSB], writes=[rpt])
                            S.op("act", lambda e: e.activation(out=dst[:], in_=v3(pt[:, 0:512], 4), func=AF.Copy), reads=[rpt], writes=[rdst])

                        chk(4)

                        def hop(Tt, h):
                            return Tt[(h % 2) * 64:(h % 2) * 64 + 64, h // 2, :]

                        def rw_group(g):
                            hs = [g, g + 2, g + 4, g + 6]
                            P0, rP0 = PQ[g][0]; Q0, rQ0 = PQ[g][1]

                            def mm4(p, A, Bm):
                                return lambda e: [e.matmul(p[:, j * 128:(j + 1) * 128], lhsT=hop(A, h), rhs=hop(Bm, h), start=True, stop=True)
                                                  for j, h in enumerate(hs) if (VAR != 3 or h % 2 == 0) and (VAR != 4 or h % 2 == 1)][-1]
                            for (A, rA, Bm, rB, dst, rdst, mask, eng) in (
                                    (ABT, rABT, BTT, rBTT, P0[:], rP0, ML, "dve"),
                                    (BTT, rBTT, ABT, rABT, Q0[:], rQ0, MU, "dve"),
                                    (ABT, rABT, KTT, rKTT, AAK[:, 4 * g:4 * g + 4, :], rAAK, ML, "dve"),
                                    (KTT, rKTT, RBT, rRBT, ARKT[:, 4 * g:4 * g + 4, :], rARKT, MUI, "dve"),
                                    (BTT, rBTT, RBT, rRBT, ARBT[:, 4 * g:4 * g + 4, :], rARBT, MUI, "dve")):
                                p, rp = pf("g%d" % g)
                                yield S.op("pe", mm4(p, A, Bm), reads=[rA, rB], writes=[rp])
                                if VAR in (1, 3, 4):
                                    pass
                                elif VAR == 2:
                                    for j4 in range(4):
                                        yield S.op(eng, lambda e: e.tensor_tensor(out=dst[:, j4, :], in0=p[:, j4 * 128:(j4 + 1) * 128], in1=mask, op=ALU.mult),
                                             reads=[rp, rCST], writes=[rdst])
                                else:
                                    yield S.op(eng, lambda e: e.tensor_tensor(out=dst, in0=v3(p[:], 4), in1=bcm(mask, 4), op=ALU.mult),
                                         reads=[rp, rCST], writes=[rdst])
                            Tc, rTc = TT[g][0]
                            yield S.op("pool", lambda e: e.tensor_tensor(out=Tc[:], in0=Q0[:], in1=bcm(IDB, 4), op=ALU.add),
                                 reads=[rQ0, rCSB], writes=[rTc])
                            Pp, rPp, Qp, rQp = P0, rP0, Q0, rQ0
                            ti = 0
                            for lev in range(1, 7):
                                Pn, rPn = PQ[g][2 * (lev % 2)]
                                Qn, rQn = PQ[g][2 * (lev % 2) + 1]
                                p, rp = pf("g%d" % g)
                                yield S.op("pe", lambda e: [e.matmul(p[:, j * 128:(j + 1) * 128], lhsT=Qp[:, j, :], rhs=Pp[:, j, :], start=True, stop=True)
                                                      for j in range(4)][-1], reads=[rPp, rQp], writes=[rp])
                                yield S.op("act", lambda e: e.activation(out=Pn[:], in_=v3(p[:], 4), func=AF.Copy), reads=[rp], writes=[rPn])
                                if lev < 6:
                                    p2, rp2 = pf("g%d" % g)
                                    yield S.op("pe", lambda e: [e.matmul(p2[:, j * 128:(j + 1) * 128], lhsT=Pp[:, j, :], rhs=Qp[:, j, :], start=True, stop=True)
                                                          for j in range(4)][-1], reads=[rPp, rQp], writes=[rp2])
                                    yield S.op("act", lambda e: e.activation(out=Qn[:], in_=v3(p2[:], 4), func=AF.Copy), reads=[rp2], writes=[rQn])
                                Tn, rTn = TT[g][(ti + 1) % 2]
                                p3, rp3 = pf("g%d" % g)
                                yield S.op("pe", lambda e: [e.matmul(p3[:, j * 128:(j + 1) * 128], lhsT=Pn[:, j, :], rhs=Tc[:, j, :], start=True, stop=True)
                                                      for j in range(4)][-1], reads=[rPn, rTc], writes=[rp3])
                                yield S.op("dve", lambda e: e.tensor_tensor(out=Tn[:], in0=v3(p3[:], 4), in1=Tc[:], op=ALU.add),
                                     reads=[rp3, rTc], writes=[rTn])
                                Tc, rTc = Tn, rTn
                                ti += 1
                                Pp, rPp, Qp, rQp = Pn, rPn, Qn, rQn
                            p, rp = pf("g%d" % g)
                            yield S.op("pe", lambda e: [e.matmul(p[g * 64:g * 64 + 64, j * 128:(j + 1) * 128], lhsT=ABAR[:, h * 64:(h + 1) * 64], rhs=Tc[:, j, :],
                                                           start=True, stop=True) for j, h in enumerate(hs)][-1],
                                 reads=[rABAR, rTc], writes=[rp])
                            yield S.op("act", lambda e: e.activation(out=ABPT[g * 64:g * 64 + 64, :, :], in_=v3(p[g * 64:g * 64 + 64, :], 4), func=AF.Copy),
                                 reads=[rp], writes=[rABPT])
                            p, rp = pf("g%d" % g)
                            yield S.op("pe", lambda e: [e.matmul(p[:, j * 128:(j + 1) * 128], lhsT=AAK[:, 4 * g + j, :], rhs=Tc[:, j, :],
                                                           start=True, stop=True) for j, h in enumerate(hs)][-1],
                                 reads=[rAAK, rTc], writes=[rp])
                            yield S.op("dve", lambda e: e.tensor_copy(out=AAKPT[:, 4 * g:4 * g + 4, :], in_=v3(p[:], 4)), reads=[rp], writes=[rAAKPT])


                        def chain_R():
                            yield from interleave(rw_group(0), rw_group(1))
                            yield S.op("act", lambda e: e.activation(out=HB[:], in_=HFs[:], func=AF.Copy), reads=[rHFs], writes=[rHB])
                            pu, rpu = pf("r")

                            def fu(e):
                                last = None
                                for h in range(8):
                                    e.matmul(pu[:, h * 64:(h + 1) * 64], lhsT=hop(ABPT, h), rhs=hop(HB, h), start=True, stop=False)
                                    last = e.matmul(pu[:, h * 64:(h + 1) * 64], lhsT=AAKPT[:, (h % 2) * 4 + h // 2, :], rhs=VB[:, h * 64:(h + 1) * 64], start=False, stop=True)
                                return last
                            yield S.op("pe", fu, reads=[rABPT, rHB, rAAKPT, rVB], writes=[rpu])
                            yield S.op("act", lambda e: e.activation(out=UB[:], in_=pu[:], func=AF.Copy), reads=[rpu], writes=[rUB])
                            py, rpy = pf("r")

                            def fy(e):
                                last = None
                                for h in range(8):
                                    o = py[:, h * 64:(h + 1) * 64]
                                    e.matmul(o, lhsT=hop(RBT, h), rhs=hop(HB, h), start=True, stop=False)
                                    e.matmul(o, lhsT=ARBT[:, (h % 2) * 4 + h // 2, :], rhs=UB[:, h * 64:(h + 1) * 64], start=False, stop=False)
                                    last = e.matmul(o, lhsT=ARKT[:, (h % 2) * 4 + h // 2, :], rhs=VB[:, h * 64:(h + 1) * 64], start=False, stop=True)
                                return last
                            yield S.op("pe", fy, reads=[rRBT, rHB, rARBT, rUB, rARKT, rVB], writes=[rpy])
                            ph, rph = pf("r")

                            def fh(e):
                                last = None
                                for h in range(8):
                                    o = ph[(h % 2) * 64:(h % 2) * 64 + 64, (h // 2) * 64:(h // 2 + 1) * 64]
                                    e.matmul(o, lhsT=BTIL[:, h * 64:(h + 1) * 64], rhs=UB[:, h * 64:(h + 1) * 64], start=True, stop=False)
                                    last = e.matmul(o, lhsT=KTIL[:, h * 64:(h + 1) * 64], rhs=VB[:, h * 64:(h + 1) * 64], start=False, stop=True)
                                return last
                            yield S.op("pe", fh, reads=[rBTIL, rKTIL, rUB, rVB], writes=[rph])
                            yield S.op("dve", lambda e: e.tensor_tensor(out=HFs[:], in0=v3(ph[:, 0:256], 4), in1=HFs[:], op=ALU.add),
                                 reads=[rph, rHFs], writes=[rHFs])
                            yield S.op("dve", lambda e: e.tensor_tensor(out=HFs[:], in0=HFs[:], in1=bc3(WL[:], 4, 64), op=ALU.mult),
                                 reads=[rHFs, rWL], writes=[rHFs])
                            yield S.op("act", lambda e: e.activation(out=YF[:], in_=py[:], func=AF.Copy), reads=[rpy], writes=[rYF])
                            dbg_out("y", YF[:], rYF, r0)
                            yield S.op("dve", lambda e: e.tensor_reduce(out=S8[:, 32:40], in_=v3(YF[:], 8), axis=AX.X, op=ALU.add), reads=[rYF], writes=[rS8])
                            yield S.op("dve", lambda e: e.tensor_scalar(out=S8[:, 32:40], in0=S8[:, 32:40], scalar1=1.0 / 64, scalar2=None, op0=ALU.mult),
                                 reads=[rS8], writes=[rS8])
                            yield S.op("dve", lambda e: e.tensor_tensor(out=v3(YF[:], 8), in0=v3(YF[:], 8), in1=bc3(S8[:, 32:40], 8, 64), op=ALU.subtract),
                                 reads=[rYF, rS8], writes=[rYF])
                            yield S.op("pool", lambda e: e.tensor_tensor(out=TB[:], in0=YF[:], in1=YF[:], op=ALU.mult), reads=[rYF], writes=[rTB])
                            yield S.op("dve", lambda e: e.tensor_reduce(out=S8[:, 40:48], in_=v3(TB[:], 8), axis=AX.X, op=ALU.add), reads=[rTB], writes=[rS8])
                            yield S.op("dve", lambda e: e.tensor_scalar(out=S8[:, 40:48], in0=S8[:, 40:48], scalar1=1.0 / 64, scalar2=64e-5,
                                                                  op0=ALU.mult, op1=ALU.add), reads=[rS8], writes=[rS8])
                            yield S.op("act", lambda e: e.activation(out=S8[:, 40:48], in_=S8[:, 40:48], func=AF.Sqrt), reads=[rS8], writes=[rS8])
                            yield S.op("dve", lambda e: e.reciprocal(out=S8[:, 48:56], in_=S8[:, 40:48]), reads=[rS8], writes=[rS8])
                            yield S.op("dve", lambda e: e.tensor_tensor(out=v3(YF[:], 8), in0=v3(YF[:], 8), in1=bc3(S8[:, 48:56], 8, 64), op=ALU.mult),
                                 reads=[rYF, rS8], writes=[rYF])
                            yield S.op("pool", lambda e: e.tensor_tensor(out=YF[:], in0=YF[:], in1=LWV, op=ALU.mult), reads=[rYF, rVEC], writes=[rYF])
                            yield S.op("pool", lambda e: e.tensor_tensor(out=YF[:], in0=YF[:], in1=LBV, op=ALU.add), reads=[rYF, rVEC], writes=[rYF])
                            yield S.op("pool", lambda e: e.tensor_tensor(out=YF[:], in0=YF[:], in1=BON[:], op=ALU.add), reads=[rYF, rBON], writes=[rYF])
                            pg, rpg = pf("r")
                            yield S.op("pe", lambda e: e.matmul(pg[:], lhsT=LT[:, 128:256], rhs=WG[:], start=True, stop=True), reads=[rLT, rWG], writes=[rpg])
                            if dbg:
                                yield S.op("dve", lambda e: e.tensor_tensor(out=DBG[:], in0=YF[:], in1=pg[:], op=ALU.mult), reads=[rYF, rpg], writes=[rDBG])
                                dbg_out("yrw", DBG[:], rDBG, r0)
                            yield S.op("dve", lambda e: e.tensor_tensor(out=MIX[:, 0:512], in0=YF[:], in1=pg[:], op=ALU.mult), reads=[rYF, rpg], writes=[rMIX])


                        def chain_M():
                            pqk, rpqk = pf("m")
                            for b in range(4):
                                yield S.op("pe", proj_feat(pqk[:, b * 128:(b + 1) * 128], MLO - 0 + b * 128 if False else 0, False) if False else
                                     (lambda e, b=b: [e.matmul(pqk[:, b * 128:(b + 1) * 128], lhsT=WIN[:, k, MLO + b * 128:MLO + (b + 1) * 128], rhs=cur(k),
                                                              start=(k == 0), stop=(k == 7)) for k in range(8)][-1]),
                                     reads=[rHTc, rWIN], writes=[rpqk])
                            if c == 0:
                                yield S.op("pool", lambda e: e.memset(QKC[:, :, 0:3], 0.0), writes=[rQKC])
                            else:
                                yield S.op("pool", lambda e: e.tensor_copy(out=QKC[:, :, 0:3], in_=QKC[:, :, 128:131]), reads=[rQKC], writes=[rQKC])
                            yield S.op("act", lambda e: e.activation(out=QKC[:, :, 3:131], in_=v3(pqk[:], 4), func=AF.Copy), reads=[rpqk], writes=[rQKC])
                            for b in range(4):
                                eng = "dve"
                                yield S.op(eng, lambda e, b=b: e.tensor_scalar(out=ACC[:, b, :], in0=QKC[:, b, 0:128], scalar1=CW[:, b, 0:1], scalar2=CB[:, b:b + 1],
                                                                         op0=ALU.mult, op1=ALU.add), reads=[rQKC, rCW, rCB], writes=[rACC])
                                for j in range(1, 4):
                                    yield S.op(eng, lambda e, b=b, j=j: e.scalar_tensor_tensor(out=ACC[:, b, :], in0=QKC[:, b, j:j + 128], scalar=CW[:, b, j:j + 1],
                                                                                        in1=ACC[:, b, :], op0=ALU.mult, op1=ALU.add),
                                         reads=[rQKC, rCW, rACC], writes=[rACC])
                            yield S.op("act", lambda e: e.activation(out=QKT[:], in_=ACC, func=AF.Silu), reads=[rACC], writes=[rQKT])
                            pv, rpv = pf("m")
                            yield S.op("pe", proj_tok(pv[:], MLO + 512 - 0, 512, False) if False else
                                 (lambda e: [e.matmul(pv[:], lhsT=cur(k), rhs=WIN[:, k, MLO + 512:MLO + 1024], start=(k == 0), stop=(k == 7)) for k in range(8)][-1]),
                                 reads=[rHTc, rWIN], writes=[rpv])
                            yield S.op("act", lambda e: e.activation(out=VE[:, :, 0:128], in_=v3(pv[:], 4), func=AF.Copy), reads=[rpv], writes=[rVE])
                            po, rpo = pf("m")
                            yield S.op("pe", lambda e: [e.matmul(po[:], lhsT=cur(k), rhs=WIN[:, k, MLO + 1024:MLO + 1536], start=(k == 0), stop=(k == 7)) for k in range(8)][-1],
                                 reads=[rHTc, rWIN], writes=[rpo])
                            yield S.op("act", lambda e: e.activation(out=SO[:], in_=po[:], func=AF.Sigmoid), reads=[rpo], writes=[rSO])
                            pgt, rpgt = pf("m")
                            yield S.op("pe", lambda e: [e.matmul(pgt[:, 0:8], lhsT=cur(k), rhs=WIN[:, k, MLO + 1536:MLO + 1544], start=(k == 0), stop=(k == 7)) for k in range(8)][-1],
                                 reads=[rHTc, rWIN], writes=[rpgt])
                            yield S.op("dve", lambda e: e.tensor_tensor(out=G8[:, 0:8], in0=pgt[:, 0:8], in1=GB[:], op=ALU.add), reads=[rpgt, rGB], writes=[rG8])
                            yield S.op("act", lambda e: e.activation(out=G8[:, 0:8], in_=G8[:, 0:8], func=AF.Tanh, scale=1.0 / 15), reads=[rG8], writes=[rG8])
                            yield S.op("act", lambda e: e.activation(out=G8[:, 8:12], in_=G8[:, 4:8], func=AF.Exp, scale=-15.0), reads=[rG8], writes=[rG8])
                            yield S.op("dve", lambda e: e.tensor_scalar(out=G8[:, 8:12], in0=G8[:, 8:12], scalar1=1.0, scalar2=None, op0=ALU.add), reads=[rG8], writes=[rG8])
                            yield S.op("act", lambda e: e.activation(out=G8[:, 12:16], in_=G8[:, 8:12], func=AF.Ln), reads=[rG8], writes=[rG8])
                            pb, rpb = pf("m")
                            yield S.op("pe", lambda e: e.matmul(pb[:, 0:4], lhsT=MUI, rhs=G8[:, 12:16], start=True, stop=True), reads=[rCST, rG8], writes=[rpb])
                            yield S.op("pe", lambda e: e.matmul(pb[:, 4:8], lhsT=ONES, rhs=G8[:, 12:16], start=True, stop=True), reads=[rCST, rG8], writes=[rpb])
                            yield S.op("dve", lambda e: e.scalar_tensor_tensor(out=G8[:, 16:20], in0=G8[:, 0:4], scalar=15.0, in1=pb[:, 0:4], op0=ALU.mult, op1=ALU.add),
                                 reads=[rG8, rpb], writes=[rG8])
                            yield S.op("dve", lambda e: e.tensor_tensor(out=DG, in0=bcm(IDF, 4), in1=bc3(G8[:, 16:20], 4, 128), op=ALU.mult),
                                 reads=[rCST, rG8], writes=[rDG])
                            pgm, rpgm = pf("m")
                            yield S.op("pe", lambda e: e.matmul(pgm[:], lhsT=ONES, rhs=TM[:], start=True, stop=True),
                                 reads=[rCST, rDG], writes=[rpgm])
                            yield S.op("dve", lambda e: e.tensor_reduce(out=G8[:, 20:24], in_=v3(pgm[:], 4), axis=AX.X, op=ALU.max), reads=[rpgm], writes=[rG8])
                            yield S.op("dve", lambda e: e.tensor_tensor(out=G8[:, 20:24], in0=G8[:, 20:24], in1=Ms[:], op=ALU.max), reads=[rG8, rMs], writes=[rG8])
                            yield S.op("dve", lambda e: e.tensor_tensor(out=G8[:, 40:44], in0=G8[:, 16:20], in1=G8[:, 20:24], op=ALU.subtract), reads=[rG8], writes=[rG8])
                            yield S.op("act", lambda e: e.activation(out=G8[:, 24:28], in_=G8[:, 40:44], func=AF.Exp), reads=[rG8], writes=[rG8])
                            yield S.op("dve", lambda e: e.tensor_tensor(out=G8[:, 44:48], in0=Ms[:], in1=G8[:, 20:24], op=ALU.subtract), reads=[rG8, rMs], writes=[rG8])
                            yield S.op("act", lambda e: e.activation(out=G8[:, 28:32], in_=G8[:, 44:48], func=AF.Exp), reads=[rG8], writes=[rG8])
                            yield S.op("dve", lambda e: e.tensor_scalar(out=G8[:, 36:40], in0=G8[:, 28:32], scalar1=0.125, scalar2=None, op0=ALU.mult), reads=[rG8], writes=[rG8])
                            yield S.op("dve", lambda e: e.tensor_tensor(out=G8[:, 40:44], in0=pb[:, 0:4], in1=G8[:, 20:24], op=ALU.subtract), reads=[rpb, rG8], writes=[rG8])
                            yield S.op("act", lambda e: e.activation(out=G8[:, 32:36], in_=G8[:, 40:44], func=AF.Exp), reads=[rG8], writes=[rG8])
                            yield S.op("dve", lambda e: e.tensor_tensor(out=Ms[:], in0=G8[:, 20:24], in1=pb[:, 4:8], op=ALU.subtract), reads=[rG8, rpb], writes=[rMs])
                            pt, rpt = ptb()
                            yield S.op("pe", lambda e: [e.transpose(out=pt[:, b * 128:(b + 1) * 128], in_=QKT[:, 2 + b, :], identity=IDB) for b in range(2)][-1],
                                 reads=[rQKT, rCSB], writes=[rpt])
                            yield S.op("dve", lambda e: e.tensor_tensor(out=KP[:], in0=v3(pt[:, 0:256], 4), in1=bc3(G8[:, 24:28], 4, 64), op=ALU.mult),
                                 reads=[rpt, rG8], writes=[rKP])
                            psts = [pf("m"), pf("m")]
                            for par in range(2):
                                pst, rpst = psts[par]
                                yield S.op("pe", lambda e: [e.matmul(pst[:, (h // 2) * 128:(h // 2 + 1) * 128], lhsT=QKT[par * 64:par * 64 + 64, 2 + h // 2, :],
                                                               rhs=QKT[par * 64:par * 64 + 64, h // 2, :], start=True, stop=True) for h in (par, par + 2)][-1],
                                     reads=[rQKT], writes=[rpst])
                            for h in range(4):
                                pst, rpst = psts[h % 2]
                                yield S.op("dve", lambda e, h=h: e.scalar_tensor_tensor(out=PTB[:, h, :], in0=pst[:, (h // 2) * 128:(h // 2 + 1) * 128], scalar=G8[:, 24 + h:25 + h],
                                                                                 in1=MUI8[:], op0=ALU.mult, op1=ALU.mult), reads=[rpst, rG8, rMUI8], writes=[rPTB])
                            for h in range(4):
                                po_ = (h % 2) * 64
                                yield S.op("dve", lambda e, h=h: e.tensor_scalar(out=CBF[po_:po_ + 64, h // 2, :], in0=CFs[po_:po_ + 64, h // 2, :],
                                                                            scalar1=G8[po_:po_ + 64, 36 + h:37 + h], scalar2=None, op0=ALU.mult),
                                     reads=[rCFs, rG8], writes=[rCBF])
                            pn = [pf("m"), pf("m")]
                            for i2 in range(2):
                                pnn, rpnn = pn[i2]

                                def fn(e, i2=i2, pnn=pnn):
                                    last = None
                                    for j in range(2):
                                        h = 2 * i2 + j
                                        o = pnn[:, j * 129:(j + 1) * 129]
                                        e.matmul(o, lhsT=PTB[:, h, :], rhs=VE[:, h, :], start=True, stop=False)
                                        last = e.matmul(o, lhsT=QKT[(h % 2) * 64:(h % 2) * 64 + 64, h // 2, :], rhs=CBF[(h % 2) * 64:(h % 2) * 64 + 64, h // 2, :], start=False, stop=True)
                                    return last
                                yield S.op("pe", fn, reads=[rPTB, rVE, rQKT, rCBF], writes=[rpnn])
                            for i2 in range(2):
                                pnn, rpnn = pn[i2]
                                yield S.op("dve", lambda e, i2=i2, pnn=pnn: e.tensor_copy(out=S8[:, 56 + 2 * i2:58 + 2 * i2],
                                                                                  in_=pnn[:, 0:258].rearrange("p (a b) -> p a b", a=2)[:, :, 128:129].rearrange("p a b -> p (a b)")),
                                     reads=[rpnn], writes=[rS8])
                            yield S.op("dve", lambda e: e.tensor_scalar(out=G8[:, 40:44], in0=S8[:, 56:60], scalar1=-1.0, scalar2=None, op0=ALU.mult), reads=[rS8], writes=[rG8])
                            yield S.op("dve", lambda e: e.tensor_tensor(out=S8[:, 56:60], in0=S8[:, 56:60], in1=G8[:, 40:44], op=ALU.max), reads=[rS8, rG8], writes=[rS8])
                            yield S.op("dve", lambda e: e.tensor_tensor(out=S8[:, 56:60], in0=S8[:, 56:60], in1=G8[:, 32:36], op=ALU.max), reads=[rS8, rG8], writes=[rS8])
                            yield S.op("dve", lambda e: e.reciprocal(out=S8[:, 60:64], in_=S8[:, 56:60]), reads=[rS8], writes=[rS8])
                            for i2 in range(2):
                                pnn, rpnn = pn[i2]
                                yield S.op("dve", lambda e, i2=i2, pnn=pnn: e.tensor_tensor(out=HM[:, 2 * i2:2 * i2 + 2, :],
                                                                                    in0=pnn[:, 0:258].rearrange("p (a b) -> p a b", a=2)[:, :, 0:128],
                                                                                    in1=bc3(S8[:, 60 + 2 * i2:62 + 2 * i2], 2, 128), op=ALU.mult),
                                     reads=[rpnn, rS8], writes=[rHM])
                            pcc, rpcc = pf("m")
                            yield S.op("pe", lambda e: [e.matmul(pcc[(h % 2) * 64:(h % 2) * 64 + 64, (h // 2) * 129:(h // 2 + 1) * 129], lhsT=KP[:, h, :], rhs=VE[:, h, :],
                                                           start=True, stop=True) for h in range(4)][-1], reads=[rKP, rVE], writes=[rpcc])
                            for h in range(4):
                                po_ = (h % 2) * 64
                                yield S.op("dve", lambda e, h=h: e.scalar_tensor_tensor(out=CFs[po_:po_ + 64, h // 2, :], in0=CFs[po_:po_ + 64, h // 2, :],
                                                                                 scalar=G8[po_:po_ + 64, 28 + h:29 + h],
                                                                                 in1=pcc[po_:po_ + 64, (h // 2) * 129:(h // 2 + 1) * 129], op0=ALU.mult, op1=ALU.add),
                                     reads=[rCFs, rG8, rpcc], writes=[rCFs])
                            HM2 = HM_[:]
                            dbg_out("hm", HM2, rHM, r0)
                            yield S.op("pool", lambda e: e.tensor_tensor(out=TM[:], in0=HM2, in1=HM2, op=ALU.mult), reads=[rHM], writes=[rTM])
                            yield S.op("dve", lambda e: e.tensor_reduce(out=G8[:, 40:44], in_=v3(TM[:], 4), axis=AX.X, op=ALU.add), reads=[rTM], writes=[rG8])
                            yield S.op("dve", lambda e: e.tensor_scalar(out=G8[:, 40:44], in0=G8[:, 40:44], scalar1=1.0 / 128, scalar2=1e-6, op0=ALU.mult, op1=ALU.add),
                                 reads=[rG8], writes=[rG8])
                            yield S.op("act", lambda e: e.activation(out=G8[:, 40:44], in_=G8[:, 40:44], func=AF.Sqrt), reads=[rG8], writes=[rG8])
                            yield S.op("dve", lambda e: e.reciprocal(out=G8[:, 44:48], in_=G8[:, 40:44]), reads=[rG8], writes=[rG8])
                            yield S.op("dve", lambda e: e.tensor_tensor(out=HM, in0=HM, in1=bc3(G8[:, 44:48], 4, 128), op=ALU.mult), reads=[rHM, rG8], writes=[rHM])
                            yield S.op("pool", lambda e: e.tensor_tensor(out=TM[:], in0=HM2, in1=MHG, op=ALU.mult), reads=[rHM, rVEC], writes=[rTM])
                            if dbg:
                                yield S.op("dve", lambda e: e.tensor_tensor(out=DBG[:], in0=TM[:], in1=SO[:], op=ALU.mult), reads=[rTM, rSO], writes=[rDBG])
                                dbg_out("yml", DBG[:], rDBG, r0)
                            yield S.op("pool", lambda e: e.tensor_tensor(out=MIX[:, 512:1024], in0=TM[:], in1=SO[:], op=ALU.mult), reads=[rTM, rSO], writes=[rMIX])


                        nxt = [early(ti + 1)] if ti + 1 < len(tiles) else []
                        for _ in interleave(chain_R(), chain_M(), *nxt):
                            pass
                        chk(7)
                        pt, rpt = ptb()
                        S.op("pe", lambda e: [e.transpose(out=pt[:, k * 128:(k + 1) * 128], in_=MIX[:, k * 128:(k + 1) * 128], identity=IDB) for k in range(8)][-1],
                             reads=[rMIX, rCSB], writes=[rpt])
                        S.op("act", lambda e: e.activation(out=MIXT[:], in_=v3(pt[:], 8), func=AF.Copy), reads=[rpt], writes=[rMIXT])
                        for g in range(2):
                            p, rp = pf()
                            S.op("pe", lambda e, g=g, p=p: [e.matmul(p[:], lhsT=MIXT[:, k, :], rhs=WOUT[:, k, g * 512:(g + 1) * 512], start=(k == 0), stop=(k == 7))
                                                          for k in range(8)][-1], reads=[rMIXT, rWOUT], writes=[rp])
                            S.op("dve", lambda e, g=g, p=p: e.tensor_tensor(out=Xc[:, g * 512:(g + 1) * 512], in0=p[:], in1=Xc[:, g * 512:(g + 1) * 512], op=ALU.add),
                                 reads=[rp, rXc], writes=[rXc])
                        S.dma("sp", x1_d[r0:r0 + 128, :], Xc[:], reads=[rXc])
                        chk(8)
                S.barrier()
            S.barrier()

        es2 = ExitStack()
        with es2:
            sb2, _ = mk(es2)
            TOKS = 512 if T % 512 == 0 else 256
            NSUB = TOKS // 128
            WUP, rWUP = sb2("WUP", [128, 8, 2 * DFF], BF16)
            WDN, rWDN = sb2("WDN", [128, NB, D], BF16)
            FCW, rFCW = sb2("FCW", [128, NB, 3])
            FCB, rFCB = sb2("FCB", [128, NB])
            GF, rGF = sb2("GF", [128, D])
            S.dma("sp", FCW[:].rearrange("p b j -> p (b j)"), ffn_conv_w, writes=[rFCW])
            S.dma("sp", FCB[:], ffn_conv_b, writes=[rFCB])
            S.dma("sp", GF[:], norm_f_g.partition_broadcast(128), writes=[rGF])
            esC = ExitStack()
            with esC:
                sbC, _ = mk(esC)
                G2, rG2 = sbC("G2", [128, 8])
                STG2 = [sbC(f"STH{i}", [128, DFF]) for i in range(2)]
                S.dma("sp", G2[:], norm2_g, writes=[rG2])
                i = 0
                for k in range(8):
                    for hlf in range(2):
                        st, rst = STG2[i % 2]; i += 1
                        S.dma("sp", st[:], w_ffn_up[k * 128:(k + 1) * 128, hlf * DFF:(hlf + 1) * DFF], writes=[rst])
                        eng = "act" if hlf == 0 else "dve"
                        if eng == "act":
                            S.op("act", lambda e: e.activation(out=WUP[:, k, hlf * DFF:(hlf + 1) * DFF], in_=st[:], func=AF.Copy, scale=G2[:, k:k + 1]),
                                 reads=[rst, rG2], writes=[rWUP])
                        else:
                            S.op("dve", lambda e: e.tensor_scalar(out=WUP[:, k, hlf * DFF:(hlf + 1) * DFF], in0=st[:], scalar1=G2[:, k:k + 1], scalar2=None, op0=ALU.mult),
                                 reads=[rst, rG2], writes=[rWUP])
                for b in range(0, NB, 2):
                    st, rst = STG2[i % 2]; i += 1
                    S.dma("sp", st[:, 0:2 * D].rearrange("p (a n) -> p a n", a=2), w_ffn_down[b * 128:(b + 2) * 128, :].rearrange("(a p) n -> p a n", p=128),
                          writes=[rst])
                    S.op("pool" if (b // 2) % 2 else "dve", lambda e: e.tensor_copy(out=WDN[:, b:b + 2, :], in_=st[:, 0:2 * D].rearrange("p (a n) -> p a n", a=2)),
                         reads=[rst], writes=[rWDN])
                S.barrier()
            chk(9)
            esD = ExitStack()
            with esD:
                sbD, _ = mk(esD)
                XS = [sbD(f"XS{i}", [128, D]) for i in range(NSUB)]
                HN2, rHN2 = sbD("HN2", [128, D], BF16)
                H2T, rH2T = sbD("H2T", [128, 8, TOKS], BF16)
                GT, rGT = sbD("GT", [128, NB, TOKS], BF16)
                AC = [sbD(f"AC{i}", [128, TOKS + 2]) for i in range(2)]
                AQ = [sbD(f"AQ{i}", [128, TOKS]) for i in range(2)]
                CAR, rCAR = sbD("CAR", [128, NB, 2])
                ST2, rST2 = sbD("ST2", [128, 8])
                it = 0
                for s in range(NSEQ):
                    S.op("pool", lambda e: e.memset(CAR[:], 0.0), writes=[rCAR])
                    for c in range(T // TOKS):
                        r0 = s * T + c * TOKS
                        for sub in range(NSUB):
                            Xs, rXs = XS[sub]
                            S.dma("sp", Xs[:], x1_d[r0 + sub * 128:r0 + (sub + 1) * 128, :], writes=[rXs])
                            rmsnorm_to_bf16(Xs[:], rXs, HN2, rHN2, ST2, rST2)
                            pt, rpt = ptb()
                            S.op("pe", lambda e: [e.transpose(out=pt[:, k * 128:(k + 1) * 128], in_=HN2[:, k * 128:(k + 1) * 128], identity=IDB) for k in range(8)][-1],
                                 reads=[rHN2, rCSB], writes=[rpt])
                            S.op("dve", lambda e, sub=sub: e.tensor_copy(out=H2T[:, :, sub * 128:(sub + 1) * 128], in_=v3(pt[:], 8)), reads=[rpt], writes=[rH2T])
                        for b in range(NB):
                            ACb, rACb = AC[b % 2]
                            AQb, rAQb = AQ[b % 2]
                            pa, rpa = pf()
                            pbk, rpbk = pf()
                            S.op("pe", lambda e, b=b, pa=pa: [e.matmul(pa[:, 0:TOKS], lhsT=WUP[:, k, b * 128:(b + 1) * 128], rhs=H2T[:, k, :], start=(k == 0), stop=(k == 7))
                                                             for k in range(8)][-1], reads=[rWUP, rH2T], writes=[rpa])
                            S.op("pe", lambda e, b=b, pbk=pbk: [e.matmul(pbk[:, 0:TOKS], lhsT=WUP[:, k, DFF + b * 128:DFF + (b + 1) * 128], rhs=H2T[:, k, :], start=(k == 0), stop=(k == 7))
                                                               for k in range(8)][-1], reads=[rWUP, rH2T], writes=[rpbk])
                            S.op("pool", lambda e, b=b: e.tensor_copy(out=ACb[:, 0:2], in_=CAR[:, b, :]), reads=[rCAR], writes=[rACb])
                            S.op("act", lambda e: e.activation(out=ACb[:, 2:TOKS + 2], in_=pa[:, 0:TOKS], func=AF.Copy), reads=[rpa], writes=[rACb])
                            S.op("pool", lambda e, b=b: e.tensor_copy(out=CAR[:, b, :], in_=ACb[:, TOKS:TOKS + 2]), reads=[rACb], writes=[rCAR])
                            S.op("dve", lambda e, b=b: e.tensor_scalar(out=AQb[:], in0=ACb[:, 0:TOKS], scalar1=FCW[:, b, 0:1], scalar2=FCB[:, b:b + 1], op0=ALU.mult, op1=ALU.add),
                                 reads=[rACb, rFCW, rFCB], writes=[rAQb])
                            S.op("dve", lambda e, b=b: e.scalar_tensor_tensor(out=AQb[:], in0=ACb[:, 1:TOKS + 1], scalar=FCW[:, b, 1:2], in1=AQb[:], op0=ALU.mult, op1=ALU.add),
                                 reads=[rACb, rFCW, rAQb], writes=[rAQb])
                            S.op("dve", lambda e, b=b: e.scalar_tensor_tensor(out=AQb[:], in0=ACb[:, 2:TOKS + 2], scalar=FCW[:, b, 2:3], in1=AQb[:], op0=ALU.mult, op1=ALU.add),
                                 reads=[rACb, rFCW, rAQb], writes=[rAQb])
                            S.op("act", lambda e: e.activation(out=AQb[:], in_=AQb[:], func=AF.Silu), reads=[rAQb], writes=[rAQb])
                            S.op("dve", lambda e, b=b: e.tensor_tensor(out=GT[:, b, :], in0=AQb[:], in1=pbk[:, 0:TOKS], op=ALU.mult), reads=[rAQb, rpbk], writes=[rGT])
                        for sub in range(NSUB):
                            Xs, rXs = XS[sub]
                            X2c, rX2c = Xs, rXs
                            for g in range(2):
                                p, rp = pf()
                                S.op("pe", lambda e, g=g, p=p, sub=sub: [e.matmul(p[:], lhsT=GT[:, b, sub * 128:(sub + 1) * 128], rhs=WDN[:, b, g * 512:(g + 1) * 512],
                                                                                 start=(b == 0), stop=(b == NB - 1)) for b in range(NB)][-1], reads=[rGT, rWDN], writes=[rp])
                                S.op("dve", lambda e, g=g, p=p: e.tensor_tensor(out=X2c[:, g * 512:(g + 1) * 512], in0=p[:], in1=Xs[:, g * 512:(g + 1) * 512], op=ALU.add),
                                     reads=[rp, rXs], writes=[rX2c])
                            S.op("pool", lambda e: e.memset(ST2[:, 4:5], 0.0), writes=[rST2])
                            S.op("act", lambda e: e.activation(out=HN2[:], in_=X2c[:], func=AF.Square, accum_out=ST2[:, 4:5]), reads=[rX2c, rST2], writes=[rHN2, rST2])
                            S.op("dve", lambda e: e.tensor_scalar(out=ST2[:, 5:6], in0=ST2[:, 4:5], scalar1=1.0 / D, scalar2=1e-6, op0=ALU.mult, op1=ALU.add),
                                 reads=[rST2], writes=[rST2])
                            S.op("act", lambda e: e.activation(out=ST2[:, 6:7], in_=ST2[:, 5:6], func=AF.Sqrt), reads=[rST2], writes=[rST2])
                            S.op("dve", lambda e: e.reciprocal(out=ST2[:, 7:8], in_=ST2[:, 6:7]), reads=[rST2], writes=[rST2])
                            S.op("act", lambda e: e.activation(out=X2c[:], in_=X2c[:], func=AF.Copy, scale=ST2[:, 7:8]), reads=[rX2c, rST2], writes=[rX2c])
                            S.op("pool", lambda e: e.tensor_tensor(out=X2c[:], in0=X2c[:], in1=GF[:], op=ALU.mult), reads=[rX2c, rGF], writes=[rX2c])
                            S.dma("sp", out_d[r0 + sub * 128:r0 + (sub + 1) * 128, :], X2c[:], reads=[rX2c])
                S.barrier()
            S.barrier()
    return nc


def make_consts():
    c = np.zeros((128, 640), np.float32)
    i = np.arange(128)
    c[:, 0:128] = np.eye(128)
    c[:, 128:256] = (i[:, None] < i[None, :])
    c[:, 256:384] = (i[:, None] <= i[None, :])
    c[:, 384:512] = (i[:, None] > i[None, :])
    c[:, 512:640] = 1.0
    return c


def make_in_maps(inputs, n_cores, nseq):
    f = lambda a: np.ascontiguousarray(np.asarray(a, np.float32))
    x = f(inputs["x"])
    T = x.shape[1]
    shared = {}
    for k, v in inputs.items():
        if k == "x":
            continue
        a = f(v)
        if k != "norm_f_g":
            a = a[0]
        if k == "r_k":
            a = a.reshape(512)
        elif k in ("norm1_g", "norm2_g", "qk_conv_b", "ffn_conv_b"):
            a = a.reshape(-1, 128).T
        elif k in ("qk_conv_w", "ffn_conv_w"):
            j = a.shape[0]
            a = a.reshape(j, -1, 128).transpose(2, 1, 0).reshape(128, -1)
        shared[k] = np.ascontiguousarray(a)
    shared["consts"] = make_consts()
    maps = []
    for c in range(n_cores):
        m = dict(shared)
        m["x"] = np.ascontiguousarray(x[c * nseq:(c + 1) * nseq].reshape(nseq * T, D))
        maps.append(m)
    return maps


def kernel(**inputs):
    x = np.asarray(inputs["x"])
    B, T, _ = x.shape
    nseq = B // N_CORES
    nc = build(nseq, T)
    maps = make_in_maps(inputs, N_CORES, nseq)
    res = run_bass_kernel_spmd(nc, maps, core_ids=list(range(N_CORES)))
    out = np.concatenate([r["out"].reshape(nseq, T, D) for r in res.results], axis=0)
    return out.astype(np.float32)
```

```python
import numpy as np
from contextlib import ExitStack
import concourse.bass as bass
import concourse.mybir as mybir
from concourse.bass_utils import run_bass_kernel_spmd

F32 = mybir.dt.float32
BF16 = mybir.dt.bfloat16
ALU = mybir.AluOpType
AF = mybir.ActivationFunctionType
AX = mybir.AxisListType

N_CORES = 8
D = 1024
N_IN = 3336
RWC = 1792
MLO = 2 * RWC
WIN_COLS = 2 * RWC + 1544
DFF = 2816
NB = DFF // 128
CDEC = float(np.exp(-0.5))


class Res:
    __slots__ = ("name", "lw", "rd")

    def __init__(self, name):
        self.name = name
        self.lw = None
        self.rd = []


class Sched:
    EPOCH = 30000
    NDMA = 24

    def __init__(self, nc, es):
        self.nc = nc
        self.es = es
        self.engs = {"pe": nc.tensor, "act": nc.scalar, "dve": nc.vector,
                     "pool": nc.gpsimd, "sp": nc.sync}
        self.sems = {e: [] for e in self.engs}
        self.cnt = {e: 0 for e in self.engs}
        self.waited = {e: {} for e in self.engs}
        self.dma_sems = [es.enter_context(nc.semaphore(f"dq{i}")) for i in range(self.NDMA)]
        self.dma_cnt = [0] * self.NDMA
        self.dma_i = 0
        for e in self.engs:
            self._new_epoch(e)

    def _new_epoch(self, e):
        s = self.es.enter_context(self.nc.semaphore(f"s_{e}_{len(self.sems[e])}"))
        self.sems[e].append(s)
        self.cnt[e] = 0

    def _wait(self, e, dep):
        if dep[0] == "dma":
            _, idx, val = dep
            key = ("dma", idx)
            sem = self.dma_sems[idx]
        else:
            de, ep, val = dep
            key = (de, ep)
            sem = self.sems[de][ep]
        if self.waited[e].get(key, 0) >= val:
            return
        self.engs[e].wait_ge(sem, val)
        self.waited[e][key] = val

    def _deps(self, e, reads, writes):
        deps = []
        for r in reads:
            if r.lw is not None and not (r.lw[0] == e and e == "pe"):
                deps.append(r.lw)
        for w in writes:
            if w.lw is not None and w.lw[0] != e:
                deps.append(w.lw)
            for d in w.rd:
                if d[0] != e:
                    deps.append(d)
        return deps

    def _mark(self, tag, reads, writes):
        for r in reads:
            r.rd.append(tag)
            if len(r.rd) > 64:
                r.rd = r.rd[-48:]
        for w in writes:
            w.lw = tag
            w.rd = []

    def op(self, e, fn, reads=(), writes=()):
        for d in self._deps(e, reads, writes):
            self._wait(e, d)
        if self.cnt[e] >= self.EPOCH:
            self._new_epoch(e)
        ins = fn(self.engs[e])
        ep = len(self.sems[e]) - 1
        ins.then_inc(self.sems[e][ep], 1)
        self.cnt[e] += 1
        tag = (e, ep, self.cnt[e])
        self._mark(tag, reads, writes)
        return tag

    def dma(self, q, out, in_, reads=(), writes=(), slow=False):
        for d in self._deps(q, reads, writes):
            self._wait(q, d)
        idx = self.dma_i
        self.dma_i = (self.dma_i + 1) % self.NDMA
        kw = {"allow_slow_non_contiguous": True} if slow else {}
        self.engs[q].dma_start(out=out, in_=in_, **kw).then_inc(self.dma_sems[idx], 16)
        self.dma_cnt[idx] += 16
        tag = ("dma", idx, self.dma_cnt[idx])
        self._mark(tag, reads, writes)
        return tag

    def barrier(self):
        for e in self.engs:
            for d in self.engs:
                if d != e:
                    ep = len(self.sems[d]) - 1
                    if self.cnt[d] > 0:
                        self._wait(e, (d, ep, self.cnt[d]))
                    elif ep > 0:
                        self._wait(e, (d, ep - 1, self.EPOCH))
            for i in range(self.NDMA):
                if self.dma_cnt[i]:
                    self._wait(e, ("dma", i, self.dma_cnt[i]))


import os
VAR = int(os.environ.get('KVAR', '0'))


class _Stop(Exception):
    pass


def build(NSEQ, T, dbg=False, stop=0):
    nc = bass.Bass("TRN2", target_bir_lowering=False)
    try:
        _build(nc, NSEQ, T, dbg, stop)
    except _Stop:
        pass
    return nc


def _build(nc, NSEQ, T, dbg, stop):
    NT = T // 128
    NTOK = NSEQ * T
    di = lambda n, s: nc.dram_tensor(n, s, F32, kind="ExternalInput").ap()
    x_d = di("x", [NTOK, D])
    norm1_g = di("norm1_g", [128, 8]); w_in = di("w_in", [D, N_IN]); rw_mu = di("rw_mu", [RWC])
    w0 = di("w0", [512]); w_up_decay = di("w_up_decay", [64, 512]); a0 = di("a0", [512])
    w_up_a = di("w_up_a", [64, 512]); w_up_g = di("w_up_g", [128, 512]); k_k = di("k_k", [512])
    k_a = di("k_a", [512]); r_k = di("r_k", [512]); lnx_w = di("lnx_w", [512]); lnx_b = di("lnx_b", [512])
    qk_conv_w = di("qk_conv_w", [128, 16]); qk_conv_b = di("qk_conv_b", [128, 4])
    i_bias = di("i_bias", [4]); f_bias = di("f_bias", [4]); mh_norm_g = di("mh_norm_g", [128, 4])
    w_out = di("w_out", [D, D]); norm2_g = di("norm2_g", [128, 8]); w_ffn_up = di("w_ffn_up", [D, 2 * DFF])
    ffn_conv_w = di("ffn_conv_w", [128, NB * 3]); ffn_conv_b = di("ffn_conv_b", [128, NB])
    w_ffn_down = di("w_ffn_down", [DFF, D]); norm_f_g = di("norm_f_g", [D])
    consts = di("consts", [128, 640])
    out_d = nc.dram_tensor("out", [NTOK, D], F32, kind="ExternalOutput").ap()
    x1_d = nc.dram_tensor("x1s", [NTOK, D], F32, kind="Internal").ap()
    dbg_d = {}
    if dbg:
        for nm in ("yrw", "yml", "y", "hm"):
            dbg_d[nm] = nc.dram_tensor("d_" + nm, [NTOK, 512], F32, kind="ExternalOutput").ap()

    es0 = ExitStack()
    with es0:
        S = Sched(nc, es0)

        def chk(n):
            if stop == n:
                S.barrier()
                raise _Stop()

        def mk(es):
            def sb(n, s, d=F32):
                return es.enter_context(nc.sbuf_tensor(n, s, d)), Res(n)

            def ps(n, s, d=F32):
                return es.enter_context(nc.psum_tensor(n, s, d)), Res(n)
            return sb, ps

        sb0, ps0 = mk(es0)
        CST, rCST = sb0("CST", [128, 640])
        S.dma("sp", CST[:], consts, writes=[rCST])
        IDF = CST[:, 0:128]; MU = CST[:, 128:256]; MUI = CST[:, 256:384]; ML = CST[:, 384:512]; ONES = CST[:, 512:640]
        CSB, rCSB = sb0("CSB", [128, 128], BF16)
        S.op("dve", lambda e: e.tensor_copy(out=CSB[:], in_=CST[:, 0:128]), reads=[rCST], writes=[rCSB])
        IDB = CSB[:, 0:128]
        MUI8, rMUI8 = sb0("MUI8", [128, 128])
        S.op("dve", lambda e: e.tensor_scalar(out=MUI8[:], in0=MUI, scalar1=0.125, scalar2=None, op0=ALU.mult),
             reads=[rCST], writes=[rMUI8])
        NPF = 6
        PF = [ps0(f"PF{i}", [128, 512]) for i in range(NPF)]
        PT = [ps0(f"PT{i}", [128, 1024], BF16) for i in range(2)]
        pf_i = [0]
        pt_i = [0]

        POOLS = {"all": [0, 1, 2, 3, 4, 5], "g0": [0, 1], "g1": [2, 3], "m": [4, 5], "r": [0, 1, 2, 3]}
        pool_i = {k: 0 for k in POOLS}

        def pf(pool="all"):
            lst = POOLS[pool]
            p = PF[lst[pool_i[pool] % len(lst)]]
            pool_i[pool] += 1
            return p

        def interleave(*gens):
            gens = list(gens)
            while gens:
                for gg in list(gens):
                    try:
                        next(gg)
                        yield
                    except StopIteration:
                        gens.remove(gg)

        def ptb():
            return PT[0]

        def bc3(ap2, a, b):
            return ap2.rearrange("p (a o) -> p a o", o=1).to_broadcast([ap2.shape[0], a, b])

        def bcm(ap2, a):
            return ap2.rearrange("p (o n) -> p o n", o=1).to_broadcast([ap2.shape[0], a, ap2.shape[1]])

        def v3(ap, a):
            return ap.rearrange("p (a b) -> p a b", a=a)

        def rmsnorm_to_bf16(X, rX, HN, rHN, ST, rST):
            S.op("pool", lambda e: e.memset(ST[:, 0:1], 0.0), writes=[rST])
            S.op("act", lambda e: e.activation(out=HN[:], in_=X, func=AF.Square, accum_out=ST[:, 0:1]),
                 reads=[rX, rST], writes=[rHN, rST])
            S.op("dve", lambda e: e.tensor_scalar(out=ST[:, 1:2], in0=ST[:, 0:1], scalar1=1.0 / D, scalar2=1e-6,
                                                  op0=ALU.mult, op1=ALU.add), reads=[rST], writes=[rST])
            S.op("act", lambda e: e.activation(out=ST[:, 2:3], in_=ST[:, 1:2], func=AF.Sqrt), reads=[rST], writes=[rST])
            S.op("dve", lambda e: e.reciprocal(out=ST[:, 3:4], in_=ST[:, 2:3]), reads=[rST], writes=[rST])
            S.op("act", lambda e: e.activation(out=HN[:], in_=X, func=AF.Copy, scale=ST[:, 3:4]),
                 reads=[rX, rST], writes=[rHN])

        es1 = ExitStack()
        with es1:
            sb1, _ = mk(es1)
            WIN, rWIN = sb1("WIN", [128, 8, WIN_COLS], BF16)
            WOUT, rWOUT = sb1("WOUT", [128, 8, D], BF16)
            WLOR, rWLOR = sb1("WLOR", [128, 512], BF16)
            WG, rWG = sb1("WG", [128, 512], BF16)
            VEC, rVEC = sb1("VEC", [128, 7, 512])
            MHG4, rMHG4 = sb1("MHG4", [128, 4])
            CW, rCW = sb1("CW", [128, 4, 4])
            CB, rCB = sb1("CB", [128, 4])
            GB, rGB = sb1("GB", [128, 8])
            for i, v in enumerate((w0, a0, k_k, k_a, r_k, lnx_w, lnx_b)):
                S.dma("sp", VEC[:, i, :], v.partition_broadcast(128), writes=[rVEC])
            S.dma("sp", CW[:].rearrange("p b j -> p (b j)"), qk_conv_w, writes=[rCW])
            S.dma("sp", CB[:], qk_conv_b, writes=[rCB])
            S.dma("sp", GB[:, 0:4], i_bias.partition_broadcast(128), writes=[rGB])
            S.dma("sp", GB[:, 4:8], f_bias.partition_broadcast(128), writes=[rGB])
            W0V = VEC[:, 0, :]; A0V = VEC[:, 1, :]; KKV = VEC[:, 2, :]; KAV = VEC[:, 3, :]
            RKV = VEC[:, 4, :]; LWV = VEC[:, 5, :]; LBV = VEC[:, 6, :]
            S.dma("sp", MHG4[:], mh_norm_g, writes=[rMHG4])
            esA = ExitStack()
            with esA:
                sbA, _ = mk(esA)
                MUT, rMUT = sbA("MUT", [128, RWC])
                OMM, rOMM = sbA("OMM", [128, RWC])
                G1, rG1 = sbA("G1", [128, 8])
                STG = [sbA(f"STG{i}", [128, N_IN]) for i in range(2)]
                S.dma("sp", MUT[:], rw_mu.partition_broadcast(128), writes=[rMUT])
                S.dma("sp", G1[:], norm1_g, writes=[rG1])
                S.op("dve", lambda e: e.tensor_scalar(out=OMM[:], in0=MUT[:], scalar1=-1.0, scalar2=1.0,
                                                      op0=ALU.mult, op1=ALU.add), reads=[rMUT], writes=[rOMM])
                for k in range(8):
                    st, rst = STG[k % 2]
                    S.dma("sp", st[:], w_in[k * 128:(k + 1) * 128, :], writes=[rst])
                    S.op("act", lambda e: e.activation(out=st[:], in_=st[:], func=AF.Copy, scale=G1[:, k:k + 1]),
                         reads=[rst, rG1], writes=[rst])
                    S.op("dve", lambda e: e.tensor_tensor(out=WIN[:, k, 0:RWC], in0=st[:, 0:RWC], in1=OMM[:], op=ALU.mult),
                         reads=[rst, rOMM], writes=[rWIN])
                    S.op("pool", lambda e: e.tensor_tensor(out=WIN[:, k, RWC:2 * RWC], in0=st[:, 0:RWC], in1=MUT[:], op=ALU.mult),
                         reads=[rst, rMUT], writes=[rWIN])
                    S.op("act", lambda e: e.activation(out=WIN[:, k, MLO:WIN_COLS], in_=st[:, RWC:N_IN], func=AF.Copy),
                         reads=[rst], writes=[rWIN])
                for k in range(8):
                    st, rst = STG[k % 2]
                    S.dma("sp", st[:, 0:D], w_out[k * 128:(k + 1) * 128, :], writes=[rst])
                    if k < 4:
                        S.op("dve", lambda e: e.tensor_copy(out=WOUT[:, k, :], in_=st[:, 0:D]), reads=[rst], writes=[rWOUT])
                    else:
                        S.op("dve", lambda e: e.tensor_scalar(out=WOUT[:, k, :], in0=st[:, 0:D], scalar1=MHG4[:, k - 4:k - 3], scalar2=None, op0=ALU.mult),
                             reads=[rst, rMHG4], writes=[rWOUT])
                st, rst = STG[0]
                S.dma("sp", st[0:64, 0:512], w_up_decay, writes=[rst])
                S.dma("sp", st[64:128, 0:512], w_up_a, writes=[rst])
                S.dma("sp", st[:, 512:1024], w_up_g, writes=[rst])
                S.op("dve", lambda e: e.tensor_copy(out=WLOR[:], in_=st[:, 0:512]), reads=[rst], writes=[rWLOR])
                S.op("dve", lambda e: e.tensor_copy(out=WG[:], in_=st[:, 512:1024]), reads=[rst], writes=[rWG])
                S.barrier()
            chk(1)
            esB = ExitStack()
            with esB:
                sbB, _ = mk(esB)
                X = [sbB(f"X{i}", [128, D]) for i in range(2)]
                HN, rHN = sbB("HN", [128, D], BF16)
                HT = [sbB(f"HT{i}", [128, 8, 129], BF16) for i in range(2)]
                ST, rST = sbB("ST", [128, 8])
                R, rR = sbB("R", [128, 512]); K, rK = sbB("K", [128, 512]); V, rV = sbB("V", [128, 512])
                SG, rSG = sbB("SG", [128, 512]); AA, rAA = sbB("AA", [128, 512])
                ECL, rECL = sbB("ECL", [128, 512]); ENCL, rENCL = sbB("ENCL", [128, 512])
                KKN, rKKN = sbB("KKN", [128, 512]); KM, rKM = sbB("KM", [128, 512])
                TA, rTA = sbB("TA", [128, 512]); TB, rTB = sbB("TB", [128, 512])
                ECLM, rECLM = TA, rTA
                BON, rBON = sbB("BON", [128, 512])
                S8, rS8 = sbB("S8", [128, 64])
                WL2 = [sbB(f"WL{i}", [128, 4]) for i in range(2)]
                S8E, rS8E = sbB("S8E", [128, 24])
                RBAR, rRBAR = sbB("RBAR", [128, 512], BF16); ABAR, rABAR = sbB("ABAR", [128, 512], BF16)
                BTIL, rBTIL = sbB("BTIL", [128, 512], BF16); KTIL, rKTIL = sbB("KTIL", [128, 512], BF16)
                VB, rVB = sbB("VB", [128, 512], BF16)
                RBT, rRBT = sbB("RBT", [128, 4, 128], BF16); ABT, rABT = sbB("ABT", [128, 4, 128], BF16)
                BTT, rBTT = sbB("BTT", [128, 4, 128], BF16); KTT, rKTT = sbB("KTT", [128, 4, 128], BF16)
                AAK, rAAK = sbB("AAK", [128, 8, 128], BF16); ARKT, rARKT = sbB("ARKT", [128, 8, 128], BF16)
                ARBT, rARBT = sbB("ARBT", [128, 8, 128], BF16)
                PQ = [[sbB(f"PQ{g}{i}", [128, 4, 128], BF16) for i in range(4)] for g in range(2)]
                TT = [[sbB(f"TT{g}{i}", [128, 4, 128], BF16) for i in range(2)] for g in range(2)]
                ABPT, rABPT = sbB("ABPT", [128, 4, 128], BF16); AAKPT, rAAKPT = sbB("AAKPT", [128, 8, 128], BF16)
                UB, rUB = sbB("UB", [128, 512], BF16)
                YF, rYF = sbB("YF", [128, 512]); TB2, rTB2 = sbB("TB2", [128, 512])
                HF = [sbB("HF", [128, 4, 64])] * NSEQ
                HB, rHB = sbB("HB", [128, 4, 64], BF16)
                QKC, rQKC = sbB("QKC", [128, 4, 131])
                ACC_, rACC = sbB("ACC", [128, 512]); ACC = v3(ACC_[:], 4)
                QKT, rQKT = sbB("QKT", [128, 4, 128], BF16)
                KP, rKP = sbB("KP", [128, 4, 64], BF16)
                VE, rVE = sbB("VE", [128, 4, 129], BF16)
                G8, rG8 = sbB("G8", [128, 48])
                TM, rTM = ACC_, rACC; DG, rDG = v3(TM[:], 4), rTM
                MST = [sbB("MST", [128, 4])] * NSEQ
                CF = [sbB("CF", [128, 2, 129])] * NSEQ
                CBF, rCBF = sbB("CBF", [128, 2, 129], BF16)
                PTB, rPTB = sbB("PTB", [128, 4, 128], BF16)
                HM_, rHM = sbB("HM", [128, 512]); HM = v3(HM_[:], 4)
                SO, rSO = sbB("SO", [128, 512])
                MIX, rMIX = sbB("MIX", [128, D], BF16)
                MIXT, rMIXT = AAKPT, rAAKPT
                DBG, rDBG = sbB("DBG", [128, 512]) if dbg else (None, None)

                S.op("pool", lambda e: e.memset(VE[:], 1.0), writes=[rVE])

                def dbg_out(nm, ap, rr, r0):
                    if dbg:
                        S.dma("sp", dbg_d[nm][r0:r0 + 128, :], ap, reads=[rr])

                LT2 = [sbB(f"LTb{i}", [128, 256], BF16) for i in range(2)]
                tiles = [(s_, c_) for s_ in range(NSEQ) for c_ in range(NT)]
                EP, rEP = PT[1]
                EPF = EP[:].bitcast(F32)

                def mk_proj(HTx):
                    cur = lambda k: HTx[:, k, 1:129]
                    prv = lambda k: HTx[:, k, 0:128]

                    def proj_tok(p, col, n, shifted):
                        def f(e):
                            last = None
                            nm = 16 if shifted else 8
                            i = 0
                            for k in range(8):
                                last = e.matmul(p, lhsT=cur(k), rhs=WIN[:, k, col:col + n], start=(i == 0), stop=(i == nm - 1)); i += 1
                                if shifted:
                                    last = e.matmul(p, lhsT=prv(k), rhs=WIN[:, k, RWC + col:RWC + col + n], start=False, stop=(i == nm - 1)); i += 1
                            return last
                        return f

                    def proj_feat(p, col, shifted):
                        def f(e):
                            last = None
                            nm = 16 if shifted else 8
                            i = 0
                            for k in range(8):
                                last = e.matmul(p, lhsT=WIN[:, k, col:col + 128], rhs=cur(k), start=(i == 0), stop=(i == nm - 1)); i += 1
                                if shifted:
                                    last = e.matmul(p, lhsT=WIN[:, k, RWC + col:RWC + col + 128], rhs=prv(k), start=False, stop=(i == nm - 1)); i += 1
                            return last
                        return f
                    return cur, prv, proj_tok, proj_feat

                def early(ti):
                    s_, c_ = tiles[ti]
                    r0_ = s_ * T + c_ * 128
                    Xn, rXn = X[ti % 2]
                    HTn, rHTn = HT[ti % 2]
                    HTq, rHTq = HT[(ti + 1) % 2]
                    LTn, rLTn = LT2[ti % 2]
                    _, _, ptok, pfeat = mk_proj(HTn)
                    S.dma("sp", Xn[:], x_d[r0_:r0_ + 128, :], writes=[rXn])
                    yield
                    rmsnorm_to_bf16(Xn[:], rXn, HN, rHN, ST, rST)
                    yield
                    yield S.op("pe", lambda e: [e.transpose(out=EP[:, k * 128:(k + 1) * 128], in_=HN[:, k * 128:(k + 1) * 128],
                                                            identity=IDB) for k in range(8)][-1], reads=[rHN, rCSB], writes=[rEP])
                    yield S.op("dve", lambda e: e.tensor_copy(out=HTn[:, :, 1:129], in_=v3(EP[:], 8)), reads=[rEP], writes=[rHTn])
                    if c_ == 0:
                        yield S.op("pool", lambda e: e.memset(HTn[:, :, 0:1], 0.0), writes=[rHTn])
                    else:
                        yield S.op("pool", lambda e: e.tensor_copy(out=HTn[:, :, 0:1], in_=HTq[:, :, 128:129]), reads=[rHTq], writes=[rHTn])
                    yield S.op("pe", pfeat(EPF[:, 0:128], 1536, True), reads=[rHTn, rWIN], writes=[rEP])
                    yield S.op("pe", pfeat(EPF[:, 128:256], 1664, True), reads=[rHTn, rWIN], writes=[rEP])
                    yield S.op("act", lambda e: e.activation(out=LTn[0:64, 0:128], in_=EPF[0:64, 0:128], func=AF.Tanh), reads=[rEP], writes=[rLTn])
                    yield S.op("act", lambda e: e.activation(out=LTn[:, 128:256], in_=EPF[:, 128:256], func=AF.Sigmoid), reads=[rEP], writes=[rLTn])
                    yield S.op("act", lambda e: e.activation(out=LTn[64:128, 0:128], in_=EPF[64:128, 0:128], func=AF.Copy), reads=[rEP], writes=[rLTn])
                    for g_, (dst, rdst) in ((1, (K, rK)), (0, (R, rR)), (2, (V, rV))):
                        yield S.op("pe", ptok(EPF, g_ * 512, 512, True), reads=[rHTn, rWIN], writes=[rEP])
                        yield S.op("act", lambda e: e.activation(out=dst[:], in_=EPF, func=AF.Copy), reads=[rEP], writes=[rdst])
                    WLn, rWLn = WL2[ti % 2]
                    pw, rpw = EPF, rEP
                    yield S.op("pe", lambda e: e.matmul(pw[:], lhsT=LTn[0:64, 0:128], rhs=WLOR[0:64, :], start=True, stop=True),
                         reads=[rLTn, rWLOR], writes=[rpw])
                    yield S.op("dve", lambda e: e.tensor_tensor(out=SG[:], in0=pw[:], in1=W0V, op=ALU.add), reads=[rpw, rVEC], writes=[rSG])
                    yield S.op("act", lambda e: e.activation(out=SG[:], in_=SG[:], func=AF.Sigmoid), reads=[rSG], writes=[rSG])
                    pa, rpa = EPF, rEP
                    yield S.op("pe", lambda e: e.matmul(pa[:], lhsT=LTn[64:128, 0:128], rhs=WLOR[64:128, :], start=True, stop=True),
                         reads=[rLTn, rWLOR], writes=[rpa])
                    yield S.op("dve", lambda e: e.tensor_tensor(out=AA[:], in0=pa[:], in1=A0V, op=ALU.add), reads=[rpa, rVEC], writes=[rAA])
                    yield S.op("act", lambda e: e.activation(out=AA[:], in_=AA[:], func=AF.Sigmoid), reads=[rAA], writes=[rAA])
                    pc, rpc = EPF, rEP
                    yield S.op("pe", lambda e: e.matmul(pc[:], lhsT=MUI, rhs=SG[:], start=True, stop=True), reads=[rCST, rSG], writes=[rpc])
                    yield S.op("act", lambda e: e.activation(out=ECL[:], in_=pc[:], func=AF.Exp, scale=-CDEC), reads=[rpc], writes=[rECL])
                    yield S.op("act", lambda e: e.activation(out=ENCL[:], in_=pc[:], func=AF.Exp, scale=CDEC), reads=[rpc], writes=[rENCL])
                    yield S.op("dve", lambda e: e.tensor_tensor(out=TA[:], in0=pc[:], in1=SG[:], op=ALU.subtract), reads=[rpc, rSG], writes=[rTA])
                    yield S.op("act", lambda e: e.activation(out=ECLM[:], in_=TA[:], func=AF.Exp, scale=-CDEC), reads=[rTA], writes=[rECLM])
                    pwl, rpwl = EPF, rEP
                    yield S.op("pe", lambda e: [e.matmul(pwl[(h % 2) * 64:(h % 2) * 64 + 64, h // 2:h // 2 + 1], lhsT=SG[:, h * 64:(h + 1) * 64], rhs=ONES[:, 0:1], start=True, stop=True)
                                          for h in range(8)][-1], reads=[rSG, rCST], writes=[rpwl])
                    yield S.op("act", lambda e: e.activation(out=WLn[:], in_=pwl[:, 0:4], func=AF.Exp, scale=-CDEC), reads=[rpwl], writes=[rWLn])
                    yield S.op("dve", lambda e: e.tensor_tensor(out=KKN[:], in0=K[:], in1=KKV, op=ALU.mult), reads=[rK, rVEC], writes=[rKKN])
                    yield S.op("pool", lambda e: e.tensor_tensor(out=TB[:], in0=KKN[:], in1=KKN[:], op=ALU.mult), reads=[rKKN], writes=[rTB])
                    yield S.op("dve", lambda e: e.tensor_reduce(out=S8E[:, 0:8], in_=v3(TB[:], 8), axis=AX.X, op=ALU.add), reads=[rTB], writes=[rS8E])
                    yield S.op("act", lambda e: e.activation(out=S8E[:, 8:16], in_=S8E[:, 0:8], func=AF.Sqrt), reads=[rS8E], writes=[rS8E])
                    yield S.op("dve", lambda e: e.tensor_scalar(out=S8E[:, 8:16], in0=S8E[:, 8:16], scalar1=1e-12, scalar2=None, op0=ALU.max),
                         reads=[rS8E], writes=[rS8E])
                    yield S.op("dve", lambda e: e.reciprocal(out=S8E[:, 16:24], in_=S8E[:, 8:16]), reads=[rS8E], writes=[rS8E])
                    yield S.op("dve", lambda e: e.tensor_tensor(out=v3(KKN[:], 8), in0=v3(KKN[:], 8), in1=bc3(S8E[:, 16:24], 8, 64), op=ALU.mult),
                         reads=[rKKN, rS8E], writes=[rKKN])
                    yield S.op("dve", lambda e: e.scalar_tensor_tensor(out=TB[:], in0=AA[:], scalar=-1.0, in1=KAV, op0=ALU.add, op1=ALU.mult),
                         reads=[rAA, rVEC], writes=[rTB])
                    yield S.op("dve", lambda e: e.scalar_tensor_tensor(out=KM[:], in0=TB[:], scalar=1.0, in1=K[:], op0=ALU.add, op1=ALU.mult),
                         reads=[rTB, rK], writes=[rKM])

                for ti, (s, c) in enumerate(tiles):
                    if True:
                        HFs, rHFs = HF[s]
                        CFs, rCFs = CF[s]
                        Ms, rMs = MST[s]
                        if c == 0:
                            S.op("pool", lambda e: e.memset(HFs[:], 0.0), writes=[rHFs])
                            S.op("pool", lambda e: e.memset(CFs[:], 0.0), writes=[rCFs])
                            S.op("pool", lambda e: e.memset(Ms[:], 0.0), writes=[rMs])
                        r0 = s * T + c * 128
                        Xc, rXc = X[ti % 2]
                        HTc, rHTc = HT[ti % 2]
                        LT, rLT = LT2[ti % 2]
                        WL, rWL = WL2[ti % 2]
                        cur, prv, proj_tok, proj_feat = mk_proj(HTc)
                        if ti == 0:
                            for _ in early(0):
                                pass
                        chk(2)
                        S.op("dve", lambda e: e.tensor_tensor(out=RBAR[:], in0=R[:], in1=ECL[:], op=ALU.mult), reads=[rR, rECL], writes=[rRBAR])
                        S.op("dve", lambda e: e.scalar_tensor_tensor(out=ABAR[:], in0=KKN[:], scalar=-1.0, in1=ECLM[:], op0=ALU.mult, op1=ALU.mult),
                             reads=[rKKN, rECLM], writes=[rABAR])
                        S.op("pool", lambda e: e.tensor_tensor(out=TA[:], in0=KKN[:], in1=AA[:], op=ALU.mult), reads=[rKKN, rAA], writes=[rTA])
                        S.op("pool", lambda e: e.tensor_tensor(out=BTIL[:], in0=TA[:], in1=ENCL[:], op=ALU.mult), reads=[rTA, rENCL], writes=[rBTIL])
                        S.op("dve", lambda e: e.tensor_tensor(out=KTIL[:], in0=KM[:], in1=ENCL[:], op=ALU.mult), reads=[rKM, rENCL], writes=[rKTIL])
                        S.op("act", lambda e: e.activation(out=VB[:], in_=V[:], func=AF.Copy), reads=[rV], writes=[rVB])
                        S.op("pool", lambda e: e.tensor_tensor(out=TB[:], in0=R[:], in1=KM[:], op=ALU.mult), reads=[rR, rKM], writes=[rTB])
                        S.op("pool", lambda e: e.tensor_tensor(out=TB[:], in0=TB[:], in1=RKV, op=ALU.mult), reads=[rTB, rVEC], writes=[rTB])
                        S.op("dve", lambda e: e.tensor_reduce(out=S8[:, 24:32], in_=v3(TB[:], 8), axis=AX.X, op=ALU.add), reads=[rTB], writes=[rS8])
                        S.op("dve", lambda e: e.tensor_tensor(out=v3(BON[:], 8), in0=v3(V[:], 8), in1=bc3(S8[:, 24:32], 8, 64), op=ALU.mult),
                             reads=[rV, rS8], writes=[rBON])
                        for src, rsrc, dst, rdst in ((RBAR, rRBAR, RBT, rRBT), (ABAR, rABAR, ABT, rABT),
                                                     (BTIL, rBTIL, BTT, rBTT), (KTIL, rKTIL, KTT, rKTT)):
                            pt, rpt = ptb()
                            S.op("pe", lambda e: [e.transpose(out=pt[:, b * 128:(b + 1) * 128], in_=src[:, b * 128:(b + 1) * 128],
                                                              identity=IDB) for b in range(4)][-1], reads=[rsrc, rCSB], writes=[rpt])
                            S.op("act", lambda e: e.activation(out=dst[:], in_=v3(pt[:, 0:512], 4), func=AF.Copy), reads=[rpt], writes=[rdst])

                        chk(4)

                        def hop(Tt, h):
                            return Tt[(h % 2) * 64:(h % 2) * 64 + 64, h // 2, :]

                        def rw_group(g):
                            hs = [g, g + 2, g + 4, g + 6]
                            P0, rP0 = PQ[g][0]; Q0, rQ0 = PQ[g][1]

                            def mm4(p, A, Bm):
                                return lambda e: [e.matmul(p[:, j * 128:(j + 1) * 128], lhsT=hop(A, h), rhs=hop(Bm, h), start=True, stop=True)
                                                  for j, h in enumerate(hs) if (VAR != 3 or h % 2 == 0) and (VAR != 4 or h % 2 == 1)][-1]
                            for (A, rA, Bm, rB, dst, rdst, mask, eng) in (
                                    (ABT, rABT, BTT, rBTT, P0[:], rP0, ML, "dve"),
                                    (BTT, rBTT, ABT, rABT, Q0[:], rQ0, MU, "dve"),
                                    (ABT, rABT, KTT, rKTT, AAK[:, 4 * g:4 * g + 4, :], rAAK, ML, "dve"),
                                    (KTT, rKTT, RBT, rRBT, ARKT[:, 4 * g:4 * g + 4, :], rARKT, MUI, "dve"),
                                    (BTT, rBTT, RBT, rRBT, ARBT[:, 4 * g:4 * g + 4, :], rARBT, MUI, "dve")):
                                p, rp = pf("g%d" % g)
                                yield S.op("pe", mm4(p, A, Bm), reads=[rA, rB], writes=[rp])
                                if VAR in (1, 3, 4):
                                    pass
                                elif VAR == 2:
                                    for j4 in range(4):
                                        yield S.op(eng, lambda e: e.tensor_tensor(out=dst[:, j4, :], in0=p[:, j4 * 128:(j4 + 1) * 128], in1=mask, op=ALU.mult),
                                             reads=[rp, rCST], writes=[rdst])
                                else:
                                    yield S.op(eng, lambda e: e.tensor_tensor(out=dst, in0=v3(p[:], 4), in1=bcm(mask, 4), op=ALU.mult),
                                         reads=[rp, rCST], writes=[rdst])
                            Tc, rTc = TT[g][0]
                            yield S.op("pool", lambda e: e.tensor_tensor(out=Tc[:], in0=Q0[:], in1=bcm(IDB, 4), op=ALU.add),
                                 reads=[rQ0, rCSB], writes=[rTc])
                            Pp, rPp, Qp, rQp = P0, rP0, Q0, rQ0
                            ti = 0
                            for lev in range(1, 7):
                                Pn, rPn = PQ[g][2 * (lev % 2)]
                                Qn, rQn = PQ[g][2 * (lev % 2) + 1]
                                p, rp = pf("g%d" % g)
                                yield S.op("pe", lambda e: [e.matmul(p[:, j * 128:(j + 1) * 128], lhsT=Qp[:, j, :], rhs=Pp[:, j, :], start=True, stop=True)
                                                      for j in range(4)][-1], reads=[rPp, rQp], writes=[rp])
                                yield S.op("act", lambda e: e.activation(out=Pn[:], in_=v3(p[:], 4), func=AF.Copy), reads=[rp], writes=[rPn])
                                if lev < 6:
                                    p2, rp2 = pf("g%d" % g)
                                    yield S.op("pe", lambda e: [e.matmul(p2[:, j * 128:(j + 1) * 128], lhsT=Pp[:, j, :], rhs=Qp[:, j, :], start=True, stop=True)
                                                          for j in range(4)][-1], reads=[rPp, rQp], writes=[rp2])
                                    yield S.op("act", lambda e: e.activation(out=Qn[:], in_=v3(p2[:], 4), func=AF.Copy), reads=[rp2], writes=[rQn])
                                Tn, rTn = TT[g][(ti + 1) % 2]
                                p3, rp3 = pf("g%d" % g)
                                yield S.op("pe", lambda e: [e.matmul(p3[:, j * 128:(j + 1) * 128], lhsT=Pn[:, j, :], rhs=Tc[:, j, :], start=True, stop=True)
                                                      for j in range(4)][-1], reads=[rPn, rTc], writes=[rp3])
                                yield S.op("dve", lambda e: e.tensor_tensor(out=Tn[:], in0=v3(p3[:], 4), in1=Tc[:], op=ALU.add),
                                     reads=[rp3, rTc], writes=[rTn])
                                Tc, rTc = Tn, rTn
                                ti += 1
                                Pp, rPp, Qp, rQp = Pn, rPn, Qn, rQn
                            p, rp = pf("g%d" % g)
                            yield S.op("pe", lambda e: [e.matmul(p[g * 64:g * 64 + 64, j * 128:(j + 1) * 128], lhsT=ABAR[:, h * 64:(h + 1) * 64], rhs=Tc[:, j, :],
                                                           start=True, stop=True) for j, h in enumerate(hs)][-1],
                                 reads=[rABAR, rTc], writes=[rp])
                            yield S.op("act", lambda e: e.activation(out=ABPT[g * 64:g * 64 + 64, :, :], in_=v3(p[g * 64:g * 64 + 64, :], 4), func=AF.Copy),
                                 reads=[rp], writes=[rABPT])
                            p, rp = pf("g%d" % g)
                            yield S.op("pe", lambda e: [e.matmul(p[:, j * 128:(j + 1) * 128], lhsT=AAK[:, 4 * g + j, :], rhs=Tc[:, j, :],
                                                           start=True, stop=True) for j, h in enumerate(hs)][-1],
                                 reads=[rAAK, rTc], writes=[rp])
                            yield S.op("dve", lambda e: e.tensor_copy(out=AAKPT[:, 4 * g:4 * g + 4, :], in_=v3(p[:], 4)), reads=[rp], writes=[rAAKPT])


                        def chain_R():
                            yield from interleave(rw_group(0), rw_group(1))
                            yield S.op("act", lambda e: e.activation(out=HB[:], in_=HFs[:], func=AF.Copy), reads=[rHFs], writes=[rHB])
                            pu, rpu = pf("r")

                            def fu(e):
                                last = None
                                for h in range(8):
                                    e.matmul(pu[:, h * 64:(h + 1) * 64], lhsT=hop(ABPT, h), rhs=hop(HB, h), start=True, stop=False)
                                    last = e.matmul(pu[:, h * 64:(h + 1) * 64], lhsT=AAKPT[:, (h % 2) * 4 + h // 2, :], rhs=VB[:, h * 64:(h + 1) * 64], start=False, stop=True)
                                return last
                            yield S.op("pe", fu, reads=[rABPT, rHB, rAAKPT, rVB], writes=[rpu])
                            yield S.op("act", lambda e: e.activation(out=UB[:], in_=pu[:], func=AF.Copy), reads=[rpu], writes=[rUB])
                            py, rpy = pf("r")

                            def fy(e):
                                last = None
                                for h in range(8):
                                    o = py[:, h * 64:(h + 1) * 64]
                                    e.matmul(o, lhsT=hop(RBT, h), rhs=hop(HB, h), start=True, stop=False)
                                    e.matmul(o, lhsT=ARBT[:, (h % 2) * 4 + h // 2, :], rhs=UB[:, h * 64:(h + 1) * 64], start=False, stop=False)
                                    last = e.matmul(o, lhsT=ARKT[:, (h % 2) * 4 + h // 2, :], rhs=VB[:, h * 64:(h + 1) * 64], start=False, stop=True)
                                return last
                            yield S.op("pe", fy, reads=[rRBT, rHB, rARBT, rUB, rARKT, rVB], writes=[rpy])
                            ph, rph = pf("r")

                            def fh(e):
                                last = None
                                for h in range(8):
                                    o = ph[(h % 2) * 64:(h % 2) * 64 + 64, (h // 2) * 64:(h // 2 + 1) * 64]
                                    e.matmul(o, lhsT=BTIL[:, h * 64:(h + 1) * 64], rhs=UB[:, h * 64:(h + 1) * 64], start=True, stop=False)
                                    last = e.matmul(o, lhsT=KTIL[:, h * 64:(h + 1) * 64], rhs=VB[:, h * 64:(h + 1) * 64], start=False, stop=True)
                                return last
                            yield S.op("pe", fh, reads=[rBTIL, rKTIL, rUB, rVB], writes=[rph])
                            yield S.op("dve", lambda e: e.tensor_tensor(out=HFs[:], in0=v3(ph[:, 0:256], 4), in1=HFs[:], op=ALU.add),
                                 reads=[rph, rHFs], writes=[rHFs])
                            yield S.op("dve", lambda e: e.tensor_tensor(out=HFs[:], in0=HFs[:], in1=bc3(WL[:], 4, 64), op=ALU.mult),
                                 reads=[rHFs, rWL], writes=[rHFs])
                            yield S.op("act", lambda e: e.activation(out=YF[:], in_=py[:], func=AF.Copy), reads=[rpy], writes=[rYF])
                            dbg_out("y", YF[:], rYF, r0)
                            yield S.op("dve", lambda e: e.tensor_reduce(out=S8[:, 32:40], in_=v3(YF[:], 8), axis=AX.X, op=ALU.add), reads=[rYF], writes=[rS8])
                            yield S.op("dve", lambda e: e.tensor_scalar(out=S8[:, 32:40], in0=S8[:, 32:40], scalar1=1.0 / 64, scalar2=None, op0=ALU.mult),
                                 reads=[rS8], writes=[rS8])
                            yield S.op("dve", lambda e: e.tensor_tensor(out=v3(YF[:], 8), in0=v3(YF[:], 8), in1=bc3(S8[:, 32:40], 8, 64), op=ALU.subtract),
                                 reads=[rYF, rS8], writes=[rYF])
                            yield S.op("pool", lambda e: e.tensor_tensor(out=TB2[:], in0=YF[:], in1=YF[:], op=ALU.mult), reads=[rYF], writes=[rTB2])
                            yield S.op("dve", lambda e: e.tensor_reduce(out=S8[:, 40:48], in_=v3(TB2[:], 8), axis=AX.X, op=ALU.add), reads=[rTB2], writes=[rS8])
                            yield S.op("dve", lambda e: e.tensor_scalar(out=S8[:, 40:48], in0=S8[:, 40:48], scalar1=1.0 / 64, scalar2=64e-5,
                                                                  op0=ALU.mult, op1=ALU.add), reads=[rS8], writes=[rS8])
                            yield S.op("act", lambda e: e.activation(out=S8[:, 40:48], in_=S8[:, 40:48], func=AF.Sqrt), reads=[rS8], writes=[rS8])
                            yield S.op("dve", lambda e: e.reciprocal(out=S8[:, 48:56], in_=S8[:, 40:48]), reads=[rS8], writes=[rS8])
                            yield S.op("dve", lambda e: e.tensor_tensor(out=v3(YF[:], 8), in0=v3(YF[:], 8), in1=bc3(S8[:, 48:56], 8, 64), op=ALU.mult),
                                 reads=[rYF, rS8], writes=[rYF])
                            yield S.op("pool", lambda e: e.tensor_tensor(out=YF[:], in0=YF[:], in1=LWV, op=ALU.mult), reads=[rYF, rVEC], writes=[rYF])
                            yield S.op("pool", lambda e: e.tensor_tensor(out=YF[:], in0=YF[:], in1=LBV, op=ALU.add), reads=[rYF, rVEC], writes=[rYF])
                            yield S.op("pool", lambda e: e.tensor_tensor(out=YF[:], in0=YF[:], in1=BON[:], op=ALU.add), reads=[rYF, rBON], writes=[rYF])
                            pg, rpg = pf("r")
                            yield S.op("pe", lambda e: e.matmul(pg[:], lhsT=LT[:, 128:256], rhs=WG[:], start=True, stop=True), reads=[rLT, rWG], writes=[rpg])
                            if dbg:
                                yield S.op("dve", lambda e: e.tensor_tensor(out=DBG[:], in0=YF[:], in1=pg[:], op=ALU.mult), reads=[rYF, rpg], writes=[rDBG])
                                dbg_out("yrw", DBG[:], rDBG, r0)
                            yield S.op("dve", lambda e: e.tensor_tensor(out=MIX[:, 0:512], in0=YF[:], in1=pg[:], op=ALU.mult), reads=[rYF, rpg], writes=[rMIX])


                        def chain_M():
                            pqk, rpqk = pf("m")
                            for b in range(4):
                                yield S.op("pe", proj_feat(pqk[:, b * 128:(b + 1) * 128], MLO - 0 + b * 128 if False else 0, False) if False else
                                     (lambda e, b=b: [e.matmul(pqk[:, b * 128:(b + 1) * 128], lhsT=WIN[:, k, MLO + b * 128:MLO + (b + 1) * 128], rhs=cur(k),
                                                              start=(k == 0), stop=(k == 7)) for k in range(8)][-1]),
                                     reads=[rHTc, rWIN], writes=[rpqk])
                            if c == 0:
                                yield S.op("pool", lambda e: e.memset(QKC[:, :, 0:3], 0.0), writes=[rQKC])
                            else:
                                yield S.op("pool", lambda e: e.tensor_copy(out=QKC[:, :, 0:3], in_=QKC[:, :, 128:131]), reads=[rQKC], writes=[rQKC])
                            yield S.op("act", lambda e: e.activation(out=QKC[:, :, 3:131], in_=v3(pqk[:], 4), func=AF.Copy), reads=[rpqk], writes=[rQKC])
                            for b in range(4):
                                eng = "dve"
                                yield S.op(eng, lambda e, b=b: e.tensor_scalar(out=ACC[:, b, :], in0=QKC[:, b, 0:128], scalar1=CW[:, b, 0:1], scalar2=CB[:, b:b + 1],
                                                                         op0=ALU.mult, op1=ALU.add), reads=[rQKC, rCW, rCB], writes=[rACC])
                                for j in range(1, 4):
                                    yield S.op(eng, lambda e, b=b, j=j: e.scalar_tensor_tensor(out=ACC[:, b, :], in0=QKC[:, b, j:j + 128], scalar=CW[:, b, j:j + 1],
                                                                                        in1=ACC[:, b, :], op0=ALU.mult, op1=ALU.add),
                                         reads=[rQKC, rCW, rACC], writes=[rACC])
                            yield S.op("act", lambda e: e.activation(out=QKT[:], in_=ACC, func=AF.Silu), reads=[rACC], writes=[rQKT])
                            pv, rpv = pf("m")
                            yield S.op("pe", proj_tok(pv[:], MLO + 512 - 0, 512, False) if False else
                                 (lambda e: [e.matmul(pv[:], lhsT=cur(k), rhs=WIN[:, k, MLO + 512:MLO + 1024], start=(k == 0), stop=(k == 7)) for k in range(8)][-1]),
                                 reads=[rHTc, rWIN], writes=[rpv])
                            yield S.op("act", lambda e: e.activation(out=VE[:, :, 0:128], in_=v3(pv[:], 4), func=AF.Copy), reads=[rpv], writes=[rVE])
                            po, rpo = pf("m")
                            yield S.op("pe", lambda e: [e.matmul(po[:], lhsT=cur(k), rhs=WIN[:, k, MLO + 1024:MLO + 1536], start=(k == 0), stop=(k == 7)) for k in range(8)][-1],
                                 reads=[rHTc, rWIN], writes=[rpo])
                            yield S.op("act", lambda e: e.activation(out=SO[:], in_=po[:], func=AF.Sigmoid), reads=[rpo], writes=[rSO])
                            pgt, rpgt = pf("m")
                            yield S.op("pe", lambda e: [e.matmul(pgt[:, 0:8], lhsT=cur(k), rhs=WIN[:, k, MLO + 1536:MLO + 1544], start=(k == 0), stop=(k == 7)) for k in range(8)][-1],
                                 reads=[rHTc, rWIN], writes=[rpgt])
                            yield S.op("dve", lambda e: e.tensor_tensor(out=G8[:, 0:8], in0=pgt[:, 0:8], in1=GB[:], op=ALU.add), reads=[rpgt, rGB], writes=[rG8])
                            yield S.op("act", lambda e: e.activation(out=G8[:, 0:8], in_=G8[:, 0:8], func=AF.Tanh, scale=1.0 / 15), reads=[rG8], writes=[rG8])
                            yield S.op("act", lambda e: e.activation(out=G8[:, 8:12], in_=G8[:, 4:8], func=AF.Exp, scale=-15.0), reads=[rG8], writes=[rG8])
                            yield S.op("dve", lambda e: e.tensor_scalar(out=G8[:, 8:12], in0=G8[:, 8:12], scalar1=1.0, scalar2=None, op0=ALU.add), reads=[rG8], writes=[rG8])
                            yield S.op("act", lambda e: e.activation(out=G8[:, 12:16], in_=G8[:, 8:12], func=AF.Ln), reads=[rG8], writes=[rG8])
                            pb, rpb = pf("m")
                            yield S.op("pe", lambda e: e.matmul(pb[:, 0:4], lhsT=MUI, rhs=G8[:, 12:16], start=True, stop=True), reads=[rCST, rG8], writes=[rpb])
                            yield S.op("pe", lambda e: e.matmul(pb[:, 4:8], lhsT=ONES, rhs=G8[:, 12:16], start=True, stop=True), reads=[rCST, rG8], writes=[rpb])
                            yield S.op("dve", lambda e: e.scalar_tensor_tensor(out=G8[:, 16:20], in0=G8[:, 0:4], scalar=15.0, in1=pb[:, 0:4], op0=ALU.mult, op1=ALU.add),
                                 reads=[rG8, rpb], writes=[rG8])
                            yield S.op("dve", lambda e: e.tensor_tensor(out=DG, in0=bcm(IDF, 4), in1=bc3(G8[:, 16:20], 4, 128), op=ALU.mult),
                                 reads=[rCST, rG8], writes=[rDG])
                            pgm, rpgm = pf("m")
                            yield S.op("pe", lambda e: e.matmul(pgm[:], lhsT=ONES, rhs=TM[:], start=True, stop=True),
                                 reads=[rCST, rDG], writes=[rpgm])
                            yield S.op("dve", lambda e: e.tensor_reduce(out=G8[:, 20:24], in_=v3(pgm[:], 4), axis=AX.X, op=ALU.max), reads=[rpgm], writes=[rG8])
                            yield S.op("dve", lambda e: e.tensor_tensor(out=G8[:, 20:24], in0=G8[:, 20:24], in1=Ms[:], op=ALU.max), reads=[rG8, rMs], writes=[rG8])
                            yield S.op("dve", lambda e: e.tensor_tensor(out=G8[:, 40:44], in0=G8[:, 16:20], in1=G8[:, 20:24], op=ALU.subtract), reads=[rG8], writes=[rG8])
                            yield S.op("act", lambda e: e.activation(out=G8[:, 24:28], in_=G8[:, 40:44], func=AF.Exp), reads=[rG8], writes=[rG8])
                            yield S.op("dve", lambda e: e.tensor_tensor(out=G8[:, 44:48], in0=Ms[:], in1=G8[:, 20:24], op=ALU.subtract), reads=[rG8, rMs], writes=[rG8])
                            yield S.op("act", lambda e: e.activation(out=G8[:, 28:32], in_=G8[:, 44:48], func=AF.Exp), reads=[rG8], writes=[rG8])
                            yield S.op("dve", lambda e: e.tensor_scalar(out=G8[:, 36:40], in0=G8[:, 28:32], scalar1=0.125, scalar2=None, op0=ALU.mult), reads=[rG8], writes=[rG8])
                            yield S.op("dve", lambda e: e.tensor_tensor(out=G8[:, 40:44], in0=pb[:, 0:4], in1=G8[:, 20:24], op=ALU.subtract), reads=[rpb, rG8], writes=[rG8])
                            yield S.op("act", lambda e: e.activation(out=G8[:, 32:36], in_=G8[:, 40:44], func=AF.Exp), reads=[rG8], writes=[rG8])
                            yield S.op("dve", lambda e: e.tensor_tensor(out=Ms[:], in0=G8[:, 20:24], in1=pb[:, 4:8], op=ALU.subtract), reads=[rG8, rpb], writes=[rMs])
                            pt, rpt = ptb()
                            yield S.op("pe", lambda e: [e.transpose(out=pt[:, b * 128:(b + 1) * 128], in_=QKT[:, 2 + b, :], identity=IDB) for b in range(2)][-1],
                                 reads=[rQKT, rCSB], writes=[rpt])
                            yield S.op("dve", lambda e: e.tensor_tensor(out=KP[:], in0=v3(pt[:, 0:256], 4), in1=bc3(G8[:, 24:28], 4, 64), op=ALU.mult),
                                 reads=[rpt, rG8], writes=[rKP])
                            psts = [pf("m"), pf("m")]
                            for par in range(2):
                                pst, rpst = psts[par]
                                yield S.op("pe", lambda e: [e.matmul(pst[:, (h // 2) * 128:(h // 2 + 1) * 128], lhsT=QKT[par * 64:par * 64 + 64, 2 + h // 2, :],
                                                               rhs=QKT[par * 64:par * 64 + 64, h // 2, :], start=True, stop=True) for h in (par, par + 2)][-1],
                                     reads=[rQKT], writes=[rpst])
                            for h in range(4):
                                pst, rpst = psts[h % 2]
                                yield S.op("dve", lambda e, h=h: e.scalar_tensor_tensor(out=PTB[:, h, :], in0=pst[:, (h // 2) * 128:(h // 2 + 1) * 128], scalar=G8[:, 24 + h:25 + h],
                                                                                 in1=MUI8[:], op0=ALU.mult, op1=ALU.mult), reads=[rpst, rG8, rMUI8], writes=[rPTB])
                            for h in range(4):
                                po_ = (h % 2) * 64
                                yield S.op("dve", lambda e, h=h: e.tensor_scalar(out=CBF[po_:po_ + 64, h // 2, :], in0=CFs[po_:po_ + 64, h // 2, :],
                                                                            scalar1=G8[po_:po_ + 64, 36 + h:37 + h], scalar2=None, op0=ALU.mult),
                                     reads=[rCFs, rG8], writes=[rCBF])
                            pn = [pf("m"), pf("m")]
                            for i2 in range(2):
                                pnn, rpnn = pn[i2]

                                def fn(e, i2=i2, pnn=pnn):
                                    last = None
                                    for j in range(2):
                                        h = 2 * i2 + j
                                        o = pnn[:, j * 129:(j + 1) * 129]
                                        e.matmul(o, lhsT=PTB[:, h, :], rhs=VE[:, h, :], start=True, stop=False)
                                        last = e.matmul(o, lhsT=QKT[(h % 2) * 64:(h % 2) * 64 + 64, h // 2, :], rhs=CBF[(h % 2) * 64:(h % 2) * 64 + 64, h // 2, :], start=False, stop=True)
                                    return last
                                yield S.op("pe", fn, reads=[rPTB, rVE, rQKT, rCBF], writes=[rpnn])
                            for i2 in range(2):
                                pnn, rpnn = pn[i2]
                                yield S.op("dve", lambda e, i2=i2, pnn=pnn: e.tensor_copy(out=S8[:, 56 + 2 * i2:58 + 2 * i2],
                                                                                  in_=pnn[:, 0:258].rearrange("p (a b) -> p a b", a=2)[:, :, 128:129].rearrange("p a b -> p (a b)")),
                                     reads=[rpnn], writes=[rS8])
                            yield S.op("dve", lambda e: e.tensor_scalar(out=G8[:, 40:44], in0=S8[:, 56:60], scalar1=-1.0, scalar2=None, op0=ALU.mult), reads=[rS8], writes=[rG8])
                            yield S.op("dve", lambda e: e.tensor_tensor(out=S8[:, 56:60], in0=S8[:, 56:60], in1=G8[:, 40:44], op=ALU.max), reads=[rS8, rG8], writes=[rS8])
                            yield S.op("dve", lambda e: e.tensor_tensor(out=S8[:, 56:60], in0=S8[:, 56:60], in1=G8[:, 32:36], op=ALU.max), reads=[rS8, rG8], writes=[rS8])
                            yield S.op("dve", lambda e: e.reciprocal(out=S8[:, 60:64], in_=S8[:, 56:60]), reads=[rS8], writes=[rS8])
                            for i2 in range(2):
                                pnn, rpnn = pn[i2]
                                yield S.op("dve", lambda e, i2=i2, pnn=pnn: e.tensor_tensor(out=HM[:, 2 * i2:2 * i2 + 2, :],
                                                                                    in0=pnn[:, 0:258].rearrange("p (a b) -> p a b", a=2)[:, :, 0:128],
                                                                                    in1=bc3(S8[:, 60 + 2 * i2:62 + 2 * i2], 2, 128), op=ALU.mult),
                                     reads=[rpnn, rS8], writes=[rHM])
                            pcc, rpcc = pf("m")
                            yield S.op("pe", lambda e: [e.matmul(pcc[(h % 2) * 64:(h % 2) * 64 + 64, (h // 2) * 129:(h // 2 + 1) * 129], lhsT=KP[:, h, :], rhs=VE[:, h, :],
                                                           start=True, stop=True) for h in range(4)][-1], reads=[rKP, rVE], writes=[rpcc])
                            for h in range(4):
                                po_ = (h % 2) * 64
                                yield S.op("dve", lambda e, h=h: e.scalar_tensor_tensor(out=CFs[po_:po_ + 64, h // 2, :], in0=CFs[po_:po_ + 64, h // 2, :],
                                                                                 scalar=G8[po_:po_ + 64, 28 + h:29 + h],
                                                                                 in1=pcc[po_:po_ + 64, (h // 2) * 129:(h // 2 + 1) * 129], op0=ALU.mult, op1=ALU.add),
                                     reads=[rCFs, rG8, rpcc], writes=[rCFs])
                            HM2 = HM_[:]
                            dbg_out("hm", HM2, rHM, r0)
                            yield S.op("pool", lambda e: e.tensor_tensor(out=TM[:], in0=HM2, in1=HM2, op=ALU.mult), reads=[rHM], writes=[rTM])
                            yield S.op("dve", lambda e: e.tensor_reduce(out=G8[:, 40:44], in_=v3(TM[:], 4), axis=AX.X, op=ALU.add), reads=[rTM], writes=[rG8])
                            yield S.op("dve", lambda e: e.tensor_scalar(out=G8[:, 40:44], in0=G8[:, 40:44], scalar1=1.0 / 128, scalar2=1e-6, op0=ALU.mult, op1=ALU.add),
                                 reads=[rG8], writes=[rG8])
                            yield S.op("act", lambda e: e.activation(out=G8[:, 40:44], in_=G8[:, 40:44], func=AF.Sqrt), reads=[rG8], writes=[rG8])
                            yield S.op("dve", lambda e: e.reciprocal(out=G8[:, 44:48], in_=G8[:, 40:44]), reads=[rG8], writes=[rG8])
                            yield S.op("dve", lambda e: e.tensor_tensor(out=HM, in0=HM, in1=bc3(G8[:, 44:48], 4, 128), op=ALU.mult), reads=[rHM, rG8], writes=[rHM])
                            if dbg:
                                yield S.op("dve", lambda e: e.tensor_tensor(out=DBG[:], in0=HM2, in1=SO[:], op=ALU.mult), reads=[rHM, rSO], writes=[rDBG])
                                dbg_out("yml", DBG[:], rDBG, r0)
                            yield S.op("pool", lambda e: e.tensor_tensor(out=MIX[:, 512:1024], in0=HM2, in1=SO[:], op=ALU.mult), reads=[rHM, rSO], writes=[rMIX])


                        nxt = [early(ti + 1)] if ti + 1 < len(tiles) else []
                        for _ in interleave(chain_R(), chain_M(), *nxt):
                            pass
                        chk(7)
                        pt, rpt = ptb()
                        S.op("pe", lambda e: [e.transpose(out=pt[:, k * 128:(k + 1) * 128], in_=MIX[:, k * 128:(k + 1) * 128], identity=IDB) for k in range(8)][-1],
                             reads=[rMIX, rCSB], writes=[rpt])
                        S.op("act", lambda e: e.activation(out=MIXT[:], in_=v3(pt[:], 8), func=AF.Copy), reads=[rpt], writes=[rMIXT])
                        for g in range(2):
                            p, rp = pf()
                            S.op("pe", lambda e, g=g, p=p: [e.matmul(p[:], lhsT=MIXT[:, k, :], rhs=WOUT[:, k, g * 512:(g + 1) * 512], start=(k == 0), stop=(k == 7))
                                                          for k in range(8)][-1], reads=[rMIXT, rWOUT], writes=[rp])
                            S.op("dve", lambda e, g=g, p=p: e.tensor_tensor(out=Xc[:, g * 512:(g + 1) * 512], in0=p[:], in1=Xc[:, g * 512:(g + 1) * 512], op=ALU.add),
                                 reads=[rp, rXc], writes=[rXc])
                        S.dma("sp", x1_d[r0:r0 + 128, :], Xc[:], reads=[rXc])
                        chk(8)
                S.barrier()
            S.barrier()

        es2 = ExitStack()
        with es2:
            sb2, _ = mk(es2)
            TOKS = 512 if T % 512 == 0 else 256
            NSUB = TOKS // 128
            WUP, rWUP = sb2("WUP", [128, 8, 2 * DFF], BF16)
            WDN, rWDN = sb2("WDN", [128, NB, D], BF16)
            FCW, rFCW = sb2("FCW", [128, NB, 3])
            FCB, rFCB = sb2("FCB", [128, NB])
            GF, rGF = sb2("GF", [128, D])
            S.dma("sp", FCW[:].rearrange("p b j -> p (b j)"), ffn_conv_w, writes=[rFCW])
            S.dma("sp", FCB[:], ffn_conv_b, writes=[rFCB])
            S.dma("sp", GF[:], norm_f_g.partition_broadcast(128), writes=[rGF])
            esC = ExitStack()
            with esC:
                sbC, _ = mk(esC)
                G2, rG2 = sbC("G2", [128, 8])
                STG2 = [sbC(f"STH{i}", [128, DFF]) for i in range(2)]
                S.dma("sp", G2[:], norm2_g, writes=[rG2])
                i = 0
                for k in range(8):
                    for hlf in range(2):
                        st, rst = STG2[i % 2]; i += 1
                        S.dma("sp", st[:], w_ffn_up[k * 128:(k + 1) * 128, hlf * DFF:(hlf + 1) * DFF], writes=[rst])
                        eng = "act" if hlf == 0 else "dve"
                        if eng == "act":
                            S.op("act", lambda e: e.activation(out=WUP[:, k, hlf * DFF:(hlf + 1) * DFF], in_=st[:], func=AF.Copy, scale=G2[:, k:k + 1]),
                                 reads=[rst, rG2], writes=[rWUP])
                        else:
                            S.op("dve", lambda e: e.tensor_scalar(out=WUP[:, k, hlf * DFF:(hlf + 1) * DFF], in0=st[:], scalar1=G2[:, k:k + 1], scalar2=None, op0=ALU.mult),
                                 reads=[rst, rG2], writes=[rWUP])
                for b in range(0, NB, 2):
                    st, rst = STG2[i % 2]; i += 1
                    S.dma("sp", st[:, 0:2 * D].rearrange("p (a n) -> p a n", a=2), w_ffn_down[b * 128:(b + 2) * 128, :].rearrange("(a p) n -> p a n", p=128),
                          writes=[rst])
                    S.op("pool" if (b // 2) % 2 else "dve", lambda e: e.tensor_copy(out=WDN[:, b:b + 2, :], in_=st[:, 0:2 * D].rearrange("p (a n) -> p a n", a=2)),
                         reads=[rst], writes=[rWDN])
                S.barrier()
            chk(9)
            esD = ExitStack()
            with esD:
                sbD, _ = mk(esD)
                XS = [sbD(f"XS{i}", [128, D]) for i in range(NSUB)]
                HN2, rHN2 = sbD("HN2", [128, D], BF16)
                H2T, rH2T = sbD("H2T", [128, 8, TOKS], BF16)
                GT, rGT = sbD("GT", [128, NB, TOKS], BF16)
                AC = [sbD(f"AC{i}", [128, TOKS + 2]) for i in range(2)]
                AQ = [sbD(f"AQ{i}", [128, TOKS]) for i in range(2)]
                CAR, rCAR = sbD("CAR", [128, NB, 2])
                ST2, rST2 = sbD("ST2", [128, 8])
                it = 0
                for s in range(NSEQ):
                    S.op("pool", lambda e: e.memset(CAR[:], 0.0), writes=[rCAR])
                    for c in range(T // TOKS):
                        r0 = s * T + c * TOKS
                        for sub in range(NSUB):
                            Xs, rXs = XS[sub]
                            S.dma("sp", Xs[:], x1_d[r0 + sub * 128:r0 + (sub + 1) * 128, :], writes=[rXs])
                            rmsnorm_to_bf16(Xs[:], rXs, HN2, rHN2, ST2, rST2)
                            pt, rpt = ptb()
                            S.op("pe", lambda e: [e.transpose(out=pt[:, k * 128:(k + 1) * 128], in_=HN2[:, k * 128:(k + 1) * 128], identity=IDB) for k in range(8)][-1],
                                 reads=[rHN2, rCSB], writes=[rpt])
                            S.op("dve", lambda e, sub=sub: e.tensor_copy(out=H2T[:, :, sub * 128:(sub + 1) * 128], in_=v3(pt[:], 8)), reads=[rpt], writes=[rH2T])
                        for b in range(NB):
                            ACb, rACb = AC[b % 2]
                            AQb, rAQb = AQ[b % 2]
                            pa, rpa = pf()
                            pbk, rpbk = pf()
                            S.op("pe", lambda e, b=b, pa=pa: [e.matmul(pa[:, 0:TOKS], lhsT=WUP[:, k, b * 128:(b + 1) * 128], rhs=H2T[:, k, :], start=(k == 0), stop=(k == 7))
                                                             for k in range(8)][-1], reads=[rWUP, rH2T], writes=[rpa])
                            S.op("pe", lambda e, b=b, pbk=pbk: [e.matmul(pbk[:, 0:TOKS], lhsT=WUP[:, k, DFF + b * 128:DFF + (b + 1) * 128], rhs=H2T[:, k, :], start=(k == 0), stop=(k == 7))
                                                               for k in range(8)][-1], reads=[rWUP, rH2T], writes=[rpbk])
                            S.op("pool", lambda e, b=b: e.tensor_copy(out=ACb[:, 0:2], in_=CAR[:, b, :]), reads=[rCAR], writes=[rACb])
                            S.op("act", lambda e: e.activation(out=ACb[:, 2:TOKS + 2], in_=pa[:, 0:TOKS], func=AF.Copy), reads=[rpa], writes=[rACb])
                            S.op("pool", lambda e, b=b: e.tensor_copy(out=CAR[:, b, :], in_=ACb[:, TOKS:TOKS + 2]), reads=[rACb], writes=[rCAR])
                            S.op("dve", lambda e, b=b: e.tensor_scalar(out=AQb[:], in0=ACb[:, 0:TOKS], scalar1=FCW[:, b, 0:1], scalar2=FCB[:, b:b + 1], op0=ALU.mult, op1=ALU.add),
                                 reads=[rACb, rFCW, rFCB], writes=[rAQb])
                            S.op("dve", lambda e, b=b: e.scalar_tensor_tensor(out=AQb[:], in0=ACb[:, 1:TOKS + 1], scalar=FCW[:, b, 1:2], in1=AQb[:], op0=ALU.mult, op1=ALU.add),
                                 reads=[rACb, rFCW, rAQb], writes=[rAQb])
                            S.op("dve", lambda e, b=b: e.scalar_tensor_tensor(out=AQb[:], in0=ACb[:, 2:TOKS + 2], scalar=FCW[:, b, 2:3], in1=AQb[:], op0=ALU.mult, op1=ALU.add),
                                 reads=[rACb, rFCW, rAQb], writes=[rAQb])
                            S.op("act", lambda e: e.activation(out=AQb[:], in_=AQb[:], func=AF.Silu), reads=[rAQb], writes=[rAQb])
                            S.op("dve", lambda e, b=b: e.tensor_tensor(out=GT[:, b, :], in0=AQb[:], in1=pbk[:, 0:TOKS], op=ALU.mult), reads=[rAQb, rpbk], writes=[rGT])
                        for sub in range(NSUB):
                            Xs, rXs = XS[sub]
                            X2c, rX2c = Xs, rXs
                            for g in range(2):
                                p, rp = pf()
                                S.op("pe", lambda e, g=g, p=p, sub=sub: [e.matmul(p[:], lhsT=GT[:, b, sub * 128:(sub + 1) * 128], rhs=WDN[:, b, g * 512:(g + 1) * 512],
                                                                                 start=(b == 0), stop=(b == NB - 1)) for b in range(NB)][-1], reads=[rGT, rWDN], writes=[rp])
                                S.op("dve", lambda e, g=g, p=p: e.tensor_tensor(out=X2c[:, g * 512:(g + 1) * 512], in0=p[:], in1=Xs[:, g * 512:(g + 1) * 512], op=ALU.add),
                                     reads=[rp, rXs], writes=[rX2c])
                            S.op("pool", lambda e: e.memset(ST2[:, 4:5], 0.0), writes=[rST2])
                            S.op("act", lambda e: e.activation(out=HN2[:], in_=X2c[:], func=AF.Square, accum_out=ST2[:, 4:5]), reads=[rX2c, rST2], writes=[rHN2, rST2])
                            S.op("dve", lambda e: e.tensor_scalar(out=ST2[:, 5:6], in0=ST2[:, 4:5], scalar1=1.0 / D, scalar2=1e-6, op0=ALU.mult, op1=ALU.add),
                                 reads=[rST2], writes=[rST2])
                            S.op("act", lambda e: e.activation(out=ST2[:, 6:7], in_=ST2[:, 5:6], func=AF.Sqrt), reads=[rST2], writes=[rST2])
                            S.op("dve", lambda e: e.reciprocal(out=ST2[:, 7:8], in_=ST2[:, 6:7]), reads=[rST2], writes=[rST2])
                            S.op("act", lambda e: e.activation(out=X2c[:], in_=X2c[:], func=AF.Copy, scale=ST2[:, 7:8]), reads=[rX2c, rST2], writes=[rX2c])
                            S.op("pool", lambda e: e.tensor_tensor(out=X2c[:], in0=X2c[:], in1=GF[:], op=ALU.mult), reads=[rX2c, rGF], writes=[rX2c])
                            S.dma("sp", out_d[r0 + sub * 128:r0 + (sub + 1) * 128, :], X2c[:], reads=[rX2c])
                S.barrier()
            S.barrier()
    return nc


def make_consts():
    c = np.zeros((128, 640), np.float32)
    i = np.arange(128)
    c[:, 0:128] = np.eye(128)
    c[:, 128:256] = (i[:, None] < i[None, :])
    c[:, 256:384] = (i[:, None] <= i[None, :])
    c[:, 384:512] = (i[:, None] > i[None, :])
    c[:, 512:640] = 1.0
    return c


def make_in_maps(inputs, n_cores, nseq):
    f = lambda a: np.ascontiguousarray(np.asarray(a, np.float32))
    x = f(inputs["x"])
    T = x.shape[1]
    shared = {}
    for k, v in inputs.items():
        if k == "x":
            continue
        a = f(v)
        if k != "norm_f_g":
            a = a[0]
        if k == "r_k":
            a = a.reshape(512)
        elif k in ("norm1_g", "norm2_g", "qk_conv_b", "ffn_conv_b", "mh_norm_g"):
            a = a.reshape(-1, 128).T
        elif k in ("qk_conv_w", "ffn_conv_w"):
            j = a.shape[0]
            a = a.reshape(j, -1, 128).transpose(2, 1, 0).reshape(128, -1)
        shared[k] = np.ascontiguousarray(a)
    shared["consts"] = make_consts()
    maps = []
    for c in range(n_cores):
        m = dict(shared)
        m["x"] = np.ascontiguousarray(x[c * nseq:(c + 1) * nseq].reshape(nseq * T, D))
        maps.append(m)
    return maps


def kernel(**inputs):
    x = np.asarray(inputs["x"])
    B, T, _ = x.shape
    nseq = B // N_CORES
    nc = build(nseq, T)
    maps = make_in_maps(inputs, N_CORES, nseq)
    res = run_bass_kernel_spmd(nc, maps, core_ids=list(range(N_CORES)))
    out = np.concatenate([r["out"].reshape(nseq, T, D) for r in res.results], axis=0)
    return out.astype(np.float32)
```

```python
import numpy as np
from contextlib import ExitStack
import concourse.bass as bass
import concourse.mybir as mybir
from concourse.bass_utils import run_bass_kernel_spmd

F32 = mybir.dt.float32
BF16 = mybir.dt.bfloat16
ALU = mybir.AluOpType
AF = mybir.ActivationFunctionType
AX = mybir.AxisListType

N_CORES = 8
D = 1024
N_IN = 3336
RWC = 1792
MLO = 2 * RWC
WIN_COLS = 2 * RWC + 1544
DFF = 2816
NB = DFF // 128
CDEC = float(np.exp(-0.5))


class Res:
    __slots__ = ("name", "lw", "rd")

    def __init__(self, name):
        self.name = name
        self.lw = None
        self.rd = []


class Sched:
    EPOCH = 30000
    NDMA = 24

    def __init__(self, nc, es):
        self.nc = nc
        self.es = es
        self.engs = {"pe": nc.tensor, "act": nc.scalar, "dve": nc.vector,
                     "pool": nc.gpsimd, "sp": nc.sync}
        self.sems = {e: [] for e in self.engs}
        self.cnt = {e: 0 for e in self.engs}
        self.waited = {e: {} for e in self.engs}
        self.dma_sems = [es.enter_context(nc.semaphore(f"dq{i}")) for i in range(self.NDMA)]
        self.dma_cnt = [0] * self.NDMA
        self.dma_i = 0
        for e in self.engs:
            self._new_epoch(e)

    def _new_epoch(self, e):
        s = self.es.enter_context(self.nc.semaphore(f"s_{e}_{len(self.sems[e])}"))
        self.sems[e].append(s)
        self.cnt[e] = 0

    def _wait(self, e, dep):
        if dep[0] == "dma":
            _, idx, val = dep
            key = ("dma", idx)
            sem = self.dma_sems[idx]
        else:
            de, ep, val = dep
            key = (de, ep)
            sem = self.sems[de][ep]
        if self.waited[e].get(key, 0) >= val:
            return
        self.engs[e].wait_ge(sem, val)
        self.waited[e][key] = val

    def _deps(self, e, reads, writes):
        deps = []
        for r in reads:
            if r.lw is not None and not (r.lw[0] == e and e == "pe"):
                deps.append(r.lw)
        for w in writes:
            if w.lw is not None and w.lw[0] != e:
                deps.append(w.lw)
            for d in w.rd:
                if d[0] != e:
                    deps.append(d)
        return deps

    def _mark(self, tag, reads, writes):
        for r in reads:
            r.rd.append(tag)
            if len(r.rd) > 64:
                r.rd = r.rd[-48:]
        for w in writes:
            w.lw = tag
            w.rd = []

    def op(self, e, fn, reads=(), writes=()):
        for d in self._deps(e, reads, writes):
            self._wait(e, d)
        if self.cnt[e] >= self.EPOCH:
            self._new_epoch(e)
        ins = fn(self.engs[e])
        ep = len(self.sems[e]) - 1
        ins.then_inc(self.sems[e][ep], 1)
        self.cnt[e] += 1
        tag = (e, ep, self.cnt[e])
        self._mark(tag, reads, writes)
        return tag

    def dma(self, q, out, in_, reads=(), writes=(), slow=False):
        for d in self._deps(q, reads, writes):
            self._wait(q, d)
        idx = self.dma_i
        self.dma_i = (self.dma_i + 1) % self.NDMA
        kw = {"allow_slow_non_contiguous": True} if slow else {}
        self.engs[q].dma_start(out=out, in_=in_, **kw).then_inc(self.dma_sems[idx], 16)
        self.dma_cnt[idx] += 16
        tag = ("dma", idx, self.dma_cnt[idx])
        self._mark(tag, reads, writes)
        return tag

    def barrier(self):
        for e in self.engs:
            for d in self.engs:
                if d != e:
                    ep = len(self.sems[d]) - 1
                    if self.cnt[d] > 0:
                        self._wait(e, (d, ep, self.cnt[d]))
                    elif ep > 0:
                        self._wait(e, (d, ep - 1, self.EPOCH))
            for i in range(self.NDMA):
                if self.dma_cnt[i]:
                    self._wait(e, ("dma", i, self.dma_cnt[i]))


VAR = 0


class _Stop(Exception):
    pass


def build(NSEQ, T, dbg=False, stop=0):
    nc = bass.Bass("TRN2", target_bir_lowering=False)
    try:
        _build(nc, NSEQ, T, dbg, stop)
    except _Stop:
        pass
    return nc


def _build(nc, NSEQ, T, dbg, stop):
    NT = T // 128
    NTOK = NSEQ * T
    di = lambda n, s: nc.dram_tensor(n, s, F32, kind="ExternalInput").ap()
    x_d = di("x", [NTOK, D])
    norm1_g = di("norm1_g", [128, 8]); w_in = di("w_in", [D, N_IN]); rw_mu = di("rw_mu", [RWC])
    w0 = di("w0", [512]); w_up_decay = di("w_up_decay", [64, 512]); a0 = di("a0", [512])
    w_up_a = di("w_up_a", [64, 512]); w_up_g = di("w_up_g", [128, 512]); k_k = di("k_k", [512])
    k_a = di("k_a", [512]); r_k = di("r_k", [512]); lnx_w = di("lnx_w", [512]); lnx_b = di("lnx_b", [512])
    qk_conv_w = di("qk_conv_w", [128, 16]); qk_conv_b = di("qk_conv_b", [128, 4])
    i_bias = di("i_bias", [4]); f_bias = di("f_bias", [4]); mh_norm_g = di("mh_norm_g", [128, 4])
    w_out = di("w_out", [D, D]); norm2_g = di("norm2_g", [128, 8]); w_ffn_up = di("w_ffn_up", [D, 2 * DFF])
    ffn_conv_w = di("ffn_conv_w", [128, NB * 3]); ffn_conv_b = di("ffn_conv_b", [128, NB])
    w_ffn_down = di("w_ffn_down", [DFF, D]); norm_f_g = di("norm_f_g", [D])
    consts = di("consts", [128, 640])
    out_d = nc.dram_tensor("out", [NTOK, D], F32, kind="ExternalOutput").ap()
    x1_d = nc.dram_tensor("x1s", [NTOK, D], F32, kind="Internal").ap()
    dbg_d = {}
    if dbg:
        for nm in ("yrw", "yml", "y", "hm"):
            dbg_d[nm] = nc.dram_tensor("d_" + nm, [NTOK, 512], F32, kind="ExternalOutput").ap()

    es0 = ExitStack()
    with es0:
        S = Sched(nc, es0)

        def chk(n):
            if stop == n:
                S.barrier()
                raise _Stop()

        def mk(es):
            def sb(n, s, d=F32):
                return es.enter_context(nc.sbuf_tensor(n, s, d)), Res(n)

            def ps(n, s, d=F32):
                return es.enter_context(nc.psum_tensor(n, s, d)), Res(n)
            return sb, ps

        sb0, ps0 = mk(es0)
        CST, rCST = sb0("CST", [128, 640])
        S.dma("sp", CST[:], consts, writes=[rCST])
        IDF = CST[:, 0:128]; MU = CST[:, 128:256]; MUI = CST[:, 256:384]; ML = CST[:, 384:512]; ONES = CST[:, 512:640]
        CSB, rCSB = sb0("CSB", [128, 128], BF16)
        S.op("dve", lambda e: e.tensor_copy(out=CSB[:], in_=CST[:, 0:128]), reads=[rCST], writes=[rCSB])
        IDB = CSB[:, 0:128]
        MUI8, rMUI8 = sb0("MUI8", [128, 128])
        S.op("dve", lambda e: e.tensor_scalar(out=MUI8[:], in0=MUI, scalar1=0.125, scalar2=None, op0=ALU.mult),
             reads=[rCST], writes=[rMUI8])
        NPF = 6
        PF = [ps0(f"PF{i}", [128, 512]) for i in range(NPF)]
        PT = [ps0(f"PT{i}", [128, 1024], BF16) for i in range(2)]
        pf_i = [0]
        pt_i = [0]

        POOLS = {"all": [0, 1, 2, 3, 4, 5], "g0": [0, 1], "g1": [2, 3], "m": [4, 5], "r": [0, 1, 2, 3]}
        pool_i = {k: 0 for k in POOLS}

        def pf(pool="all"):
            lst = POOLS[pool]
            p = PF[lst[pool_i[pool] % len(lst)]]
            pool_i[pool] += 1
            return p

        def interleave(*gens):
            gens = list(gens)
            while gens:
                for gg in list(gens):
                    try:
                        next(gg)
                        yield
                    except StopIteration:
                        gens.remove(gg)

        def ptb():
            return PT[0]

        def bc3(ap2, a, b):
            return ap2.rearrange("p (a o) -> p a o", o=1).to_broadcast([ap2.shape[0], a, b])

        def bcm(ap2, a):
            return ap2.rearrange("p (o n) -> p o n", o=1).to_broadcast([ap2.shape[0], a, ap2.shape[1]])

        def v3(ap, a):
            return ap.rearrange("p (a b) -> p a b", a=a)

        def rmsnorm_to_bf16(X, rX, HN, rHN, ST, rST):
            S.op("pool", lambda e: e.memset(ST[:, 0:1], 0.0), writes=[rST])
            S.op("act", lambda e: e.activation(out=HN[:], in_=X, func=AF.Square, accum_out=ST[:, 0:1]),
                 reads=[rX, rST], writes=[rHN, rST])
            S.op("dve", lambda e: e.tensor_scalar(out=ST[:, 1:2], in0=ST[:, 0:1], scalar1=1.0 / D, scalar2=1e-6,
                                                  op0=ALU.mult, op1=ALU.add), reads=[rST], writes=[rST])
            S.op("act", lambda e: e.activation(out=ST[:, 2:3], in_=ST[:, 1:2], func=AF.Sqrt), reads=[rST], writes=[rST])
            S.op("dve", lambda e: e.reciprocal(out=ST[:, 3:4], in_=ST[:, 2:3]), reads=[rST], writes=[rST])
            S.op("act", lambda e: e.activation(out=HN[:], in_=X, func=AF.Copy, scale=ST[:, 3:4]),
                 reads=[rX, rST], writes=[rHN])

        es1 = ExitStack()
        with es1:
            sb1, _ = mk(es1)
            WIN, rWIN = sb1("WIN", [128, 8, WIN_COLS], BF16)
            WOUT, rWOUT = sb1("WOUT", [128, 8, D], BF16)
            WLOR, rWLOR = sb1("WLOR", [128, 512], BF16)
            WG, rWG = sb1("WG", [128, 512], BF16)
            VEC, rVEC = sb1("VEC", [128, 7, 512])
            MHG4, rMHG4 = sb1("MHG4", [128, 4])
            CW, rCW = sb1("CW", [128, 4, 4])
            CB, rCB = sb1("CB", [128, 4])
            GB, rGB = sb1("GB", [128, 8])
            for i, v in enumerate((w0, a0, k_k, k_a, r_k, lnx_w, lnx_b)):
                S.dma("sp", VEC[:, i, :], v.partition_broadcast(128), writes=[rVEC])
            S.dma("sp", CW[:].rearrange("p b j -> p (b j)"), qk_conv_w, writes=[rCW])
            S.dma("sp", CB[:], qk_conv_b, writes=[rCB])
            S.dma("sp", GB[:, 0:4], i_bias.partition_broadcast(128), writes=[rGB])
            S.dma("sp", GB[:, 4:8], f_bias.partition_broadcast(128), writes=[rGB])
            W0V = VEC[:, 0, :]; A0V = VEC[:, 1, :]; KKV = VEC[:, 2, :]; KAV = VEC[:, 3, :]
            RKV = VEC[:, 4, :]; LWV = VEC[:, 5, :]; LBV = VEC[:, 6, :]
            S.dma("sp", MHG4[:], mh_norm_g, writes=[rMHG4])
            esA = ExitStack()
            with esA:
                sbA, _ = mk(esA)
                MUT, rMUT = sbA("MUT", [128, RWC])
                OMM, rOMM = sbA("OMM", [128, RWC])
                G1, rG1 = sbA("G1", [128, 8])
                STG = [sbA(f"STG{i}", [128, N_IN]) for i in range(2)]
                S.dma("sp", MUT[:], rw_mu.partition_broadcast(128), writes=[rMUT])
                S.dma("sp", G1[:], norm1_g, writes=[rG1])
                S.op("dve", lambda e: e.tensor_scalar(out=OMM[:], in0=MUT[:], scalar1=-1.0, scalar2=1.0,
                                                      op0=ALU.mult, op1=ALU.add), reads=[rMUT], writes=[rOMM])
                for k in range(8):
                    st, rst = STG[k % 2]
                    S.dma("sp", st[:], w_in[k * 128:(k + 1) * 128, :], writes=[rst])
                    S.op("act", lambda e: e.activation(out=st[:], in_=st[:], func=AF.Copy, scale=G1[:, k:k + 1]),
                         reads=[rst, rG1], writes=[rst])
                    S.op("dve", lambda e: e.tensor_tensor(out=WIN[:, k, 0:RWC], in0=st[:, 0:RWC], in1=OMM[:], op=ALU.mult),
                         reads=[rst, rOMM], writes=[rWIN])
                    S.op("pool", lambda e: e.tensor_tensor(out=WIN[:, k, RWC:2 * RWC], in0=st[:, 0:RWC], in1=MUT[:], op=ALU.mult),
                         reads=[rst, rMUT], writes=[rWIN])
                    S.op("act", lambda e: e.activation(out=WIN[:, k, MLO:WIN_COLS], in_=st[:, RWC:N_IN], func=AF.Copy),
                         reads=[rst], writes=[rWIN])
                for k in range(8):
                    st, rst = STG[k % 2]
                    S.dma("sp", st[:, 0:D], w_out[k * 128:(k + 1) * 128, :], writes=[rst])
                    if k < 4:
                        S.op("dve", lambda e: e.tensor_copy(out=WOUT[:, k, :], in_=st[:, 0:D]), reads=[rst], writes=[rWOUT])
                    else:
                        S.op("dve", lambda e: e.tensor_scalar(out=WOUT[:, k, :], in0=st[:, 0:D], scalar1=MHG4[:, k - 4:k - 3], scalar2=None, op0=ALU.mult),
                             reads=[rst, rMHG4], writes=[rWOUT])
                st, rst = STG[0]
                S.dma("sp", st[0:64, 0:512], w_up_decay, writes=[rst])
                S.dma("sp", st[64:128, 0:512], w_up_a, writes=[rst])
                S.dma("sp", st[:, 512:1024], w_up_g, writes=[rst])
                S.op("dve", lambda e: e.tensor_copy(out=WLOR[:], in_=st[:, 0:512]), reads=[rst], writes=[rWLOR])
                S.op("dve", lambda e: e.tensor_copy(out=WG[:], in_=st[:, 512:1024]), reads=[rst], writes=[rWG])
                S.barrier()
            chk(1)
            esB = ExitStack()
            with esB:
                sbB, _ = mk(esB)
                X = [sbB(f"X{i}", [128, D]) for i in range(2)]
                HN, rHN = sbB("HN", [128, D], BF16)
                HT = [sbB(f"HT{i}", [128, 8, 129], BF16) for i in range(2)]
                ST, rST = sbB("ST", [128, 8])
                R, rR = sbB("R", [128, 512]); K, rK = sbB("K", [128, 512]); V, rV = sbB("V", [128, 512])
                SG, rSG = sbB("SG", [128, 512]); AA, rAA = sbB("AA", [128, 512])
                ECL, rECL = sbB("ECL", [128, 512]); ENCL, rENCL = sbB("ENCL", [128, 512])
                KKN, rKKN = sbB("KKN", [128, 512]); KM, rKM = sbB("KM", [128, 512])
                TA, rTA = sbB("TA", [128, 512]); TB, rTB = sbB("TB", [128, 512])
                ECLM, rECLM = TA, rTA
                BON, rBON = sbB("BON", [128, 512])
                S8, rS8 = sbB("S8", [128, 64])
                WL2 = [sbB(f"WL{i}", [128, 4]) for i in range(2)]
                S8E, rS8E = sbB("S8E", [128, 24])
                RBAR, rRBAR = sbB("RBAR", [128, 512], BF16); ABAR, rABAR = sbB("ABAR", [128, 512], BF16)
                BTIL, rBTIL = sbB("BTIL", [128, 512], BF16); KTIL, rKTIL = sbB("KTIL", [128, 512], BF16)
                VB, rVB = sbB("VB", [128, 512], BF16)
                RBT, rRBT = sbB("RBT", [128, 4, 128], BF16); ABT, rABT = sbB("ABT", [128, 4, 128], BF16)
                BTT, rBTT = sbB("BTT", [128, 4, 128], BF16); KTT, rKTT = sbB("KTT", [128, 4, 128], BF16)
                AAK, rAAK = sbB("AAK", [128, 8, 128], BF16); ARKT, rARKT = sbB("ARKT", [128, 8, 128], BF16)
                ARBT, rARBT = sbB("ARBT", [128, 8, 128], BF16)
                PQ = [[sbB(f"PQ{g}{i}", [128, 4, 128], BF16) for i in range(4)] for g in range(2)]
                TT = [[sbB(f"TT{g}{i}", [128, 4, 128], BF16) for i in range(2)] for g in range(2)]
                ABPT, rABPT = sbB("ABPT", [128, 4, 128], BF16); AAKPT, rAAKPT = sbB("AAKPT", [128, 8, 128], BF16)
                UB, rUB = sbB("UB", [128, 512], BF16)
                YF, rYF = sbB("YF", [128, 512]); TB2, rTB2 = sbB("TB2", [128, 512])
                HF = [sbB("HF", [128, 4, 64])] * NSEQ
                HB, rHB = sbB("HB", [128, 4, 64], BF16)
                QKC, rQKC = sbB("QKC", [128, 4, 131])
                ACC_, rACC = sbB("ACC", [128, 512]); ACC = v3(ACC_[:], 4)
                QKT, rQKT = sbB("QKT", [128, 4, 128], BF16)
                KP, rKP = sbB("KP", [128, 4, 64], BF16)
                VE, rVE = sbB("VE", [128, 4, 129], BF16)
                G8, rG8 = sbB("G8", [128, 48])
                TM, rTM = ACC_, rACC; DG, rDG = v3(TM[:], 4), rTM
                MST = [sbB("MST", [128, 4])] * NSEQ
                CF = [sbB("CF", [128, 2, 129])] * NSEQ
                CBF, rCBF = sbB("CBF", [128, 2, 129], BF16)
                PTB, rPTB = sbB("PTB", [128, 4, 128], BF16)
                HM_, rHM = sbB("HM", [128, 512]); HM = v3(HM_[:], 4)
                SO, rSO = sbB("SO", [128, 512])
                MIX, rMIX = sbB("MIX", [128, D], BF16)
                MIXT, rMIXT = AAKPT, rAAKPT
                DBG, rDBG = sbB("DBG", [128, 512]) if dbg else (None, None)

                S.op("pool", lambda e: e.memset(VE[:], 1.0), writes=[rVE])

                def dbg_out(nm, ap, rr, r0):
                    if dbg:
                        S.dma("sp", dbg_d[nm][r0:r0 + 128, :], ap, reads=[rr])

                LT2 = [sbB(f"LTb{i}", [128, 256], BF16) for i in range(2)]
                tiles = [(s_, c_) for s_ in range(NSEQ) for c_ in range(NT)]
                EP, rEP = PT[1]
                EPF = EP[:].bitcast(F32)

                def mk_proj(HTx):
                    cur = lambda k: HTx[:, k, 1:129]
                    prv = lambda k: HTx[:, k, 0:128]

                    def proj_tok(p, col, n, shifted):
                        def f(e):
                            last = None
                            nm = 16 if shifted else 8
                            i = 0
                            for k in range(8):
                                last = e.matmul(p, lhsT=cur(k), rhs=WIN[:, k, col:col + n], start=(i == 0), stop=(i == nm - 1)); i += 1
                                if shifted:
                                    last = e.matmul(p, lhsT=prv(k), rhs=WIN[:, k, RWC + col:RWC + col + n], start=False, stop=(i == nm - 1)); i += 1
                            return last
                        return f

                    def proj_feat(p, col, shifted):
                        def f(e):
                            last = None
                            nm = 16 if shifted else 8
                            i = 0
                            for k in range(8):
                                last = e.matmul(p, lhsT=WIN[:, k, col:col + 128], rhs=cur(k), start=(i == 0), stop=(i == nm - 1)); i += 1
                                if shifted:
                                    last = e.matmul(p, lhsT=WIN[:, k, RWC + col:RWC + col + 128], rhs=prv(k), start=False, stop=(i == nm - 1)); i += 1
                            return last
                        return f
                    return cur, prv, proj_tok, proj_feat

                def early(ti):
                    s_, c_ = tiles[ti]
                    r0_ = s_ * T + c_ * 128
                    Xn, rXn = X[ti % 2]
                    HTn, rHTn = HT[ti % 2]
                    HTq, rHTq = HT[(ti + 1) % 2]
                    LTn, rLTn = LT2[ti % 2]
                    _, _, ptok, pfeat = mk_proj(HTn)
                    S.dma("sp", Xn[:], x_d[r0_:r0_ + 128, :], writes=[rXn])
                    yield
                    rmsnorm_to_bf16(Xn[:], rXn, HN, rHN, ST, rST)
                    yield
                    yield S.op("pe", lambda e: [e.transpose(out=EP[:, k * 128:(k + 1) * 128], in_=HN[:, k * 128:(k + 1) * 128],
                                                            identity=IDB) for k in range(8)][-1], reads=[rHN, rCSB], writes=[rEP])
                    yield S.op("dve", lambda e: e.tensor_copy(out=HTn[:, :, 1:129], in_=v3(EP[:], 8)), reads=[rEP], writes=[rHTn])
                    if c_ == 0:
                        yield S.op("pool", lambda e: e.memset(HTn[:, :, 0:1], 0.0), writes=[rHTn])
                    else:
                        yield S.op("pool", lambda e: e.tensor_copy(out=HTn[:, :, 0:1], in_=HTq[:, :, 128:129]), reads=[rHTq], writes=[rHTn])
                    yield S.op("pe", pfeat(EPF[:, 0:128], 1536, True), reads=[rHTn, rWIN], writes=[rEP])
                    yield S.op("pe", pfeat(EPF[:, 128:256], 1664, True), reads=[rHTn, rWIN], writes=[rEP])
                    yield S.op("act", lambda e: e.activation(out=LTn[0:64, 0:128], in_=EPF[0:64, 0:128], func=AF.Tanh), reads=[rEP], writes=[rLTn])
                    yield S.op("act", lambda e: e.activation(out=LTn[:, 128:256], in_=EPF[:, 128:256], func=AF.Sigmoid), reads=[rEP], writes=[rLTn])
                    yield S.op("act", lambda e: e.activation(out=LTn[64:128, 0:128], in_=EPF[64:128, 0:128], func=AF.Copy), reads=[rEP], writes=[rLTn])
                    for g_, (dst, rdst) in ((1, (K, rK)), (0, (R, rR)), (2, (V, rV))):
                        yield S.op("pe", ptok(EPF, g_ * 512, 512, True), reads=[rHTn, rWIN], writes=[rEP])
                        yield S.op("act", lambda e: e.activation(out=dst[:], in_=EPF, func=AF.Copy), reads=[rEP], writes=[rdst])
                    WLn, rWLn = WL2[ti % 2]
                    pw, rpw = EPF, rEP
                    yield S.op("pe", lambda e: e.matmul(pw[:], lhsT=LTn[0:64, 0:128], rhs=WLOR[0:64, :], start=True, stop=True),
                         reads=[rLTn, rWLOR], writes=[rpw])
                    yield S.op("dve", lambda e: e.tensor_tensor(out=SG[:], in0=pw[:], in1=W0V, op=ALU.add), reads=[rpw, rVEC], writes=[rSG])
                    yield S.op("act", lambda e: e.activation(out=SG[:], in_=SG[:], func=AF.Sigmoid), reads=[rSG], writes=[rSG])
                    pa, rpa = EPF, rEP
                    yield S.op("pe", lambda e: e.matmul(pa[:], lhsT=LTn[64:128, 0:128], rhs=WLOR[64:128, :], start=True, stop=True),
                         reads=[rLTn, rWLOR], writes=[rpa])
                    yield S.op("dve", lambda e: e.tensor_tensor(out=AA[:], in0=pa[:], in1=A0V, op=ALU.add), reads=[rpa, rVEC], writes=[rAA])
                    yield S.op("act", lambda e: e.activation(out=AA[:], in_=AA[:], func=AF.Sigmoid), reads=[rAA], writes=[rAA])
                    pc, rpc = EPF, rEP
                    yield S.op("pe", lambda e: e.matmul(pc[:], lhsT=MUI, rhs=SG[:], start=True, stop=True), reads=[rCST, rSG], writes=[rpc])
                    yield S.op("act", lambda e: e.activation(out=ECL[:], in_=pc[:], func=AF.Exp, scale=-CDEC), reads=[rpc], writes=[rECL])
                    yield S.op("act", lambda e: e.activation(out=ENCL[:], in_=pc[:], func=AF.Exp, scale=CDEC), reads=[rpc], writes=[rENCL])
                    yield S.op("dve", lambda e: e.tensor_tensor(out=TA[:], in0=pc[:], in1=SG[:], op=ALU.subtract), reads=[rpc, rSG], writes=[rTA])
                    yield S.op("act", lambda e: e.activation(out=ECLM[:], in_=TA[:], func=AF.Exp, scale=-CDEC), reads=[rTA], writes=[rECLM])
                    pwl, rpwl = EPF, rEP
                    yield S.op("pe", lambda e: [e.matmul(pwl[(h % 2) * 64:(h % 2) * 64 + 64, h // 2:h // 2 + 1], lhsT=SG[:, h * 64:(h + 1) * 64], rhs=ONES[:, 0:1], start=True, stop=True)
                                          for h in range(8)][-1], reads=[rSG, rCST], writes=[rpwl])
                    yield S.op("act", lambda e: e.activation(out=WLn[:], in_=pwl[:, 0:4], func=AF.Exp, scale=-CDEC), reads=[rpwl], writes=[rWLn])
                    yield S.op("dve", lambda e: e.tensor_tensor(out=KKN[:], in0=K[:], in1=KKV, op=ALU.mult), reads=[rK, rVEC], writes=[rKKN])
                    yield S.op("pool", lambda e: e.tensor_tensor(out=TB[:], in0=KKN[:], in1=KKN[:], op=ALU.mult), reads=[rKKN], writes=[rTB])
                    yield S.op("dve", lambda e: e.tensor_reduce(out=S8E[:, 0:8], in_=v3(TB[:], 8), axis=AX.X, op=ALU.add), reads=[rTB], writes=[rS8E])
                    yield S.op("act", lambda e: e.activation(out=S8E[:, 8:16], in_=S8E[:, 0:8], func=AF.Sqrt), reads=[rS8E], writes=[rS8E])
                    yield S.op("dve", lambda e: e.tensor_scalar(out=S8E[:, 8:16], in0=S8E[:, 8:16], scalar1=1e-12, scalar2=None, op0=ALU.max),
                         reads=[rS8E], writes=[rS8E])
                    yield S.op("dve", lambda e: e.reciprocal(out=S8E[:, 16:24], in_=S8E[:, 8:16]), reads=[rS8E], writes=[rS8E])
                    yield S.op("dve", lambda e: e.tensor_tensor(out=v3(KKN[:], 8), in0=v3(KKN[:], 8), in1=bc3(S8E[:, 16:24], 8, 64), op=ALU.mult),
                         reads=[rKKN, rS8E], writes=[rKKN])
                    yield S.op("dve", lambda e: e.scalar_tensor_tensor(out=TB[:], in0=AA[:], scalar=-1.0, in1=KAV, op0=ALU.add, op1=ALU.mult),
                         reads=[rAA, rVEC], writes=[rTB])
                    yield S.op("dve", lambda e: e.scalar_tensor_tensor(out=KM[:], in0=TB[:], scalar=1.0, in1=K[:], op0=ALU.add, op1=ALU.mult),
                         reads=[rTB, rK], writes=[rKM])

                for ti, (s, c) in enumerate(tiles):
                    if True:
                        HFs, rHFs = HF[s]
                        CFs, rCFs = CF[s]
                        Ms, rMs = MST[s]
                        if c == 0:
                            S.op("pool", lambda e: e.memset(HFs[:], 0.0), writes=[rHFs])
                            S.op("pool", lambda e: e.memset(CFs[:], 0.0), writes=[rCFs])
                            S.op("pool", lambda e: e.memset(Ms[:], 0.0), writes=[rMs])
                        r0 = s * T + c * 128
                        Xc, rXc = X[ti % 2]
                        HTc, rHTc = HT[ti % 2]
                        LT, rLT = LT2[ti % 2]
                        WL, rWL = WL2[ti % 2]
                        cur, prv, proj_tok, proj_feat = mk_proj(HTc)
                        if ti == 0:
                            for _ in early(0):
                                pass
                        chk(2)
                        S.op("dve", lambda e: e.tensor_tensor(out=RBAR[:], in0=R[:], in1=ECL[:], op=ALU.mult), reads=[rR, rECL], writes=[rRBAR])
                        S.op("dve", lambda e: e.scalar_tensor_tensor(out=ABAR[:], in0=KKN[:], scalar=-1.0, in1=ECLM[:], op0=ALU.mult, op1=ALU.mult),
                             reads=[rKKN, rECLM], writes=[rABAR])
                        S.op("pool", lambda e: e.tensor_tensor(out=TA[:], in0=KKN[:], in1=AA[:], op=ALU.mult), reads=[rKKN, rAA], writes=[rTA])
                        S.op("pool", lambda e: e.tensor_tensor(out=BTIL[:], in0=TA[:], in1=ENCL[:], op=ALU.mult), reads=[rTA, rENCL], writes=[rBTIL])
                        S.op("dve", lambda e: e.tensor_tensor(out=KTIL[:], in0=KM[:], in1=ENCL[:], op=ALU.mult), reads=[rKM, rENCL], writes=[rKTIL])
                        S.op("act", lambda e: e.activation(out=VB[:], in_=V[:], func=AF.Copy), reads=[rV], writes=[rVB])
                        S.op("pool", lambda e: e.tensor_tensor(out=TB[:], in0=R[:], in1=KM[:], op=ALU.mult), reads=[rR, rKM], writes=[rTB])
                        S.op("pool", lambda e: e.tensor_tensor(out=TB[:], in0=TB[:], in1=RKV, op=ALU.mult), reads=[rTB, rVEC], writes=[rTB])
                        S.op("dve", lambda e: e.tensor_reduce(out=S8[:, 24:32], in_=v3(TB[:], 8), axis=AX.X, op=ALU.add), reads=[rTB], writes=[rS8])
                        S.op("dve", lambda e: e.tensor_tensor(out=v3(BON[:], 8), in0=v3(V[:], 8), in1=bc3(S8[:, 24:32], 8, 64), op=ALU.mult),
                             reads=[rV, rS8], writes=[rBON])
                        for src, rsrc, dst, rdst in ((RBAR, rRBAR, RBT, rRBT), (ABAR, rABAR, ABT, rABT),
                                                     (BTIL, rBTIL, BTT, rBTT), (KTIL, rKTIL, KTT, rKTT)):
                            pt, rpt = ptb()
                            S.op("pe", lambda e: [e.transpose(out=pt[:, b * 128:(b + 1) * 128], in_=src[:, b * 128:(b + 1) * 128],
                                                              identity=IDB) for b in range(4)][-1], reads=[rsrc, rCSB], writes=[rpt])
                            S.op("act", lambda e: e.activation(out=dst[:], in_=v3(pt[:, 0:512], 4), func=AF.Copy), reads=[rpt], writes=[rdst])

                        chk(4)

                        def hop(Tt, h):
                            return Tt[(h % 2) * 64:(h % 2) * 64 + 64, h // 2, :]

                        def rw_group(g):
                            hs = [g, g + 2, g + 4, g + 6]
                            P0, rP0 = PQ[g][0]; Q0, rQ0 = PQ[g][1]

                            def mm4(p, A, Bm):
                                return lambda e: [e.matmul(p[:, j * 128:(j + 1) * 128], lhsT=hop(A, h), rhs=hop(Bm, h), start=True, stop=True)
                                                  for j, h in enumerate(hs) if (VAR != 3 or h % 2 == 0) and (VAR != 4 or h % 2 == 1)][-1]
                            for (A, rA, Bm, rB, dst, rdst, mask, eng) in (
                                    (ABT, rABT, BTT, rBTT, P0[:], rP0, ML, "dve"),
                                    (BTT, rBTT, ABT, rABT, Q0[:], rQ0, MU, "dve"),
                                    (ABT, rABT, KTT, rKTT, AAK[:, 4 * g:4 * g + 4, :], rAAK, ML, "dve"),
                                    (KTT, rKTT, RBT, rRBT, ARKT[:, 4 * g:4 * g + 4, :], rARKT, MUI, "dve"),
                                    (BTT, rBTT, RBT, rRBT, ARBT[:, 4 * g:4 * g + 4, :], rARBT, MUI, "dve")):
                                p, rp = pf("g%d" % g)
                                yield S.op("pe", mm4(p, A, Bm), reads=[rA, rB], writes=[rp])
                                if VAR in (1, 3, 4):
                                    pass
                                elif VAR == 2:
                                    for j4 in range(4):
                                        yield S.op(eng, lambda e: e.tensor_tensor(out=dst[:, j4, :], in0=p[:, j4 * 128:(j4 + 1) * 128], in1=mask, op=ALU.mult),
                                             reads=[rp, rCST], writes=[rdst])
                                else:
                                    yield S.op(eng, lambda e: e.tensor_tensor(out=dst, in0=v3(p[:], 4), in1=bcm(mask, 4), op=ALU.mult),
                                         reads=[rp, rCST], writes=[rdst])
                            Tc, rTc = TT[g][0]
                            yield S.op("pool", lambda e: e.tensor_tensor(out=Tc[:], in0=Q0[:], in1=bcm(IDB, 4), op=ALU.add),
                                 reads=[rQ0, rCSB], writes=[rTc])
                            Pp, rPp, Qp, rQp = P0, rP0, Q0, rQ0
                            ti = 0
                            for lev in range(1, 7):
                                Pn, rPn = PQ[g][2 * (lev % 2)]
                                Qn, rQn = PQ[g][2 * (lev % 2) + 1]
                                p, rp = pf("g%d" % g)
                                yield S.op("pe", lambda e: [e.matmul(p[:, j * 128:(j + 1) * 128], lhsT=Qp[:, j, :], rhs=Pp[:, j, :], start=True, stop=True)
                                                      for j in range(4)][-1], reads=[rPp, rQp], writes=[rp])
                                yield S.op("act", lambda e: e.activation(out=Pn[:], in_=v3(p[:], 4), func=AF.Copy), reads=[rp], writes=[rPn])
                                if lev < 6:
                                    p2, rp2 = pf("g%d" % g)
                                    yield S.op("pe", lambda e: [e.matmul(p2[:, j * 128:(j + 1) * 128], lhsT=Pp[:, j, :], rhs=Qp[:, j, :], start=True, stop=True)
                                                          for j in range(4)][-1], reads=[rPp, rQp], writes=[rp2])
                                    yield S.op("act", lambda e: e.activation(out=Qn[:], in_=v3(p2[:], 4), func=AF.Copy), reads=[rp2], writes=[rQn])
                                Tn, rTn = TT[g][(ti + 1) % 2]
                                p3, rp3 = pf("g%d" % g)
                                yield S.op("pe", lambda e: [e.matmul(p3[:, j * 128:(j + 1) * 128], lhsT=Pn[:, j, :], rhs=Tc[:, j, :], start=True, stop=True)
                                                      for j in range(4)][-1], reads=[rPn, rTc], writes=[rp3])
                                yield S.op("dve", lambda e: e.tensor_tensor(out=Tn[:], in0=v3(p3[:], 4), in1=Tc[:], op=ALU.add),
                                     reads=[rp3, rTc], writes=[rTn])
                                Tc, rTc = Tn, rTn
                                ti += 1
                                Pp, rPp, Qp, rQp = Pn, rPn, Qn, rQn
                            p, rp = pf("g%d" % g)
                            yield S.op("pe", lambda e: [e.matmul(p[g * 64:g * 64 + 64, j * 128:(j + 1) * 128], lhsT=ABAR[:, h * 64:(h + 1) * 64], rhs=Tc[:, j, :],
                                                           start=True, stop=True) for j, h in enumerate(hs)][-1],
                                 reads=[rABAR, rTc], writes=[rp])
                            yield S.op("act", lambda e: e.activation(out=ABPT[g * 64:g * 64 + 64, :, :], in_=v3(p[g * 64:g * 64 + 64, :], 4), func=AF.Copy),
                                 reads=[rp], writes=[rABPT])
                            p, rp = pf("g%d" % g)
                            yield S.op("pe", lambda e: [e.matmul(p[:, j * 128:(j + 1) * 128], lhsT=AAK[:, 4 * g + j, :], rhs=Tc[:, j, :],
                                                           start=True, stop=True) for j, h in enumerate(hs)][-1],
                                 reads=[rAAK, rTc], writes=[rp])
                            yield S.op("dve", lambda e: e.tensor_copy(out=AAKPT[:, 4 * g:4 * g + 4, :], in_=v3(p[:], 4)), reads=[rp], writes=[rAAKPT])


                        def chain_R():
                            yield from interleave(rw_group(0), rw_group(1))
                            yield S.op("act", lambda e: e.activation(out=HB[:], in_=HFs[:], func=AF.Copy), reads=[rHFs], writes=[rHB])
                            pu, rpu = pf("r")

                            def fu(e):
                                last = None
                                for h in range(8):
                                    e.matmul(pu[:, h * 64:(h + 1) * 64], lhsT=hop(ABPT, h), rhs=hop(HB, h), start=True, stop=False)
                                    last = e.matmul(pu[:, h * 64:(h + 1) * 64], lhsT=AAKPT[:, (h % 2) * 4 + h // 2, :], rhs=VB[:, h * 64:(h + 1) * 64], start=False, stop=True)
                                return last
                            yield S.op("pe", fu, reads=[rABPT, rHB, rAAKPT, rVB], writes=[rpu])
                            yield S.op("act", lambda e: e.activation(out=UB[:], in_=pu[:], func=AF.Copy), reads=[rpu], writes=[rUB])
                            py, rpy = pf("r")

                            def fy(e):
                                last = None
                                for h in range(8):
                                    o = py[:, h * 64:(h + 1) * 64]
                                    e.matmul(o, lhsT=hop(RBT, h), rhs=hop(HB, h), start=True, stop=False)
                                    e.matmul(o, lhsT=ARBT[:, (h % 2) * 4 + h // 2, :], rhs=UB[:, h * 64:(h + 1) * 64], start=False, stop=False)
                                    last = e.matmul(o, lhsT=ARKT[:, (h % 2) * 4 + h // 2, :], rhs=VB[:, h * 64:(h + 1) * 64], start=False, stop=True)
                                return last
                            yield S.op("pe", fy, reads=[rRBT, rHB, rARBT, rUB, rARKT, rVB], writes=[rpy])
                            ph, rph = pf("r")

                            def fh(e):
                                last = None
                                for h in range(8):
                                    o = ph[(h % 2) * 64:(h % 2) * 64 + 64, (h // 2) * 64:(h // 2 + 1) * 64]
                                    e.matmul(o, lhsT=BTIL[:, h * 64:(h + 1) * 64], rhs=UB[:, h * 64:(h + 1) * 64], start=True, stop=False)
                                    last = e.matmul(o, lhsT=KTIL[:, h * 64:(h + 1) * 64], rhs=VB[:, h * 64:(h + 1) * 64], start=False, stop=True)
                                return last
                            yield S.op("pe", fh, reads=[rBTIL, rKTIL, rUB, rVB], writes=[rph])
                            yield S.op("dve", lambda e: e.tensor_tensor(out=HFs[:], in0=v3(ph[:, 0:256], 4), in1=HFs[:], op=ALU.add),
                                 reads=[rph, rHFs], writes=[rHFs])
                            yield S.op("dve", lambda e: e.tensor_tensor(out=HFs[:], in0=HFs[:], in1=bc3(WL[:], 4, 64), op=ALU.mult),
                                 reads=[rHFs, rWL], writes=[rHFs])
                            yield S.op("act", lambda e: e.activation(out=YF[:], in_=py[:], func=AF.Copy), reads=[rpy], writes=[rYF])
                            dbg_out("y", YF[:], rYF, r0)
                            yield S.op("dve", lambda e: e.tensor_reduce(out=S8[:, 32:40], in_=v3(YF[:], 8), axis=AX.X, op=ALU.add), reads=[rYF], writes=[rS8])
                            yield S.op("dve", lambda e: e.tensor_scalar(out=S8[:, 32:40], in0=S8[:, 32:40], scalar1=1.0 / 64, scalar2=None, op0=ALU.mult),
                                 reads=[rS8], writes=[rS8])
                            yield S.op("dve", lambda e: e.tensor_tensor(out=v3(YF[:], 8), in0=v3(YF[:], 8), in1=bc3(S8[:, 32:40], 8, 64), op=ALU.subtract),
                                 reads=[rYF, rS8], writes=[rYF])
                            yield S.op("pool", lambda e: e.tensor_tensor(out=TB2[:], in0=YF[:], in1=YF[:], op=ALU.mult), reads=[rYF], writes=[rTB2])
                            yield S.op("dve", lambda e: e.tensor_reduce(out=S8[:, 40:48], in_=v3(TB2[:], 8), axis=AX.X, op=ALU.add), reads=[rTB2], writes=[rS8])
                            yield S.op("dve", lambda e: e.tensor_scalar(out=S8[:, 40:48], in0=S8[:, 40:48], scalar1=1.0 / 64, scalar2=64e-5,
                                                                  op0=ALU.mult, op1=ALU.add), reads=[rS8], writes=[rS8])
                            yield S.op("act", lambda e: e.activation(out=S8[:, 40:48], in_=S8[:, 40:48], func=AF.Sqrt), reads=[rS8], writes=[rS8])
                            yield S.op("dve", lambda e: e.reciprocal(out=S8[:, 48:56], in_=S8[:, 40:48]), reads=[rS8], writes=[rS8])
                            yield S.op("dve", lambda e: e.tensor_tensor(out=v3(YF[:], 8), in0=v3(YF[:], 8), in1=bc3(S8[:, 48:56], 8, 64), op=ALU.mult),
                                 reads=[rYF, rS8], writes=[rYF])
                            yield S.op("pool", lambda e: e.tensor_tensor(out=YF[:], in0=YF[:], in1=LWV, op=ALU.mult), reads=[rYF, rVEC], writes=[rYF])
                            yield S.op("pool", lambda e: e.tensor_tensor(out=YF[:], in0=YF[:], in1=LBV, op=ALU.add), reads=[rYF, rVEC], writes=[rYF])
                            yield S.op("pool", lambda e: e.tensor_tensor(out=YF[:], in0=YF[:], in1=BON[:], op=ALU.add), reads=[rYF, rBON], writes=[rYF])
                            pg, rpg = pf("r")
                            yield S.op("pe", lambda e: e.matmul(pg[:], lhsT=LT[:, 128:256], rhs=WG[:], start=True, stop=True), reads=[rLT, rWG], writes=[rpg])
                            if dbg:
                                yield S.op("dve", lambda e: e.tensor_tensor(out=DBG[:], in0=YF[:], in1=pg[:], op=ALU.mult), reads=[rYF, rpg], writes=[rDBG])
                                dbg_out("yrw", DBG[:], rDBG, r0)
                            yield S.op("dve", lambda e: e.tensor_tensor(out=MIX[:, 0:512], in0=YF[:], in1=pg[:], op=ALU.mult), reads=[rYF, rpg], writes=[rMIX])


                        def chain_M():
                            pqk, rpqk = pf("m")
                            for b in range(4):
                                yield S.op("pe", proj_feat(pqk[:, b * 128:(b + 1) * 128], MLO - 0 + b * 128 if False else 0, False) if False else
                                     (lambda e, b=b: [e.matmul(pqk[:, b * 128:(b + 1) * 128], lhsT=WIN[:, k, MLO + b * 128:MLO + (b + 1) * 128], rhs=cur(k),
                                                              start=(k == 0), stop=(k == 7)) for k in range(8)][-1]),
                                     reads=[rHTc, rWIN], writes=[rpqk])
                            if c == 0:
                                yield S.op("pool", lambda e: e.memset(QKC[:, :, 0:3], 0.0), writes=[rQKC])
                            else:
                                yield S.op("pool", lambda e: e.tensor_copy(out=QKC[:, :, 0:3], in_=QKC[:, :, 128:131]), reads=[rQKC], writes=[rQKC])
                            yield S.op("act", lambda e: e.activation(out=QKC[:, :, 3:131], in_=v3(pqk[:], 4), func=AF.Copy), reads=[rpqk], writes=[rQKC])
                            for b in range(4):
                                eng = "dve"
                                yield S.op(eng, lambda e, b=b: e.tensor_scalar(out=ACC[:, b, :], in0=QKC[:, b, 0:128], scalar1=CW[:, b, 0:1], scalar2=CB[:, b:b + 1],
                                                                         op0=ALU.mult, op1=ALU.add), reads=[rQKC, rCW, rCB], writes=[rACC])
                                for j in range(1, 4):
                                    yield S.op(eng, lambda e, b=b, j=j: e.scalar_tensor_tensor(out=ACC[:, b, :], in0=QKC[:, b, j:j + 128], scalar=CW[:, b, j:j + 1],
                                                                                        in1=ACC[:, b, :], op0=ALU.mult, op1=ALU.add),
                                         reads=[rQKC, rCW, rACC], writes=[rACC])
                            yield S.op("act", lambda e: e.activation(out=QKT[:], in_=ACC, func=AF.Silu), reads=[rACC], writes=[rQKT])
                            pv, rpv = pf("m")
                            yield S.op("pe", proj_tok(pv[:], MLO + 512 - 0, 512, False) if False else
                                 (lambda e: [e.matmul(pv[:], lhsT=cur(k), rhs=WIN[:, k, MLO + 512:MLO + 1024], start=(k == 0), stop=(k == 7)) for k in range(8)][-1]),
                                 reads=[rHTc, rWIN], writes=[rpv])
                            yield S.op("act", lambda e: e.activation(out=VE[:, :, 0:128], in_=v3(pv[:], 4), func=AF.Copy), reads=[rpv], writes=[rVE])
                            po, rpo = pf("m")
                            yield S.op("pe", lambda e: [e.matmul(po[:], lhsT=cur(k), rhs=WIN[:, k, MLO + 1024:MLO + 1536], start=(k == 0), stop=(k == 7)) for k in range(8)][-1],
                                 reads=[rHTc, rWIN], writes=[rpo])
                            yield S.op("act", lambda e: e.activation(out=SO[:], in_=po[:], func=AF.Sigmoid), reads=[rpo], writes=[rSO])
                            pgt, rpgt = pf("m")
                            yield S.op("pe", lambda e: [e.matmul(pgt[:, 0:8], lhsT=cur(k), rhs=WIN[:, k, MLO + 1536:MLO + 1544], start=(k == 0), stop=(k == 7)) for k in range(8)][-1],
                                 reads=[rHTc, rWIN], writes=[rpgt])
                            yield S.op("dve", lambda e: e.tensor_tensor(out=G8[:, 0:8], in0=pgt[:, 0:8], in1=GB[:], op=ALU.add), reads=[rpgt, rGB], writes=[rG8])
                            yield S.op("act", lambda e: e.activation(out=G8[:, 0:8], in_=G8[:, 0:8], func=AF.Tanh, scale=1.0 / 15), reads=[rG8], writes=[rG8])
                            yield S.op("act", lambda e: e.activation(out=G8[:, 8:12], in_=G8[:, 4:8], func=AF.Exp, scale=-15.0), reads=[rG8], writes=[rG8])
                            yield S.op("dve", lambda e: e.tensor_scalar(out=G8[:, 8:12], in0=G8[:, 8:12], scalar1=1.0, scalar2=None, op0=ALU.add), reads=[rG8], writes=[rG8])
                            yield S.op("act", lambda e: e.activation(out=G8[:, 12:16], in_=G8[:, 8:12], func=AF.Ln), reads=[rG8], writes=[rG8])
                            pb, rpb = pf("m")
                            yield S.op("pe", lambda e: e.matmul(pb[:, 0:4], lhsT=MUI, rhs=G8[:, 12:16], start=True, stop=True), reads=[rCST, rG8], writes=[rpb])
                            yield S.op("pe", lambda e: e.matmul(pb[:, 4:8], lhsT=ONES, rhs=G8[:, 12:16], start=True, stop=True), reads=[rCST, rG8], writes=[rpb])
                            yield S.op("dve", lambda e: e.scalar_tensor_tensor(out=G8[:, 16:20], in0=G8[:, 0:4], scalar=15.0, in1=pb[:, 0:4], op0=ALU.mult, op1=ALU.add),
                                 reads=[rG8, rpb], writes=[rG8])
                            yield S.op("dve", lambda e: e.tensor_tensor(out=DG, in0=bcm(IDF, 4), in1=bc3(G8[:, 16:20], 4, 128), op=ALU.mult),
                                 reads=[rCST, rG8], writes=[rDG])
                            pgm, rpgm = pf("m")
                            yield S.op("pe", lambda e: e.matmul(pgm[:], lhsT=ONES, rhs=TM[:], start=True, stop=True),
                                 reads=[rCST, rDG], writes=[rpgm])
                            yield S.op("dve", lambda e: e.tensor_reduce(out=G8[:, 20:24], in_=v3(pgm[:], 4), axis=AX.X, op=ALU.max), reads=[rpgm], writes=[rG8])
                            yield S.op("dve", lambda e: e.tensor_tensor(out=G8[:, 20:24], in0=G8[:, 20:24], in1=Ms[:], op=ALU.max), reads=[rG8, rMs], writes=[rG8])
                            yield S.op("dve", lambda e: e.tensor_tensor(out=G8[:, 40:44], in0=G8[:, 16:20], in1=G8[:, 20:24], op=ALU.subtract), reads=[rG8], writes=[rG8])
                            yield S.op("act", lambda e: e.activation(out=G8[:, 24:28], in_=G8[:, 40:44], func=AF.Exp), reads=[rG8], writes=[rG8])
                            yield S.op("dve", lambda e: e.tensor_tensor(out=G8[:, 44:48], in0=Ms[:], in1=G8[:, 20:24], op=ALU.subtract), reads=[rG8, rMs], writes=[rG8])
                            yield S.op("act", lambda e: e.activation(out=G8[:, 28:32], in_=G8[:, 44:48], func=AF.Exp), reads=[rG8], writes=[rG8])
                            yield S.op("dve", lambda e: e.tensor_scalar(out=G8[:, 36:40], in0=G8[:, 28:32], scalar1=0.125, scalar2=None, op0=ALU.mult), reads=[rG8], writes=[rG8])
                            yield S.op("dve", lambda e: e.tensor_tensor(out=G8[:, 40:44], in0=pb[:, 0:4], in1=G8[:, 20:24], op=ALU.subtract), reads=[rpb, rG8], writes=[rG8])
                            yield S.op("act", lambda e: e.activation(out=G8[:, 32:36], in_=G8[:, 40:44], func=AF.Exp), reads=[rG8], writes=[rG8])
                            yield S.op("dve", lambda e: e.tensor_tensor(out=Ms[:], in0=G8[:, 20:24], in1=pb[:, 4:8], op=ALU.subtract), reads=[rG8, rpb], writes=[rMs])
                            pt, rpt = ptb()
                            yield S.op("pe", lambda e: [e.transpose(out=pt[:, b * 128:(b + 1) * 128], in_=QKT[:, 2 + b, :], identity=IDB) for b in range(2)][-1],
                                 reads=[rQKT, rCSB], writes=[rpt])
                            yield S.op("dve", lambda e: e.tensor_tensor(out=KP[:], in0=v3(pt[:, 0:256], 4), in1=bc3(G8[:, 24:28], 4, 64), op=ALU.mult),
                                 reads=[rpt, rG8], writes=[rKP])
                            psts = [pf("m"), pf("m")]
                            for par in range(2):
                                pst, rpst = psts[par]
                                yield S.op("pe", lambda e: [e.matmul(pst[:, (h // 2) * 128:(h // 2 + 1) * 128], lhsT=QKT[par * 64:par * 64 + 64, 2 + h // 2, :],
                                                               rhs=QKT[par * 64:par * 64 + 64, h // 2, :], start=True, stop=True) for h in (par, par + 2)][-1],
                                     reads=[rQKT], writes=[rpst])
                            for h in range(4):
                                pst, rpst = psts[h % 2]
                                yield S.op("dve", lambda e, h=h: e.scalar_tensor_tensor(out=PTB[:, h, :], in0=pst[:, (h // 2) * 128:(h // 2 + 1) * 128], scalar=G8[:, 24 + h:25 + h],
                                                                                 in1=MUI8[:], op0=ALU.mult, op1=ALU.mult), reads=[rpst, rG8, rMUI8], writes=[rPTB])
                            for h in range(4):
                                po_ = (h % 2) * 64
                                yield S.op("dve", lambda e, h=h: e.tensor_scalar(out=CBF[po_:po_ + 64, h // 2, :], in0=CFs[po_:po_ + 64, h // 2, :],
                                                                            scalar1=G8[po_:po_ + 64, 36 + h:37 + h], scalar2=None, op0=ALU.mult),
                                     reads=[rCFs, rG8], writes=[rCBF])
                            pn = [pf("m"), pf("m")]
                            for i2 in range(2):
                                pnn, rpnn = pn[i2]

                                def fn(e, i2=i2, pnn=pnn):
                                    last = None
                                    for j in range(2):
                                        h = 2 * i2 + j
                                        o = pnn[:, j * 129:(j + 1) * 129]
                                        e.matmul(o, lhsT=PTB[:, h, :], rhs=VE[:, h, :], start=True, stop=False)
                                        last = e.matmul(o, lhsT=QKT[(h % 2) * 64:(h % 2) * 64 + 64, h // 2, :], rhs=CBF[(h % 2) * 64:(h % 2) * 64 + 64, h // 2, :], start=False, stop=True)
                                    return last
                                yield S.op("pe", fn, reads=[rPTB, rVE, rQKT, rCBF], writes=[rpnn])
                            for i2 in range(2):
                                pnn, rpnn = pn[i2]
                                yield S.op("dve", lambda e, i2=i2, pnn=pnn: e.tensor_copy(out=S8[:, 56 + 2 * i2:58 + 2 * i2],
                                                                                  in_=pnn[:, 0:258].rearrange("p (a b) -> p a b", a=2)[:, :, 128:129].rearrange("p a b -> p (a b)")),
                                     reads=[rpnn], writes=[rS8])
                            yield S.op("dve", lambda e: e.tensor_scalar(out=G8[:, 40:44], in0=S8[:, 56:60], scalar1=-1.0, scalar2=None, op0=ALU.mult), reads=[rS8], writes=[rG8])
                            yield S.op("dve", lambda e: e.tensor_tensor(out=S8[:, 56:60], in0=S8[:, 56:60], in1=G8[:, 40:44], op=ALU.max), reads=[rS8, rG8], writes=[rS8])
                            yield S.op("dve", lambda e: e.tensor_tensor(out=S8[:, 56:60], in0=S8[:, 56:60], in1=G8[:, 32:36], op=ALU.max), reads=[rS8, rG8], writes=[rS8])
                            yield S.op("dve", lambda e: e.reciprocal(out=S8[:, 60:64], in_=S8[:, 56:60]), reads=[rS8], writes=[rS8])
                            for i2 in range(2):
                                pnn, rpnn = pn[i2]
                                yield S.op("dve", lambda e, i2=i2, pnn=pnn: e.tensor_tensor(out=HM[:, 2 * i2:2 * i2 + 2, :],
                                                                                    in0=pnn[:, 0:258].rearrange("p (a b) -> p a b", a=2)[:, :, 0:128],
                                                                                    in1=bc3(S8[:, 60 + 2 * i2:62 + 2 * i2], 2, 128), op=ALU.mult),
                                     reads=[rpnn, rS8], writes=[rHM])
                            pcc, rpcc = pf("m")
                            yield S.op("pe", lambda e: [e.matmul(pcc[(h % 2) * 64:(h % 2) * 64 + 64, (h // 2) * 129:(h // 2 + 1) * 129], lhsT=KP[:, h, :], rhs=VE[:, h, :],
                                                           start=True, stop=True) for h in range(4)][-1], reads=[rKP, rVE], writes=[rpcc])
                            for h in range(4):
                                po_ = (h % 2) * 64
                                yield S.op("dve", lambda e, h=h: e.scalar_tensor_tensor(out=CFs[po_:po_ + 64, h // 2, :], in0=CFs[po_:po_ + 64, h // 2, :],
                                                                                 scalar=G8[po_:po_ + 64, 28 + h:29 + h],
                                                                                 in1=pcc[po_:po_ + 64, (h // 2) * 129:(h // 2 + 1) * 129], op0=ALU.mult, op1=ALU.add),
                                     reads=[rCFs, rG8, rpcc], writes=[rCFs])
                            HM2 = HM_[:]
                            dbg_out("hm", HM2, rHM, r0)
                            yield S.op("pool", lambda e: e.tensor_tensor(out=TM[:], in0=HM2, in1=HM2, op=ALU.mult), reads=[rHM], writes=[rTM])
                            yield S.op("dve", lambda e: e.tensor_reduce(out=G8[:, 40:44], in_=v3(TM[:], 4), axis=AX.X, op=ALU.add), reads=[rTM], writes=[rG8])
                            yield S.op("dve", lambda e: e.tensor_scalar(out=G8[:, 40:44], in0=G8[:, 40:44], scalar1=1.0 / 128, scalar2=1e-6, op0=ALU.mult, op1=ALU.add),
                                 reads=[rG8], writes=[rG8])
                            yield S.op("act", lambda e: e.activation(out=G8[:, 40:44], in_=G8[:, 40:44], func=AF.Sqrt), reads=[rG8], writes=[rG8])
                            yield S.op("dve", lambda e: e.reciprocal(out=G8[:, 44:48], in_=G8[:, 40:44]), reads=[rG8], writes=[rG8])
                            yield S.op("dve", lambda e: e.tensor_tensor(out=HM, in0=HM, in1=bc3(G8[:, 44:48], 4, 128), op=ALU.mult), reads=[rHM, rG8], writes=[rHM])
                            if dbg:
                                yield S.op("dve", lambda e: e.tensor_tensor(out=DBG[:], in0=HM2, in1=SO[:], op=ALU.mult), reads=[rHM, rSO], writes=[rDBG])
                                dbg_out("yml", DBG[:], rDBG, r0)
                            yield S.op("pool", lambda e: e.tensor_tensor(out=MIX[:, 512:1024], in0=HM2, in1=SO[:], op=ALU.mult), reads=[rHM, rSO], writes=[rMIX])


                        def speed(gen, n):
                            while True:
                                for _k in range(n):
                                    try:
                                        next(gen)
                                    except StopIteration:
                                        return
                                yield
                        NP_, NR_, NM_ = 1, 4, 3
                        nxt = [speed(early(ti + 1), NP_)] if ti + 1 < len(tiles) else []
                        for _ in interleave(speed(chain_R(), NR_), speed(chain_M(), NM_), *nxt):
                            pass
                        chk(7)
                        pt, rpt = ptb()
                        S.op("pe", lambda e: [e.transpose(out=pt[:, k * 128:(k + 1) * 128], in_=MIX[:, k * 128:(k + 1) * 128], identity=IDB) for k in range(8)][-1],
                             reads=[rMIX, rCSB], writes=[rpt])
                        S.op("act", lambda e: e.activation(out=MIXT[:], in_=v3(pt[:], 8), func=AF.Copy), reads=[rpt], writes=[rMIXT])
                        for g in range(2):
                            p, rp = pf()
                            S.op("pe", lambda e, g=g, p=p: [e.matmul(p[:], lhsT=MIXT[:, k, :], rhs=WOUT[:, k, g * 512:(g + 1) * 512], start=(k == 0), stop=(k == 7))
                                                          for k in range(8)][-1], reads=[rMIXT, rWOUT], writes=[rp])
                            S.op("dve", lambda e, g=g, p=p: e.tensor_tensor(out=Xc[:, g * 512:(g + 1) * 512], in0=p[:], in1=Xc[:, g * 512:(g + 1) * 512], op=ALU.add),
                                 reads=[rp, rXc], writes=[rXc])
                        S.dma("sp", x1_d[r0:r0 + 128, :], Xc[:], reads=[rXc])
                        chk(8)
                S.barrier()
            S.barrier()

        es2 = ExitStack()
        with es2:
            sb2, _ = mk(es2)
            TOKS = 512 if T % 512 == 0 else 256
            NSUB = TOKS // 128
            WUP, rWUP = sb2("WUP", [128, 8, 2 * DFF], BF16)
            WDN, rWDN = sb2("WDN", [128, NB, D], BF16)
            FCW, rFCW = sb2("FCW", [128, NB, 3])
            FCB, rFCB = sb2("FCB", [128, NB])
            GF, rGF = sb2("GF", [128, D])
            S.dma("sp", FCW[:].rearrange("p b j -> p (b j)"), ffn_conv_w, writes=[rFCW])
            S.dma("sp", FCB[:], ffn_conv_b, writes=[rFCB])
            S.dma("sp", GF[:], norm_f_g.partition_broadcast(128), writes=[rGF])
            esC = ExitStack()
            with esC:
                sbC, _ = mk(esC)
                G2, rG2 = sbC("G2", [128, 8])
                STG2 = [sbC(f"STH{i}", [128, DFF]) for i in range(2)]
                S.dma("sp", G2[:], norm2_g, writes=[rG2])
                i = 0
                for k in range(8):
                    for hlf in range(2):
                        st, rst = STG2[i % 2]; i += 1
                        S.dma("sp", st[:], w_ffn_up[k * 128:(k + 1) * 128, hlf * DFF:(hlf + 1) * DFF], writes=[rst])
                        eng = "act" if hlf == 0 else "dve"
                        if eng == "act":
                            S.op("act", lambda e: e.activation(out=WUP[:, k, hlf * DFF:(hlf + 1) * DFF], in_=st[:], func=AF.Copy, scale=G2[:, k:k + 1]),
                                 reads=[rst, rG2], writes=[rWUP])
                        else:
                            S.op("dve", lambda e: e.tensor_scalar(out=WUP[:, k, hlf * DFF:(hlf + 1) * DFF], in0=st[:], scalar1=G2[:, k:k + 1], scalar2=None, op0=ALU.mult),
                                 reads=[rst, rG2], writes=[rWUP])
                for b in range(0, NB, 2):
                    st, rst = STG2[i % 2]; i += 1
                    S.dma("sp", st[:, 0:2 * D].rearrange("p (a n) -> p a n", a=2), w_ffn_down[b * 128:(b + 2) * 128, :].rearrange("(a p) n -> p a n", p=128),
                          writes=[rst])
                    S.op("pool" if (b // 2) % 2 else "dve", lambda e: e.tensor_copy(out=WDN[:, b:b + 2, :], in_=st[:, 0:2 * D].rearrange("p (a n) -> p a n", a=2)),
                         reads=[rst], writes=[rWDN])
                S.barrier()
            chk(9)
            esD = ExitStack()
            with esD:
                sbD, _ = mk(esD)
                XS = [sbD(f"XS{i}", [128, D]) for i in range(NSUB)]
                HN2, rHN2 = sbD("HN2", [128, D], BF16)
                H2T, rH2T = sbD("H2T", [128, 8, TOKS], BF16)
                GT, rGT = sbD("GT", [128, NB, TOKS], BF16)
                AC = [sbD(f"AC{i}", [128, TOKS + 2]) for i in range(2)]
                AQ = [sbD(f"AQ{i}", [128, TOKS]) for i in range(2)]
                CAR, rCAR = sbD("CAR", [128, NB, 2])
                ST2, rST2 = sbD("ST2", [128, 8])
                it = 0
                for s in range(NSEQ):
                    S.op("pool", lambda e: e.memset(CAR[:], 0.0), writes=[rCAR])
                    for c in range(T // TOKS):
                        r0 = s * T + c * TOKS
                        for sub in range(NSUB):
                            Xs, rXs = XS[sub]
                            S.dma("sp", Xs[:], x1_d[r0 + sub * 128:r0 + (sub + 1) * 128, :], writes=[rXs])
                            rmsnorm_to_bf16(Xs[:], rXs, HN2, rHN2, ST2, rST2)
                            pt, rpt = ptb()
                            S.op("pe", lambda e: [e.transpose(out=pt[:, k * 128:(k + 1) * 128], in_=HN2[:, k * 128:(k + 1) * 128], identity=IDB) for k in range(8)][-1],
                                 reads=[rHN2, rCSB], writes=[rpt])
                            S.op("dve", lambda e, sub=sub: e.tensor_copy(out=H2T[:, :, sub * 128:(sub + 1) * 128], in_=v3(pt[:], 8)), reads=[rpt], writes=[rH2T])
                        for b in range(NB):
                            ACb, rACb = AC[b % 2]
                            AQb, rAQb = AQ[b % 2]
                            pa, rpa = pf()
                            pbk, rpbk = pf()
                            S.op("pe", lambda e, b=b, pa=pa: [e.matmul(pa[:, 0:TOKS], lhsT=WUP[:, k, b * 128:(b + 1) * 128], rhs=H2T[:, k, :], start=(k == 0), stop=(k == 7))
                                                             for k in range(8)][-1], reads=[rWUP, rH2T], writes=[rpa])
                            S.op("pe", lambda e, b=b, pbk=pbk: [e.matmul(pbk[:, 0:TOKS], lhsT=WUP[:, k, DFF + b * 128:DFF + (b + 1) * 128], rhs=H2T[:, k, :], start=(k == 0), stop=(k == 7))
                                                               for k in range(8)][-1], reads=[rWUP, rH2T], writes=[rpbk])
                            S.op("pool", lambda e, b=b: e.tensor_copy(out=ACb[:, 0:2], in_=CAR[:, b, :]), reads=[rCAR], writes=[rACb])
                            S.op("act", lambda e: e.activation(out=ACb[:, 2:TOKS + 2], in_=pa[:, 0:TOKS], func=AF.Copy), reads=[rpa], writes=[rACb])
                            S.op("pool", lambda e, b=b: e.tensor_copy(out=CAR[:, b, :], in_=ACb[:, TOKS:TOKS + 2]), reads=[rACb], writes=[rCAR])
                            S.op("dve", lambda e, b=b: e.tensor_scalar(out=AQb[:], in0=ACb[:, 0:TOKS], scalar1=FCW[:, b, 0:1], scalar2=FCB[:, b:b + 1], op0=ALU.mult, op1=ALU.add),
                                 reads=[rACb, rFCW, rFCB], writes=[rAQb])
                            S.op("dve", lambda e, b=b: e.scalar_tensor_tensor(out=AQb[:], in0=ACb[:, 1:TOKS + 1], scalar=FCW[:, b, 1:2], in1=AQb[:], op0=ALU.mult, op1=ALU.add),
                                 reads=[rACb, rFCW, rAQb], writes=[rAQb])
                            S.op("dve", lambda e, b=b: e.scalar_tensor_tensor(out=AQb[:], in0=ACb[:, 2:TOKS + 2], scalar=FCW[:, b, 2:3], in1=AQb[:], op0=ALU.mult, op1=ALU.add),
                                 reads=[rACb, rFCW, rAQb], writes=[rAQb])
                            S.op("act", lambda e: e.activation(out=AQb[:], in_=AQb[:], func=AF.Silu), reads=[rAQb], writes=[rAQb])
                            S.op("dve", lambda e, b=b: e.tensor_tensor(out=GT[:, b, :], in0=AQb[:], in1=pbk[:, 0:TOKS], op=ALU.mult), reads=[rAQb, rpbk], writes=[rGT])
                        for sub in range(NSUB):
                            Xs, rXs = XS[sub]
                            X2c, rX2c = Xs, rXs
                            for g in range(2):
                                p, rp = pf()
                                S.op("pe", lambda e, g=g, p=p, sub=sub: [e.matmul(p[:], lhsT=GT[:, b, sub * 128:(sub + 1) * 128], rhs=WDN[:, b, g * 512:(g + 1) * 512],
                                                                                 start=(b == 0), stop=(b == NB - 1)) for b in range(NB)][-1], reads=[rGT, rWDN], writes=[rp])
                                S.op("dve", lambda e, g=g, p=p: e.tensor_tensor(out=X2c[:, g * 512:(g + 1) * 512], in0=p[:], in1=Xs[:, g * 512:(g + 1) * 512], op=ALU.add),
                                     reads=[rp, rXs], writes=[rX2c])
                            S.op("pool", lambda e: e.memset(ST2[:, 4:5], 0.0), writes=[rST2])
                            S.op("act", lambda e: e.activation(out=HN2[:], in_=X2c[:], func=AF.Square, accum_out=ST2[:, 4:5]), reads=[rX2c, rST2], writes=[rHN2, rST2])
                            S.op("dve", lambda e: e.tensor_scalar(out=ST2[:, 5:6], in0=ST2[:, 4:5], scalar1=1.0 / D, scalar2=1e-6, op0=ALU.mult, op1=ALU.add),
                                 reads=[rST2], writes=[rST2])
                            S.op("act", lambda e: e.activation(out=ST2[:, 6:7], in_=ST2[:, 5:6], func=AF.Sqrt), reads=[rST2], writes=[rST2])
                            S.op("dve", lambda e: e.reciprocal(out=ST2[:, 7:8], in_=ST2[:, 6:7]), reads=[rST2], writes=[rST2])
                            S.op("act", lambda e: e.activation(out=X2c[:], in_=X2c[:], func=AF.Copy, scale=ST2[:, 7:8]), reads=[rX2c, rST2], writes=[rX2c])
                            S.op("pool", lambda e: e.tensor_tensor(out=X2c[:], in0=X2c[:], in1=GF[:], op=ALU.mult), reads=[rX2c, rGF], writes=[rX2c])
                            S.dma("sp", out_d[r0 + sub * 128:r0 + (sub + 1) * 128, :], X2c[:], reads=[rX2c])
                S.barrier()
            S.barrier()
    return nc


def make_consts():
    c = np.zeros((128, 640), np.float32)
    i = np.arange(128)
    c[:, 0:128] = np.eye(128)
    c[:, 128:256] = (i[:, None] < i[None, :])
    c[:, 256:384] = (i[:, None] <= i[None, :])
    c[:, 384:512] = (i[:, None] > i[None, :])
    c[:, 512:640] = 1.0
    return c


def make_in_maps(inputs, n_cores, nseq):
    f = lambda a: np.ascontiguousarray(np.asarray(a, np.float32))
    x = f(inputs["x"])
    T = x.shape[1]
    shared = {}
    for k, v in inputs.items():
        if k == "x":
            continue
        a = f(v)
        if k != "norm_f_g":
            a = a[0]
        if k == "r_k":
            a = a.reshape(512)
        elif k in ("norm1_g", "norm2_g", "qk_conv_b", "ffn_conv_b", "mh_norm_g"):
            a = a.reshape(-1, 128).T
        elif k in ("qk_conv_w", "ffn_conv_w"):
            j = a.shape[0]
            a = a.reshape(j, -1, 128).transpose(2, 1, 0).reshape(128, -1)
        shared[k] = np.ascontiguousarray(a)
    shared["consts"] = make_consts()
    maps = []
    for c in range(n_cores):
        m = dict(shared)
        m["x"] = np.ascontiguousarray(x[c * nseq:(c + 1) * nseq].reshape(nseq * T, D))
        maps.append(m)
    return maps


def kernel(**inputs):
    x = np.asarray(inputs["x"])
    B, T, _ = x.shape
    nseq = B // N_CORES
    nc = build(nseq, T)
    maps = make_in_maps(inputs, N_CORES, nseq)
    res = run_bass_kernel_spmd(nc, maps, core_ids=list(range(N_CORES)))
    out = np.concatenate([r["out"].reshape(nseq, T, D) for r in res.results], axis=0)
    return out.astype(np.float32)
```

```python
import numpy as np
from contextlib import ExitStack
import concourse.bass as bass
import concourse.mybir as mybir
from concourse.bass_utils import run_bass_kernel_spmd

F32 = mybir.dt.float32
BF16 = mybir.dt.bfloat16
ALU = mybir.AluOpType
AF = mybir.ActivationFunctionType
AX = mybir.AxisListType

N_CORES = 8
D = 1024
N_IN = 3336
RWC = 1792
MLO = 2 * RWC
WIN_COLS = 2 * RWC + 1544
DFF = 2816
NB = DFF // 128
CDEC = float(np.exp(-0.5))


class Res:
    __slots__ = ("name", "lw", "rd")

    def __init__(self, name):
        self.name = name
        self.lw = None
        self.rd = []


class Sched:
    EPOCH = 30000
    NDMA = 24

    def __init__(self, nc, es):
        self.nc = nc
        self.es = es
        self.engs = {"pe": nc.tensor, "act": nc.scalar, "dve": nc.vector,
                     "pool": nc.gpsimd, "sp": nc.sync}
        self.sems = {e: [] for e in self.engs}
        self.cnt = {e: 0 for e in self.engs}
        self.waited = {e: {} for e in self.engs}
        self.dma_sems = [es.enter_context(nc.semaphore(f"dq{i}")) for i in range(self.NDMA)]
        self.dma_cnt = [0] * self.NDMA
        self.dma_i = 0
        for e in self.engs:
            self._new_epoch(e)

    def _new_epoch(self, e):
        s = self.es.enter_context(self.nc.semaphore(f"s_{e}_{len(self.sems[e])}"))
        self.sems[e].append(s)
        self.cnt[e] = 0

    def _wait(self, e, dep):
        if dep[0] == "dma":
            _, idx, val = dep
            key = ("dma", idx)
            sem = self.dma_sems[idx]
        else:
            de, ep, val = dep
            key = (de, ep)
            sem = self.sems[de][ep]
        if self.waited[e].get(key, 0) >= val:
            return
        self.engs[e].wait_ge(sem, val)
        self.waited[e][key] = val

    def _deps(self, e, reads, writes):
        deps = []
        for r in reads:
            if r.lw is not None and not (r.lw[0] == e and e == "pe"):
                deps.append(r.lw)
        for w in writes:
            if w.lw is not None and w.lw[0] != e:
                deps.append(w.lw)
            for d in w.rd:
                if d[0] != e:
                    deps.append(d)
        return deps

    def _mark(self, tag, reads, writes):
        for r in reads:
            r.rd.append(tag)
            if len(r.rd) > 64:
                r.rd = r.rd[-48:]
        for w in writes:
            w.lw = tag
            w.rd = []

    def op(self, e, fn, reads=(), writes=()):
        for d in self._deps(e, reads, writes):
            self._wait(e, d)
        if self.cnt[e] >= self.EPOCH:
            self._new_epoch(e)
        ins = fn(self.engs[e])
        ep = len(self.sems[e]) - 1
        ins.then_inc(self.sems[e][ep], 1)
        self.cnt[e] += 1
        tag = (e, ep, self.cnt[e])
        self._mark(tag, reads, writes)
        return tag

    def dma(self, q, out, in_, reads=(), writes=(), slow=False):
        for d in self._deps(q, reads, writes):
            self._wait(q, d)
        idx = self.dma_i
        self.dma_i = (self.dma_i + 1) % self.NDMA
        kw = {"allow_slow_non_contiguous": True} if slow else {}
        self.engs[q].dma_start(out=out, in_=in_, **kw).then_inc(self.dma_sems[idx], 16)
        self.dma_cnt[idx] += 16
        tag = ("dma", idx, self.dma_cnt[idx])
        self._mark(tag, reads, writes)
        return tag

    def barrier(self):
        for e in self.engs:
            for d in self.engs:
                if d != e:
                    ep = len(self.sems[d]) - 1
                    if self.cnt[d] > 0:
                        self._wait(e, (d, ep, self.cnt[d]))
                    elif ep > 0:
                        self._wait(e, (d, ep - 1, self.EPOCH))
            for i in range(self.NDMA):
                if self.dma_cnt[i]:
                    self._wait(e, ("dma", i, self.dma_cnt[i]))


VAR = 0


class _Stop(Exception):
    pass


def build(NSEQ, T, dbg=False, stop=0):
    nc = bass.Bass("TRN2", target_bir_lowering=False)
    try:
        _build(nc, NSEQ, T, dbg, stop)
    except _Stop:
        pass
    return nc


def _build(nc, NSEQ, T, dbg, stop):
    NT = T // 128
    NTOK = NSEQ * T
    di = lambda n, s: nc.dram_tensor(n, s, F32, kind="ExternalInput").ap()
    x_d = di("x", [NTOK, D])
    norm1_g = di("norm1_g", [128, 8]); w_in = di("w_in", [D, N_IN]); rw_mu = di("rw_mu", [RWC])
    w0 = di("w0", [512]); w_up_decay = di("w_up_decay", [64, 512]); a0 = di("a0", [512])
    w_up_a = di("w_up_a", [64, 512]); w_up_g = di("w_up_g", [128, 512]); k_k = di("k_k", [512])
    k_a = di("k_a", [512]); r_k = di("r_k", [512]); lnx_w = di("lnx_w", [512]); lnx_b = di("lnx_b", [512])
    qk_conv_w = di("qk_conv_w", [128, 16]); qk_conv_b = di("qk_conv_b", [128, 4])
    i_bias = di("i_bias", [4]); f_bias = di("f_bias", [4]); mh_norm_g = di("mh_norm_g", [128, 4])
    w_out = di("w_out", [D, D]); norm2_g = di("norm2_g", [128, 8]); w_ffn_up = di("w_ffn_up", [D, 2 * DFF])
    ffn_conv_w = di("ffn_conv_w", [128, NB * 3]); ffn_conv_b = di("ffn_conv_b", [128, NB])
    w_ffn_down = di("w_ffn_down", [DFF, D]); norm_f_g = di("norm_f_g", [D])
    consts = di("consts", [128, 640])
    out_d = nc.dram_tensor("out", [NTOK, D], F32, kind="ExternalOutput").ap()
    x1_d = nc.dram_tensor("x1s", [NTOK, D], F32, kind="Internal").ap()
    dbg_d = {}
    if dbg:
        for nm in ("yrw", "yml", "y", "hm"):
            dbg_d[nm] = nc.dram_tensor("d_" + nm, [NTOK, 512], F32, kind="ExternalOutput").ap()

    es0 = ExitStack()
    with es0:
        S = Sched(nc, es0)

        def chk(n):
            if stop == n:
                S.barrier()
                raise _Stop()

        def mk(es):
            def sb(n, s, d=F32):
                return es.enter_context(nc.sbuf_tensor(n, s, d)), Res(n)

            def ps(n, s, d=F32):
                return es.enter_context(nc.psum_tensor(n, s, d)), Res(n)
            return sb, ps

        sb0, ps0 = mk(es0)
        CST, rCST = sb0("CST", [128, 640])
        S.dma("sp", CST[:], consts, writes=[rCST])
        IDF = CST[:, 0:128]; MU = CST[:, 128:256]; MUI = CST[:, 256:384]; ML = CST[:, 384:512]; ONES = CST[:, 512:640]
        CSB, rCSB = sb0("CSB", [128, 128], BF16)
        S.op("dve", lambda e: e.tensor_copy(out=CSB[:], in_=CST[:, 0:128]), reads=[rCST], writes=[rCSB])
        IDB = CSB[:, 0:128]
        MUI8, rMUI8 = sb0("MUI8", [128, 128])
        S.op("dve", lambda e: e.tensor_scalar(out=MUI8[:], in0=MUI, scalar1=0.125, scalar2=None, op0=ALU.mult),
             reads=[rCST], writes=[rMUI8])
        NPF = 6
        PF = [ps0(f"PF{i}", [128, 512]) for i in range(NPF)]
        PT = [ps0(f"PT{i}", [128, 1024], BF16) for i in range(2)]
        pf_i = [0]
        pt_i = [0]

        POOLS = {"all": [0, 1, 2, 3, 4, 5], "g0": [0, 1], "g1": [2, 3], "m": [4, 5], "r": [0, 1, 2, 3]}
        pool_i = {k: 0 for k in POOLS}

        def pf(pool="all"):
            lst = POOLS[pool]
            p = PF[lst[pool_i[pool] % len(lst)]]
            pool_i[pool] += 1
            return p

        def interleave(*gens):
            gens = list(gens)
            while gens:
                for gg in list(gens):
                    try:
                        next(gg)
                        yield
                    except StopIteration:
                        gens.remove(gg)

        def ptb():
            return PT[0]

        def bc3(ap2, a, b):
            return ap2.rearrange("p (a o) -> p a o", o=1).to_broadcast([ap2.shape[0], a, b])

        def bcm(ap2, a):
            return ap2.rearrange("p (o n) -> p o n", o=1).to_broadcast([ap2.shape[0], a, ap2.shape[1]])

        def v3(ap, a):
            return ap.rearrange("p (a b) -> p a b", a=a)

        def rmsnorm_to_bf16(X, rX, HN, rHN, ST, rST):
            S.op("pool", lambda e: e.memset(ST[:, 0:1], 0.0), writes=[rST])
            S.op("act", lambda e: e.activation(out=HN[:], in_=X, func=AF.Square, accum_out=ST[:, 0:1]),
                 reads=[rX, rST], writes=[rHN, rST])
            S.op("dve", lambda e: e.tensor_scalar(out=ST[:, 1:2], in0=ST[:, 0:1], scalar1=1.0 / D, scalar2=1e-6,
                                                  op0=ALU.mult, op1=ALU.add), reads=[rST], writes=[rST])
            S.op("act", lambda e: e.activation(out=ST[:, 2:3], in_=ST[:, 1:2], func=AF.Sqrt), reads=[rST], writes=[rST])
            S.op("dve", lambda e: e.reciprocal(out=ST[:, 3:4], in_=ST[:, 2:3]), reads=[rST], writes=[rST])
            S.op("act", lambda e: e.activation(out=HN[:], in_=X, func=AF.Copy, scale=ST[:, 3:4]),
                 reads=[rX, rST], writes=[rHN])

        es1 = ExitStack()
        with es1:
            sb1, _ = mk(es1)
            WIN, rWIN = sb1("WIN", [128, 8, WIN_COLS], BF16)
            WOUT, rWOUT = sb1("WOUT", [128, 8, D], BF16)
            WLOR, rWLOR = sb1("WLOR", [128, 512], BF16)
            WG, rWG = sb1("WG", [128, 512], BF16)
            VEC, rVEC = sb1("VEC", [128, 7, 512])
            MHG4, rMHG4 = sb1("MHG4", [128, 4])
            CW, rCW = sb1("CW", [128, 4, 4])
            CB, rCB = sb1("CB", [128, 4])
            GB, rGB = sb1("GB", [128, 8])
            for i, v in enumerate((w0, a0, k_k, k_a, r_k, lnx_w, lnx_b)):
                S.dma("sp", VEC[:, i, :], v.partition_broadcast(128), writes=[rVEC])
            S.dma("sp", CW[:].rearrange("p b j -> p (b j)"), qk_conv_w, writes=[rCW])
            S.dma("sp", CB[:], qk_conv_b, writes=[rCB])
            S.dma("sp", GB[:, 0:4], i_bias.partition_broadcast(128), writes=[rGB])
            S.dma("sp", GB[:, 4:8], f_bias.partition_broadcast(128), writes=[rGB])
            W0V = VEC[:, 0, :]; A0V = VEC[:, 1, :]; KKV = VEC[:, 2, :]; KAV = VEC[:, 3, :]
            RKV = VEC[:, 4, :]; LWV = VEC[:, 5, :]; LBV = VEC[:, 6, :]
            S.dma("sp", MHG4[:], mh_norm_g, writes=[rMHG4])
            esA = ExitStack()
            with esA:
                sbA, _ = mk(esA)
                MUT, rMUT = sbA("MUT", [128, RWC])
                OMM, rOMM = sbA("OMM", [128, RWC])
                G1, rG1 = sbA("G1", [128, 8])
                STG = [sbA(f"STG{i}", [128, N_IN]) for i in range(2)]
                S.dma("sp", MUT[:], rw_mu.partition_broadcast(128), writes=[rMUT])
                S.dma("sp", G1[:], norm1_g, writes=[rG1])
                S.op("dve", lambda e: e.tensor_scalar(out=OMM[:], in0=MUT[:], scalar1=-1.0, scalar2=1.0,
                                                      op0=ALU.mult, op1=ALU.add), reads=[rMUT], writes=[rOMM])
                for k in range(8):
                    st, rst = STG[k % 2]
                    S.dma("sp", st[:], w_in[k * 128:(k + 1) * 128, :], writes=[rst])
                    S.op("act", lambda e: e.activation(out=st[:], in_=st[:], func=AF.Copy, scale=G1[:, k:k + 1]),
                         reads=[rst, rG1], writes=[rst])
                    S.op("dve", lambda e: e.tensor_tensor(out=WIN[:, k, 0:RWC], in0=st[:, 0:RWC], in1=OMM[:], op=ALU.mult),
                         reads=[rst, rOMM], writes=[rWIN])
                    S.op("pool", lambda e: e.tensor_tensor(out=WIN[:, k, RWC:2 * RWC], in0=st[:, 0:RWC], in1=MUT[:], op=ALU.mult),
                         reads=[rst, rMUT], writes=[rWIN])
                    S.op("act", lambda e: e.activation(out=WIN[:, k, MLO:WIN_COLS], in_=st[:, RWC:N_IN], func=AF.Copy),
                         reads=[rst], writes=[rWIN])
                for k in range(8):
                    st, rst = STG[k % 2]
                    S.dma("sp", st[:, 0:D], w_out[k * 128:(k + 1) * 128, :], writes=[rst])
                    if k < 4:
                        S.op("dve", lambda e: e.tensor_copy(out=WOUT[:, k, :], in_=st[:, 0:D]), reads=[rst], writes=[rWOUT])
                    else:
                        S.op("dve", lambda e: e.tensor_scalar(out=WOUT[:, k, :], in0=st[:, 0:D], scalar1=MHG4[:, k - 4:k - 3], scalar2=None, op0=ALU.mult),
                             reads=[rst, rMHG4], writes=[rWOUT])
                st, rst = STG[0]
                S.dma("sp", st[0:64, 0:512], w_up_decay, writes=[rst])
                S.dma("sp", st[64:128, 0:512], w_up_a, writes=[rst])
                S.dma("sp", st[:, 512:1024], w_up_g, writes=[rst])
                S.op("dve", lambda e: e.tensor_copy(out=WLOR[:], in_=st[:, 0:512]), reads=[rst], writes=[rWLOR])
                S.op("dve", lambda e: e.tensor_copy(out=WG[:], in_=st[:, 512:1024]), reads=[rst], writes=[rWG])
                S.barrier()
            chk(1)
            esB = ExitStack()
            with esB:
                sbB, _ = mk(esB)
                X = [sbB(f"X{i}", [128, D]) for i in range(2)]
                HN, rHN = sbB("HN", [128, D], BF16)
                HT = [sbB(f"HT{i}", [128, 8, 129], BF16) for i in range(2)]
                ST, rST = sbB("ST", [128, 8])
                R, rR = sbB("R", [128, 512]); K, rK = sbB("K", [128, 512]); V, rV = sbB("V", [128, 512])
                SG, rSG = sbB("SG", [128, 512]); AA, rAA = sbB("AA", [128, 512])
                ECL, rECL = sbB("ECL", [128, 512]); ENCL, rENCL = sbB("ENCL", [128, 512])
                KKN, rKKN = sbB("KKN", [128, 512]); KM, rKM = sbB("KM", [128, 512])
                TA, rTA = sbB("TA", [128, 512]); TB, rTB = sbB("TB", [128, 512])
                ECLM, rECLM = TA, rTA
                BON, rBON = sbB("BON", [128, 512])
                S8, rS8 = sbB("S8", [128, 64])
                WL2 = [sbB(f"WL{i}", [128, 4]) for i in range(2)]
                S8E, rS8E = sbB("S8E", [128, 24])
                RBAR, rRBAR = sbB("RBAR", [128, 512], BF16); ABAR, rABAR = sbB("ABAR", [128, 512], BF16)
                BTIL, rBTIL = sbB("BTIL", [128, 512], BF16); KTIL, rKTIL = sbB("KTIL", [128, 512], BF16)
                VB, rVB = sbB("VB", [128, 512], BF16)
                RBT, rRBT = sbB("RBT", [128, 4, 128], BF16); ABT, rABT = sbB("ABT", [128, 4, 128], BF16)
                BTT, rBTT = sbB("BTT", [128, 4, 128], BF16); KTT, rKTT = sbB("KTT", [128, 4, 128], BF16)
                AAK, rAAK = sbB("AAK", [128, 8, 128], BF16); ARKT, rARKT = sbB("ARKT", [128, 8, 128], BF16)
                ARBT, rARBT = sbB("ARBT", [128, 8, 128], BF16)
                PQ = [[sbB(f"PQ{g}{i}", [128, 4, 128], BF16) for i in range(4)] for g in range(2)]
                TT = [[sbB(f"TT{g}{i}", [128, 4, 128], BF16) for i in range(2)] for g in range(2)]
                ABPT, rABPT = sbB("ABPT", [128, 4, 128], BF16); AAKPT, rAAKPT = sbB("AAKPT", [128, 8, 128], BF16)
                UB, rUB = sbB("UB", [128, 512], BF16)
                YF, rYF = sbB("YF", [128, 512]); TB2, rTB2 = sbB("TB2", [128, 512])
                HF = [sbB("HF", [128, 4, 64])] * NSEQ
                HB, rHB = sbB("HB", [128, 4, 64], BF16)
                QKC, rQKC = sbB("QKC", [128, 4, 131])
                ACC_, rACC = sbB("ACC", [128, 512]); ACC = v3(ACC_[:], 4)
                QKT, rQKT = sbB("QKT", [128, 4, 128], BF16)
                KP, rKP = sbB("KP", [128, 4, 64], BF16)
                VE, rVE = sbB("VE", [128, 4, 129], BF16)
                G8, rG8 = sbB("G8", [128, 48])
                TM, rTM = ACC_, rACC; DG, rDG = v3(TM[:], 4), rTM
                MST = [sbB("MST", [128, 4])] * NSEQ
                CF = [sbB("CF", [128, 2, 129])] * NSEQ
                CBF, rCBF = sbB("CBF", [128, 2, 129], BF16)
                PTB, rPTB = sbB("PTB", [128, 4, 128], BF16)
                HM_, rHM = sbB("HM", [128, 512]); HM = v3(HM_[:], 4)
                SO, rSO = sbB("SO", [128, 512])
                MIX, rMIX = sbB("MIX", [128, D], BF16)
                MIXT, rMIXT = AAKPT, rAAKPT
                DBG, rDBG = sbB("DBG", [128, 512]) if dbg else (None, None)

                S.op("pool", lambda e: e.memset(VE[:], 1.0), writes=[rVE])

                def dbg_out(nm, ap, rr, r0):
                    if dbg:
                        S.dma("sp", dbg_d[nm][r0:r0 + 128, :], ap, reads=[rr])

                LT2 = [sbB(f"LTb{i}", [128, 256], BF16) for i in range(2)]
                tiles = [(s_, c_) for s_ in range(NSEQ) for c_ in range(NT)]
                EP, rEP = PT[1]
                EPF = EP[:].bitcast(F32)

                def mk_proj(HTx):
                    cur = lambda k: HTx[:, k, 1:129]
                    prv = lambda k: HTx[:, k, 0:128]

                    def proj_tok(p, col, n, shifted):
                        def f(e):
                            last = None
                            nm = 16 if shifted else 8
                            i = 0
                            for k in range(8):
                                last = e.matmul(p, lhsT=cur(k), rhs=WIN[:, k, col:col + n], start=(i == 0), stop=(i == nm - 1)); i += 1
                                if shifted:
                                    last = e.matmul(p, lhsT=prv(k), rhs=WIN[:, k, RWC + col:RWC + col + n], start=False, stop=(i == nm - 1)); i += 1
                            return last
                        return f

                    def proj_feat(p, col, shifted):
                        def f(e):
                            last = None
                            nm = 16 if shifted else 8
                            i = 0
                            for k in range(8):
                                last = e.matmul(p, lhsT=WIN[:, k, col:col + 128], rhs=cur(k), start=(i == 0), stop=(i == nm - 1)); i += 1
                                if shifted:
                                    last = e.matmul(p, lhsT=WIN[:, k, RWC + col:RWC + col + 128], rhs=prv(k), start=False, stop=(i == nm - 1)); i += 1
                            return last
                        return f
                    return cur, prv, proj_tok, proj_feat

                def early(ti):
                    s_, c_ = tiles[ti]
                    r0_ = s_ * T + c_ * 128
                    Xn, rXn = X[ti % 2]
                    HTn, rHTn = HT[ti % 2]
                    HTq, rHTq = HT[(ti + 1) % 2]
                    LTn, rLTn = LT2[ti % 2]
                    _, _, ptok, pfeat = mk_proj(HTn)
                    S.dma("sp", Xn[:], x_d[r0_:r0_ + 128, :], writes=[rXn])
                    yield
                    rmsnorm_to_bf16(Xn[:], rXn, HN, rHN, ST, rST)
                    yield
                    yield S.op("pe", lambda e: [e.transpose(out=EP[:, k * 128:(k + 1) * 128], in_=HN[:, k * 128:(k + 1) * 128],
                                                            identity=IDB) for k in range(8)][-1], reads=[rHN, rCSB], writes=[rEP])
                    yield S.op("dve", lambda e: e.tensor_copy(out=HTn[:, :, 1:129], in_=v3(EP[:], 8)), reads=[rEP], writes=[rHTn])
                    if c_ == 0:
                        yield S.op("pool", lambda e: e.memset(HTn[:, :, 0:1], 0.0), writes=[rHTn])
                    else:
                        yield S.op("pool", lambda e: e.tensor_copy(out=HTn[:, :, 0:1], in_=HTq[:, :, 128:129]), reads=[rHTq], writes=[rHTn])
                    yield S.op("pe", pfeat(EPF[:, 0:128], 1536, True), reads=[rHTn, rWIN], writes=[rEP])
                    yield S.op("pe", pfeat(EPF[:, 128:256], 1664, True), reads=[rHTn, rWIN], writes=[rEP])
                    yield S.op("act", lambda e: e.activation(out=LTn[0:64, 0:128], in_=EPF[0:64, 0:128], func=AF.Tanh), reads=[rEP], writes=[rLTn])
                    yield S.op("act", lambda e: e.activation(out=LTn[:, 128:256], in_=EPF[:, 128:256], func=AF.Sigmoid), reads=[rEP], writes=[rLTn])
                    yield S.op("act", lambda e: e.activation(out=LTn[64:128, 0:128], in_=EPF[64:128, 0:128], func=AF.Copy), reads=[rEP], writes=[rLTn])
                    for g_, (dst, rdst) in ((1, (K, rK)), (0, (R, rR)), (2, (V, rV))):
                        yield S.op("pe", ptok(EPF, g_ * 512, 512, True), reads=[rHTn, rWIN], writes=[rEP])
                        yield S.op("act", lambda e: e.activation(out=dst[:], in_=EPF, func=AF.Copy), reads=[rEP], writes=[rdst])
                    WLn, rWLn = WL2[ti % 2]
                    pw, rpw = EPF, rEP
                    yield S.op("pe", lambda e: e.matmul(pw[:], lhsT=LTn[0:64, 0:128], rhs=WLOR[0:64, :], start=True, stop=True),
                         reads=[rLTn, rWLOR], writes=[rpw])
                    yield S.op("dve", lambda e: e.tensor_tensor(out=SG[:], in0=pw[:], in1=W0V, op=ALU.add), reads=[rpw, rVEC], writes=[rSG])
                    yield S.op("act", lambda e: e.activation(out=SG[:], in_=SG[:], func=AF.Sigmoid), reads=[rSG], writes=[rSG])
                    pa, rpa = EPF, rEP
                    yield S.op("pe", lambda e: e.matmul(pa[:], lhsT=LTn[64:128, 0:128], rhs=WLOR[64:128, :], start=True, stop=True),
                         reads=[rLTn, rWLOR], writes=[rpa])
                    yield S.op("dve", lambda e: e.tensor_tensor(out=AA[:], in0=pa[:], in1=A0V, op=ALU.add), reads=[rpa, rVEC], writes=[rAA])
                    yield S.op("act", lambda e: e.activation(out=AA[:], in_=AA[:], func=AF.Sigmoid), reads=[rAA], writes=[rAA])
                    pc, rpc = EPF, rEP
                    yield S.op("pe", lambda e: e.matmul(pc[:], lhsT=MUI, rhs=SG[:], start=True, stop=True), reads=[rCST, rSG], writes=[rpc])
                    yield S.op("act", lambda e: e.activation(out=ECL[:], in_=pc[:], func=AF.Exp, scale=-CDEC), reads=[rpc], writes=[rECL])
                    yield S.op("act", lambda e: e.activation(out=ENCL[:], in_=pc[:], func=AF.Exp, scale=CDEC), reads=[rpc], writes=[rENCL])
                    yield S.op("dve", lambda e: e.tensor_tensor(out=TA[:], in0=pc[:], in1=SG[:], op=ALU.subtract), reads=[rpc, rSG], writes=[rTA])
                    yield S.op("act", lambda e: e.activation(out=ECLM[:], in_=TA[:], func=AF.Exp, scale=-CDEC), reads=[rTA], writes=[rECLM])
                    pwl, rpwl = EPF, rEP
                    yield S.op("pe", lambda e: [e.matmul(pwl[(h % 2) * 64:(h % 2) * 64 + 64, h // 2:h // 2 + 1], lhsT=SG[:, h * 64:(h + 1) * 64], rhs=ONES[:, 0:1], start=True, stop=True)
                                          for h in range(8)][-1], reads=[rSG, rCST], writes=[rpwl])
                    yield S.op("act", lambda e: e.activation(out=WLn[:], in_=pwl[:, 0:4], func=AF.Exp, scale=-CDEC), reads=[rpwl], writes=[rWLn])
                    yield S.op("dve", lambda e: e.tensor_tensor(out=KKN[:], in0=K[:], in1=KKV, op=ALU.mult), reads=[rK, rVEC], writes=[rKKN])
                    yield S.op("pool", lambda e: e.tensor_tensor(out=TB[:], in0=KKN[:], in1=KKN[:], op=ALU.mult), reads=[rKKN], writes=[rTB])
                    yield S.op("dve", lambda e: e.tensor_reduce(out=S8E[:, 0:8], in_=v3(TB[:], 8), axis=AX.X, op=ALU.add), reads=[rTB], writes=[rS8E])
                    yield S.op("act", lambda e: e.activation(out=S8E[:, 8:16], in_=S8E[:, 0:8], func=AF.Sqrt), reads=[rS8E], writes=[rS8E])
                    yield S.op("dve", lambda e: e.tensor_scalar(out=S8E[:, 8:16], in0=S8E[:, 8:16], scalar1=1e-12, scalar2=None, op0=ALU.max),
                         reads=[rS8E], writes=[rS8E])
                    yield S.op("dve", lambda e: e.reciprocal(out=S8E[:, 16:24], in_=S8E[:, 8:16]), reads=[rS8E], writes=[rS8E])
                    yield S.op("dve", lambda e: e.tensor_tensor(out=v3(KKN[:], 8), in0=v3(KKN[:], 8), in1=bc3(S8E[:, 16:24], 8, 64), op=ALU.mult),
                         reads=[rKKN, rS8E], writes=[rKKN])
                    yield S.op("dve", lambda e: e.scalar_tensor_tensor(out=TB[:], in0=AA[:], scalar=-1.0, in1=KAV, op0=ALU.add, op1=ALU.mult),
                         reads=[rAA, rVEC], writes=[rTB])
                    yield S.op("dve", lambda e: e.scalar_tensor_tensor(out=KM[:], in0=TB[:], scalar=1.0, in1=K[:], op0=ALU.add, op1=ALU.mult),
                         reads=[rTB, rK], writes=[rKM])

                def L_late():
                    yield S.op("dve", lambda e: e.tensor_tensor(out=RBAR[:], in0=R[:], in1=ECL[:], op=ALU.mult), reads=[rR, rECL], writes=[rRBAR])
                    yield S.op("dve", lambda e: e.scalar_tensor_tensor(out=ABAR[:], in0=KKN[:], scalar=-1.0, in1=ECLM[:], op0=ALU.mult, op1=ALU.mult),
                         reads=[rKKN, rECLM], writes=[rABAR])
                    yield S.op("pool", lambda e: e.tensor_tensor(out=TA[:], in0=KKN[:], in1=AA[:], op=ALU.mult), reads=[rKKN, rAA], writes=[rTA])
                    yield S.op("pool", lambda e: e.tensor_tensor(out=BTIL[:], in0=TA[:], in1=ENCL[:], op=ALU.mult), reads=[rTA, rENCL], writes=[rBTIL])
                    yield S.op("dve", lambda e: e.tensor_tensor(out=KTIL[:], in0=KM[:], in1=ENCL[:], op=ALU.mult), reads=[rKM, rENCL], writes=[rKTIL])
                    yield S.op("act", lambda e: e.activation(out=VB[:], in_=V[:], func=AF.Copy), reads=[rV], writes=[rVB])
                    yield S.op("pool", lambda e: e.tensor_tensor(out=TB[:], in0=R[:], in1=KM[:], op=ALU.mult), reads=[rR, rKM], writes=[rTB])
                    yield S.op("pool", lambda e: e.tensor_tensor(out=TB[:], in0=TB[:], in1=RKV, op=ALU.mult), reads=[rTB, rVEC], writes=[rTB])
                    yield S.op("dve", lambda e: e.tensor_reduce(out=S8[:, 24:32], in_=v3(TB[:], 8), axis=AX.X, op=ALU.add), reads=[rTB], writes=[rS8])
                    yield S.op("dve", lambda e: e.tensor_tensor(out=v3(BON[:], 8), in0=v3(V[:], 8), in1=bc3(S8[:, 24:32], 8, 64), op=ALU.mult),
                         reads=[rV, rS8], writes=[rBON])
                    for src, rsrc, dst, rdst in ((RBAR, rRBAR, RBT, rRBT), (ABAR, rABAR, ABT, rABT),
                                                 (BTIL, rBTIL, BTT, rBTT), (KTIL, rKTIL, KTT, rKTT)):
                        pt, rpt = ptb()
                        yield S.op("pe", lambda e: [e.transpose(out=pt[:, b * 128:(b + 1) * 128], in_=src[:, b * 128:(b + 1) * 128],
                                                          identity=IDB) for b in range(4)][-1], reads=[rsrc, rCSB], writes=[rpt])
                        yield S.op("act", lambda e: e.activation(out=dst[:], in_=v3(pt[:, 0:512], 4), func=AF.Copy), reads=[rpt], writes=[rdst])


                for ti, (s, c) in enumerate(tiles):
                    if True:
                        HFs, rHFs = HF[s]
                        CFs, rCFs = CF[s]
                        Ms, rMs = MST[s]
                        if c == 0:
                            S.op("pool", lambda e: e.memset(HFs[:], 0.0), writes=[rHFs])
                            S.op("pool", lambda e: e.memset(CFs[:], 0.0), writes=[rCFs])
                            S.op("pool", lambda e: e.memset(Ms[:], 0.0), writes=[rMs])
                        r0 = s * T + c * 128
                        Xc, rXc = X[ti % 2]
                        HTc, rHTc = HT[ti % 2]
                        LT, rLT = LT2[ti % 2]
                        WL, rWL = WL2[ti % 2]
                        cur, prv, proj_tok, proj_feat = mk_proj(HTc)
                        if ti == 0:
                            for _ in early(0):
                                pass
                            for _ in L_late():
                                pass
                        chk(2)
                        chk(4)

                        def hop(Tt, h):
                            return Tt[(h % 2) * 64:(h % 2) * 64 + 64, h // 2, :]

                        def rw_group(g):
                            hs = [g, g + 2, g + 4, g + 6]
                            P0, rP0 = PQ[g][0]; Q0, rQ0 = PQ[g][1]

                            def mm4(p, A, Bm):
                                return lambda e: [e.matmul(p[:, j * 128:(j + 1) * 128], lhsT=hop(A, h), rhs=hop(Bm, h), start=True, stop=True)
                                                  for j, h in enumerate(hs) if (VAR != 3 or h % 2 == 0) and (VAR != 4 or h % 2 == 1)][-1]
                            for (A, rA, Bm, rB, dst, rdst, mask, eng) in (
                                    (ABT, rABT, BTT, rBTT, P0[:], rP0, ML, "dve"),
                                    (BTT, rBTT, ABT, rABT, Q0[:], rQ0, MU, "dve"),
                                    (ABT, rABT, KTT, rKTT, AAK[:, 4 * g:4 * g + 4, :], rAAK, ML, "dve"),
                                    (KTT, rKTT, RBT, rRBT, ARKT[:, 4 * g:4 * g + 4, :], rARKT, MUI, "dve"),
                                    (BTT, rBTT, RBT, rRBT, ARBT[:, 4 * g:4 * g + 4, :], rARBT, MUI, "dve")):
                                p, rp = pf("g%d" % g)
                                yield S.op("pe", mm4(p, A, Bm), reads=[rA, rB], writes=[rp])
                                if VAR in (1, 3, 4):
                                    pass
                                elif VAR == 2:
                                    for j4 in range(4):
                                        yield S.op(eng, lambda e: e.tensor_tensor(out=dst[:, j4, :], in0=p[:, j4 * 128:(j4 + 1) * 128], in1=mask, op=ALU.mult),
                                             reads=[rp, rCST], writes=[rdst])
                                else:
                                    yield S.op(eng, lambda e: e.tensor_tensor(out=dst, in0=v3(p[:], 4), in1=bcm(mask, 4), op=ALU.mult),
                                         reads=[rp, rCST], writes=[rdst])
                            Tc, rTc = TT[g][0]
                            yield S.op("pool", lambda e: e.tensor_tensor(out=Tc[:], in0=Q0[:], in1=bcm(IDB, 4), op=ALU.add),
                                 reads=[rQ0, rCSB], writes=[rTc])
                            Pp, rPp, Qp, rQp = P0, rP0, Q0, rQ0
                            ti = 0
                            for lev in range(1, 7):
                                Pn, rPn = PQ[g][2 * (lev % 2)]
                                Qn, rQn = PQ[g][2 * (lev % 2) + 1]
                                p, rp = pf("g%d" % g)
                                yield S.op("pe", lambda e: [e.matmul(p[:, j * 128:(j + 1) * 128], lhsT=Qp[:, j, :], rhs=Pp[:, j, :], start=True, stop=True)
                                                      for j in range(4)][-1], reads=[rPp, rQp], writes=[rp])
                                yield S.op("act", lambda e: e.activation(out=Pn[:], in_=v3(p[:], 4), func=AF.Copy), reads=[rp], writes=[rPn])
                                if lev < 6:
                                    p2, rp2 = pf("g%d" % g)
                                    yield S.op("pe", lambda e: [e.matmul(p2[:, j * 128:(j + 1) * 128], lhsT=Pp[:, j, :], rhs=Qp[:, j, :], start=True, stop=True)
                                                          for j in range(4)][-1], reads=[rPp, rQp], writes=[rp2])
                                    yield S.op("act", lambda e: e.activation(out=Qn[:], in_=v3(p2[:], 4), func=AF.Copy), reads=[rp2], writes=[rQn])
                                Tn, rTn = TT[g][(ti + 1) % 2]
                                p3, rp3 = pf("g%d" % g)
                                yield S.op("pe", lambda e: [e.matmul(p3[:, j * 128:(j + 1) * 128], lhsT=Pn[:, j, :], rhs=Tc[:, j, :], start=True, stop=True)
                                                      for j in range(4)][-1], reads=[rPn, rTc], writes=[rp3])
                                yield S.op("dve", lambda e: e.tensor_tensor(out=Tn[:], in0=v3(p3[:], 4), in1=Tc[:], op=ALU.add),
                                     reads=[rp3, rTc], writes=[rTn])
                                Tc, rTc = Tn, rTn
                                ti += 1
                                Pp, rPp, Qp, rQp = Pn, rPn, Qn, rQn
                            p, rp = pf("g%d" % g)
                            yield S.op("pe", lambda e: [e.matmul(p[g * 64:g * 64 + 64, j * 128:(j + 1) * 128], lhsT=ABAR[:, h * 64:(h + 1) * 64], rhs=Tc[:, j, :],
                                                           start=True, stop=True) for j, h in enumerate(hs)][-1],
                                 reads=[rABAR, rTc], writes=[rp])
                            yield S.op("act", lambda e: e.activation(out=ABPT[g * 64:g * 64 + 64, :, :], in_=v3(p[g * 64:g * 64 + 64, :], 4), func=AF.Copy),
                                 reads=[rp], writes=[rABPT])
                            p, rp = pf("g%d" % g)
                            yield S.op("pe", lambda e: [e.matmul(p[:, j * 128:(j + 1) * 128], lhsT=AAK[:, 4 * g + j, :], rhs=Tc[:, j, :],
                                                           start=True, stop=True) for j, h in enumerate(hs)][-1],
                                 reads=[rAAK, rTc], writes=[rp])
                            yield S.op("dve", lambda e: e.tensor_copy(out=AAKPT[:, 4 * g:4 * g + 4, :], in_=v3(p[:], 4)), reads=[rp], writes=[rAAKPT])


                        def chain_R():
                            yield from interleave(rw_group(0), rw_group(1))
                            yield S.op("act", lambda e: e.activation(out=HB[:], in_=HFs[:], func=AF.Copy), reads=[rHFs], writes=[rHB])
                            pu, rpu = pf("r")

                            def fu(e):
                                last = None
                                for h in range(8):
                                    e.matmul(pu[:, h * 64:(h + 1) * 64], lhsT=hop(ABPT, h), rhs=hop(HB, h), start=True, stop=False)
                                    last = e.matmul(pu[:, h * 64:(h + 1) * 64], lhsT=AAKPT[:, (h % 2) * 4 + h // 2, :], rhs=VB[:, h * 64:(h + 1) * 64], start=False, stop=True)
                                return last
                            yield S.op("pe", fu, reads=[rABPT, rHB, rAAKPT, rVB], writes=[rpu])
                            yield S.op("act", lambda e: e.activation(out=UB[:], in_=pu[:], func=AF.Copy), reads=[rpu], writes=[rUB])
                            py, rpy = pf("r")

                            def fy(e):
                                last = None
                                for h in range(8):
                                    o = py[:, h * 64:(h + 1) * 64]
                                    e.matmul(o, lhsT=hop(RBT, h), rhs=hop(HB, h), start=True, stop=False)
                                    e.matmul(o, lhsT=ARBT[:, (h % 2) * 4 + h // 2, :], rhs=UB[:, h * 64:(h + 1) * 64], start=False, stop=False)
                                    last = e.matmul(o, lhsT=ARKT[:, (h % 2) * 4 + h // 2, :], rhs=VB[:, h * 64:(h + 1) * 64], start=False, stop=True)
                                return last
                            yield S.op("pe", fy, reads=[rRBT, rHB, rARBT, rUB, rARKT, rVB], writes=[rpy])
                            ph, rph = pf("r")

                            def fh(e):
                                last = None
                                for h in range(8):
                                    o = ph[(h % 2) * 64:(h % 2) * 64 + 64, (h // 2) * 64:(h // 2 + 1) * 64]
                                    e.matmul(o, lhsT=BTIL[:, h * 64:(h + 1) * 64], rhs=UB[:, h * 64:(h + 1) * 64], start=True, stop=False)
                                    last = e.matmul(o, lhsT=KTIL[:, h * 64:(h + 1) * 64], rhs=VB[:, h * 64:(h + 1) * 64], start=False, stop=True)
                                return last
                            yield S.op("pe", fh, reads=[rBTIL, rKTIL, rUB, rVB], writes=[rph])
                            yield S.op("dve", lambda e: e.tensor_tensor(out=HFs[:], in0=v3(ph[:, 0:256], 4), in1=HFs[:], op=ALU.add),
                                 reads=[rph, rHFs], writes=[rHFs])
                            yield S.op("dve", lambda e: e.tensor_tensor(out=HFs[:], in0=HFs[:], in1=bc3(WL[:], 4, 64), op=ALU.mult),
                                 reads=[rHFs, rWL], writes=[rHFs])
                            yield S.op("act", lambda e: e.activation(out=YF[:], in_=py[:], func=AF.Copy), reads=[rpy], writes=[rYF])
                            dbg_out("y", YF[:], rYF, r0)
                            yield S.op("dve", lambda e: e.tensor_reduce(out=S8[:, 32:40], in_=v3(YF[:], 8), axis=AX.X, op=ALU.add), reads=[rYF], writes=[rS8])
                            yield S.op("dve", lambda e: e.tensor_scalar(out=S8[:, 32:40], in0=S8[:, 32:40], scalar1=1.0 / 64, scalar2=None, op0=ALU.mult),
                                 reads=[rS8], writes=[rS8])
                            yield S.op("dve", lambda e: e.tensor_tensor(out=v3(YF[:], 8), in0=v3(YF[:], 8), in1=bc3(S8[:, 32:40], 8, 64), op=ALU.subtract),
                                 reads=[rYF, rS8], writes=[rYF])
                            yield S.op("pool", lambda e: e.tensor_tensor(out=TB2[:], in0=YF[:], in1=YF[:], op=ALU.mult), reads=[rYF], writes=[rTB2])
                            yield S.op("dve", lambda e: e.tensor_reduce(out=S8[:, 40:48], in_=v3(TB2[:], 8), axis=AX.X, op=ALU.add), reads=[rTB2], writes=[rS8])
                            yield S.op("dve", lambda e: e.tensor_scalar(out=S8[:, 40:48], in0=S8[:, 40:48], scalar1=1.0 / 64, scalar2=64e-5,
                                                                  op0=ALU.mult, op1=ALU.add), reads=[rS8], writes=[rS8])
                            yield S.op("act", lambda e: e.activation(out=S8[:, 40:48], in_=S8[:, 40:48], func=AF.Sqrt), reads=[rS8], writes=[rS8])
                            yield S.op("dve", lambda e: e.reciprocal(out=S8[:, 48:56], in_=S8[:, 40:48]), reads=[rS8], writes=[rS8])
                            yield S.op("dve", lambda e: e.tensor_tensor(out=v3(YF[:], 8), in0=v3(YF[:], 8), in1=bc3(S8[:, 48:56], 8, 64), op=ALU.mult),
                                 reads=[rYF, rS8], writes=[rYF])
                            yield S.op("pool", lambda e: e.tensor_tensor(out=YF[:], in0=YF[:], in1=LWV, op=ALU.mult), reads=[rYF, rVEC], writes=[rYF])
                            yield S.op("pool", lambda e: e.tensor_tensor(out=YF[:], in0=YF[:], in1=LBV, op=ALU.add), reads=[rYF, rVEC], writes=[rYF])
                            yield S.op("pool", lambda e: e.tensor_tensor(out=YF[:], in0=YF[:], in1=BON[:], op=ALU.add), reads=[rYF, rBON], writes=[rYF])
                            pg, rpg = pf("r")
                            yield S.op("pe", lambda e: e.matmul(pg[:], lhsT=LT[:, 128:256], rhs=WG[:], start=True, stop=True), reads=[rLT, rWG], writes=[rpg])
                            if dbg:
                                yield S.op("dve", lambda e: e.tensor_tensor(out=DBG[:], in0=YF[:], in1=pg[:], op=ALU.mult), reads=[rYF, rpg], writes=[rDBG])
                                dbg_out("yrw", DBG[:], rDBG, r0)
                            yield S.op("dve", lambda e: e.tensor_tensor(out=MIX[:, 0:512], in0=YF[:], in1=pg[:], op=ALU.mult), reads=[rYF, rpg], writes=[rMIX])


                        def chain_M():
                            pqk, rpqk = pf("m")
                            for b in range(4):
                                yield S.op("pe", proj_feat(pqk[:, b * 128:(b + 1) * 128], MLO - 0 + b * 128 if False else 0, False) if False else
                                     (lambda e, b=b: [e.matmul(pqk[:, b * 128:(b + 1) * 128], lhsT=WIN[:, k, MLO + b * 128:MLO + (b + 1) * 128], rhs=cur(k),
                                                              start=(k == 0), stop=(k == 7)) for k in range(8)][-1]),
                                     reads=[rHTc, rWIN], writes=[rpqk])
                            if c == 0:
                                yield S.op("pool", lambda e: e.memset(QKC[:, :, 0:3], 0.0), writes=[rQKC])
                            else:
                                yield S.op("pool", lambda e: e.tensor_copy(out=QKC[:, :, 0:3], in_=QKC[:, :, 128:131]), reads=[rQKC], writes=[rQKC])
                            yield S.op("act", lambda e: e.activation(out=QKC[:, :, 3:131], in_=v3(pqk[:], 4), func=AF.Copy), reads=[rpqk], writes=[rQKC])
                            for b in range(4):
                                eng = "dve"
                                yield S.op(eng, lambda e, b=b: e.tensor_scalar(out=ACC[:, b, :], in0=QKC[:, b, 0:128], scalar1=CW[:, b, 0:1], scalar2=CB[:, b:b + 1],
                                                                         op0=ALU.mult, op1=ALU.add), reads=[rQKC, rCW, rCB], writes=[rACC])
                                for j in range(1, 4):
                                    yield S.op(eng, lambda e, b=b, j=j: e.scalar_tensor_tensor(out=ACC[:, b, :], in0=QKC[:, b, j:j + 128], scalar=CW[:, b, j:j + 1],
                                                                                        in1=ACC[:, b, :], op0=ALU.mult, op1=ALU.add),
                                         reads=[rQKC, rCW, rACC], writes=[rACC])
                            yield S.op("act", lambda e: e.activation(out=QKT[:], in_=ACC, func=AF.Silu), reads=[rACC], writes=[rQKT])
                            pv, rpv = pf("m")
                            yield S.op("pe", proj_tok(pv[:], MLO + 512 - 0, 512, False) if False else
                                 (lambda e: [e.matmul(pv[:], lhsT=cur(k), rhs=WIN[:, k, MLO + 512:MLO + 1024], start=(k == 0), stop=(k == 7)) for k in range(8)][-1]),
                                 reads=[rHTc, rWIN], writes=[rpv])
                            yield S.op("act", lambda e: e.activation(out=VE[:, :, 0:128], in_=v3(pv[:], 4), func=AF.Copy), reads=[rpv], writes=[rVE])
                            po, rpo = pf("m")
                            yield S.op("pe", lambda e: [e.matmul(po[:], lhsT=cur(k), rhs=WIN[:, k, MLO + 1024:MLO + 1536], start=(k == 0), stop=(k == 7)) for k in range(8)][-1],
                                 reads=[rHTc, rWIN], writes=[rpo])
                            yield S.op("act", lambda e: e.activation(out=SO[:], in_=po[:], func=AF.Sigmoid), reads=[rpo], writes=[rSO])
                            pgt, rpgt = pf("m")
                            yield S.op("pe", lambda e: [e.matmul(pgt[:, 0:8], lhsT=cur(k), rhs=WIN[:, k, MLO + 1536:MLO + 1544], start=(k == 0), stop=(k == 7)) for k in range(8)][-1],
                                 reads=[rHTc, rWIN], writes=[rpgt])
                            yield S.op("dve", lambda e: e.tensor_tensor(out=G8[:, 0:8], in0=pgt[:, 0:8], in1=GB[:], op=ALU.add), reads=[rpgt, rGB], writes=[rG8])
                            yield S.op("act", lambda e: e.activation(out=G8[:, 0:8], in_=G8[:, 0:8], func=AF.Tanh, scale=1.0 / 15), reads=[rG8], writes=[rG8])
                            yield S.op("act", lambda e: e.activation(out=G8[:, 8:12], in_=G8[:, 4:8], func=AF.Exp, scale=-15.0), reads=[rG8], writes=[rG8])
                            yield S.op("dve", lambda e: e.tensor_scalar(out=G8[:, 8:12], in0=G8[:, 8:12], scalar1=1.0, scalar2=None, op0=ALU.add), reads=[rG8], writes=[rG8])
                            yield S.op("act", lambda e: e.activation(out=G8[:, 12:16], in_=G8[:, 8:12], func=AF.Ln), reads=[rG8], writes=[rG8])
                            pb, rpb = pf("m")
                            yield S.op("pe", lambda e: e.matmul(pb[:, 0:4], lhsT=MUI, rhs=G8[:, 12:16], start=True, stop=True), reads=[rCST, rG8], writes=[rpb])
                            yield S.op("pe", lambda e: e.matmul(pb[:, 4:8], lhsT=ONES, rhs=G8[:, 12:16], start=True, stop=True), reads=[rCST, rG8], writes=[rpb])
                            yield S.op("dve", lambda e: e.scalar_tensor_tensor(out=G8[:, 16:20], in0=G8[:, 0:4], scalar=15.0, in1=pb[:, 0:4], op0=ALU.mult, op1=ALU.add),
                                 reads=[rG8, rpb], writes=[rG8])
                            yield S.op("dve", lambda e: e.tensor_tensor(out=DG, in0=bcm(IDF, 4), in1=bc3(G8[:, 16:20], 4, 128), op=ALU.mult),
                                 reads=[rCST, rG8], writes=[rDG])
                            pgm, rpgm = pf("m")
                            yield S.op("pe", lambda e: e.matmul(pgm[:], lhsT=ONES, rhs=TM[:], start=True, stop=True),
                                 reads=[rCST, rDG], writes=[rpgm])
                            yield S.op("dve", lambda e: e.tensor_reduce(out=G8[:, 20:24], in_=v3(pgm[:], 4), axis=AX.X, op=ALU.max), reads=[rpgm], writes=[rG8])
                            yield S.op("dve", lambda e: e.tensor_tensor(out=G8[:, 20:24], in0=G8[:, 20:24], in1=Ms[:], op=ALU.max), reads=[rG8, rMs], writes=[rG8])
                            yield S.op("dve", lambda e: e.tensor_tensor(out=G8[:, 40:44], in0=G8[:, 16:20], in1=G8[:, 20:24], op=ALU.subtract), reads=[rG8], writes=[rG8])
                            yield S.op("act", lambda e: e.activation(out=G8[:, 24:28], in_=G8[:, 40:44], func=AF.Exp), reads=[rG8], writes=[rG8])
                            yield S.op("dve", lambda e: e.tensor_tensor(out=G8[:, 44:48], in0=Ms[:], in1=G8[:, 20:24], op=ALU.subtract), reads=[rG8, rMs], writes=[rG8])
                            yield S.op("act", lambda e: e.activation(out=G8[:, 28:32], in_=G8[:, 44:48], func=AF.Exp), reads=[rG8], writes=[rG8])
                            yield S.op("dve", lambda e: e.tensor_scalar(out=G8[:, 36:40], in0=G8[:, 28:32], scalar1=0.125, scalar2=None, op0=ALU.mult), reads=[rG8], writes=[rG8])
                            yield S.op("dve", lambda e: e.tensor_tensor(out=G8[:, 40:44], in0=pb[:, 0:4], in1=G8[:, 20:24], op=ALU.subtract), reads=[rpb, rG8], writes=[rG8])
                            yield S.op("act", lambda e: e.activation(out=G8[:, 32:36], in_=G8[:, 40:44], func=AF.Exp), reads=[rG8], writes=[rG8])
                            yield S.op("dve", lambda e: e.tensor_tensor(out=Ms[:], in0=G8[:, 20:24], in1=pb[:, 4:8], op=ALU.subtract), reads=[rG8, rpb], writes=[rMs])
                            pt, rpt = ptb()
                            yield S.op("pe", lambda e: [e.transpose(out=pt[:, b * 128:(b + 1) * 128], in_=QKT[:, 2 + b, :], identity=IDB) for b in range(2)][-1],
                                 reads=[rQKT, rCSB], writes=[rpt])
                            yield S.op("dve", lambda e: e.tensor_tensor(out=KP[:], in0=v3(pt[:, 0:256], 4), in1=bc3(G8[:, 24:28], 4, 64), op=ALU.mult),
                                 reads=[rpt, rG8], writes=[rKP])
                            psts = [pf("m"), pf("m")]
                            for par in range(2):
                                pst, rpst = psts[par]
                                yield S.op("pe", lambda e: [e.matmul(pst[:, (h // 2) * 128:(h // 2 + 1) * 128], lhsT=QKT[par * 64:par * 64 + 64, 2 + h // 2, :],
                                                               rhs=QKT[par * 64:par * 64 + 64, h // 2, :], start=True, stop=True) for h in (par, par + 2)][-1],
                                     reads=[rQKT], writes=[rpst])
                            for h in range(4):
                                pst, rpst = psts[h % 2]
                                yield S.op("dve", lambda e, h=h: e.scalar_tensor_tensor(out=PTB[:, h, :], in0=pst[:, (h // 2) * 128:(h // 2 + 1) * 128], scalar=G8[:, 24 + h:25 + h],
                                                                                 in1=MUI8[:], op0=ALU.mult, op1=ALU.mult), reads=[rpst, rG8, rMUI8], writes=[rPTB])
                            for h in range(4):
                                po_ = (h % 2) * 64
                                yield S.op("dve", lambda e, h=h: e.tensor_scalar(out=CBF[po_:po_ + 64, h // 2, :], in0=CFs[po_:po_ + 64, h // 2, :],
                                                                            scalar1=G8[po_:po_ + 64, 36 + h:37 + h], scalar2=None, op0=ALU.mult),
                                     reads=[rCFs, rG8], writes=[rCBF])
                            pn = [pf("m"), pf("m")]
                            for i2 in range(2):
                                pnn, rpnn = pn[i2]

                                def fn(e, i2=i2, pnn=pnn):
                                    last = None
                                    for j in range(2):
                                        h = 2 * i2 + j
                                        o = pnn[:, j * 129:(j + 1) * 129]
                                        e.matmul(o, lhsT=PTB[:, h, :], rhs=VE[:, h, :], start=True, stop=False)
                                        last = e.matmul(o, lhsT=QKT[(h % 2) * 64:(h % 2) * 64 + 64, h // 2, :], rhs=CBF[(h % 2) * 64:(h % 2) * 64 + 64, h // 2, :], start=False, stop=True)
                                    return last
                                yield S.op("pe", fn, reads=[rPTB, rVE, rQKT, rCBF], writes=[rpnn])
                            for i2 in range(2):
                                pnn, rpnn = pn[i2]
                                yield S.op("dve", lambda e, i2=i2, pnn=pnn: e.tensor_copy(out=S8[:, 56 + 2 * i2:58 + 2 * i2],
                                                                                  in_=pnn[:, 0:258].rearrange("p (a b) -> p a b", a=2)[:, :, 128:129].rearrange("p a b -> p (a b)")),
                                     reads=[rpnn], writes=[rS8])
                            yield S.op("dve", lambda e: e.tensor_scalar(out=G8[:, 40:44], in0=S8[:, 56:60], scalar1=-1.0, scalar2=None, op0=ALU.mult), reads=[rS8], writes=[rG8])
                            yield S.op("dve", lambda e: e.tensor_tensor(out=S8[:, 56:60], in0=S8[:, 56:60], in1=G8[:, 40:44], op=ALU.max), reads=[rS8, rG8], writes=[rS8])
                            yield S.op("dve", lambda e: e.tensor_tensor(out=S8[:, 56:60], in0=S8[:, 56:60], in1=G8[:, 32:36], op=ALU.max), reads=[rS8, rG8], writes=[rS8])
                            yield S.op("dve", lambda e: e.reciprocal(out=S8[:, 60:64], in_=S8[:, 56:60]), reads=[rS8], writes=[rS8])
                            for i2 in range(2):
                                pnn, rpnn = pn[i2]
                                yield S.op("dve", lambda e, i2=i2, pnn=pnn: e.tensor_tensor(out=HM[:, 2 * i2:2 * i2 + 2, :],
                                                                                    in0=pnn[:, 0:258].rearrange("p (a b) -> p a b", a=2)[:, :, 0:128],
                                                                                    in1=bc3(S8[:, 60 + 2 * i2:62 + 2 * i2], 2, 128), op=ALU.mult),
                                     reads=[rpnn, rS8], writes=[rHM])
                            pcc, rpcc = pf("m")
                            yield S.op("pe", lambda e: [e.matmul(pcc[(h % 2) * 64:(h % 2) * 64 + 64, (h // 2) * 129:(h // 2 + 1) * 129], lhsT=KP[:, h, :], rhs=VE[:, h, :],
                                                           start=True, stop=True) for h in range(4)][-1], reads=[rKP, rVE], writes=[rpcc])
                            for h in range(4):
                                po_ = (h % 2) * 64
                                yield S.op("dve", lambda e, h=h: e.scalar_tensor_tensor(out=CFs[po_:po_ + 64, h // 2, :], in0=CFs[po_:po_ + 64, h // 2, :],
                                                                                 scalar=G8[po_:po_ + 64, 28 + h:29 + h],
                                                                                 in1=pcc[po_:po_ + 64, (h // 2) * 129:(h // 2 + 1) * 129], op0=ALU.mult, op1=ALU.add),
                                     reads=[rCFs, rG8, rpcc], writes=[rCFs])
                            HM2 = HM_[:]
                            dbg_out("hm", HM2, rHM, r0)
                            yield S.op("pool", lambda e: e.tensor_tensor(out=TM[:], in0=HM2, in1=HM2, op=ALU.mult), reads=[rHM], writes=[rTM])
                            yield S.op("dve", lambda e: e.tensor_reduce(out=G8[:, 40:44], in_=v3(TM[:], 4), axis=AX.X, op=ALU.add), reads=[rTM], writes=[rG8])
                            yield S.op("dve", lambda e: e.tensor_scalar(out=G8[:, 40:44], in0=G8[:, 40:44], scalar1=1.0 / 128, scalar2=1e-6, op0=ALU.mult, op1=ALU.add),
                                 reads=[rG8], writes=[rG8])
                            yield S.op("act", lambda e: e.activation(out=G8[:, 40:44], in_=G8[:, 40:44], func=AF.Sqrt), reads=[rG8], writes=[rG8])
                            yield S.op("dve", lambda e: e.reciprocal(out=G8[:, 44:48], in_=G8[:, 40:44]), reads=[rG8], writes=[rG8])
                            yield S.op("dve", lambda e: e.tensor_tensor(out=HM, in0=HM, in1=bc3(G8[:, 44:48], 4, 128), op=ALU.mult), reads=[rHM, rG8], writes=[rHM])
                            if dbg:
                                yield S.op("dve", lambda e: e.tensor_tensor(out=DBG[:], in0=HM2, in1=SO[:], op=ALU.mult), reads=[rHM, rSO], writes=[rDBG])
                                dbg_out("yml", DBG[:], rDBG, r0)
                            yield S.op("pool", lambda e: e.tensor_tensor(out=MIX[:, 512:1024], in0=HM2, in1=SO[:], op=ALU.mult), reads=[rHM, rSO], writes=[rMIX])


                        def speed(gen, n):
                            while True:
                                for _k in range(n):
                                    try:
                                        next(gen)
                                    except StopIteration:
                                        return
                                yield
                        NP_, NR_, NM_ = 1, 4, 3
                        nxt = [speed(early(ti + 1), NP_)] if ti + 1 < len(tiles) else []
                        for _ in interleave(speed(chain_R(), NR_), speed(chain_M(), NM_), *nxt):
                            pass
                        chk(7)
                        def w_out_chain():
                            pt_, rpt = PF[5]; pt = pt_[:].bitcast(BF16)
                            yield S.op("pe", lambda e: [e.transpose(out=pt[:, k * 128:(k + 1) * 128], in_=MIX[:, k * 128:(k + 1) * 128], identity=IDB) for k in range(8)][-1],
                                 reads=[rMIX, rCSB], writes=[rpt])
                            yield S.op("act", lambda e: e.activation(out=MIXT[:], in_=v3(pt[:], 8), func=AF.Copy), reads=[rpt], writes=[rMIXT])
                            for g in range(2):
                                p, rp = PF[3 + g]
                                yield S.op("pe", lambda e, g=g, p=p: [e.matmul(p[:], lhsT=MIXT[:, k, :], rhs=WOUT[:, k, g * 512:(g + 1) * 512], start=(k == 0), stop=(k == 7))
                                                              for k in range(8)][-1], reads=[rMIXT, rWOUT], writes=[rp])
                                yield S.op("dve", lambda e, g=g, p=p: e.tensor_tensor(out=Xc[:, g * 512:(g + 1) * 512], in0=p[:], in1=Xc[:, g * 512:(g + 1) * 512], op=ALU.add),
                                     reads=[rp, rXc], writes=[rXc])
                            S.dma("sp", x1_d[r0:r0 + 128, :], Xc[:], reads=[rXc])

                            yield
                        for _ in interleave(speed(w_out_chain(), 2), *([speed(L_late(), 3)] if ti + 1 < len(tiles) else [])):
                            pass
                        chk(8)
                S.barrier()
            S.barrier()

        es2 = ExitStack()
        with es2:
            sb2, _ = mk(es2)
            TOKS = 512 if T % 512 == 0 else 256
            NSUB = TOKS // 128
            WUP, rWUP = sb2("WUP", [128, 8, 2 * DFF], BF16)
            WDN, rWDN = sb2("WDN", [128, NB, D], BF16)
            FCW, rFCW = sb2("FCW", [128, NB, 3])
            FCB, rFCB = sb2("FCB", [128, NB])
            GF, rGF = sb2("GF", [128, D])
            S.dma("sp", FCW[:].rearrange("p b j -> p (b j)"), ffn_conv_w, writes=[rFCW])
            S.dma("sp", FCB[:], ffn_conv_b, writes=[rFCB])
            S.dma("sp", GF[:], norm_f_g.partition_broadcast(128), writes=[rGF])
            esC = ExitStack()
            with esC:
                sbC, _ = mk(esC)
                G2, rG2 = sbC("G2", [128, 8])
                STG2 = [sbC(f"STH{i}", [128, DFF]) for i in range(2)]
                S.dma("sp", G2[:], norm2_g, writes=[rG2])
                i = 0
                for k in range(8):
                    for hlf in range(2):
                        st, rst = STG2[i % 2]; i += 1
                        S.dma("sp", st[:], w_ffn_up[k * 128:(k + 1) * 128, hlf * DFF:(hlf + 1) * DFF], writes=[rst])
                        eng = "act" if hlf == 0 else "dve"
                        if eng == "act":
                            S.op("act", lambda e: e.activation(out=WUP[:, k, hlf * DFF:(hlf + 1) * DFF], in_=st[:], func=AF.Copy, scale=G2[:, k:k + 1]),
                                 reads=[rst, rG2], writes=[rWUP])
                        else:
                            S.op("dve", lambda e: e.tensor_scalar(out=WUP[:, k, hlf * DFF:(hlf + 1) * DFF], in0=st[:], scalar1=G2[:, k:k + 1], scalar2=None, op0=ALU.mult),
                                 reads=[rst, rG2], writes=[rWUP])
                for b in range(0, NB, 2):
                    st, rst = STG2[i % 2]; i += 1
                    S.dma("sp", st[:, 0:2 * D].rearrange("p (a n) -> p a n", a=2), w_ffn_down[b * 128:(b + 2) * 128, :].rearrange("(a p) n -> p a n", p=128),
                          writes=[rst])
                    S.op("pool" if (b // 2) % 2 else "dve", lambda e: e.tensor_copy(out=WDN[:, b:b + 2, :], in_=st[:, 0:2 * D].rearrange("p (a n) -> p a n", a=2)),
                         reads=[rst], writes=[rWDN])
                S.barrier()
            chk(9)
            esD = ExitStack()
            with esD:
                sbD, _ = mk(esD)
                XS = [sbD(f"XS{i}", [128, D]) for i in range(NSUB)]
                HN2, rHN2 = sbD("HN2", [128, D], BF16)
                H2T, rH2T = sbD("H2T", [128, 8, TOKS], BF16)
                GT, rGT = sbD("GT", [128, NB, TOKS], BF16)
                AC = [sbD(f"AC{i}", [128, TOKS + 2]) for i in range(2)]
                AQ = [sbD(f"AQ{i}", [128, TOKS]) for i in range(2)]
                CAR, rCAR = sbD("CAR", [128, NB, 2])
                ST2, rST2 = sbD("ST2", [128, 8])
                it = 0
                for s in range(NSEQ):
                    S.op("pool", lambda e: e.memset(CAR[:], 0.0), writes=[rCAR])
                    for c in range(T // TOKS):
                        r0 = s * T + c * TOKS
                        for sub in range(NSUB):
                            Xs, rXs = XS[sub]
                            S.dma("sp", Xs[:], x1_d[r0 + sub * 128:r0 + (sub + 1) * 128, :], writes=[rXs])
                            rmsnorm_to_bf16(Xs[:], rXs, HN2, rHN2, ST2, rST2)
                            pt, rpt = ptb()
                            S.op("pe", lambda e: [e.transpose(out=pt[:, k * 128:(k + 1) * 128], in_=HN2[:, k * 128:(k + 1) * 128], identity=IDB) for k in range(8)][-1],
                                 reads=[rHN2, rCSB], writes=[rpt])
                            S.op("dve", lambda e, sub=sub: e.tensor_copy(out=H2T[:, :, sub * 128:(sub + 1) * 128], in_=v3(pt[:], 8)), reads=[rpt], writes=[rH2T])
                        for b in range(NB):
                            ACb, rACb = AC[b % 2]
                            AQb, rAQb = AQ[b % 2]
                            pa, rpa = pf()
                            pbk, rpbk = pf()
                            S.op("pe", lambda e, b=b, pa=pa: [e.matmul(pa[:, 0:TOKS], lhsT=WUP[:, k, b * 128:(b + 1) * 128], rhs=H2T[:, k, :], start=(k == 0), stop=(k == 7))
                                                             for k in range(8)][-1], reads=[rWUP, rH2T], writes=[rpa])
                            S.op("pe", lambda e, b=b, pbk=pbk: [e.matmul(pbk[:, 0:TOKS], lhsT=WUP[:, k, DFF + b * 128:DFF + (b + 1) * 128], rhs=H2T[:, k, :], start=(k == 0), stop=(k == 7))
                                                               for k in range(8)][-1], reads=[rWUP, rH2T], writes=[rpbk])
                            S.op("pool", lambda e, b=b: e.tensor_copy(out=ACb[:, 0:2], in_=CAR[:, b, :]), reads=[rCAR], writes=[rACb])
                            S.op("act", lambda e: e.activation(out=ACb[:, 2:TOKS + 2], in_=pa[:, 0:TOKS], func=AF.Copy), reads=[rpa], writes=[rACb])
                            S.op("pool", lambda e, b=b: e.tensor_copy(out=CAR[:, b, :], in_=ACb[:, TOKS:TOKS + 2]), reads=[rACb], writes=[rCAR])
                            S.op("dve", lambda e, b=b: e.tensor_scalar(out=AQb[:], in0=ACb[:, 0:TOKS], scalar1=FCW[:, b, 0:1], scalar2=FCB[:, b:b + 1], op0=ALU.mult, op1=ALU.add),
                                 reads=[rACb, rFCW, rFCB], writes=[rAQb])
                            S.op("dve", lambda e, b=b: e.scalar_tensor_tensor(out=AQb[:], in0=ACb[:, 1:TOKS + 1], scalar=FCW[:, b, 1:2], in1=AQb[:], op0=ALU.mult, op1=ALU.add),
                                 reads=[rACb, rFCW, rAQb], writes=[rAQb])
                            S.op("dve", lambda e, b=b: e.scalar_tensor_tensor(out=AQb[:], in0=ACb[:, 2:TOKS + 2], scalar=FCW[:, b, 2:3], in1=AQb[:], op0=ALU.mult, op1=ALU.add),
                                 reads=[rACb, rFCW, rAQb], writes=[rAQb])
                            S.op("act", lambda e: e.activation(out=AQb[:], in_=AQb[:], func=AF.Silu), reads=[rAQb], writes=[rAQb])
                            S.op("dve", lambda e, b=b: e.tensor_tensor(out=GT[:, b, :], in0=AQb[:], in1=pbk[:, 0:TOKS], op=ALU.mult), reads=[rAQb, rpbk], writes=[rGT])
                        for sub in range(NSUB):
                            Xs, rXs = XS[sub]
                            X2c, rX2c = Xs, rXs
                            for g in range(2):
                                p, rp = pf()
                                S.op("pe", lambda e, g=g, p=p, sub=sub: [e.matmul(p[:], lhsT=GT[:, b, sub * 128:(sub + 1) * 128], rhs=WDN[:, b, g * 512:(g + 1) * 512],
                                                                                 start=(b == 0), stop=(b == NB - 1)) for b in range(NB)][-1], reads=[rGT, rWDN], writes=[rp])
                                S.op("dve", lambda e, g=g, p=p: e.tensor_tensor(out=X2c[:, g * 512:(g + 1) * 512], in0=p[:], in1=Xs[:, g * 512:(g + 1) * 512], op=ALU.add),
                                     reads=[rp, rXs], writes=[rX2c])
                            S.op("pool", lambda e: e.memset(ST2[:, 4:5], 0.0), writes=[rST2])
                            S.op("act", lambda e: e.activation(out=HN2[:], in_=X2c[:], func=AF.Square, accum_out=ST2[:, 4:5]), reads=[rX2c, rST2], writes=[rHN2, rST2])
                            S.op("dve", lambda e: e.tensor_scalar(out=ST2[:, 5:6], in0=ST2[:, 4:5], scalar1=1.0 / D, scalar2=1e-6, op0=ALU.mult, op1=ALU.add),
                                 reads=[rST2], writes=[rST2])
                            S.op("act", lambda e: e.activation(out=ST2[:, 6:7], in_=ST2[:, 5:6], func=AF.Sqrt), reads=[rST2], writes=[rST2])
                            S.op("dve", lambda e: e.reciprocal(out=ST2[:, 7:8], in_=ST2[:, 6:7]), reads=[rST2], writes=[rST2])
                            S.op("act", lambda e: e.activation(out=X2c[:], in_=X2c[:], func=AF.Copy, scale=ST2[:, 7:8]), reads=[rX2c, rST2], writes=[rX2c])
                            S.op("pool", lambda e: e.tensor_tensor(out=X2c[:], in0=X2c[:], in1=GF[:], op=ALU.mult), reads=[rX2c, rGF], writes=[rX2c])
                            S.dma("sp", out_d[r0 + sub * 128:r0 + (sub + 1) * 128, :], X2c[:], reads=[rX2c])
                S.barrier()
            S.barrier()
    return nc


def make_consts():
    c = np.zeros((128, 640), np.float32)
    i = np.arange(128)
    c[:, 0:128] = np.eye(128)
    c[:, 128:256] = (i[:, None] < i[None, :])
    c[:, 256:384] = (i[:, None] <= i[None, :])
    c[:, 384:512] = (i[:, None] > i[None, :])
    c[:, 512:640] = 1.0
    return c


def make_in_maps(inputs, n_cores, nseq):
    f = lambda a: np.ascontiguousarray(np.asarray(a, np.float32))
    x = f(inputs["x"])
    T = x.shape[1]
    shared = {}
    for k, v in inputs.items():
        if k == "x":
            continue
        a = f(v)
        if k != "norm_f_g":
            a = a[0]
        if k == "r_k":
            a = a.reshape(512)
        elif k in ("norm1_g", "norm2_g", "qk_conv_b", "ffn_conv_b", "mh_norm_g"):
            a = a.reshape(-1, 128).T
        elif k in ("qk_conv_w", "ffn_conv_w"):
            j = a.shape[0]
            a = a.reshape(j, -1, 128).transpose(2, 1, 0).reshape(128, -1)
        shared[k] = np.ascontiguousarray(a)
    shared["consts"] = make_consts()
    maps = []
    for c in range(n_cores):
        m = dict(shared)
        m["x"] = np.ascontiguousarray(x[c * nseq:(c + 1) * nseq].reshape(nseq * T, D))
        maps.append(m)
    return maps


def kernel(**inputs):
    x = np.asarray(inputs["x"])
    B, T, _ = x.shape
    nseq = B // N_CORES
    nc = build(nseq, T)
    maps = make_in_maps(inputs, N_CORES, nseq)
    res = run_bass_kernel_spmd(nc, maps, core_ids=list(range(N_CORES)))
    out = np.concatenate([r["out"].reshape(nseq, T, D) for r in res.results], axis=0)
    return out.astype(np.float32)
```

```python
import numpy as np
from contextlib import ExitStack
import concourse.bass as bass
import concourse.mybir as mybir
from concourse.bass_utils import run_bass_kernel_spmd

F32 = mybir.dt.float32
BF16 = mybir.dt.bfloat16
ALU = mybir.AluOpType
AF = mybir.ActivationFunctionType
AX = mybir.AxisListType

N_CORES = 8
D = 1024
N_IN = 3336
RWC = 1792
MLO = 2 * RWC
WIN_COLS = 2 * RWC + 1544
DFF = 2816
NB = DFF // 128
CDEC = float(np.exp(-0.5))


class Res:
    __slots__ = ("name", "lw", "rd")

    def __init__(self, name):
        self.name = name
        self.lw = None
        self.rd = []


class Sched:
    EPOCH = 30000
    NDMA = 24

    def __init__(self, nc, es):
        self.nc = nc
        self.es = es
        self.engs = {"pe": nc.tensor, "act": nc.scalar, "dve": nc.vector,
                     "pool": nc.gpsimd, "sp": nc.sync}
        self.sems = {e: [] for e in self.engs}
        self.cnt = {e: 0 for e in self.engs}
        self.waited = {e: {} for e in self.engs}
        self.dma_sems = [es.enter_context(nc.semaphore(f"dq{i}")) for i in range(self.NDMA)]
        self.dma_cnt = [0] * self.NDMA
        self.dma_i = 0
        for e in self.engs:
            self._new_epoch(e)

    def _new_epoch(self, e):
        s = self.es.enter_context(self.nc.semaphore(f"s_{e}_{len(self.sems[e])}"))
        self.sems[e].append(s)
        self.cnt[e] = 0

    def _wait(self, e, dep):
        if dep[0] == "dma":
            _, idx, val = dep
            key = ("dma", idx)
            sem = self.dma_sems[idx]
        else:
            de, ep, val = dep
            key = (de, ep)
            sem = self.sems[de][ep]
        if self.waited[e].get(key, 0) >= val:
            return
        self.engs[e].wait_ge(sem, val)
        self.waited[e][key] = val

    def _deps(self, e, reads, writes):
        deps = []
        for r in reads:
            if r.lw is not None and not (r.lw[0] == e and e == "pe"):
                deps.append(r.lw)
        for w in writes:
            if w.lw is not None and w.lw[0] != e:
                deps.append(w.lw)
            for d in w.rd:
                if d[0] != e:
                    deps.append(d)
        return deps

    def _mark(self, tag, reads, writes):
        for r in reads:
            r.rd.append(tag)
            if len(r.rd) > 64:
                r.rd = r.rd[-48:]
        for w in writes:
            w.lw = tag
            w.rd = []

    def op(self, e, fn, reads=(), writes=()):
        for d in self._deps(e, reads, writes):
            self._wait(e, d)
        if self.cnt[e] >= self.EPOCH:
            self._new_epoch(e)
        ins = fn(self.engs[e])
        ep = len(self.sems[e]) - 1
        ins.then_inc(self.sems[e][ep], 1)
        self.cnt[e] += 1
        tag = (e, ep, self.cnt[e])
        self._mark(tag, reads, writes)
        return tag

    def dma(self, q, out, in_, reads=(), writes=(), slow=False):
        for d in self._deps(q, reads, writes):
            self._wait(q, d)
        idx = self.dma_i
        self.dma_i = (self.dma_i + 1) % self.NDMA
        kw = {"allow_slow_non_contiguous": True} if slow else {}
        self.engs[q].dma_start(out=out, in_=in_, **kw).then_inc(self.dma_sems[idx], 16)
        self.dma_cnt[idx] += 16
        tag = ("dma", idx, self.dma_cnt[idx])
        self._mark(tag, reads, writes)
        return tag

    def barrier(self):
        for e in self.engs:
            for d in self.engs:
                if d != e:
                    ep = len(self.sems[d]) - 1
                    if self.cnt[d] > 0:
                        self._wait(e, (d, ep, self.cnt[d]))
                    elif ep > 0:
                        self._wait(e, (d, ep - 1, self.EPOCH))
            for i in range(self.NDMA):
                if self.dma_cnt[i]:
                    self._wait(e, ("dma", i, self.dma_cnt[i]))


VAR = 0


class _Stop(Exception):
    pass


def build(NSEQ, T, dbg=False, stop=0):
    nc = bass.Bass("TRN2", target_bir_lowering=False)
    try:
        _build(nc, NSEQ, T, dbg, stop)
    except _Stop:
        pass
    return nc


def _build(nc, NSEQ, T, dbg, stop):
    NT = T // 128
    NTOK = NSEQ * T
    di = lambda n, s: nc.dram_tensor(n, s, F32, kind="ExternalInput").ap()
    x_d = di("x", [NTOK, D])
    norm1_g = di("norm1_g", [128, 8]); w_in = di("w_in", [D, N_IN]); rw_mu = di("rw_mu", [RWC])
    w0 = di("w0", [512]); w_up_decay = di("w_up_decay", [64, 512]); a0 = di("a0", [512])
    w_up_a = di("w_up_a", [64, 512]); w_up_g = di("w_up_g", [128, 512]); k_k = di("k_k", [512])
    k_a = di("k_a", [512]); r_k = di("r_k", [512]); lnx_w = di("lnx_w", [512]); lnx_b = di("lnx_b", [512])
    qk_conv_w = di("qk_conv_w", [128, 16]); qk_conv_b = di("qk_conv_b", [128, 4])
    i_bias = di("i_bias", [4]); f_bias = di("f_bias", [4]); mh_norm_g = di("mh_norm_g", [128, 4])
    w_out = di("w_out", [D, D]); norm2_g = di("norm2_g", [128, 8]); w_ffn_up = di("w_ffn_up", [D, 2 * DFF])
    ffn_conv_w = di("ffn_conv_w", [128, NB * 3]); ffn_conv_b = di("ffn_conv_b", [128, NB])
    w_ffn_down = di("w_ffn_down", [DFF, D]); norm_f_g = di("norm_f_g", [D])
    consts = di("consts", [128, 640])
    out_d = nc.dram_tensor("out", [NTOK, D], F32, kind="ExternalOutput").ap()
    x1_d = nc.dram_tensor("x1s", [NTOK, D], F32, kind="Internal").ap()
    dbg_d = {}
    if dbg:
        for nm in ("yrw", "yml", "y", "hm"):
            dbg_d[nm] = nc.dram_tensor("d_" + nm, [NTOK, 512], F32, kind="ExternalOutput").ap()

    es0 = ExitStack()
    with es0:
        S = Sched(nc, es0)

        def chk(n):
            if stop == n:
                S.barrier()
                raise _Stop()

        def mk(es):
            def sb(n, s, d=F32):
                return es.enter_context(nc.sbuf_tensor(n, s, d)), Res(n)

            def ps(n, s, d=F32):
                return es.enter_context(nc.psum_tensor(n, s, d)), Res(n)
            return sb, ps

        sb0, ps0 = mk(es0)
        CST, rCST = sb0("CST", [128, 640])
        S.dma("sp", CST[:], consts, writes=[rCST])
        IDF = CST[:, 0:128]; MU = CST[:, 128:256]; MUI = CST[:, 256:384]; ML = CST[:, 384:512]; ONES = CST[:, 512:640]
        CSB, rCSB = sb0("CSB", [128, 128], BF16)
        S.op("dve", lambda e: e.tensor_copy(out=CSB[:], in_=CST[:, 0:128]), reads=[rCST], writes=[rCSB])
        IDB = CSB[:, 0:128]
        MUI8, rMUI8 = sb0("MUI8", [128, 128])
        S.op("dve", lambda e: e.tensor_scalar(out=MUI8[:], in0=MUI, scalar1=0.125, scalar2=None, op0=ALU.mult),
             reads=[rCST], writes=[rMUI8])
        NPF = 6
        PF = [ps0(f"PF{i}", [128, 512]) for i in range(NPF)]
        PT = [ps0(f"PT{i}", [128, 1024], BF16) for i in range(2)]
        pf_i = [0]
        pt_i = [0]

        POOLS = {"all": [0, 1, 2, 3, 4, 5], "g0": [0, 1], "g1": [2, 3], "m": [4, 5], "r": [0, 1, 2, 3]}
        pool_i = {k: 0 for k in POOLS}

        def pf(pool="all"):
            lst = POOLS[pool]
            p = PF[lst[pool_i[pool] % len(lst)]]
            pool_i[pool] += 1
            return p

        def interleave(*gens):
            gens = list(gens)
            while gens:
                for gg in list(gens):
                    try:
                        next(gg)
                        yield
                    except StopIteration:
                        gens.remove(gg)

        def ptb():
            return PT[0]

        def bc3(ap2, a, b):
            return ap2.rearrange("p (a o) -> p a o", o=1).to_broadcast([ap2.shape[0], a, b])

        def bcm(ap2, a):
            return ap2.rearrange("p (o n) -> p o n", o=1).to_broadcast([ap2.shape[0], a, ap2.shape[1]])

        def v3(ap, a):
            return ap.rearrange("p (a b) -> p a b", a=a)

        def rmsnorm_to_bf16(X, rX, HN, rHN, ST, rST):
            S.op("pool", lambda e: e.memset(ST[:, 0:1], 0.0), writes=[rST])
            S.op("act", lambda e: e.activation(out=HN[:], in_=X, func=AF.Square, accum_out=ST[:, 0:1]),
                 reads=[rX, rST], writes=[rHN, rST])
            S.op("dve", lambda e: e.tensor_scalar(out=ST[:, 1:2], in0=ST[:, 0:1], scalar1=1.0 / D, scalar2=1e-6,
                                                  op0=ALU.mult, op1=ALU.add), reads=[rST], writes=[rST])
            S.op("act", lambda e: e.activation(out=ST[:, 2:3], in_=ST[:, 1:2], func=AF.Sqrt), reads=[rST], writes=[rST])
            S.op("dve", lambda e: e.reciprocal(out=ST[:, 3:4], in_=ST[:, 2:3]), reads=[rST], writes=[rST])
            S.op("act", lambda e: e.activation(out=HN[:], in_=X, func=AF.Copy, scale=ST[:, 3:4]),
                 reads=[rX, rST], writes=[rHN])

        es1 = ExitStack()
        with es1:
            sb1, _ = mk(es1)
            WIN, rWIN = sb1("WIN", [128, 8, WIN_COLS], BF16)
            WOUT, rWOUT = sb1("WOUT", [128, 8, D], BF16)
            WLOR, rWLOR = sb1("WLOR", [128, 512], BF16)
            WG, rWG = sb1("WG", [128, 512], BF16)
            VEC, rVEC = sb1("VEC", [128, 7, 512])
            MHG4, rMHG4 = sb1("MHG4", [128, 4])
            CW, rCW = sb1("CW", [128, 4, 4])
            CB, rCB = sb1("CB", [128, 4])
            GB, rGB = sb1("GB", [128, 8])
            for i, v in enumerate((w0, a0, k_k, k_a, r_k, lnx_w, lnx_b)):
                S.dma("sp", VEC[:, i, :], v.partition_broadcast(128), writes=[rVEC])
            S.dma("sp", CW[:].rearrange("p b j -> p (b j)"), qk_conv_w, writes=[rCW])
            S.dma("sp", CB[:], qk_conv_b, writes=[rCB])
            S.dma("sp", GB[:, 0:4], i_bias.partition_broadcast(128), writes=[rGB])
            S.dma("sp", GB[:, 4:8], f_bias.partition_broadcast(128), writes=[rGB])
            W0V = VEC[:, 0, :]; A0V = VEC[:, 1, :]; KKV = VEC[:, 2, :]; KAV = VEC[:, 3, :]
            RKV = VEC[:, 4, :]; LWV = VEC[:, 5, :]; LBV = VEC[:, 6, :]
            S.dma("sp", MHG4[:], mh_norm_g, writes=[rMHG4])
            esA = ExitStack()
            with esA:
                sbA, _ = mk(esA)
                MUT, rMUT = sbA("MUT", [128, RWC])
                OMM, rOMM = sbA("OMM", [128, RWC])
                G1, rG1 = sbA("G1", [128, 8])
                STG = [sbA(f"STG{i}", [128, N_IN]) for i in range(2)]
                S.dma("sp", MUT[:], rw_mu.partition_broadcast(128), writes=[rMUT])
                S.dma("sp", G1[:], norm1_g, writes=[rG1])
                S.op("dve", lambda e: e.tensor_scalar(out=OMM[:], in0=MUT[:], scalar1=-1.0, scalar2=1.0,
                                                      op0=ALU.mult, op1=ALU.add), reads=[rMUT], writes=[rOMM])
                for k in range(8):
                    st, rst = STG[k % 2]
                    S.dma("sp", st[:], w_in[k * 128:(k + 1) * 128, :], writes=[rst])
                    S.op("act", lambda e: e.activation(out=st[:], in_=st[:], func=AF.Copy, scale=G1[:, k:k + 1]),
                         reads=[rst, rG1], writes=[rst])
                    S.op("dve", lambda e: e.tensor_tensor(out=WIN[:, k, 0:RWC], in0=st[:, 0:RWC], in1=OMM[:], op=ALU.mult),
                         reads=[rst, rOMM], writes=[rWIN])
                    S.op("pool", lambda e: e.tensor_tensor(out=WIN[:, k, RWC:2 * RWC], in0=st[:, 0:RWC], in1=MUT[:], op=ALU.mult),
                         reads=[rst, rMUT], writes=[rWIN])
                    S.op("act", lambda e: e.activation(out=WIN[:, k, MLO:WIN_COLS], in_=st[:, RWC:N_IN], func=AF.Copy),
                         reads=[rst], writes=[rWIN])
                for k in range(8):
                    st, rst = STG[k % 2]
                    S.dma("sp", st[:, 0:D], w_out[k * 128:(k + 1) * 128, :], writes=[rst])
                    if k < 4:
                        S.op("dve", lambda e: e.tensor_copy(out=WOUT[:, k, :], in_=st[:, 0:D]), reads=[rst], writes=[rWOUT])
                    else:
                        S.op("dve", lambda e: e.tensor_scalar(out=WOUT[:, k, :], in0=st[:, 0:D], scalar1=MHG4[:, k - 4:k - 3], scalar2=None, op0=ALU.mult),
                             reads=[rst, rMHG4], writes=[rWOUT])
                st, rst = STG[0]
                S.dma("sp", st[0:64, 0:512], w_up_decay, writes=[rst])
                S.dma("sp", st[64:128, 0:512], w_up_a, writes=[rst])
                S.dma("sp", st[:, 512:1024], w_up_g, writes=[rst])
                S.op("dve", lambda e: e.tensor_copy(out=WLOR[:], in_=st[:, 0:512]), reads=[rst], writes=[rWLOR])
                S.op("dve", lambda e: e.tensor_copy(out=WG[:], in_=st[:, 512:1024]), reads=[rst], writes=[rWG])
                S.barrier()
            chk(1)
            esB = ExitStack()
            with esB:
                sbB, _ = mk(esB)
                X = [sbB(f"X{i}", [128, D]) for i in range(2)]
                HN, rHN = sbB("HN", [128, D], BF16)
                HT = [sbB(f"HT{i}", [128, 8, 129], BF16) for i in range(2)]
                ST, rST = sbB("ST", [128, 8])
                R, rR = sbB("R", [128, 512]); K, rK = sbB("K", [128, 512]); V, rV = sbB("V", [128, 512])
                SG, rSG = sbB("SG", [128, 512]); AA, rAA = sbB("AA", [128, 512])
                ECL, rECL = sbB("ECL", [128, 512]); ENCL, rENCL = sbB("ENCL", [128, 512])
                KKN, rKKN = sbB("KKN", [128, 512]); KM, rKM = sbB("KM", [128, 512])
                TA, rTA = sbB("TA", [128, 512]); TB, rTB = sbB("TB", [128, 512])
                ECLM, rECLM = TA, rTA
                BON, rBON = sbB("BON", [128, 512])
                S8, rS8 = sbB("S8", [128, 64])
                WL2 = [sbB(f"WL{i}", [128, 4]) for i in range(2)]
                S8E, rS8E = sbB("S8E", [128, 24])
                RBAR, rRBAR = sbB("RBAR", [128, 512], BF16); ABAR, rABAR = sbB("ABAR", [128, 512], BF16)
                BTIL, rBTIL = sbB("BTIL", [128, 512], BF16); KTIL, rKTIL = sbB("KTIL", [128, 512], BF16)
                VB, rVB = sbB("VB", [128, 512], BF16)
                RBT, rRBT = sbB("RBT", [128, 4, 128], BF16); ABT, rABT = sbB("ABT", [128, 4, 128], BF16)
                BTT, rBTT = sbB("BTT", [128, 4, 128], BF16); KTT, rKTT = sbB("KTT", [128, 4, 128], BF16)
                AAK, rAAK = sbB("AAK", [128, 8, 128], BF16); ARKT, rARKT = sbB("ARKT", [128, 8, 128], BF16)
                ARBT, rARBT = sbB("ARBT", [128, 8, 128], BF16)
                PQ = [[sbB(f"PQ{g}{i}", [128, 4, 128], BF16) for i in range(4)] for g in range(2)]
                TT = [[sbB(f"TT{g}{i}", [128, 4, 128], BF16) for i in range(2)] for g in range(2)]
                ABPT, rABPT = sbB("ABPT", [128, 4, 128], BF16); AAKPT, rAAKPT = sbB("AAKPT", [128, 8, 128], BF16)
                UB, rUB = sbB("UB", [128, 512], BF16)
                YF, rYF = sbB("YF", [128, 512]); TB2, rTB2 = sbB("TB2", [128, 512])
                HF = [sbB("HF", [128, 4, 64])] * NSEQ
                HB, rHB = sbB("HB", [128, 4, 64], BF16)
                QKC, rQKC = sbB("QKC", [128, 4, 131])
                ACC_, rACC = sbB("ACC", [128, 512]); ACC = v3(ACC_[:], 4)
                QKT, rQKT = sbB("QKT", [128, 4, 128], BF16)
                KP, rKP = sbB("KP", [128, 4, 64], BF16)
                VE, rVE = sbB("VE", [128, 4, 129], BF16)
                G8, rG8 = sbB("G8", [128, 48])
                TM, rTM = ACC_, rACC; DG, rDG = v3(TM[:], 4), rTM
                MST = [sbB("MST", [128, 4])] * NSEQ
                CF = [sbB("CF", [128, 2, 129])] * NSEQ
                CBF, rCBF = sbB("CBF", [128, 2, 129], BF16)
                PTB, rPTB = sbB("PTB", [128, 4, 128], BF16)
                HM_, rHM = sbB("HM", [128, 512]); HM = v3(HM_[:], 4)
                SO, rSO = sbB("SO", [128, 512])
                MIX, rMIX = sbB("MIX", [128, D], BF16)
                MIXT, rMIXT = AAKPT, rAAKPT
                DBG, rDBG = sbB("DBG", [128, 512]) if dbg else (None, None)

                S.op("pool", lambda e: e.memset(VE[:], 1.0), writes=[rVE])

                def dbg_out(nm, ap, rr, r0):
                    if dbg:
                        S.dma("sp", dbg_d[nm][r0:r0 + 128, :], ap, reads=[rr])

                LT2 = [sbB(f"LTb{i}", [128, 256], BF16) for i in range(2)]
                tiles = [(s_, c_) for s_ in range(NSEQ) for c_ in range(NT)]
                EP, rEP = PT[1]
                EPF = EP[:].bitcast(F32)

                def mk_proj(HTx):
                    cur = lambda k: HTx[:, k, 1:129]
                    prv = lambda k: HTx[:, k, 0:128]

                    def proj_tok(p, col, n, shifted):
                        def f(e):
                            last = None
                            nm = 16 if shifted else 8
                            i = 0
                            for k in range(8):
                                last = e.matmul(p, lhsT=cur(k), rhs=WIN[:, k, col:col + n], start=(i == 0), stop=(i == nm - 1)); i += 1
                                if shifted:
                                    last = e.matmul(p, lhsT=prv(k), rhs=WIN[:, k, RWC + col:RWC + col + n], start=False, stop=(i == nm - 1)); i += 1
                            return last
                        return f

                    def proj_feat(p, col, shifted):
                        def f(e):
                            last = None
                            nm = 16 if shifted else 8
                            i = 0
                            for k in range(8):
                                last = e.matmul(p, lhsT=WIN[:, k, col:col + 128], rhs=cur(k), start=(i == 0), stop=(i == nm - 1)); i += 1
                                if shifted:
                                    last = e.matmul(p, lhsT=WIN[:, k, RWC + col:RWC + col + 128], rhs=prv(k), start=False, stop=(i == nm - 1)); i += 1
                            return last
                        return f
                    return cur, prv, proj_tok, proj_feat

                def early(ti):
                    s_, c_ = tiles[ti]
                    r0_ = s_ * T + c_ * 128
                    Xn, rXn = X[ti % 2]
                    HTn, rHTn = HT[ti % 2]
                    HTq, rHTq = HT[(ti + 1) % 2]
                    LTn, rLTn = LT2[ti % 2]
                    _, _, ptok, pfeat = mk_proj(HTn)
                    S.dma("sp", Xn[:], x_d[r0_:r0_ + 128, :], writes=[rXn])
                    yield
                    rmsnorm_to_bf16(Xn[:], rXn, HN, rHN, ST, rST)
                    yield
                    yield S.op("pe", lambda e: [e.transpose(out=EP[:, k * 128:(k + 1) * 128], in_=HN[:, k * 128:(k + 1) * 128],
                                                            identity=IDB) for k in range(8)][-1], reads=[rHN, rCSB], writes=[rEP])
                    yield S.op("dve", lambda e: e.tensor_copy(out=HTn[:, :, 1:129], in_=v3(EP[:], 8)), reads=[rEP], writes=[rHTn])
                    if c_ == 0:
                        yield S.op("pool", lambda e: e.memset(HTn[:, :, 0:1], 0.0), writes=[rHTn])
                    else:
                        yield S.op("pool", lambda e: e.tensor_copy(out=HTn[:, :, 0:1], in_=HTq[:, :, 128:129]), reads=[rHTq], writes=[rHTn])
                    yield S.op("pe", pfeat(EPF[:, 0:128], 1536, True), reads=[rHTn, rWIN], writes=[rEP])
                    yield S.op("pe", pfeat(EPF[:, 128:256], 1664, True), reads=[rHTn, rWIN], writes=[rEP])
                    yield S.op("act", lambda e: e.activation(out=LTn[0:64, 0:128], in_=EPF[0:64, 0:128], func=AF.Tanh), reads=[rEP], writes=[rLTn])
                    yield S.op("act", lambda e: e.activation(out=LTn[:, 128:256], in_=EPF[:, 128:256], func=AF.Sigmoid), reads=[rEP], writes=[rLTn])
                    yield S.op("act", lambda e: e.activation(out=LTn[64:128, 0:128], in_=EPF[64:128, 0:128], func=AF.Copy), reads=[rEP], writes=[rLTn])
                    for g_, (dst, rdst) in ((1, (K, rK)), (0, (R, rR)), (2, (V, rV))):
                        yield S.op("pe", ptok(EPF, g_ * 512, 512, True), reads=[rHTn, rWIN], writes=[rEP])
                        yield S.op("act", lambda e: e.activation(out=dst[:], in_=EPF, func=AF.Copy), reads=[rEP], writes=[rdst])
                    WLn, rWLn = WL2[ti % 2]
                    pw, rpw = EPF, rEP
                    yield S.op("pe", lambda e: e.matmul(pw[:], lhsT=LTn[0:64, 0:128], rhs=WLOR[0:64, :], start=True, stop=True),
                         reads=[rLTn, rWLOR], writes=[rpw])
                    yield S.op("dve", lambda e: e.tensor_tensor(out=SG[:], in0=pw[:], in1=W0V, op=ALU.add), reads=[rpw, rVEC], writes=[rSG])
                    yield S.op("act", lambda e: e.activation(out=SG[:], in_=SG[:], func=AF.Sigmoid), reads=[rSG], writes=[rSG])
                    pa, rpa = EPF, rEP
                    yield S.op("pe", lambda e: e.matmul(pa[:], lhsT=LTn[64:128, 0:128], rhs=WLOR[64:128, :], start=True, stop=True),
                         reads=[rLTn, rWLOR], writes=[rpa])
                    yield S.op("dve", lambda e: e.tensor_tensor(out=AA[:], in0=pa[:], in1=A0V, op=ALU.add), reads=[rpa, rVEC], writes=[rAA])
                    yield S.op("act", lambda e: e.activation(out=AA[:], in_=AA[:], func=AF.Sigmoid), reads=[rAA], writes=[rAA])
                    pc, rpc = EPF, rEP
                    yield S.op("pe", lambda e: e.matmul(pc[:], lhsT=MUI, rhs=SG[:], start=True, stop=True), reads=[rCST, rSG], writes=[rpc])
                    yield S.op("act", lambda e: e.activation(out=ECL[:], in_=pc[:], func=AF.Exp, scale=-CDEC), reads=[rpc], writes=[rECL])
                    yield S.op("act", lambda e: e.activation(out=ENCL[:], in_=pc[:], func=AF.Exp, scale=CDEC), reads=[rpc], writes=[rENCL])
                    yield S.op("dve", lambda e: e.tensor_tensor(out=TA[:], in0=pc[:], in1=SG[:], op=ALU.subtract), reads=[rpc, rSG], writes=[rTA])
                    yield S.op("act", lambda e: e.activation(out=ECLM[:], in_=TA[:], func=AF.Exp, scale=-CDEC), reads=[rTA], writes=[rECLM])
                    pwl, rpwl = EPF, rEP
                    yield S.op("pe", lambda e: [e.matmul(pwl[(h % 2) * 64:(h % 2) * 64 + 64, h // 2:h // 2 + 1], lhsT=SG[:, h * 64:(h + 1) * 64], rhs=ONES[:, 0:1], start=True, stop=True)
                                          for h in range(8)][-1], reads=[rSG, rCST], writes=[rpwl])
                    yield S.op("act", lambda e: e.activation(out=WLn[:], in_=pwl[:, 0:4], func=AF.Exp, scale=-CDEC), reads=[rpwl], writes=[rWLn])
                    yield S.op("dve", lambda e: e.tensor_tensor(out=KKN[:], in0=K[:], in1=KKV, op=ALU.mult), reads=[rK, rVEC], writes=[rKKN])
                    yield S.op("pool", lambda e: e.tensor_tensor(out=TB[:], in0=KKN[:], in1=KKN[:], op=ALU.mult), reads=[rKKN], writes=[rTB])
                    yield S.op("dve", lambda e: e.tensor_reduce(out=S8E[:, 0:8], in_=v3(TB[:], 8), axis=AX.X, op=ALU.add), reads=[rTB], writes=[rS8E])
                    yield S.op("act", lambda e: e.activation(out=S8E[:, 8:16], in_=S8E[:, 0:8], func=AF.Sqrt), reads=[rS8E], writes=[rS8E])
                    yield S.op("dve", lambda e: e.tensor_scalar(out=S8E[:, 8:16], in0=S8E[:, 8:16], scalar1=1e-12, scalar2=None, op0=ALU.max),
                         reads=[rS8E], writes=[rS8E])
                    yield S.op("dve", lambda e: e.reciprocal(out=S8E[:, 16:24], in_=S8E[:, 8:16]), reads=[rS8E], writes=[rS8E])
                    yield S.op("dve", lambda e: e.tensor_tensor(out=v3(KKN[:], 8), in0=v3(KKN[:], 8), in1=bc3(S8E[:, 16:24], 8, 64), op=ALU.mult),
                         reads=[rKKN, rS8E], writes=[rKKN])
                    yield S.op("dve", lambda e: e.scalar_tensor_tensor(out=TB[:], in0=AA[:], scalar=-1.0, in1=KAV, op0=ALU.add, op1=ALU.mult),
                         reads=[rAA, rVEC], writes=[rTB])
                    yield S.op("dve", lambda e: e.scalar_tensor_tensor(out=KM[:], in0=TB[:], scalar=1.0, in1=K[:], op0=ALU.add, op1=ALU.mult),
                         reads=[rTB, rK], writes=[rKM])

                def L_late():
                    yield S.op("dve", lambda e: e.tensor_tensor(out=RBAR[:], in0=R[:], in1=ECL[:], op=ALU.mult), reads=[rR, rECL], writes=[rRBAR])
                    yield S.op("dve", lambda e: e.scalar_tensor_tensor(out=ABAR[:], in0=KKN[:], scalar=-1.0, in1=ECLM[:], op0=ALU.mult, op1=ALU.mult),
                         reads=[rKKN, rECLM], writes=[rABAR])
                    yield S.op("pool", lambda e: e.tensor_tensor(out=TA[:], in0=KKN[:], in1=AA[:], op=ALU.mult), reads=[rKKN, rAA], writes=[rTA])
                    yield S.op("pool", lambda e: e.tensor_tensor(out=BTIL[:], in0=TA[:], in1=ENCL[:], op=ALU.mult), reads=[rTA, rENCL], writes=[rBTIL])
                    yield S.op("dve", lambda e: e.tensor_tensor(out=KTIL[:], in0=KM[:], in1=ENCL[:], op=ALU.mult), reads=[rKM, rENCL], writes=[rKTIL])
                    yield S.op("act", lambda e: e.activation(out=VB[:], in_=V[:], func=AF.Copy), reads=[rV], writes=[rVB])
                    yield S.op("pool", lambda e: e.tensor_tensor(out=TB[:], in0=R[:], in1=KM[:], op=ALU.mult), reads=[rR, rKM], writes=[rTB])
                    yield S.op("pool", lambda e: e.tensor_tensor(out=TB[:], in0=TB[:], in1=RKV, op=ALU.mult), reads=[rTB, rVEC], writes=[rTB])
                    yield S.op("dve", lambda e: e.tensor_reduce(out=S8[:, 24:32], in_=v3(TB[:], 8), axis=AX.X, op=ALU.add), reads=[rTB], writes=[rS8])
                    yield S.op("dve", lambda e: e.tensor_tensor(out=v3(BON[:], 8), in0=v3(V[:], 8), in1=bc3(S8[:, 24:32], 8, 64), op=ALU.mult),
                         reads=[rV, rS8], writes=[rBON])
                    for ti_, (src, rsrc, dst, rdst) in enumerate(((RBAR, rRBAR, RBT, rRBT), (ABAR, rABAR, ABT, rABT),
                                                 (BTIL, rBTIL, BTT, rBTT), (KTIL, rKTIL, KTT, rKTT))):
                        if ti_ == 0:
                            pt, rpt = PT[0]
                        else:
                            pt_, rpt = PF[ti_ - 1]
                            pt = pt_[:].bitcast(BF16)
                        yield S.op("pe", lambda e: [e.transpose(out=pt[:, b * 128:(b + 1) * 128], in_=src[:, b * 128:(b + 1) * 128],
                                                          identity=IDB) for b in range(4)][-1], reads=[rsrc, rCSB], writes=[rpt])
                        yield S.op("act", lambda e: e.activation(out=dst[:], in_=v3(pt[:, 0:512], 4), func=AF.Copy), reads=[rpt], writes=[rdst])


                for ti, (s, c) in enumerate(tiles):
                    if True:
                        HFs, rHFs = HF[s]
                        CFs, rCFs = CF[s]
                        Ms, rMs = MST[s]
                        if c == 0:
                            S.op("pool", lambda e: e.memset(HFs[:], 0.0), writes=[rHFs])
                            S.op("pool", lambda e: e.memset(CFs[:], 0.0), writes=[rCFs])
                            S.op("pool", lambda e: e.memset(Ms[:], 0.0), writes=[rMs])
                        r0 = s * T + c * 128
                        Xc, rXc = X[ti % 2]
                        HTc, rHTc = HT[ti % 2]
                        LT, rLT = LT2[ti % 2]
                        WL, rWL = WL2[ti % 2]
                        cur, prv, proj_tok, proj_feat = mk_proj(HTc)
                        if ti == 0:
                            for _ in early(0):
                                pass
                            for _ in L_late():
                                pass
                        chk(2)
                        chk(4)

                        def hop(Tt, h):
                            return Tt[(h % 2) * 64:(h % 2) * 64 + 64, h // 2, :]

                        def rw_group(g):
                            hs = [g, g + 2, g + 4, g + 6]
                            P0, rP0 = PQ[g][0]; Q0, rQ0 = PQ[g][1]

                            def mm4(p, A, Bm):
                                return lambda e: [e.matmul(p[:, j * 128:(j + 1) * 128], lhsT=hop(A, h), rhs=hop(Bm, h), start=True, stop=True)
                                                  for j, h in enumerate(hs) if (VAR != 3 or h % 2 == 0) and (VAR != 4 or h % 2 == 1)][-1]
                            for (A, rA, Bm, rB, dst, rdst, mask, eng) in (
                                    (ABT, rABT, BTT, rBTT, P0[:], rP0, ML, "dve"),
                                    (BTT, rBTT, ABT, rABT, Q0[:], rQ0, MU, "dve"),
                                    (ABT, rABT, KTT, rKTT, AAK[:, 4 * g:4 * g + 4, :], rAAK, ML, "dve"),
                                    (KTT, rKTT, RBT, rRBT, ARKT[:, 4 * g:4 * g + 4, :], rARKT, MUI, "dve"),
                                    (BTT, rBTT, RBT, rRBT, ARBT[:, 4 * g:4 * g + 4, :], rARBT, MUI, "dve")):
                                p, rp = pf("g%d" % g)
                                yield S.op("pe", mm4(p, A, Bm), reads=[rA, rB], writes=[rp])
                                if VAR in (1, 3, 4):
                                    pass
                                elif VAR == 2:
                                    for j4 in range(4):
                                        yield S.op(eng, lambda e: e.tensor_tensor(out=dst[:, j4, :], in0=p[:, j4 * 128:(j4 + 1) * 128], in1=mask, op=ALU.mult),
                                             reads=[rp, rCST], writes=[rdst])
                                else:
                                    yield S.op(eng, lambda e: e.tensor_tensor(out=dst, in0=v3(p[:], 4), in1=bcm(mask, 4), op=ALU.mult),
                                         reads=[rp, rCST], writes=[rdst])
                            Tc, rTc = TT[g][0]
                            yield S.op("pool", lambda e: e.tensor_tensor(out=Tc[:], in0=Q0[:], in1=bcm(IDB, 4), op=ALU.add),
                                 reads=[rQ0, rCSB], writes=[rTc])
                            Pp, rPp, Qp, rQp = P0, rP0, Q0, rQ0
                            ti = 0
                            for lev in range(1, 7):
                                Pn, rPn = PQ[g][2 * (lev % 2)]
                                Qn, rQn = PQ[g][2 * (lev % 2) + 1]
                                p, rp = pf("g%d" % g)
                                yield S.op("pe", lambda e: [e.matmul(p[:, j * 128:(j + 1) * 128], lhsT=Qp[:, j, :], rhs=Pp[:, j, :], start=True, stop=True)
                                                      for j in range(4)][-1], reads=[rPp, rQp], writes=[rp])
                                yield S.op("act", lambda e: e.activation(out=Pn[:], in_=v3(p[:], 4), func=AF.Copy), reads=[rp], writes=[rPn])
                                if lev < 6:
                                    p2, rp2 = pf("g%d" % g)
                                    yield S.op("pe", lambda e: [e.matmul(p2[:, j * 128:(j + 1) * 128], lhsT=Pp[:, j, :], rhs=Qp[:, j, :], start=True, stop=True)
                                                          for j in range(4)][-1], reads=[rPp, rQp], writes=[rp2])
                                    yield S.op("act", lambda e: e.activation(out=Qn[:], in_=v3(p2[:], 4), func=AF.Copy), reads=[rp2], writes=[rQn])
                                Tn, rTn = TT[g][(ti + 1) % 2]
                                p3, rp3 = pf("g%d" % g)
                                yield S.op("pe", lambda e: [e.matmul(p3[:, j * 128:(j + 1) * 128], lhsT=Pn[:, j, :], rhs=Tc[:, j, :], start=True, stop=True)
                                                      for j in range(4)][-1], reads=[rPn, rTc], writes=[rp3])
                                yield S.op("dve", lambda e: e.tensor_tensor(out=Tn[:], in0=v3(p3[:], 4), in1=Tc[:], op=ALU.add),
                                     reads=[rp3, rTc], writes=[rTn])
                                Tc, rTc = Tn, rTn
                                ti += 1
                                Pp, rPp, Qp, rQp = Pn, rPn, Qn, rQn
                            p, rp = pf("g%d" % g)
                            yield S.op("pe", lambda e: [e.matmul(p[g * 64:g * 64 + 64, j * 128:(j + 1) * 128], lhsT=ABAR[:, h * 64:(h + 1) * 64], rhs=Tc[:, j, :],
                                                           start=True, stop=True) for j, h in enumerate(hs)][-1],
                                 reads=[rABAR, rTc], writes=[rp])
                            yield S.op("act", lambda e: e.activation(out=ABPT[g * 64:g * 64 + 64, :, :], in_=v3(p[g * 64:g * 64 + 64, :], 4), func=AF.Copy),
                                 reads=[rp], writes=[rABPT])
                            p, rp = pf("g%d" % g)
                            yield S.op("pe", lambda e: [e.matmul(p[:, j * 128:(j + 1) * 128], lhsT=AAK[:, 4 * g + j, :], rhs=Tc[:, j, :],
                                                           start=True, stop=True) for j, h in enumerate(hs)][-1],
                                 reads=[rAAK, rTc], writes=[rp])
                            yield S.op("dve", lambda e: e.tensor_copy(out=AAKPT[:, 4 * g:4 * g + 4, :], in_=v3(p[:], 4)), reads=[rp], writes=[rAAKPT])


                        def chain_R():
                            yield from interleave(rw_group(0), rw_group(1))
                            yield S.op("act", lambda e: e.activation(out=HB[:], in_=HFs[:], func=AF.Copy), reads=[rHFs], writes=[rHB])
                            pu, rpu = pf("r")

                            def fu(e):
                                last = None
                                for h in range(8):
                                    e.matmul(pu[:, h * 64:(h + 1) * 64], lhsT=hop(ABPT, h), rhs=hop(HB, h), start=True, stop=False)
                                    last = e.matmul(pu[:, h * 64:(h + 1) * 64], lhsT=AAKPT[:, (h % 2) * 4 + h // 2, :], rhs=VB[:, h * 64:(h + 1) * 64], start=False, stop=True)
                                return last
                            yield S.op("pe", fu, reads=[rABPT, rHB, rAAKPT, rVB], writes=[rpu])
                            yield S.op("act", lambda e: e.activation(out=UB[:], in_=pu[:], func=AF.Copy), reads=[rpu], writes=[rUB])
                            py, rpy = pf("r")

                            def fy(e):
                                last = None
                                for h in range(8):
                                    o = py[:, h * 64:(h + 1) * 64]
                                    e.matmul(o, lhsT=hop(RBT, h), rhs=hop(HB, h), start=True, stop=False)
                                    e.matmul(o, lhsT=ARBT[:, (h % 2) * 4 + h // 2, :], rhs=UB[:, h * 64:(h + 1) * 64], start=False, stop=False)
                                    last = e.matmul(o, lhsT=ARKT[:, (h % 2) * 4 + h // 2, :], rhs=VB[:, h * 64:(h + 1) * 64], start=False, stop=True)
                                return last
                            yield S.op("pe", fy, reads=[rRBT, rHB, rARBT, rUB, rARKT, rVB], writes=[rpy])
                            ph, rph = pf("r")

                            def fh(e):
                                last = None
                                for h in range(8):
                                    o = ph[(h % 2) * 64:(h % 2) * 64 + 64, (h // 2) * 64:(h // 2 + 1) * 64]
                                    e.matmul(o, lhsT=BTIL[:, h * 64:(h + 1) * 64], rhs=UB[:, h * 64:(h + 1) * 64], start=True, stop=False)
                                    last = e.matmul(o, lhsT=KTIL[:, h * 64:(h + 1) * 64], rhs=VB[:, h * 64:(h + 1) * 64], start=False, stop=True)
                                return last
                            yield S.op("pe", fh, reads=[rBTIL, rKTIL, rUB, rVB], writes=[rph])
                            yield S.op("dve", lambda e: e.tensor_tensor(out=HFs[:], in0=v3(ph[:, 0:256], 4), in1=HFs[:], op=ALU.add),
                                 reads=[rph, rHFs], writes=[rHFs])
                            yield S.op("dve", lambda e: e.tensor_tensor(out=HFs[:], in0=HFs[:], in1=bc3(WL[:], 4, 64), op=ALU.mult),
                                 reads=[rHFs, rWL], writes=[rHFs])
                            yield S.op("act", lambda e: e.activation(out=YF[:], in_=py[:], func=AF.Copy), reads=[rpy], writes=[rYF])
                            dbg_out("y", YF[:], rYF, r0)
                            yield S.op("dve", lambda e: e.tensor_reduce(out=S8[:, 32:40], in_=v3(YF[:], 8), axis=AX.X, op=ALU.add), reads=[rYF], writes=[rS8])
                            yield S.op("dve", lambda e: e.tensor_scalar(out=S8[:, 32:40], in0=S8[:, 32:40], scalar1=1.0 / 64, scalar2=None, op0=ALU.mult),
                                 reads=[rS8], writes=[rS8])
                            yield S.op("dve", lambda e: e.tensor_tensor(out=v3(YF[:], 8), in0=v3(YF[:], 8), in1=bc3(S8[:, 32:40], 8, 64), op=ALU.subtract),
                                 reads=[rYF, rS8], writes=[rYF])
                            yield S.op("pool", lambda e: e.tensor_tensor(out=TB2[:], in0=YF[:], in1=YF[:], op=ALU.mult), reads=[rYF], writes=[rTB2])
                            yield S.op("dve", lambda e: e.tensor_reduce(out=S8[:, 40:48], in_=v3(TB2[:], 8), axis=AX.X, op=ALU.add), reads=[rTB2], writes=[rS8])
                            yield S.op("dve", lambda e: e.tensor_scalar(out=S8[:, 40:48], in0=S8[:, 40:48], scalar1=1.0 / 64, scalar2=64e-5,
                                                                  op0=ALU.mult, op1=ALU.add), reads=[rS8], writes=[rS8])
                            yield S.op("act", lambda e: e.activation(out=S8[:, 40:48], in_=S8[:, 40:48], func=AF.Sqrt), reads=[rS8], writes=[rS8])
                            yield S.op("dve", lambda e: e.reciprocal(out=S8[:, 48:56], in_=S8[:, 40:48]), reads=[rS8], writes=[rS8])
                            yield S.op("dve", lambda e: e.tensor_tensor(out=v3(YF[:], 8), in0=v3(YF[:], 8), in1=bc3(S8[:, 48:56], 8, 64), op=ALU.mult),
                                 reads=[rYF, rS8], writes=[rYF])
                            yield S.op("pool", lambda e: e.tensor_tensor(out=YF[:], in0=YF[:], in1=LWV, op=ALU.mult), reads=[rYF, rVEC], writes=[rYF])
                            yield S.op("pool", lambda e: e.tensor_tensor(out=YF[:], in0=YF[:], in1=LBV, op=ALU.add), reads=[rYF, rVEC], writes=[rYF])
                            yield S.op("pool", lambda e: e.tensor_tensor(out=YF[:], in0=YF[:], in1=BON[:], op=ALU.add), reads=[rYF, rBON], writes=[rYF])
                            pg, rpg = pf("r")
                            yield S.op("pe", lambda e: e.matmul(pg[:], lhsT=LT[:, 128:256], rhs=WG[:], start=True, stop=True), reads=[rLT, rWG], writes=[rpg])
                            if dbg:
                                yield S.op("dve", lambda e: e.tensor_tensor(out=DBG[:], in0=YF[:], in1=pg[:], op=ALU.mult), reads=[rYF, rpg], writes=[rDBG])
                                dbg_out("yrw", DBG[:], rDBG, r0)
                            yield S.op("dve", lambda e: e.tensor_tensor(out=MIX[:, 0:512], in0=YF[:], in1=pg[:], op=ALU.mult), reads=[rYF, rpg], writes=[rMIX])


                        def chain_M():
                            pqk, rpqk = pf("m")
                            for b in range(4):
                                yield S.op("pe", proj_feat(pqk[:, b * 128:(b + 1) * 128], MLO - 0 + b * 128 if False else 0, False) if False else
                                     (lambda e, b=b: [e.matmul(pqk[:, b * 128:(b + 1) * 128], lhsT=WIN[:, k, MLO + b * 128:MLO + (b + 1) * 128], rhs=cur(k),
                                                              start=(k == 0), stop=(k == 7)) for k in range(8)][-1]),
                                     reads=[rHTc, rWIN], writes=[rpqk])
                            if c == 0:
                                yield S.op("pool", lambda e: e.memset(QKC[:, :, 0:3], 0.0), writes=[rQKC])
                            else:
                                yield S.op("pool", lambda e: e.tensor_copy(out=QKC[:, :, 0:3], in_=QKC[:, :, 128:131]), reads=[rQKC], writes=[rQKC])
                            yield S.op("act", lambda e: e.activation(out=QKC[:, :, 3:131], in_=v3(pqk[:], 4), func=AF.Copy), reads=[rpqk], writes=[rQKC])
                            for b in range(4):
                                eng = "dve"
                                yield S.op(eng, lambda e, b=b: e.tensor_scalar(out=ACC[:, b, :], in0=QKC[:, b, 0:128], scalar1=CW[:, b, 0:1], scalar2=CB[:, b:b + 1],
                                                                         op0=ALU.mult, op1=ALU.add), reads=[rQKC, rCW, rCB], writes=[rACC])
                                for j in range(1, 4):
                                    yield S.op(eng, lambda e, b=b, j=j: e.scalar_tensor_tensor(out=ACC[:, b, :], in0=QKC[:, b, j:j + 128], scalar=CW[:, b, j:j + 1],
                                                                                        in1=ACC[:, b, :], op0=ALU.mult, op1=ALU.add),
                                         reads=[rQKC, rCW, rACC], writes=[rACC])
                            yield S.op("act", lambda e: e.activation(out=QKT[:], in_=ACC, func=AF.Silu), reads=[rACC], writes=[rQKT])
                            pv, rpv = pf("m")
                            yield S.op("pe", proj_tok(pv[:], MLO + 512 - 0, 512, False) if False else
                                 (lambda e: [e.matmul(pv[:], lhsT=cur(k), rhs=WIN[:, k, MLO + 512:MLO + 1024], start=(k == 0), stop=(k == 7)) for k in range(8)][-1]),
                                 reads=[rHTc, rWIN], writes=[rpv])
                            yield S.op("act", lambda e: e.activation(out=VE[:, :, 0:128], in_=v3(pv[:], 4), func=AF.Copy), reads=[rpv], writes=[rVE])
                            po, rpo = pf("m")
                            yield S.op("pe", lambda e: [e.matmul(po[:], lhsT=cur(k), rhs=WIN[:, k, MLO + 1024:MLO + 1536], start=(k == 0), stop=(k == 7)) for k in range(8)][-1],
                                 reads=[rHTc, rWIN], writes=[rpo])
                            yield S.op("act", lambda e: e.activation(out=SO[:], in_=po[:], func=AF.Sigmoid), reads=[rpo], writes=[rSO])
                            pgt, rpgt = pf("m")
                            yield S.op("pe", lambda e: [e.matmul(pgt[:, 0:8], lhsT=cur(k), rhs=WIN[:, k, MLO + 1536:MLO + 1544], start=(k == 0), stop=(k == 7)) for k in range(8)][-1],
                                 reads=[rHTc, rWIN], writes=[rpgt])
                            yield S.op("dve", lambda e: e.tensor_tensor(out=G8[:, 0:8], in0=pgt[:, 0:8], in1=GB[:], op=ALU.add), reads=[rpgt, rGB], writes=[rG8])
                            yield S.op("act", lambda e: e.activation(out=G8[:, 0:8], in_=G8[:, 0:8], func=AF.Tanh, scale=1.0 / 15), reads=[rG8], writes=[rG8])
                            yield S.op("act", lambda e: e.activation(out=G8[:, 8:12], in_=G8[:, 4:8], func=AF.Exp, scale=-15.0), reads=[rG8], writes=[rG8])
                            yield S.op("dve", lambda e: e.tensor_scalar(out=G8[:, 8:12], in0=G8[:, 8:12], scalar1=1.0, scalar2=None, op0=ALU.add), reads=[rG8], writes=[rG8])
                            yield S.op("act", lambda e: e.activation(out=G8[:, 12:16], in_=G8[:, 8:12], func=AF.Ln), reads=[rG8], writes=[rG8])
                            pb, rpb = pf("m")
                            yield S.op("pe", lambda e: e.matmul(pb[:, 0:4], lhsT=MUI, rhs=G8[:, 12:16], start=True, stop=True), reads=[rCST, rG8], writes=[rpb])
                            yield S.op("pe", lambda e: e.matmul(pb[:, 4:8], lhsT=ONES, rhs=G8[:, 12:16], start=True, stop=True), reads=[rCST, rG8], writes=[rpb])
                            yield S.op("dve", lambda e: e.scalar_tensor_tensor(out=G8[:, 16:20], in0=G8[:, 0:4], scalar=15.0, in1=pb[:, 0:4], op0=ALU.mult, op1=ALU.add),
                                 reads=[rG8, rpb], writes=[rG8])
                            yield S.op("dve", lambda e: e.tensor_tensor(out=DG, in0=bcm(IDF, 4), in1=bc3(G8[:, 16:20], 4, 128), op=ALU.mult),
                                 reads=[rCST, rG8], writes=[rDG])
                            pgm, rpgm = pf("m")
                            yield S.op("pe", lambda e: e.matmul(pgm[:], lhsT=ONES, rhs=TM[:], start=True, stop=True),
                                 reads=[rCST, rDG], writes=[rpgm])
                            yield S.op("dve", lambda e: e.tensor_reduce(out=G8[:, 20:24], in_=v3(pgm[:], 4), axis=AX.X, op=ALU.max), reads=[rpgm], writes=[rG8])
                            yield S.op("dve", lambda e: e.tensor_tensor(out=G8[:, 20:24], in0=G8[:, 20:24], in1=Ms[:], op=ALU.max), reads=[rG8, rMs], writes=[rG8])
                            yield S.op("dve", lambda e: e.tensor_tensor(out=G8[:, 40:44], in0=G8[:, 16:20], in1=G8[:, 20:24], op=ALU.subtract), reads=[rG8], writes=[rG8])
                            yield S.op("act", lambda e: e.activation(out=G8[:, 24:28], in_=G8[:, 40:44], func=AF.Exp), reads=[rG8], writes=[rG8])
                            yield S.op("dve", lambda e: e.tensor_tensor(out=G8[:, 44:48], in0=Ms[:], in1=G8[:, 20:24], op=ALU.subtract), reads=[rG8, rMs], writes=[rG8])
                            yield S.op("act", lambda e: e.activation(out=G8[:, 28:32], in_=G8[:, 44:48], func=AF.Exp), reads=[rG8], writes=[rG8])
                            yield S.op("dve", lambda e: e.tensor_scalar(out=G8[:, 36:40], in0=G8[:, 28:32], scalar1=0.125, scalar2=None, op0=ALU.mult), reads=[rG8], writes=[rG8])
                            yield S.op("dve", lambda e: e.tensor_tensor(out=G8[:, 40:44], in0=pb[:, 0:4], in1=G8[:, 20:24], op=ALU.subtract), reads=[rpb, rG8], writes=[rG8])
                            yield S.op("act", lambda e: e.activation(out=G8[:, 32:36], in_=G8[:, 40:44], func=AF.Exp), reads=[rG8], writes=[rG8])
                            yield S.op("dve", lambda e: e.tensor_tensor(out=Ms[:], in0=G8[:, 20:24], in1=pb[:, 4:8], op=ALU.subtract), reads=[rG8, rpb], writes=[rMs])
                            pt, rpt = ptb()
                            yield S.op("pe", lambda e: [e.transpose(out=pt[:, b * 128:(b + 1) * 128], in_=QKT[:, 2 + b, :], identity=IDB) for b in range(2)][-1],
                                 reads=[rQKT, rCSB], writes=[rpt])
                            yield S.op("dve", lambda e: e.tensor_tensor(out=KP[:], in0=v3(pt[:, 0:256], 4), in1=bc3(G8[:, 24:28], 4, 64), op=ALU.mult),
                                 reads=[rpt, rG8], writes=[rKP])
                            psts = [pf("m"), pf("m")]
                            for par in range(2):
                                pst, rpst = psts[par]
                                yield S.op("pe", lambda e: [e.matmul(pst[:, (h // 2) * 128:(h // 2 + 1) * 128], lhsT=QKT[par * 64:par * 64 + 64, 2 + h // 2, :],
                                                               rhs=QKT[par * 64:par * 64 + 64, h // 2, :], start=True, stop=True) for h in (par, par + 2)][-1],
                                     reads=[rQKT], writes=[rpst])
                            for h in range(4):
                                pst, rpst = psts[h % 2]
                                yield S.op("dve", lambda e, h=h: e.scalar_tensor_tensor(out=PTB[:, h, :], in0=pst[:, (h // 2) * 128:(h // 2 + 1) * 128], scalar=G8[:, 24 + h:25 + h],
                                                                                 in1=MUI8[:], op0=ALU.mult, op1=ALU.mult), reads=[rpst, rG8, rMUI8], writes=[rPTB])
                            for h in range(4):
                                po_ = (h % 2) * 64
                                yield S.op("dve", lambda e, h=h: e.tensor_scalar(out=CBF[po_:po_ + 64, h // 2, :], in0=CFs[po_:po_ + 64, h // 2, :],
                                                                            scalar1=G8[po_:po_ + 64, 36 + h:37 + h], scalar2=None, op0=ALU.mult),
                                     reads=[rCFs, rG8], writes=[rCBF])
                            pn = [pf("m"), pf("m")]
                            for i2 in range(2):
                                pnn, rpnn = pn[i2]

                                def fn(e, i2=i2, pnn=pnn):
                                    last = None
                                    for j in range(2):
                                        h = 2 * i2 + j
                                        o = pnn[:, j * 129:(j + 1) * 129]
                                        e.matmul(o, lhsT=PTB[:, h, :], rhs=VE[:, h, :], start=True, stop=False)
                                        last = e.matmul(o, lhsT=QKT[(h % 2) * 64:(h % 2) * 64 + 64, h // 2, :], rhs=CBF[(h % 2) * 64:(h % 2) * 64 + 64, h // 2, :], start=False, stop=True)
                                    return last
                                yield S.op("pe", fn, reads=[rPTB, rVE, rQKT, rCBF], writes=[rpnn])
                            for i2 in range(2):
                                pnn, rpnn = pn[i2]
                                yield S.op("dve", lambda e, i2=i2, pnn=pnn: e.tensor_copy(out=S8[:, 56 + 2 * i2:58 + 2 * i2],
                                                                                  in_=pnn[:, 0:258].rearrange("p (a b) -> p a b", a=2)[:, :, 128:129].rearrange("p a b -> p (a b)")),
                                     reads=[rpnn], writes=[rS8])
                            yield S.op("dve", lambda e: e.tensor_scalar(out=G8[:, 40:44], in0=S8[:, 56:60], scalar1=-1.0, scalar2=None, op0=ALU.mult), reads=[rS8], writes=[rG8])
                            yield S.op("dve", lambda e: e.tensor_tensor(out=S8[:, 56:60], in0=S8[:, 56:60], in1=G8[:, 40:44], op=ALU.max), reads=[rS8, rG8], writes=[rS8])
                            yield S.op("dve", lambda e: e.tensor_tensor(out=S8[:, 56:60], in0=S8[:, 56:60], in1=G8[:, 32:36], op=ALU.max), reads=[rS8, rG8], writes=[rS8])
                            yield S.op("dve", lambda e: e.reciprocal(out=S8[:, 60:64], in_=S8[:, 56:60]), reads=[rS8], writes=[rS8])
                            for i2 in range(2):
                                pnn, rpnn = pn[i2]
                                yield S.op("dve", lambda e, i2=i2, pnn=pnn: e.tensor_tensor(out=HM[:, 2 * i2:2 * i2 + 2, :],
                                                                                    in0=pnn[:, 0:258].rearrange("p (a b) -> p a b", a=2)[:, :, 0:128],
                                                                                    in1=bc3(S8[:, 60 + 2 * i2:62 + 2 * i2], 2, 128), op=ALU.mult),
                                     reads=[rpnn, rS8], writes=[rHM])
                            pcc, rpcc = pf("m")
                            yield S.op("pe", lambda e: [e.matmul(pcc[(h % 2) * 64:(h % 2) * 64 + 64, (h // 2) * 129:(h // 2 + 1) * 129], lhsT=KP[:, h, :], rhs=VE[:, h, :],
                                                           start=True, stop=True) for h in range(4)][-1], reads=[rKP, rVE], writes=[rpcc])
                            for h in range(4):
                                po_ = (h % 2) * 64
                                yield S.op("dve", lambda e, h=h: e.scalar_tensor_tensor(out=CFs[po_:po_ + 64, h // 2, :], in0=CFs[po_:po_ + 64, h // 2, :],
                                                                                 scalar=G8[po_:po_ + 64, 28 + h:29 + h],
                                                                                 in1=pcc[po_:po_ + 64, (h // 2) * 129:(h // 2 + 1) * 129], op0=ALU.mult, op1=ALU.add),
                                     reads=[rCFs, rG8, rpcc], writes=[rCFs])
                            HM2 = HM_[:]
                            dbg_out("hm", HM2, rHM, r0)
                            yield S.op("pool", lambda e: e.tensor_tensor(out=TM[:], in0=HM2, in1=HM2, op=ALU.mult), reads=[rHM], writes=[rTM])
                            yield S.op("dve", lambda e: e.tensor_reduce(out=G8[:, 40:44], in_=v3(TM[:], 4), axis=AX.X, op=ALU.add), reads=[rTM], writes=[rG8])
                            yield S.op("dve", lambda e: e.tensor_scalar(out=G8[:, 40:44], in0=G8[:, 40:44], scalar1=1.0 / 128, scalar2=1e-6, op0=ALU.mult, op1=ALU.add),
                                 reads=[rG8], writes=[rG8])
                            yield S.op("act", lambda e: e.activation(out=G8[:, 40:44], in_=G8[:, 40:44], func=AF.Sqrt), reads=[rG8], writes=[rG8])
                            yield S.op("dve", lambda e: e.reciprocal(out=G8[:, 44:48], in_=G8[:, 40:44]), reads=[rG8], writes=[rG8])
                            yield S.op("dve", lambda e: e.tensor_tensor(out=HM, in0=HM, in1=bc3(G8[:, 44:48], 4, 128), op=ALU.mult), reads=[rHM, rG8], writes=[rHM])
                            if dbg:
                                yield S.op("dve", lambda e: e.tensor_tensor(out=DBG[:], in0=HM2, in1=SO[:], op=ALU.mult), reads=[rHM, rSO], writes=[rDBG])
                                dbg_out("yml", DBG[:], rDBG, r0)
                            yield S.op("pool", lambda e: e.tensor_tensor(out=MIX[:, 512:1024], in0=HM2, in1=SO[:], op=ALU.mult), reads=[rHM, rSO], writes=[rMIX])


                        def speed(gen, n):
                            while True:
                                for _k in range(n):
                                    try:
                                        next(gen)
                                    except StopIteration:
                                        return
                                yield
                        NP_, NR_, NM_ = 1, 4, 3
                        nxt = [speed(early(ti + 1), NP_)] if ti + 1 < len(tiles) else []
                        for _ in interleave(speed(chain_R(), NR_), speed(chain_M(), NM_), *nxt):
                            pass
                        chk(7)
                        def w_out_chain():
                            pt_, rpt = PF[5]; pt = pt_[:].bitcast(BF16)
                            yield S.op("pe", lambda e: [e.transpose(out=pt[:, k * 128:(k + 1) * 128], in_=MIX[:, k * 128:(k + 1) * 128], identity=IDB) for k in range(8)][-1],
                                 reads=[rMIX, rCSB], writes=[rpt])
                            yield S.op("act", lambda e: e.activation(out=MIXT[:], in_=v3(pt[:], 8), func=AF.Copy), reads=[rpt], writes=[rMIXT])
                            for g in range(2):
                                p, rp = PF[3 + g]
                                yield S.op("pe", lambda e, g=g, p=p: [e.matmul(p[:], lhsT=MIXT[:, k, :], rhs=WOUT[:, k, g * 512:(g + 1) * 512], start=(k == 0), stop=(k == 7))
                                                              for k in range(8)][-1], reads=[rMIXT, rWOUT], writes=[rp])
                                yield S.op("dve", lambda e, g=g, p=p: e.tensor_tensor(out=Xc[:, g * 512:(g + 1) * 512], in0=p[:], in1=Xc[:, g * 512:(g + 1) * 512], op=ALU.add),
                                     reads=[rp, rXc], writes=[rXc])
                            S.dma("sp", x1_d[r0:r0 + 128, :], Xc[:], reads=[rXc])

                            yield
                        for _ in interleave(speed(w_out_chain(), 2), *([speed(L_late(), 3)] if ti + 1 < len(tiles) else [])):
                            pass
                        chk(8)
                S.barrier()
            S.barrier()

        es2 = ExitStack()
        with es2:
            sb2, _ = mk(es2)
            TOKS = 512 if T % 512 == 0 else 256
            NSUB = TOKS // 128
            WUP, rWUP = sb2("WUP", [128, 8, 2 * DFF], BF16)
            WDN, rWDN = sb2("WDN", [128, NB, D], BF16)
            FCW, rFCW = sb2("FCW", [128, NB, 3])
            FCB, rFCB = sb2("FCB", [128, NB])
            GF, rGF = sb2("GF", [128, D])
            S.dma("sp", FCW[:].rearrange("p b j -> p (b j)"), ffn_conv_w, writes=[rFCW])
            S.dma("sp", FCB[:], ffn_conv_b, writes=[rFCB])
            S.dma("sp", GF[:], norm_f_g.partition_broadcast(128), writes=[rGF])
            esC = ExitStack()
            with esC:
                sbC, _ = mk(esC)
                G2, rG2 = sbC("G2", [128, 8])
                STG2 = [sbC(f"STH{i}", [128, DFF]) for i in range(2)]
                S.dma("sp", G2[:], norm2_g, writes=[rG2])
                i = 0
                for k in range(8):
                    for hlf in range(2):
                        st, rst = STG2[i % 2]; i += 1
                        S.dma("sp", st[:], w_ffn_up[k * 128:(k + 1) * 128, hlf * DFF:(hlf + 1) * DFF], writes=[rst])
                        eng = "act" if hlf == 0 else "dve"
                        if eng == "act":
                            S.op("act", lambda e: e.activation(out=WUP[:, k, hlf * DFF:(hlf + 1) * DFF], in_=st[:], func=AF.Copy, scale=G2[:, k:k + 1]),
                                 reads=[rst, rG2], writes=[rWUP])
                        else:
                            S.op("dve", lambda e: e.tensor_scalar(out=WUP[:, k, hlf * DFF:(hlf + 1) * DFF], in0=st[:], scalar1=G2[:, k:k + 1], scalar2=None, op0=ALU.mult),
                                 reads=[rst, rG2], writes=[rWUP])
                for b in range(0, NB, 2):
                    st, rst = STG2[i % 2]; i += 1
                    S.dma("sp", st[:, 0:2 * D].rearrange("p (a n) -> p a n", a=2), w_ffn_down[b * 128:(b + 2) * 128, :].rearrange("(a p) n -> p a n", p=128),
                          writes=[rst])
                    S.op("pool" if (b // 2) % 2 else "dve", lambda e: e.tensor_copy(out=WDN[:, b:b + 2, :], in_=st[:, 0:2 * D].rearrange("p (a n) -> p a n", a=2)),
                         reads=[rst], writes=[rWDN])
                S.barrier()
            chk(9)
            esD = ExitStack()
            with esD:
                sbD, _ = mk(esD)
                XS = [sbD(f"XS{i}", [128, D]) for i in range(NSUB)]
                HN2, rHN2 = sbD("HN2", [128, D], BF16)
                H2T, rH2T = sbD("H2T", [128, 8, TOKS], BF16)
                GT, rGT = sbD("GT", [128, NB, TOKS], BF16)
                AC = [sbD(f"AC{i}", [128, TOKS + 2]) for i in range(2)]
                AQ = [sbD(f"AQ{i}", [128, TOKS]) for i in range(2)]
                CAR, rCAR = sbD("CAR", [128, NB, 2])
                ST2, rST2 = sbD("ST2", [128, 8])
                it = 0
                for s in range(NSEQ):
                    S.op("pool", lambda e: e.memset(CAR[:], 0.0), writes=[rCAR])
                    for c in range(T // TOKS):
                        r0 = s * T + c * TOKS
                        for sub in range(NSUB):
                            Xs, rXs = XS[sub]
                            S.dma("sp", Xs[:], x1_d[r0 + sub * 128:r0 + (sub + 1) * 128, :], writes=[rXs])
                            rmsnorm_to_bf16(Xs[:], rXs, HN2, rHN2, ST2, rST2)
                            pt, rpt = ptb()
                            S.op("pe", lambda e: [e.transpose(out=pt[:, k * 128:(k + 1) * 128], in_=HN2[:, k * 128:(k + 1) * 128], identity=IDB) for k in range(8)][-1],
                                 reads=[rHN2, rCSB], writes=[rpt])
                            S.op("dve", lambda e, sub=sub: e.tensor_copy(out=H2T[:, :, sub * 128:(sub + 1) * 128], in_=v3(pt[:], 8)), reads=[rpt], writes=[rH2T])
                        for b in range(NB):
                            ACb, rACb = AC[b % 2]
                            AQb, rAQb = AQ[b % 2]
                            pa, rpa = pf()
                            pbk, rpbk = pf()
                            S.op("pe", lambda e, b=b, pa=pa: [e.matmul(pa[:, 0:TOKS], lhsT=WUP[:, k, b * 128:(b + 1) * 128], rhs=H2T[:, k, :], start=(k == 0), stop=(k == 7))
                                                             for k in range(8)][-1], reads=[rWUP, rH2T], writes=[rpa])
                            S.op("pe", lambda e, b=b, pbk=pbk: [e.matmul(pbk[:, 0:TOKS], lhsT=WUP[:, k, DFF + b * 128:DFF + (b + 1) * 128], rhs=H2T[:, k, :], start=(k == 0), stop=(k == 7))
                                                               for k in range(8)][-1], reads=[rWUP, rH2T], writes=[rpbk])
                            S.op("pool", lambda e, b=b: e.tensor_copy(out=ACb[:, 0:2], in_=CAR[:, b, :]), reads=[rCAR], writes=[rACb])
                            S.op("act", lambda e: e.activation(out=ACb[:, 2:TOKS + 2], in_=pa[:, 0:TOKS], func=AF.Copy), reads=[rpa], writes=[rACb])
                            S.op("pool", lambda e, b=b: e.tensor_copy(out=CAR[:, b, :], in_=ACb[:, TOKS:TOKS + 2]), reads=[rACb], writes=[rCAR])
                            S.op("dve", lambda e, b=b: e.tensor_scalar(out=AQb[:], in0=ACb[:, 0:TOKS], scalar1=FCW[:, b, 0:1], scalar2=FCB[:, b:b + 1], op0=ALU.mult, op1=ALU.add),
                                 reads=[rACb, rFCW, rFCB], writes=[rAQb])
                            S.op("dve", lambda e, b=b: e.scalar_tensor_tensor(out=AQb[:], in0=ACb[:, 1:TOKS + 1], scalar=FCW[:, b, 1:2], in1=AQb[:], op0=ALU.mult, op1=ALU.add),
                                 reads=[rACb, rFCW, rAQb], writes=[rAQb])
                            S.op("dve", lambda e, b=b: e.scalar_tensor_tensor(out=AQb[:], in0=ACb[:, 2:TOKS + 2], scalar=FCW[:, b, 2:3], in1=AQb[:], op0=ALU.mult, op1=ALU.add),
                                 reads=[rACb, rFCW, rAQb], writes=[rAQb])
                            S.op("act", lambda e: e.activation(out=AQb[:], in_=AQb[:], func=AF.Silu), reads=[rAQb], writes=[rAQb])
                            S.op("dve", lambda e, b=b: e.tensor_tensor(out=GT[:, b, :], in0=AQb[:], in1=pbk[:, 0:TOKS], op=ALU.mult), reads=[rAQb, rpbk], writes=[rGT])
                        for sub in range(NSUB):
                            Xs, rXs = XS[sub]
                            X2c, rX2c = Xs, rXs
                            for g in range(2):
                                p, rp = pf()
                                S.op("pe", lambda e, g=g, p=p, sub=sub: [e.matmul(p[:], lhsT=GT[:, b, sub * 128:(sub + 1) * 128], rhs=WDN[:, b, g * 512:(g + 1) * 512],
                                                                                 start=(b == 0), stop=(b == NB - 1)) for b in range(NB)][-1], reads=[rGT, rWDN], writes=[rp])
                                S.op("dve", lambda e, g=g, p=p: e.tensor_tensor(out=X2c[:, g * 512:(g + 1) * 512], in0=p[:], in1=Xs[:, g * 512:(g + 1) * 512], op=ALU.add),
                                     reads=[rp, rXs], writes=[rX2c])
                            S.op("pool", lambda e: e.memset(ST2[:, 4:5], 0.0), writes=[rST2])
                            S.op("act", lambda e: e.activation(out=HN2[:], in_=X2c[:], func=AF.Square, accum_out=ST2[:, 4:5]), reads=[rX2c, rST2], writes=[rHN2, rST2])
                            S.op("dve", lambda e: e.tensor_scalar(out=ST2[:, 5:6], in0=ST2[:, 4:5], scalar1=1.0 / D, scalar2=1e-6, op0=ALU.mult, op1=ALU.add),
                                 reads=[rST2], writes=[rST2])
                            S.op("act", lambda e: e.activation(out=ST2[:, 6:7], in_=ST2[:, 5:6], func=AF.Sqrt), reads=[rST2], writes=[rST2])
                            S.op("dve", lambda e: e.reciprocal(out=ST2[:, 7:8], in_=ST2[:, 6:7]), reads=[rST2], writes=[rST2])
                            S.op("act", lambda e: e.activation(out=X2c[:], in_=X2c[:], func=AF.Copy, scale=ST2[:, 7:8]), reads=[rX2c, rST2], writes=[rX2c])
                            S.op("pool", lambda e: e.tensor_tensor(out=X2c[:], in0=X2c[:], in1=GF[:], op=ALU.mult), reads=[rX2c, rGF], writes=[rX2c])
                            S.dma("sp", out_d[r0 + sub * 128:r0 + (sub + 1) * 128, :], X2c[:], reads=[rX2c])
                S.barrier()
            S.barrier()
    return nc


def make_consts():
    c = np.zeros((128, 640), np.float32)
    i = np.arange(128)
    c[:, 0:128] = np.eye(128)
    c[:, 128:256] = (i[:, None] < i[None, :])
    c[:, 256:384] = (i[:, None] <= i[None, :])
    c[:, 384:512] = (i[:, None] > i[None, :])
    c[:, 512:640] = 1.0
    return c


def make_in_maps(inputs, n_cores, nseq):
    f = lambda a: np.ascontiguousarray(np.asarray(a, np.float32))
    x = f(inputs["x"])
    T = x.shape[1]
    shared = {}
    for k, v in inputs.items():
        if k == "x":
            continue
        a = f(v)
        if k != "norm_f_g":
            a = a[0]
        if k == "r_k":
            a = a.reshape(512)
        elif k in ("norm1_g", "norm2_g", "qk_conv_b", "ffn_conv_b", "mh_norm_g"):
            a = a.reshape(-1, 128).T
        elif k in ("qk_conv_w", "ffn_conv_w"):
            j = a.shape[0]
            a = a.reshape(j, -1, 128).transpose(2, 1, 0).reshape(128, -1)
        shared[k] = np.ascontiguousarray(a)
    shared["consts"] = make_consts()
    maps = []
    for c in range(n_cores):
        m = dict(shared)
        m["x"] = np.ascontiguousarray(x[c * nseq:(c + 1) * nseq].reshape(nseq * T, D))
        maps.append(m)
    return maps


def kernel(**inputs):
    x = np.asarray(inputs["x"])
    B, T, _ = x.shape
    nseq = B // N_CORES
    nc = build(nseq, T)
    maps = make_in_maps(inputs, N_CORES, nseq)
    res = run_bass_kernel_spmd(nc, maps, core_ids=list(range(N_CORES)))
    out = np.concatenate([r["out"].reshape(nseq, T, D) for r in res.results], axis=0)
    return out.astype(np.float32)
```

```python
import numpy as np
from contextlib import ExitStack
import concourse.bass as bass
import concourse.mybir as mybir
from concourse.bass_utils import run_bass_kernel_spmd

F32 = mybir.dt.float32
BF16 = mybir.dt.bfloat16
ALU = mybir.AluOpType
AF = mybir.ActivationFunctionType
AX = mybir.AxisListType

N_CORES = 8
D = 1024
N_IN = 3336
RWC = 1792
MLO = 2 * RWC
WIN_COLS = 2 * RWC + 1544
DFF = 2816
NB = DFF // 128
CDEC = float(np.exp(-0.5))


class Res:
    __slots__ = ("name", "lw", "rd")

    def __init__(self, name):
        self.name = name
        self.lw = None
        self.rd = []


class Sched:
    EPOCH = 30000
    NDMA = 24

    def __init__(self, nc, es):
        self.nc = nc
        self.es = es
        self.engs = {"pe": nc.tensor, "act": nc.scalar, "dve": nc.vector,
                     "pool": nc.gpsimd, "sp": nc.sync}
        self.sems = {e: [] for e in self.engs}
        self.cnt = {e: 0 for e in self.engs}
        self.waited = {e: {} for e in self.engs}
        self.dma_sems = [es.enter_context(nc.semaphore(f"dq{i}")) for i in range(self.NDMA)]
        self.dma_cnt = [0] * self.NDMA
        self.dma_i = 0
        for e in self.engs:
            self._new_epoch(e)

    def _new_epoch(self, e):
        s = self.es.enter_context(self.nc.semaphore(f"s_{e}_{len(self.sems[e])}"))
        self.sems[e].append(s)
        self.cnt[e] = 0

    def _wait(self, e, dep):
        if dep[0] == "dma":
            _, idx, val = dep
            key = ("dma", idx)
            sem = self.dma_sems[idx]
        else:
            de, ep, val = dep
            key = (de, ep)
            sem = self.sems[de][ep]
        if self.waited[e].get(key, 0) >= val:
            return
        self.engs[e].wait_ge(sem, val)
        self.waited[e][key] = val

    def _deps(self, e, reads, writes):
        deps = []
        for r in reads:
            if r.lw is not None and not (r.lw[0] == e and e == "pe"):
                deps.append(r.lw)
        for w in writes:
            if w.lw is not None and w.lw[0] != e:
                deps.append(w.lw)
            for d in w.rd:
                if d[0] != e:
                    deps.append(d)
        return deps

    def _mark(self, tag, reads, writes):
        for r in reads:
            r.rd.append(tag)
            if len(r.rd) > 64:
                r.rd = r.rd[-48:]
        for w in writes:
            w.lw = tag
            w.rd = []

    def op(self, e, fn, reads=(), writes=()):
        for d in self._deps(e, reads, writes):
            self._wait(e, d)
        if self.cnt[e] >= self.EPOCH:
            self._new_epoch(e)
        ins = fn(self.engs[e])
        ep = len(self.sems[e]) - 1
        ins.then_inc(self.sems[e][ep], 1)
        self.cnt[e] += 1
        tag = (e, ep, self.cnt[e])
        self._mark(tag, reads, writes)
        return tag

    def dma(self, q, out, in_, reads=(), writes=(), slow=False):
        for d in self._deps(q, reads, writes):
            self._wait(q, d)
        idx = self.dma_i
        self.dma_i = (self.dma_i + 1) % self.NDMA
        kw = {"allow_slow_non_contiguous": True} if slow else {}
        self.engs[q].dma_start(out=out, in_=in_, **kw).then_inc(self.dma_sems[idx], 16)
        self.dma_cnt[idx] += 16
        tag = ("dma", idx, self.dma_cnt[idx])
        self._mark(tag, reads, writes)
        return tag

    def barrier(self):
        for e in self.engs:
            for d in self.engs:
                if d != e:
                    ep = len(self.sems[d]) - 1
                    if self.cnt[d] > 0:
                        self._wait(e, (d, ep, self.cnt[d]))
                    elif ep > 0:
                        self._wait(e, (d, ep - 1, self.EPOCH))
            for i in range(self.NDMA):
                if self.dma_cnt[i]:
                    self._wait(e, ("dma", i, self.dma_cnt[i]))


VAR = 0


class _Stop(Exception):
    pass


def build(NSEQ, T, dbg=False, stop=0):
    nc = bass.Bass("TRN2", target_bir_lowering=False)
    try:
        _build(nc, NSEQ, T, dbg, stop)
    except _Stop:
        pass
    return nc


def _build(nc, NSEQ, T, dbg, stop):
    NT = T // 128
    NTOK = NSEQ * T
    di = lambda n, s: nc.dram_tensor(n, s, F32, kind="ExternalInput").ap()
    x_d = di("x", [NTOK, D])
    norm1_g = di("norm1_g", [128, 8]); w_in = di("w_in", [D, N_IN]); rw_mu = di("rw_mu", [RWC])
    w0 = di("w0", [512]); w_up_decay = di("w_up_decay", [64, 512]); a0 = di("a0", [512])
    w_up_a = di("w_up_a", [64, 512]); w_up_g = di("w_up_g", [128, 512]); k_k = di("k_k", [512])
    k_a = di("k_a", [512]); r_k = di("r_k", [512]); lnx_w = di("lnx_w", [512]); lnx_b = di("lnx_b", [512])
    qk_conv_w = di("qk_conv_w", [128, 16]); qk_conv_b = di("qk_conv_b", [128, 4])
    i_bias = di("i_bias", [4]); f_bias = di("f_bias", [4]); mh_norm_g = di("mh_norm_g", [128, 4])
    w_out = di("w_out", [D, D]); norm2_g = di("norm2_g", [128, 8]); w_ffn_up = di("w_ffn_up", [D, 2 * DFF])
    ffn_conv_w = di("ffn_conv_w", [128, NB * 3]); ffn_conv_b = di("ffn_conv_b", [128, NB])
    w_ffn_down = di("w_ffn_down", [DFF, D]); norm_f_g = di("norm_f_g", [D])
    consts = di("consts", [128, 640])
    out_d = nc.dram_tensor("out", [NTOK, D], F32, kind="ExternalOutput").ap()
    x1_d = nc.dram_tensor("x1s", [NTOK, D], F32, kind="Internal").ap()
    dbg_d = {}
    if dbg:
        for nm in ("yrw", "yml", "y", "hm"):
            dbg_d[nm] = nc.dram_tensor("d_" + nm, [NTOK, 512], F32, kind="ExternalOutput").ap()

    es0 = ExitStack()
    with es0:
        S = Sched(nc, es0)

        def chk(n):
            if stop == n:
                S.barrier()
                raise _Stop()

        def mk(es):
            def sb(n, s, d=F32):
                return es.enter_context(nc.sbuf_tensor(n, s, d)), Res(n)

            def ps(n, s, d=F32):
                return es.enter_context(nc.psum_tensor(n, s, d)), Res(n)
            return sb, ps

        sb0, ps0 = mk(es0)
        CST, rCST = sb0("CST", [128, 640])
        S.dma("sp", CST[:], consts, writes=[rCST])
        IDF = CST[:, 0:128]; MU = CST[:, 128:256]; MUI = CST[:, 256:384]; ML = CST[:, 384:512]; ONES = CST[:, 512:640]
        CSB, rCSB = sb0("CSB", [128, 128], BF16)
        S.op("dve", lambda e: e.tensor_copy(out=CSB[:], in_=CST[:, 0:128]), reads=[rCST], writes=[rCSB])
        IDB = CSB[:, 0:128]
        MUI8, rMUI8 = sb0("MUI8", [128, 128])
        S.op("dve", lambda e: e.tensor_scalar(out=MUI8[:], in0=MUI, scalar1=0.125, scalar2=None, op0=ALU.mult),
             reads=[rCST], writes=[rMUI8])
        NPF = 6
        PF = [ps0(f"PF{i}", [128, 512]) for i in range(NPF)]
        PT = [ps0(f"PT{i}", [128, 1024], BF16) for i in range(2)]
        pf_i = [0]
        pt_i = [0]

        POOLS = {"all": [0, 1, 2, 3, 4, 5], "g0": [0, 1], "g1": [2, 3], "m": [4, 5], "r": [0, 1, 2, 3]}
        pool_i = {k: 0 for k in POOLS}

        def pf(pool="all"):
            lst = POOLS[pool]
            p = PF[lst[pool_i[pool] % len(lst)]]
            pool_i[pool] += 1
            return p

        def interleave(*gens):
            gens = list(gens)
            while gens:
                for gg in list(gens):
                    try:
                        next(gg)
                        yield
                    except StopIteration:
                        gens.remove(gg)

        def ptb():
            return PT[0]

        def bc3(ap2, a, b):
            return ap2.rearrange("p (a o) -> p a o", o=1).to_broadcast([ap2.shape[0], a, b])

        def bcm(ap2, a):
            return ap2.rearrange("p (o n) -> p o n", o=1).to_broadcast([ap2.shape[0], a, ap2.shape[1]])

        def v3(ap, a):
            return ap.rearrange("p (a b) -> p a b", a=a)

        def rmsnorm_to_bf16(X, rX, HN, rHN, ST, rST):
            S.op("pool", lambda e: e.memset(ST[:, 0:1], 0.0), writes=[rST])
            S.op("act", lambda e: e.activation(out=HN[:], in_=X, func=AF.Square, accum_out=ST[:, 0:1]),
                 reads=[rX, rST], writes=[rHN, rST])
            S.op("dve", lambda e: e.tensor_scalar(out=ST[:, 1:2], in0=ST[:, 0:1], scalar1=1.0 / D, scalar2=1e-6,
                                                  op0=ALU.mult, op1=ALU.add), reads=[rST], writes=[rST])
            S.op("act", lambda e: e.activation(out=ST[:, 2:3], in_=ST[:, 1:2], func=AF.Sqrt), reads=[rST], writes=[rST])
            S.op("dve", lambda e: e.reciprocal(out=ST[:, 3:4], in_=ST[:, 2:3]), reads=[rST], writes=[rST])
            S.op("act", lambda e: e.activation(out=HN[:], in_=X, func=AF.Copy, scale=ST[:, 3:4]),
                 reads=[rX, rST], writes=[rHN])

        es1 = ExitStack()
        with es1:
            sb1, _ = mk(es1)
            WIN, rWIN = sb1("WIN", [128, 8, WIN_COLS], BF16)
            WOUT, rWOUT = sb1("WOUT", [128, 8, D], BF16)
            WLOR, rWLOR = sb1("WLOR", [128, 512], BF16)
            WG, rWG = sb1("WG", [128, 512], BF16)
            VEC, rVEC = sb1("VEC", [128, 7, 512])
            MHG4, rMHG4 = sb1("MHG4", [128, 4])
            CW, rCW = sb1("CW", [128, 4, 4])
            CB, rCB = sb1("CB", [128, 4])
            GB, rGB = sb1("GB", [128, 8])
            for i, v in enumerate((w0, a0, k_k, k_a, r_k, lnx_w, lnx_b)):
                S.dma("sp", VEC[:, i, :], v.partition_broadcast(128), writes=[rVEC])
            S.dma("sp", CW[:].rearrange("p b j -> p (b j)"), qk_conv_w, writes=[rCW])
            S.dma("sp", CB[:], qk_conv_b, writes=[rCB])
            S.dma("sp", GB[:, 0:4], i_bias.partition_broadcast(128), writes=[rGB])
            S.dma("sp", GB[:, 4:8], f_bias.partition_broadcast(128), writes=[rGB])
            W0V = VEC[:, 0, :]; A0V = VEC[:, 1, :]; KKV = VEC[:, 2, :]; KAV = VEC[:, 3, :]
            RKV = VEC[:, 4, :]; LWV = VEC[:, 5, :]; LBV = VEC[:, 6, :]
            S.dma("sp", MHG4[:], mh_norm_g, writes=[rMHG4])
            esA = ExitStack()
            with esA:
                sbA, _ = mk(esA)
                MUT, rMUT = sbA("MUT", [128, RWC])
                OMM, rOMM = sbA("OMM", [128, RWC])
                G1, rG1 = sbA("G1", [128, 8])
                STG = [sbA(f"STG{i}", [128, N_IN]) for i in range(2)]
                S.dma("sp", MUT[:], rw_mu.partition_broadcast(128), writes=[rMUT])
                S.dma("sp", G1[:], norm1_g, writes=[rG1])
                S.op("dve", lambda e: e.tensor_scalar(out=OMM[:], in0=MUT[:], scalar1=-1.0, scalar2=1.0,
                                                      op0=ALU.mult, op1=ALU.add), reads=[rMUT], writes=[rOMM])
                for k in range(8):
                    st, rst = STG[k % 2]
                    S.dma("sp", st[:], w_in[k * 128:(k + 1) * 128, :], writes=[rst])
                    S.op("act", lambda e: e.activation(out=st[:], in_=st[:], func=AF.Copy, scale=G1[:, k:k + 1]),
                         reads=[rst, rG1], writes=[rst])
                    S.op("dve", lambda e: e.tensor_tensor(out=WIN[:, k, 0:RWC], in0=st[:, 0:RWC], in1=OMM[:], op=ALU.mult),
                         reads=[rst, rOMM], writes=[rWIN])
                    S.op("pool", lambda e: e.tensor_tensor(out=WIN[:, k, RWC:2 * RWC], in0=st[:, 0:RWC], in1=MUT[:], op=ALU.mult),
                         reads=[rst, rMUT], writes=[rWIN])
                    S.op("act", lambda e: e.activation(out=WIN[:, k, MLO:WIN_COLS], in_=st[:, RWC:N_IN], func=AF.Copy),
                         reads=[rst], writes=[rWIN])
                for k in range(8):
                    st, rst = STG[k % 2]
                    S.dma("sp", st[:, 0:D], w_out[k * 128:(k + 1) * 128, :], writes=[rst])
                    if k < 4:
                        S.op("dve", lambda e: e.tensor_copy(out=WOUT[:, k, :], in_=st[:, 0:D]), reads=[rst], writes=[rWOUT])
                    else:
                        S.op("dve", lambda e: e.tensor_scalar(out=WOUT[:, k, :], in0=st[:, 0:D], scalar1=MHG4[:, k - 4:k - 3], scalar2=None, op0=ALU.mult),
                             reads=[rst, rMHG4], writes=[rWOUT])
                st, rst = STG[0]
                S.dma("sp", st[0:64, 0:512], w_up_decay, writes=[rst])
                S.dma("sp", st[64:128, 0:512], w_up_a, writes=[rst])
                S.dma("sp", st[:, 512:1024], w_up_g, writes=[rst])
                S.op("dve", lambda e: e.tensor_copy(out=WLOR[:], in_=st[:, 0:512]), reads=[rst], writes=[rWLOR])
                S.op("dve", lambda e: e.tensor_copy(out=WG[:], in_=st[:, 512:1024]), reads=[rst], writes=[rWG])
                S.barrier()
            chk(1)
            esB = ExitStack()
            with esB:
                sbB, _ = mk(esB)
                X = [sbB(f"X{i}", [128, D]) for i in range(2)]
                HN, rHN = sbB("HN", [128, D], BF16)
                HT = [sbB(f"HT{i}", [128, 8, 129], BF16) for i in range(2)]
                ST, rST = sbB("ST", [128, 8])
                R, rR = sbB("R", [128, 512]); K, rK = sbB("K", [128, 512]); V, rV = sbB("V", [128, 512])
                SG, rSG = sbB("SG", [128, 512]); AA, rAA = sbB("AA", [128, 512])
                ECL, rECL = sbB("ECL", [128, 512]); ENCL, rENCL = sbB("ENCL", [128, 512])
                KKN, rKKN = sbB("KKN", [128, 512]); KM, rKM = sbB("KM", [128, 512])
                TA, rTA = sbB("TA", [128, 512]); TB, rTB = sbB("TB", [128, 512])
                ECLM, rECLM = TA, rTA
                BON, rBON = sbB("BON", [128, 512])
                S8, rS8 = sbB("S8", [128, 64])
                WL2 = [sbB(f"WL{i}", [128, 4]) for i in range(2)]
                S8E, rS8E = sbB("S8E", [128, 24])
                RBAR, rRBAR = sbB("RBAR", [128, 512], BF16); ABAR, rABAR = sbB("ABAR", [128, 512], BF16)
                BTIL, rBTIL = sbB("BTIL", [128, 512], BF16); KTIL, rKTIL = sbB("KTIL", [128, 512], BF16)
                VB, rVB = sbB("VB", [128, 512], BF16)
                RBT, rRBT = sbB("RBT", [128, 4, 128], BF16); ABT, rABT = sbB("ABT", [128, 4, 128], BF16)
                BTT, rBTT = sbB("BTT", [128, 4, 128], BF16); KTT, rKTT = sbB("KTT", [128, 4, 128], BF16)
                AAK, rAAK = sbB("AAK", [128, 8, 128], BF16); ARKT, rARKT = sbB("ARKT", [128, 8, 128], BF16)
                ARBT, rARBT = sbB("ARBT", [128, 8, 128], BF16)
                PQ = [[sbB(f"PQ{g}{i}", [128, 4, 128], BF16) for i in range(4)] for g in range(2)]
                TT = [[sbB(f"TT{g}{i}", [128, 4, 128], BF16) for i in range(2)] for g in range(2)]
                ABPT, rABPT = sbB("ABPT", [128, 4, 128], BF16); AAKPT, rAAKPT = sbB("AAKPT", [128, 8, 128], BF16)
                UB, rUB = sbB("UB", [128, 512], BF16)
                YF, rYF = sbB("YF", [128, 512]); TB2, rTB2 = sbB("TB2", [128, 512])
                HF = [sbB("HF", [128, 4, 64])] * NSEQ
                HB, rHB = sbB("HB", [128, 4, 64], BF16)
                QKC, rQKC = sbB("QKC", [128, 4, 131])
                ACC_, rACC = sbB("ACC", [128, 512]); ACC = v3(ACC_[:], 4)
                QKT, rQKT = sbB("QKT", [128, 4, 128], BF16)
                KP, rKP = sbB("KP", [128, 4, 64], BF16)
                VE, rVE = sbB("VE", [128, 4, 129], BF16)
                G8, rG8 = sbB("G8", [128, 48])
                TM, rTM = ACC_, rACC; DG, rDG = v3(TM[:], 4), rTM
                MST = [sbB("MST", [128, 4])] * NSEQ
                CF = [sbB("CF", [128, 2, 129])] * NSEQ
                CBF, rCBF = sbB("CBF", [128, 2, 129], BF16)
                PTB, rPTB = sbB("PTB", [128, 4, 128], BF16)
                HM_, rHM = sbB("HM", [128, 512]); HM = v3(HM_[:], 4)
                SO, rSO = sbB("SO", [128, 512])
                MIX, rMIX = sbB("MIX", [128, D], BF16)
                MIXT, rMIXT = AAKPT, rAAKPT
                DBG, rDBG = sbB("DBG", [128, 512]) if dbg else (None, None)

                S.op("pool", lambda e: e.memset(VE[:], 1.0), writes=[rVE])

                def dbg_out(nm, ap, rr, r0):
                    if dbg:
                        S.dma("sp", dbg_d[nm][r0:r0 + 128, :], ap, reads=[rr])

                LT2 = [sbB(f"LTb{i}", [128, 256], BF16) for i in range(2)]
                tiles = [(s_, c_) for s_ in range(NSEQ) for c_ in range(NT)]
                EP, rEP = PT[1]
                EPF = EP[:].bitcast(F32)

                def mk_proj(HTx):
                    cur = lambda k: HTx[:, k, 1:129]
                    prv = lambda k: HTx[:, k, 0:128]

                    def proj_tok(p, col, n, shifted):
                        def f(e):
                            last = None
                            nm = 16 if shifted else 8
                            i = 0
                            for k in range(8):
                                last = e.matmul(p, lhsT=cur(k), rhs=WIN[:, k, col:col + n], start=(i == 0), stop=(i == nm - 1)); i += 1
                                if shifted:
                                    last = e.matmul(p, lhsT=prv(k), rhs=WIN[:, k, RWC + col:RWC + col + n], start=False, stop=(i == nm - 1)); i += 1
                            return last
                        return f

                    def proj_feat(p, col, shifted):
                        def f(e):
                            last = None
                            nm = 16 if shifted else 8
                            i = 0
                            for k in range(8):
                                last = e.matmul(p, lhsT=WIN[:, k, col:col + 128], rhs=cur(k), start=(i == 0), stop=(i == nm - 1)); i += 1
                                if shifted:
                                    last = e.matmul(p, lhsT=WIN[:, k, RWC + col:RWC + col + 128], rhs=prv(k), start=False, stop=(i == nm - 1)); i += 1
                            return last
                        return f
                    return cur, prv, proj_tok, proj_feat

                def early(ti):
                    s_, c_ = tiles[ti]
                    r0_ = s_ * T + c_ * 128
                    Xn, rXn = X[ti % 2]
                    HTn, rHTn = HT[ti % 2]
                    HTq, rHTq = HT[(ti + 1) % 2]
                    LTn, rLTn = LT2[ti % 2]
                    _, _, ptok, pfeat = mk_proj(HTn)
                    S.dma("sp", Xn[:], x_d[r0_:r0_ + 128, :], writes=[rXn])
                    yield
                    rmsnorm_to_bf16(Xn[:], rXn, HN, rHN, ST, rST)
                    yield
                    yield S.op("pe", lambda e: [e.transpose(out=EP[:, k * 128:(k + 1) * 128], in_=HN[:, k * 128:(k + 1) * 128],
                                                            identity=IDB) for k in range(8)][-1], reads=[rHN, rCSB], writes=[rEP])
                    yield S.op("dve", lambda e: e.tensor_copy(out=HTn[:, :, 1:129], in_=v3(EP[:], 8)), reads=[rEP], writes=[rHTn])
                    if c_ == 0:
                        yield S.op("pool", lambda e: e.memset(HTn[:, :, 0:1], 0.0), writes=[rHTn])
                    else:
                        yield S.op("pool", lambda e: e.tensor_copy(out=HTn[:, :, 0:1], in_=HTq[:, :, 128:129]), reads=[rHTq], writes=[rHTn])
                    yield S.op("pe", pfeat(EPF[:, 0:128], 1536, True), reads=[rHTn, rWIN], writes=[rEP])
                    yield S.op("pe", pfeat(EPF[:, 128:256], 1664, True), reads=[rHTn, rWIN], writes=[rEP])
                    yield S.op("act", lambda e: e.activation(out=LTn[0:64, 0:128], in_=EPF[0:64, 0:128], func=AF.Tanh), reads=[rEP], writes=[rLTn])
                    yield S.op("act", lambda e: e.activation(out=LTn[:, 128:256], in_=EPF[:, 128:256], func=AF.Sigmoid), reads=[rEP], writes=[rLTn])
                    yield S.op("act", lambda e: e.activation(out=LTn[64:128, 0:128], in_=EPF[64:128, 0:128], func=AF.Copy), reads=[rEP], writes=[rLTn])
                    for g_, (dst, rdst) in ((1, (K, rK)), (0, (R, rR)), (2, (V, rV))):
                        yield S.op("pe", ptok(EPF, g_ * 512, 512, True), reads=[rHTn, rWIN], writes=[rEP])
                        yield S.op("act", lambda e: e.activation(out=dst[:], in_=EPF, func=AF.Copy), reads=[rEP], writes=[rdst])
                    WLn, rWLn = WL2[ti % 2]
                    pw, rpw = EPF, rEP
                    yield S.op("pe", lambda e: e.matmul(pw[:], lhsT=LTn[0:64, 0:128], rhs=WLOR[0:64, :], start=True, stop=True),
                         reads=[rLTn, rWLOR], writes=[rpw])
                    yield S.op("dve", lambda e: e.tensor_tensor(out=SG[:], in0=pw[:], in1=W0V, op=ALU.add), reads=[rpw, rVEC], writes=[rSG])
                    yield S.op("act", lambda e: e.activation(out=SG[:], in_=SG[:], func=AF.Sigmoid), reads=[rSG], writes=[rSG])
                    pa, rpa = EPF, rEP
                    yield S.op("pe", lambda e: e.matmul(pa[:], lhsT=LTn[64:128, 0:128], rhs=WLOR[64:128, :], start=True, stop=True),
                         reads=[rLTn, rWLOR], writes=[rpa])
                    yield S.op("dve", lambda e: e.tensor_tensor(out=AA[:], in0=pa[:], in1=A0V, op=ALU.add), reads=[rpa, rVEC], writes=[rAA])
                    yield S.op("act", lambda e: e.activation(out=AA[:], in_=AA[:], func=AF.Sigmoid), reads=[rAA], writes=[rAA])
                    pc, rpc = EPF, rEP
                    yield S.op("pe", lambda e: e.matmul(pc[:], lhsT=MUI, rhs=SG[:], start=True, stop=True), reads=[rCST, rSG], writes=[rpc])
                    yield S.op("act", lambda e: e.activation(out=ECL[:], in_=pc[:], func=AF.Exp, scale=-CDEC), reads=[rpc], writes=[rECL])
                    yield S.op("act", lambda e: e.activation(out=ENCL[:], in_=pc[:], func=AF.Exp, scale=CDEC), reads=[rpc], writes=[rENCL])
                    yield S.op("dve", lambda e: e.tensor_tensor(out=TA[:], in0=pc[:], in1=SG[:], op=ALU.subtract), reads=[rpc, rSG], writes=[rTA])
                    yield S.op("act", lambda e: e.activation(out=ECLM[:], in_=TA[:], func=AF.Exp, scale=-CDEC), reads=[rTA], writes=[rECLM])
                    pwl, rpwl = EPF, rEP
                    yield S.op("pe", lambda e: [e.matmul(pwl[(h % 2) * 64:(h % 2) * 64 + 64, h // 2:h // 2 + 1], lhsT=SG[:, h * 64:(h + 1) * 64], rhs=ONES[:, 0:1], start=True, stop=True)
                                          for h in range(8)][-1], reads=[rSG, rCST], writes=[rpwl])
                    yield S.op("act", lambda e: e.activation(out=WLn[:], in_=pwl[:, 0:4], func=AF.Exp, scale=-CDEC), reads=[rpwl], writes=[rWLn])
                    yield S.op("dve", lambda e: e.tensor_tensor(out=KKN[:], in0=K[:], in1=KKV, op=ALU.mult), reads=[rK, rVEC], writes=[rKKN])
                    yield S.op("pool", lambda e: e.tensor_tensor(out=TB[:], in0=KKN[:], in1=KKN[:], op=ALU.mult), reads=[rKKN], writes=[rTB])
                    yield S.op("dve", lambda e: e.tensor_reduce(out=S8E[:, 0:8], in_=v3(TB[:], 8), axis=AX.X, op=ALU.add), reads=[rTB], writes=[rS8E])
                    yield S.op("act", lambda e: e.activation(out=S8E[:, 8:16], in_=S8E[:, 0:8], func=AF.Sqrt), reads=[rS8E], writes=[rS8E])
                    yield S.op("dve", lambda e: e.tensor_scalar(out=S8E[:, 8:16], in0=S8E[:, 8:16], scalar1=1e-12, scalar2=None, op0=ALU.max),
                         reads=[rS8E], writes=[rS8E])
                    yield S.op("dve", lambda e: e.reciprocal(out=S8E[:, 16:24], in_=S8E[:, 8:16]), reads=[rS8E], writes=[rS8E])
                    yield S.op("dve", lambda e: e.tensor_tensor(out=v3(KKN[:], 8), in0=v3(KKN[:], 8), in1=bc3(S8E[:, 16:24], 8, 64), op=ALU.mult),
                         reads=[rKKN, rS8E], writes=[rKKN])
                    yield S.op("dve", lambda e: e.scalar_tensor_tensor(out=TB[:], in0=AA[:], scalar=-1.0, in1=KAV, op0=ALU.add, op1=ALU.mult),
                         reads=[rAA, rVEC], writes=[rTB])
                    yield S.op("dve", lambda e: e.scalar_tensor_tensor(out=KM[:], in0=TB[:], scalar=1.0, in1=K[:], op0=ALU.add, op1=ALU.mult),
                         reads=[rTB, rK], writes=[rKM])

                def L_late():
                    yield S.op("dve", lambda e: e.tensor_tensor(out=RBAR[:], in0=R[:], in1=ECL[:], op=ALU.mult), reads=[rR, rECL], writes=[rRBAR])
                    yield S.op("dve", lambda e: e.scalar_tensor_tensor(out=ABAR[:], in0=KKN[:], scalar=-1.0, in1=ECLM[:], op0=ALU.mult, op1=ALU.mult),
                         reads=[rKKN, rECLM], writes=[rABAR])
                    yield S.op("pool", lambda e: e.tensor_tensor(out=TA[:], in0=KKN[:], in1=AA[:], op=ALU.mult), reads=[rKKN, rAA], writes=[rTA])
                    yield S.op("pool", lambda e: e.tensor_tensor(out=BTIL[:], in0=TA[:], in1=ENCL[:], op=ALU.mult), reads=[rTA, rENCL], writes=[rBTIL])
                    yield S.op("dve", lambda e: e.tensor_tensor(out=KTIL[:], in0=KM[:], in1=ENCL[:], op=ALU.mult), reads=[rKM, rENCL], writes=[rKTIL])
                    yield S.op("act", lambda e: e.activation(out=VB[:], in_=V[:], func=AF.Copy), reads=[rV], writes=[rVB])
                    yield S.op("pool", lambda e: e.tensor_tensor(out=TB[:], in0=R[:], in1=KM[:], op=ALU.mult), reads=[rR, rKM], writes=[rTB])
                    yield S.op("pool", lambda e: e.tensor_tensor(out=TB[:], in0=TB[:], in1=RKV, op=ALU.mult), reads=[rTB, rVEC], writes=[rTB])
                    yield S.op("dve", lambda e: e.tensor_reduce(out=S8[:, 24:32], in_=v3(TB[:], 8), axis=AX.X, op=ALU.add), reads=[rTB], writes=[rS8])
                    yield S.op("dve", lambda e: e.tensor_tensor(out=v3(BON[:], 8), in0=v3(V[:], 8), in1=bc3(S8[:, 24:32], 8, 64), op=ALU.mult),
                         reads=[rV, rS8], writes=[rBON])
                    yield S.op("pool", lambda e: e.tensor_tensor(out=BON[:], in0=BON[:], in1=LBV, op=ALU.add), reads=[rBON, rVEC], writes=[rBON])
                    for src, rsrc, dst, rdst in ((RBAR, rRBAR, RBT, rRBT), (ABAR, rABAR, ABT, rABT),
                                                 (BTIL, rBTIL, BTT, rBTT), (KTIL, rKTIL, KTT, rKTT)):
                        pt, rpt = ptb()
                        yield S.op("pe", lambda e: [e.transpose(out=pt[:, b * 128:(b + 1) * 128], in_=src[:, b * 128:(b + 1) * 128],
                                                          identity=IDB) for b in range(4)][-1], reads=[rsrc, rCSB], writes=[rpt])
                        yield S.op("act", lambda e: e.activation(out=dst[:], in_=v3(pt[:, 0:512], 4), func=AF.Copy), reads=[rpt], writes=[rdst])


                for ti, (s, c) in enumerate(tiles):
                    if True:
                        HFs, rHFs = HF[s]
                        CFs, rCFs = CF[s]
                        Ms, rMs = MST[s]
                        if c == 0:
                            S.op("pool", lambda e: e.memset(HFs[:], 0.0), writes=[rHFs])
                            S.op("pool", lambda e: e.memset(CFs[:], 0.0), writes=[rCFs])
                            S.op("pool", lambda e: e.memset(Ms[:], 0.0), writes=[rMs])
                        r0 = s * T + c * 128
                        Xc, rXc = X[ti % 2]
                        HTc, rHTc = HT[ti % 2]
                        LT, rLT = LT2[ti % 2]
                        WL, rWL = WL2[ti % 2]
                        cur, prv, proj_tok, proj_feat = mk_proj(HTc)
                        if ti == 0:
                            for _ in early(0):
                                pass
                            for _ in L_late():
                                pass
                        chk(2)
                        chk(4)

                        def hop(Tt, h):
                            return Tt[(h % 2) * 64:(h % 2) * 64 + 64, h // 2, :]

                        def rw_group(g):
                            hs = [g, g + 2, g + 4, g + 6]
                            P0, rP0 = PQ[g][0]; Q0, rQ0 = PQ[g][1]

                            def mm4(p, A, Bm):
                                return lambda e: [e.matmul(p[:, j * 128:(j + 1) * 128], lhsT=hop(A, h), rhs=hop(Bm, h), start=True, stop=True)
                                                  for j, h in enumerate(hs) if (VAR != 3 or h % 2 == 0) and (VAR != 4 or h % 2 == 1)][-1]
                            for (A, rA, Bm, rB, dst, rdst, mask, eng) in (
                                    (ABT, rABT, BTT, rBTT, P0[:], rP0, ML, "dve"),
                                    (BTT, rBTT, ABT, rABT, Q0[:], rQ0, MU, "dve"),
                                    (ABT, rABT, KTT, rKTT, AAK[:, 4 * g:4 * g + 4, :], rAAK, ML, "dve"),
                                    (KTT, rKTT, RBT, rRBT, ARKT[:, 4 * g:4 * g + 4, :], rARKT, MUI, "dve"),
                                    (BTT, rBTT, RBT, rRBT, ARBT[:, 4 * g:4 * g + 4, :], rARBT, MUI, "dve")):
                                p, rp = pf("g%d" % g)
                                yield S.op("pe", mm4(p, A, Bm), reads=[rA, rB], writes=[rp])
                                if VAR in (1, 3, 4):
                                    pass
                                elif VAR == 2:
                                    for j4 in range(4):
                                        yield S.op(eng, lambda e: e.tensor_tensor(out=dst[:, j4, :], in0=p[:, j4 * 128:(j4 + 1) * 128], in1=mask, op=ALU.mult),
                                             reads=[rp, rCST], writes=[rdst])
                                else:
                                    yield S.op(eng, lambda e: e.tensor_tensor(out=dst, in0=v3(p[:], 4), in1=bcm(mask, 4), op=ALU.mult),
                                         reads=[rp, rCST], writes=[rdst])
                            Tc, rTc = TT[g][0]
                            yield S.op("pool", lambda e: e.tensor_tensor(out=Tc[:], in0=Q0[:], in1=bcm(IDB, 4), op=ALU.add),
                                 reads=[rQ0, rCSB], writes=[rTc])
                            Pp, rPp, Qp, rQp = P0, rP0, Q0, rQ0
                            ti = 0
                            for lev in range(1, 7):
                                Pn, rPn = PQ[g][2 * (lev % 2)]
                                Qn, rQn = PQ[g][2 * (lev % 2) + 1]
                                p, rp = pf("g%d" % g)
                                yield S.op("pe", lambda e: [e.matmul(p[:, j * 128:(j + 1) * 128], lhsT=Qp[:, j, :], rhs=Pp[:, j, :], start=True, stop=True)
                                                      for j in range(4)][-1], reads=[rPp, rQp], writes=[rp])
                                yield S.op("act", lambda e: e.activation(out=Pn[:], in_=v3(p[:], 4), func=AF.Copy), reads=[rp], writes=[rPn])
                                if lev < 6:
                                    p2, rp2 = pf("g%d" % g)
                                    yield S.op("pe", lambda e: [e.matmul(p2[:, j * 128:(j + 1) * 128], lhsT=Pp[:, j, :], rhs=Qp[:, j, :], start=True, stop=True)
                                                          for j in range(4)][-1], reads=[rPp, rQp], writes=[rp2])
                                    yield S.op("act", lambda e: e.activation(out=Qn[:], in_=v3(p2[:], 4), func=AF.Copy), reads=[rp2], writes=[rQn])
                                Tn, rTn = TT[g][(ti + 1) % 2]
                                p3, rp3 = pf("g%d" % g)
                                yield S.op("pe", lambda e: [e.matmul(p3[:, j * 128:(j + 1) * 128], lhsT=Pn[:, j, :], rhs=Tc[:, j, :], start=True, stop=True)
                                                      for j in range(4)][-1], reads=[rPn, rTc], writes=[rp3])
                                yield S.op("dve", lambda e: e.tensor_tensor(out=Tn[:], in0=v3(p3[:], 4), in1=Tc[:], op=ALU.add),
                                     reads=[rp3, rTc], writes=[rTn])
                                Tc, rTc = Tn, rTn
                                ti += 1
                                Pp, rPp, Qp, rQp = Pn, rPn, Qn, rQn
                            p, rp = pf("g%d" % g)
                            yield S.op("pe", lambda e: [e.matmul(p[g * 64:g * 64 + 64, j * 128:(j + 1) * 128], lhsT=ABAR[:, h * 64:(h + 1) * 64], rhs=Tc[:, j, :],
                                                           start=True, stop=True) for j, h in enumerate(hs)][-1],
                                 reads=[rABAR, rTc], writes=[rp])
                            yield S.op("act", lambda e: e.activation(out=ABPT[g * 64:g * 64 + 64, :, :], in_=v3(p[g * 64:g * 64 + 64, :], 4), func=AF.Copy),
                                 reads=[rp], writes=[rABPT])
                            p, rp = pf("g%d" % g)
                            yield S.op("pe", lambda e: [e.matmul(p[:, j * 128:(j + 1) * 128], lhsT=AAK[:, 4 * g + j, :], rhs=Tc[:, j, :],
                                                           start=True, stop=True) for j, h in enumerate(hs)][-1],
                                 reads=[rAAK, rTc], writes=[rp])
                            yield S.op("dve", lambda e: e.tensor_copy(out=AAKPT[:, 4 * g:4 * g + 4, :], in_=v3(p[:], 4)), reads=[rp], writes=[rAAKPT])


                        def chain_R():
                            yield from interleave(rw_group(0), rw_group(1))
                            yield S.op("act", lambda e: e.activation(out=HB[:], in_=HFs[:], func=AF.Copy), reads=[rHFs], writes=[rHB])
                            pu, rpu = pf("r")

                            def fu(e):
                                last = None
                                for h in range(8):
                                    e.matmul(pu[:, h * 64:(h + 1) * 64], lhsT=hop(ABPT, h), rhs=hop(HB, h), start=True, stop=False)
                                    last = e.matmul(pu[:, h * 64:(h + 1) * 64], lhsT=AAKPT[:, (h % 2) * 4 + h // 2, :], rhs=VB[:, h * 64:(h + 1) * 64], start=False, stop=True)
                                return last
                            yield S.op("pe", fu, reads=[rABPT, rHB, rAAKPT, rVB], writes=[rpu])
                            yield S.op("act", lambda e: e.activation(out=UB[:], in_=pu[:], func=AF.Copy), reads=[rpu], writes=[rUB])
                            py, rpy = pf("r")

                            def fy(e):
                                last = None
                                for h in range(8):
                                    o = py[:, h * 64:(h + 1) * 64]
                                    e.matmul(o, lhsT=hop(RBT, h), rhs=hop(HB, h), start=True, stop=False)
                                    e.matmul(o, lhsT=ARBT[:, (h % 2) * 4 + h // 2, :], rhs=UB[:, h * 64:(h + 1) * 64], start=False, stop=False)
                                    last = e.matmul(o, lhsT=ARKT[:, (h % 2) * 4 + h // 2, :], rhs=VB[:, h * 64:(h + 1) * 64], start=False, stop=True)
                                return last
                            yield S.op("pe", fy, reads=[rRBT, rHB, rARBT, rUB, rARKT, rVB], writes=[rpy])
                            ph, rph = pf("r")

                            def fh(e):
                                last = None
                                for h in range(8):
                                    o = ph[(h % 2) * 64:(h % 2) * 64 + 64, (h // 2) * 64:(h // 2 + 1) * 64]
                                    e.matmul(o, lhsT=BTIL[:, h * 64:(h + 1) * 64], rhs=UB[:, h * 64:(h + 1) * 64], start=True, stop=False)
                                    last = e.matmul(o, lhsT=KTIL[:, h * 64:(h + 1) * 64], rhs=VB[:, h * 64:(h + 1) * 64], start=False, stop=True)
                                return last
                            yield S.op("pe", fh, reads=[rBTIL, rKTIL, rUB, rVB], writes=[rph])
                            yield S.op("dve", lambda e: e.tensor_tensor(out=HFs[:], in0=v3(ph[:, 0:256], 4), in1=HFs[:], op=ALU.add),
                                 reads=[rph, rHFs], writes=[rHFs])
                            yield S.op("dve", lambda e: e.tensor_tensor(out=HFs[:], in0=HFs[:], in1=bc3(WL[:], 4, 64), op=ALU.mult),
                                 reads=[rHFs, rWL], writes=[rHFs])
                            yield S.op("act", lambda e: e.activation(out=YF[:], in_=py[:], func=AF.Copy), reads=[rpy], writes=[rYF])
                            dbg_out("y", YF[:], rYF, r0)
                            yield S.op("dve", lambda e: e.tensor_reduce(out=S8[:, 32:40], in_=v3(YF[:], 8), axis=AX.X, op=ALU.add), reads=[rYF], writes=[rS8])
                            yield S.op("dve", lambda e: e.tensor_scalar(out=S8[:, 32:40], in0=S8[:, 32:40], scalar1=1.0 / 64, scalar2=None, op0=ALU.mult),
                                 reads=[rS8], writes=[rS8])
                            yield S.op("dve", lambda e: e.tensor_tensor(out=v3(YF[:], 8), in0=v3(YF[:], 8), in1=bc3(S8[:, 32:40], 8, 64), op=ALU.subtract),
                                 reads=[rYF, rS8], writes=[rYF])
                            yield S.op("pool", lambda e: e.tensor_tensor(out=TB2[:], in0=YF[:], in1=YF[:], op=ALU.mult), reads=[rYF], writes=[rTB2])
                            yield S.op("dve", lambda e: e.tensor_reduce(out=S8[:, 40:48], in_=v3(TB2[:], 8), axis=AX.X, op=ALU.add), reads=[rTB2], writes=[rS8])
                            yield S.op("dve", lambda e: e.tensor_scalar(out=S8[:, 40:48], in0=S8[:, 40:48], scalar1=1.0 / 64, scalar2=64e-5,
                                                                  op0=ALU.mult, op1=ALU.add), reads=[rS8], writes=[rS8])
                            yield S.op("act", lambda e: e.activation(out=S8[:, 40:48], in_=S8[:, 40:48], func=AF.Sqrt), reads=[rS8], writes=[rS8])
                            yield S.op("dve", lambda e: e.reciprocal(out=S8[:, 48:56], in_=S8[:, 40:48]), reads=[rS8], writes=[rS8])
                            yield S.op("dve", lambda e: e.tensor_tensor(out=v3(YF[:], 8), in0=v3(YF[:], 8), in1=bc3(S8[:, 48:56], 8, 64), op=ALU.mult),
                                 reads=[rYF, rS8], writes=[rYF])
                            yield S.op("dve", lambda e: e.tensor_tensor(out=YF[:], in0=YF[:], in1=LWV, op=ALU.mult), reads=[rYF, rVEC], writes=[rYF])
                            yield S.op("dve", lambda e: e.tensor_tensor(out=YF[:], in0=YF[:], in1=BON[:], op=ALU.add), reads=[rYF, rBON], writes=[rYF])
                            pg, rpg = pf("r")
                            yield S.op("pe", lambda e: e.matmul(pg[:], lhsT=LT[:, 128:256], rhs=WG[:], start=True, stop=True), reads=[rLT, rWG], writes=[rpg])
                            if dbg:
                                yield S.op("dve", lambda e: e.tensor_tensor(out=DBG[:], in0=YF[:], in1=pg[:], op=ALU.mult), reads=[rYF, rpg], writes=[rDBG])
                                dbg_out("yrw", DBG[:], rDBG, r0)
                            yield S.op("dve", lambda e: e.tensor_tensor(out=MIX[:, 0:512], in0=YF[:], in1=pg[:], op=ALU.mult), reads=[rYF, rpg], writes=[rMIX])


                        def chain_M():
                            pqk, rpqk = pf("m")
                            for b in range(4):
                                yield S.op("pe", proj_feat(pqk[:, b * 128:(b + 1) * 128], MLO - 0 + b * 128 if False else 0, False) if False else
                                     (lambda e, b=b: [e.matmul(pqk[:, b * 128:(b + 1) * 128], lhsT=WIN[:, k, MLO + b * 128:MLO + (b + 1) * 128], rhs=cur(k),
                                                              start=(k == 0), stop=(k == 7)) for k in range(8)][-1]),
                                     reads=[rHTc, rWIN], writes=[rpqk])
                            if c == 0:
                                yield S.op("pool", lambda e: e.memset(QKC[:, :, 0:3], 0.0), writes=[rQKC])
                            else:
                                yield S.op("pool", lambda e: e.tensor_copy(out=QKC[:, :, 0:3], in_=QKC[:, :, 128:131]), reads=[rQKC], writes=[rQKC])
                            yield S.op("act", lambda e: e.activation(out=QKC[:, :, 3:131], in_=v3(pqk[:], 4), func=AF.Copy), reads=[rpqk], writes=[rQKC])
                            for b in range(4):
                                eng = "dve"
                                yield S.op(eng, lambda e, b=b: e.tensor_scalar(out=ACC[:, b, :], in0=QKC[:, b, 0:128], scalar1=CW[:, b, 0:1], scalar2=CB[:, b:b + 1],
                                                                         op0=ALU.mult, op1=ALU.add), reads=[rQKC, rCW, rCB], writes=[rACC])
                                for j in range(1, 4):
                                    yield S.op(eng, lambda e, b=b, j=j: e.scalar_tensor_tensor(out=ACC[:, b, :], in0=QKC[:, b, j:j + 128], scalar=CW[:, b, j:j + 1],
                                                                                        in1=ACC[:, b, :], op0=ALU.mult, op1=ALU.add),
                                         reads=[rQKC, rCW, rACC], writes=[rACC])
                            yield S.op("act", lambda e: e.activation(out=QKT[:], in_=ACC, func=AF.Silu), reads=[rACC], writes=[rQKT])
                            pv, rpv = pf("m")
                            yield S.op("pe", proj_tok(pv[:], MLO + 512 - 0, 512, False) if False else
                                 (lambda e: [e.matmul(pv[:], lhsT=cur(k), rhs=WIN[:, k, MLO + 512:MLO + 1024], start=(k == 0), stop=(k == 7)) for k in range(8)][-1]),
                                 reads=[rHTc, rWIN], writes=[rpv])
                            yield S.op("act", lambda e: e.activation(out=VE[:, :, 0:128], in_=v3(pv[:], 4), func=AF.Copy), reads=[rpv], writes=[rVE])
                            po, rpo = pf("m")
                            yield S.op("pe", lambda e: [e.matmul(po[:], lhsT=cur(k), rhs=WIN[:, k, MLO + 1024:MLO + 1536], start=(k == 0), stop=(k == 7)) for k in range(8)][-1],
                                 reads=[rHTc, rWIN], writes=[rpo])
                            yield S.op("act", lambda e: e.activation(out=SO[:], in_=po[:], func=AF.Sigmoid), reads=[rpo], writes=[rSO])
                            pgt, rpgt = pf("m")
                            yield S.op("pe", lambda e: [e.matmul(pgt[:, 0:8], lhsT=cur(k), rhs=WIN[:, k, MLO + 1536:MLO + 1544], start=(k == 0), stop=(k == 7)) for k in range(8)][-1],
                                 reads=[rHTc, rWIN], writes=[rpgt])
                            yield S.op("dve", lambda e: e.tensor_tensor(out=G8[:, 0:8], in0=pgt[:, 0:8], in1=GB[:], op=ALU.add), reads=[rpgt, rGB], writes=[rG8])
                            yield S.op("act", lambda e: e.activation(out=G8[:, 0:8], in_=G8[:, 0:8], func=AF.Tanh, scale=1.0 / 15), reads=[rG8], writes=[rG8])
                            yield S.op("act", lambda e: e.activation(out=G8[:, 8:12], in_=G8[:, 4:8], func=AF.Exp, scale=-15.0), reads=[rG8], writes=[rG8])
                            yield S.op("dve", lambda e: e.tensor_scalar(out=G8[:, 8:12], in0=G8[:, 8:12], scalar1=1.0, scalar2=None, op0=ALU.add), reads=[rG8], writes=[rG8])
                            yield S.op("act", lambda e: e.activation(out=G8[:, 12:16], in_=G8[:, 8:12], func=AF.Ln), reads=[rG8], writes=[rG8])
                            pb, rpb = pf("m")
                            yield S.op("pe", lambda e: e.matmul(pb[:, 0:4], lhsT=MUI, rhs=G8[:, 12:16], start=True, stop=True), reads=[rCST, rG8], writes=[rpb])
                            yield S.op("pe", lambda e: e.matmul(pb[:, 4:8], lhsT=ONES, rhs=G8[:, 12:16], start=True, stop=True), reads=[rCST, rG8], writes=[rpb])
                            yield S.op("dve", lambda e: e.scalar_tensor_tensor(out=G8[:, 16:20], in0=G8[:, 0:4], scalar=15.0, in1=pb[:, 0:4], op0=ALU.mult, op1=ALU.add),
                                 reads=[rG8, rpb], writes=[rG8])
                            yield S.op("dve", lambda e: e.tensor_tensor(out=DG, in0=bcm(IDF, 4), in1=bc3(G8[:, 16:20], 4, 128), op=ALU.mult),
                                 reads=[rCST, rG8], writes=[rDG])
                            pgm, rpgm = pf("m")
                            yield S.op("pe", lambda e: e.matmul(pgm[:], lhsT=ONES, rhs=TM[:], start=True, stop=True),
                                 reads=[rCST, rDG], writes=[rpgm])
                            yield S.op("dve", lambda e: e.tensor_reduce(out=G8[:, 20:24], in_=v3(pgm[:], 4), axis=AX.X, op=ALU.max), reads=[rpgm], writes=[rG8])
                            yield S.op("dve", lambda e: e.tensor_tensor(out=G8[:, 20:24], in0=G8[:, 20:24], in1=Ms[:], op=ALU.max), reads=[rG8, rMs], writes=[rG8])
                            yield S.op("dve", lambda e: e.tensor_tensor(out=G8[:, 40:44], in0=G8[:, 16:20], in1=G8[:, 20:24], op=ALU.subtract), reads=[rG8], writes=[rG8])
                            yield S.op("act", lambda e: e.activation(out=G8[:, 24:28], in_=G8[:, 40:44], func=AF.Exp), reads=[rG8], writes=[rG8])
                            yield S.op("dve", lambda e: e.tensor_tensor(out=G8[:, 44:48], in0=Ms[:], in1=G8[:, 20:24], op=ALU.subtract), reads=[rG8, rMs], writes=[rG8])
                            yield S.op("act", lambda e: e.activation(out=G8[:, 28:32], in_=G8[:, 44:48], func=AF.Exp), reads=[rG8], writes=[rG8])
                            yield S.op("dve", lambda e: e.tensor_scalar(out=G8[:, 36:40], in0=G8[:, 28:32], scalar1=0.125, scalar2=None, op0=ALU.mult), reads=[rG8], writes=[rG8])
                            yield S.op("dve", lambda e: e.tensor_tensor(out=G8[:, 40:44], in0=pb[:, 0:4], in1=G8[:, 20:24], op=ALU.subtract), reads=[rpb, rG8], writes=[rG8])
                            yield S.op("act", lambda e: e.activation(out=G8[:, 32:36], in_=G8[:, 40:44], func=AF.Exp), reads=[rG8], writes=[rG8])
                            yield S.op("dve", lambda e: e.tensor_tensor(out=Ms[:], in0=G8[:, 20:24], in1=pb[:, 4:8], op=ALU.subtract), reads=[rG8, rpb], writes=[rMs])
                            pt, rpt = ptb()
                            yield S.op("pe", lambda e: [e.transpose(out=pt[:, b * 128:(b + 1) * 128], in_=QKT[:, 2 + b, :], identity=IDB) for b in range(2)][-1],
                                 reads=[rQKT, rCSB], writes=[rpt])
                            yield S.op("dve", lambda e: e.tensor_tensor(out=KP[:], in0=v3(pt[:, 0:256], 4), in1=bc3(G8[:, 24:28], 4, 64), op=ALU.mult),
                                 reads=[rpt, rG8], writes=[rKP])
                            psts = [pf("m"), pf("m")]
                            for par in range(2):
                                pst, rpst = psts[par]
                                yield S.op("pe", lambda e: [e.matmul(pst[:, (h // 2) * 128:(h // 2 + 1) * 128], lhsT=QKT[par * 64:par * 64 + 64, 2 + h // 2, :],
                                                               rhs=QKT[par * 64:par * 64 + 64, h // 2, :], start=True, stop=True) for h in (par, par + 2)][-1],
                                     reads=[rQKT], writes=[rpst])
                            for h in range(4):
                                pst, rpst = psts[h % 2]
                                yield S.op("dve", lambda e, h=h: e.scalar_tensor_tensor(out=PTB[:, h, :], in0=pst[:, (h // 2) * 128:(h // 2 + 1) * 128], scalar=G8[:, 24 + h:25 + h],
                                                                                 in1=MUI8[:], op0=ALU.mult, op1=ALU.mult), reads=[rpst, rG8, rMUI8], writes=[rPTB])
                            for h in range(4):
                                po_ = (h % 2) * 64
                                yield S.op("dve", lambda e, h=h: e.tensor_scalar(out=CBF[po_:po_ + 64, h // 2, :], in0=CFs[po_:po_ + 64, h // 2, :],
                                                                            scalar1=G8[po_:po_ + 64, 36 + h:37 + h], scalar2=None, op0=ALU.mult),
                                     reads=[rCFs, rG8], writes=[rCBF])
                            pn = [pf("m"), pf("m")]
                            for i2 in range(2):
                                pnn, rpnn = pn[i2]

                                def fn(e, i2=i2, pnn=pnn):
                                    last = None
                                    for j in range(2):
                                        h = 2 * i2 + j
                                        o = pnn[:, j * 129:(j + 1) * 129]
                                        e.matmul(o, lhsT=PTB[:, h, :], rhs=VE[:, h, :], start=True, stop=False)
                                        last = e.matmul(o, lhsT=QKT[(h % 2) * 64:(h % 2) * 64 + 64, h // 2, :], rhs=CBF[(h % 2) * 64:(h % 2) * 64 + 64, h // 2, :], start=False, stop=True)
                                    return last
                                yield S.op("pe", fn, reads=[rPTB, rVE, rQKT, rCBF], writes=[rpnn])
                            for i2 in range(2):
                                pnn, rpnn = pn[i2]
                                yield S.op("dve", lambda e, i2=i2, pnn=pnn: e.tensor_copy(out=S8[:, 56 + 2 * i2:58 + 2 * i2],
                                                                                  in_=pnn[:, 0:258].rearrange("p (a b) -> p a b", a=2)[:, :, 128:129].rearrange("p a b -> p (a b)")),
                                     reads=[rpnn], writes=[rS8])
                            yield S.op("dve", lambda e: e.tensor_scalar(out=G8[:, 40:44], in0=S8[:, 56:60], scalar1=-1.0, scalar2=None, op0=ALU.mult), reads=[rS8], writes=[rG8])
                            yield S.op("dve", lambda e: e.tensor_tensor(out=S8[:, 56:60], in0=S8[:, 56:60], in1=G8[:, 40:44], op=ALU.max), reads=[rS8, rG8], writes=[rS8])
                            yield S.op("dve", lambda e: e.tensor_tensor(out=S8[:, 56:60], in0=S8[:, 56:60], in1=G8[:, 32:36], op=ALU.max), reads=[rS8, rG8], writes=[rS8])
                            yield S.op("dve", lambda e: e.reciprocal(out=S8[:, 60:64], in_=S8[:, 56:60]), reads=[rS8], writes=[rS8])
                            for i2 in range(2):
                                pnn, rpnn = pn[i2]
                                yield S.op("dve", lambda e, i2=i2, pnn=pnn: e.tensor_tensor(out=HM[:, 2 * i2:2 * i2 + 2, :],
                                                                                    in0=pnn[:, 0:258].rearrange("p (a b) -> p a b", a=2)[:, :, 0:128],
                                                                                    in1=bc3(S8[:, 60 + 2 * i2:62 + 2 * i2], 2, 128), op=ALU.mult),
                                     reads=[rpnn, rS8], writes=[rHM])
                            pcc, rpcc = pf("m")
                            yield S.op("pe", lambda e: [e.matmul(pcc[(h % 2) * 64:(h % 2) * 64 + 64, (h // 2) * 129:(h // 2 + 1) * 129], lhsT=KP[:, h, :], rhs=VE[:, h, :],
                                                           start=True, stop=True) for h in range(4)][-1], reads=[rKP, rVE], writes=[rpcc])
                            for h in range(4):
                                po_ = (h % 2) * 64
                                yield S.op("dve", lambda e, h=h: e.scalar_tensor_tensor(out=CFs[po_:po_ + 64, h // 2, :], in0=CFs[po_:po_ + 64, h // 2, :],
                                                                                 scalar=G8[po_:po_ + 64, 28 + h:29 + h],
                                                                                 in1=pcc[po_:po_ + 64, (h // 2) * 129:(h // 2 + 1) * 129], op0=ALU.mult, op1=ALU.add),
                                     reads=[rCFs, rG8, rpcc], writes=[rCFs])
                            HM2 = HM_[:]
                            dbg_out("hm", HM2, rHM, r0)
                            yield S.op("pool", lambda e: e.tensor_tensor(out=TM[:], in0=HM2, in1=HM2, op=ALU.mult), reads=[rHM], writes=[rTM])
                            yield S.op("dve", lambda e: e.tensor_reduce(out=G8[:, 40:44], in_=v3(TM[:], 4), axis=AX.X, op=ALU.add), reads=[rTM], writes=[rG8])
                            yield S.op("dve", lambda e: e.tensor_scalar(out=G8[:, 40:44], in0=G8[:, 40:44], scalar1=1.0 / 128, scalar2=1e-6, op0=ALU.mult, op1=ALU.add),
                                 reads=[rG8], writes=[rG8])
                            yield S.op("act", lambda e: e.activation(out=G8[:, 40:44], in_=G8[:, 40:44], func=AF.Sqrt), reads=[rG8], writes=[rG8])
                            yield S.op("dve", lambda e: e.reciprocal(out=G8[:, 44:48], in_=G8[:, 40:44]), reads=[rG8], writes=[rG8])
                            yield S.op("dve", lambda e: e.tensor_tensor(out=HM, in0=HM, in1=bc3(G8[:, 44:48], 4, 128), op=ALU.mult), reads=[rHM, rG8], writes=[rHM])
                            if dbg:
                                yield S.op("dve", lambda e: e.tensor_tensor(out=DBG[:], in0=HM2, in1=SO[:], op=ALU.mult), reads=[rHM, rSO], writes=[rDBG])
                                dbg_out("yml", DBG[:], rDBG, r0)
                            yield S.op("pool", lambda e: e.tensor_tensor(out=MIX[:, 512:1024], in0=HM2, in1=SO[:], op=ALU.mult), reads=[rHM, rSO], writes=[rMIX])


                        def speed(gen, n):
                            while True:
                                for _k in range(n):
                                    try:
                                        next(gen)
                                    except StopIteration:
                                        return
                                yield
                        NP_, NR_, NM_ = 1, 4, 3
                        nxt = [speed(early(ti + 1), NP_)] if ti + 1 < len(tiles) else []
                        for _ in interleave(speed(chain_R(), NR_), speed(chain_M(), NM_), *nxt):
                            pass
                        chk(7)
                        def w_out_chain():
                            pt_, rpt = PF[5]; pt = pt_[:].bitcast(BF16)
                            yield S.op("pe", lambda e: [e.transpose(out=pt[:, k * 128:(k + 1) * 128], in_=MIX[:, k * 128:(k + 1) * 128], identity=IDB) for k in range(8)][-1],
                                 reads=[rMIX, rCSB], writes=[rpt])
                            yield S.op("act", lambda e: e.activation(out=MIXT[:], in_=v3(pt[:], 8), func=AF.Copy), reads=[rpt], writes=[rMIXT])
                            for g in range(2):
                                p, rp = PF[3 + g]
                                yield S.op("pe", lambda e, g=g, p=p: [e.matmul(p[:], lhsT=MIXT[:, k, :], rhs=WOUT[:, k, g * 512:(g + 1) * 512], start=(k == 0), stop=(k == 7))
                                                              for k in range(8)][-1], reads=[rMIXT, rWOUT], writes=[rp])
                                yield S.op("dve", lambda e, g=g, p=p: e.tensor_tensor(out=Xc[:, g * 512:(g + 1) * 512], in0=p[:], in1=Xc[:, g * 512:(g + 1) * 512], op=ALU.add),
                                     reads=[rp, rXc], writes=[rXc])
                            S.dma("sp", x1_d[r0:r0 + 128, :], Xc[:], reads=[rXc])

                            yield
                        for _ in interleave(speed(w_out_chain(), 2), *([speed(L_late(), 3)] if ti + 1 < len(tiles) else [])):
                            pass
                        chk(8)
                S.barrier()
            S.barrier()

        es2 = ExitStack()
        with es2:
            sb2, _ = mk(es2)
            TOKS = 512 if T % 512 == 0 else 256
            NSUB = TOKS // 128
            WUP, rWUP = sb2("WUP", [128, 8, 2 * DFF], BF16)
            WDN, rWDN = sb2("WDN", [128, NB, D], BF16)
            FCW, rFCW = sb2("FCW", [128, NB, 3])
            FCB, rFCB = sb2("FCB", [128, NB])
            GF, rGF = sb2("GF", [128, D])
            S.dma("sp", FCW[:].rearrange("p b j -> p (b j)"), ffn_conv_w, writes=[rFCW])
            S.dma("sp", FCB[:], ffn_conv_b, writes=[rFCB])
            S.dma("sp", GF[:], norm_f_g.partition_broadcast(128), writes=[rGF])
            esC = ExitStack()
            with esC:
                sbC, _ = mk(esC)
                G2, rG2 = sbC("G2", [128, 8])
                STG2 = [sbC(f"STH{i}", [128, DFF]) for i in range(2)]
                S.dma("sp", G2[:], norm2_g, writes=[rG2])
                i = 0
                for k in range(8):
                    for hlf in range(2):
                        st, rst = STG2[i % 2]; i += 1
                        S.dma("sp", st[:], w_ffn_up[k * 128:(k + 1) * 128, hlf * DFF:(hlf + 1) * DFF], writes=[rst])
                        eng = "act" if hlf == 0 else "dve"
                        if eng == "act":
                            S.op("act", lambda e: e.activation(out=WUP[:, k, hlf * DFF:(hlf + 1) * DFF], in_=st[:], func=AF.Copy, scale=G2[:, k:k + 1]),
                                 reads=[rst, rG2], writes=[rWUP])
                        else:
                            S.op("dve", lambda e: e.tensor_scalar(out=WUP[:, k, hlf * DFF:(hlf + 1) * DFF], in0=st[:], scalar1=G2[:, k:k + 1], scalar2=None, op0=ALU.mult),
                                 reads=[rst, rG2], writes=[rWUP])
                for b in range(0, NB, 2):
                    st, rst = STG2[i % 2]; i += 1
                    S.dma("sp", st[:, 0:2 * D].rearrange("p (a n) -> p a n", a=2), w_ffn_down[b * 128:(b + 2) * 128, :].rearrange("(a p) n -> p a n", p=128),
                          writes=[rst])
                    S.op("pool" if (b // 2) % 2 else "dve", lambda e: e.tensor_copy(out=WDN[:, b:b + 2, :], in_=st[:, 0:2 * D].rearrange("p (a n) -> p a n", a=2)),
                         reads=[rst], writes=[rWDN])
                S.barrier()
            chk(9)
            esD = ExitStack()
            with esD:
                sbD, _ = mk(esD)
                XS = [sbD(f"XS{i}", [128, D]) for i in range(NSUB)]
                HN2, rHN2 = sbD("HN2", [128, D], BF16)
                H2T, rH2T = sbD("H2T", [128, 8, TOKS], BF16)
                GT, rGT = sbD("GT", [128, NB, TOKS], BF16)
                AC = [sbD(f"AC{i}", [128, TOKS + 2]) for i in range(2)]
                AQ = [sbD(f"AQ{i}", [128, TOKS]) for i in range(2)]
                CAR, rCAR = sbD("CAR", [128, NB, 2])
                ST2, rST2 = sbD("ST2", [128, 8])
                it = 0
                for s in range(NSEQ):
                    S.op("pool", lambda e: e.memset(CAR[:], 0.0), writes=[rCAR])
                    for c in range(T // TOKS):
                        r0 = s * T + c * TOKS
                        for sub in range(NSUB):
                            Xs, rXs = XS[sub]
                            S.dma("sp", Xs[:], x1_d[r0 + sub * 128:r0 + (sub + 1) * 128, :], writes=[rXs])
                            rmsnorm_to_bf16(Xs[:], rXs, HN2, rHN2, ST2, rST2)
                            pt, rpt = ptb()
                            S.op("pe", lambda e: [e.transpose(out=pt[:, k * 128:(k + 1) * 128], in_=HN2[:, k * 128:(k + 1) * 128], identity=IDB) for k in range(8)][-1],
                                 reads=[rHN2, rCSB], writes=[rpt])
                            S.op("dve", lambda e, sub=sub: e.tensor_copy(out=H2T[:, :, sub * 128:(sub + 1) * 128], in_=v3(pt[:], 8)), reads=[rpt], writes=[rH2T])
                        for b in range(NB):
                            ACb, rACb = AC[b % 2]
                            AQb, rAQb = AQ[b % 2]
                            pa, rpa = pf()
                            pbk, rpbk = pf()
                            S.op("pe", lambda e, b=b, pa=pa: [e.matmul(pa[:, 0:TOKS], lhsT=WUP[:, k, b * 128:(b + 1) * 128], rhs=H2T[:, k, :], start=(k == 0), stop=(k == 7))
                                                             for k in range(8)][-1], reads=[rWUP, rH2T], writes=[rpa])
                            S.op("pe", lambda e, b=b, pbk=pbk: [e.matmul(pbk[:, 0:TOKS], lhsT=WUP[:, k, DFF + b * 128:DFF + (b + 1) * 128], rhs=H2T[:, k, :], start=(k == 0), stop=(k == 7))
                                                               for k in range(8)][-1], reads=[rWUP, rH2T], writes=[rpbk])
                            S.op("pool", lambda e, b=b: e.tensor_copy(out=ACb[:, 0:2], in_=CAR[:, b, :]), reads=[rCAR], writes=[rACb])
                            S.op("act", lambda e: e.activation(out=ACb[:, 2:TOKS + 2], in_=pa[:, 0:TOKS], func=AF.Copy), reads=[rpa], writes=[rACb])
                            S.op("pool", lambda e, b=b: e.tensor_copy(out=CAR[:, b, :], in_=ACb[:, TOKS:TOKS + 2]), reads=[rACb], writes=[rCAR])
                            S.op("dve", lambda e, b=b: e.tensor_scalar(out=AQb[:], in0=ACb[:, 0:TOKS], scalar1=FCW[:, b, 0:1], scalar2=FCB[:, b:b + 1], op0=ALU.mult, op1=ALU.add),
                                 reads=[rACb, rFCW, rFCB], writes=[rAQb])
                            S.op("dve", lambda e, b=b: e.scalar_tensor_tensor(out=AQb[:], in0=ACb[:, 1:TOKS + 1], scalar=FCW[:, b, 1:2], in1=AQb[:], op0=ALU.mult, op1=ALU.add),
                                 reads=[rACb, rFCW, rAQb], writes=[rAQb])
                            S.op("dve", lambda e, b=b: e.scalar_tensor_tensor(out=AQb[:], in0=ACb[:, 2:TOKS + 2], scalar=FCW[:, b, 2:3], in1=AQb[:], op0=ALU.mult, op1=ALU.add),
                                 reads=[rACb, rFCW, rAQb], writes=[rAQb])
                            S.op("act", lambda e: e.activation(out=AQb[:], in_=AQb[:], func=AF.Silu), reads=[rAQb], writes=[rAQb])
                            S.op("dve", lambda e, b=b: e.tensor_tensor(out=GT[:, b, :], in0=AQb[:], in1=pbk[:, 0:TOKS], op=ALU.mult), reads=[rAQb, rpbk], writes=[rGT])
                        for sub in range(NSUB):
                            Xs, rXs = XS[sub]
                            X2c, rX2c = Xs, rXs
                            for g in range(2):
                                p, rp = pf()
                                S.op("pe", lambda e, g=g, p=p, sub=sub: [e.matmul(p[:], lhsT=GT[:, b, sub * 128:(sub + 1) * 128], rhs=WDN[:, b, g * 512:(g + 1) * 512],
                                                                                 start=(b == 0), stop=(b == NB - 1)) for b in range(NB)][-1], reads=[rGT, rWDN], writes=[rp])
                                S.op("dve", lambda e, g=g, p=p: e.tensor_tensor(out=X2c[:, g * 512:(g + 1) * 512], in0=p[:], in1=Xs[:, g * 512:(g + 1) * 512], op=ALU.add),
                                     reads=[rp, rXs], writes=[rX2c])
                            S.op("pool", lambda e: e.memset(ST2[:, 4:5], 0.0), writes=[rST2])
                            S.op("act", lambda e: e.activation(out=HN2[:], in_=X2c[:], func=AF.Square, accum_out=ST2[:, 4:5]), reads=[rX2c, rST2], writes=[rHN2, rST2])
                            S.op("dve", lambda e: e.tensor_scalar(out=ST2[:, 5:6], in0=ST2[:, 4:5], scalar1=1.0 / D, scalar2=1e-6, op0=ALU.mult, op1=ALU.add),
                                 reads=[rST2], writes=[rST2])
                            S.op("act", lambda e: e.activation(out=ST2[:, 6:7], in_=ST2[:, 5:6], func=AF.Sqrt), reads=[rST2], writes=[rST2])
                            S.op("dve", lambda e: e.reciprocal(out=ST2[:, 7:8], in_=ST2[:, 6:7]), reads=[rST2], writes=[rST2])
                            S.op("act", lambda e: e.activation(out=X2c[:], in_=X2c[:], func=AF.Copy, scale=ST2[:, 7:8]), reads=[rX2c, rST2], writes=[rX2c])
                            S.op("pool", lambda e: e.tensor_tensor(out=X2c[:], in0=X2c[:], in1=GF[:], op=ALU.mult), reads=[rX2c, rGF], writes=[rX2c])
                            S.dma("sp", out_d[r0 + sub * 128:r0 + (sub + 1) * 128, :], X2c[:], reads=[rX2c])
                S.barrier()
            S.barrier()
    return nc


def make_consts():
    c = np.zeros((128, 640), np.float32)
    i = np.arange(128)
    c[:, 0:128] = np.eye(128)
    c[:, 128:256] = (i[:, None] < i[None, :])
    c[:, 256:384] = (i[:, None] <= i[None, :])
    c[:, 384:512] = (i[:, None] > i[None, :])
    c[:, 512:640] = 1.0
    return c


def make_in_maps(inputs, n_cores, nseq):
    f = lambda a: np.ascontiguousarray(np.asarray(a, np.float32))
    x = f(inputs["x"])
    T = x.shape[1]
    shared = {}
    for k, v in inputs.items():
        if k == "x":
            continue
        a = f(v)
        if k != "norm_f_g":
            a = a[0]
        if k == "r_k":
            a = a.reshape(512)
        elif k in ("norm1_g", "norm2_g", "qk_conv_b", "ffn_conv_b", "mh_norm_g"):
            a = a.reshape(-1, 128).T
        elif k in ("qk_conv_w", "ffn_conv_w"):
            j = a.shape[0]
            a = a.reshape(j, -1, 128).transpose(2, 1, 0).reshape(128, -1)
        shared[k] = np.ascontiguousarray(a)
    shared["consts"] = make_consts()
    maps = []
    for c in range(n_cores):
        m = dict(shared)
        m["x"] = np.ascontiguousarray(x[c * nseq:(c + 1) * nseq].reshape(nseq * T, D))
        maps.append(m)
    return maps


def kernel(**inputs):
    x = np.asarray(inputs["x"])
    B, T, _ = x.shape
    nseq = B // N_CORES
    nc = build(nseq, T)
    maps = make_in_maps(inputs, N_CORES, nseq)
    res = run_bass_kernel_spmd(nc, maps, core_ids=list(range(N_CORES)))
    out = np.concatenate([r["out"].reshape(nseq, T, D) for r in res.results], axis=0)
    return out.astype(np.float32)
```
